# Optimizing a Trainium2 kernel written in Bass

```python
import math
import jax, jax.numpy as jnp
from jax import lax
import numpy as np

D_MODEL = 2048
BATCH = 2
SEQ = 4096
DEPTH = 1

HEAD_DIM = 128
N_HEADS_SB = (D_MODEL // HEAD_DIM) // 2
N_HEADS_DSA = (D_MODEL // HEAD_DIM) - N_HEADS_SB
SB_WIDTH = N_HEADS_SB * HEAD_DIM
DSA_WIDTH = N_HEADS_DSA * HEAD_DIM
IDX_HEADS = 16
IDX_DIM = 64
TOPK_MAX = 256
ROPE_THETA = 500000.0
ROPE_FRACTION = 4
D_FF = 5504
CONV_WIDTH = 3
Q_BLOCK = 128
EPS = 1e-6
NEG_BIG = -1e30

COL_SIZES = (
    SB_WIDTH, SB_WIDTH, SB_WIDTH,
    DSA_WIDTH, HEAD_DIM, HEAD_DIM,
    IDX_HEADS * IDX_DIM, IDX_DIM,
    IDX_HEADS,
)
IN_COLS = sum(COL_SIZES)
SPLIT_POINTS = tuple(int(v) for v in np.cumsum(COL_SIZES)[:-1])

kernel_name = "hybrid_sb_dsa_convffn_adaln"


def rmsnorm(x, g):
    x32 = x.astype(jnp.float32)
    y = x32 * lax.rsqrt(jnp.mean(x32 * x32, axis=-1, keepdims=True) + EPS)
    return (y * g.astype(jnp.float32)).astype(x.dtype)


def rope_partial(x, pos):
    d = x.shape[-1]
    rd = d // ROPE_FRACTION
    half = rd // 2
    inv_freq = ROPE_THETA ** (-jnp.arange(half, dtype=jnp.float32) / half)
    ang = pos.astype(jnp.float32)[..., None] * inv_freq
    cos = jnp.cos(ang)[:, :, None, :]
    sin = jnp.sin(ang)[:, :, None, :]
    x32 = x.astype(jnp.float32)
    x1, x2, rest = x32[..., :half], x32[..., half:rd], x32[..., rd:]
    out = jnp.concatenate([x1 * cos - x2 * sin, x2 * cos + x1 * sin, rest], axis=-1)
    return out.astype(x.dtype)


def stick_breaking_attention(q, k, v):
    S = q.shape[1]
    scale = q.shape[-1] ** -0.5
    outs = []
    for start in range(0, S, Q_BLOCK):
        end = start + Q_BLOCK
        qb, kb, vb = q[:, start:end], k[:, :end], v[:, :end]
        z = jnp.einsum('bqhd,bkhd->bhqk', qb, kb).astype(jnp.float32) * scale
        t_idx = start + jnp.arange(Q_BLOCK)[:, None]
        s_idx = jnp.arange(end)[None, :]
        strict = s_idx < t_idx
        log_1m = jnp.where(strict, jax.nn.log_sigmoid(-z), 0.0)
        key_axis = log_1m.ndim - 1
        suffix = lax.cumsum(log_1m, axis=key_axis, reverse=True) - log_1m
        w = jnp.where(strict, jnp.exp(jax.nn.log_sigmoid(z) + suffix), 0.0)
        outs.append(jnp.einsum('bhqk,bkhd->bqhd', w.astype(vb.dtype), vb))
    return jnp.concatenate(outs, axis=1)


def dsa_attention(q, k, v, q_idx, k_idx, w_idx):
    S = q.shape[1]
    topk = min(TOPK_MAX, S // 4)
    scale = q.shape[-1] ** -0.5
    gather = jax.vmap(lambda arr, idx: arr[idx])
    outs = []
    for start in range(0, S, Q_BLOCK):
        end = start + Q_BLOCK
        L = max(end, topk)
        rel = jax.nn.relu(jnp.einsum('bqhd,bkd->bqhk', q_idx[:, start:end],
                                     k_idx[:, :L]).astype(jnp.float32))
        score = jnp.einsum('bqh,bqhk->bqk', w_idx[:, start:end].astype(jnp.float32), rel)
        t_idx = start + jnp.arange(Q_BLOCK)[:, None]
        causal = jnp.arange(L)[None, :] <= t_idx
        score = jnp.where(causal, score, NEG_BIG)
        _, sel = lax.top_k(score, topk)
        valid = sel <= t_idx[None]
        k_sel = gather(k, sel)
        v_sel = gather(v, sel)
        logits = jnp.einsum('bqhd,bqkd->bhqk', q[:, start:end], k_sel).astype(jnp.float32) * scale
        logits = jnp.where(valid[:, None], logits, NEG_BIG)
        p = jax.nn.softmax(logits, axis=-1)
        outs.append(jnp.einsum('bhqk,bqkd->bqhd', p.astype(v_sel.dtype), v_sel))
    return jnp.concatenate(outs, axis=1)


def conv_ffn(h, w_up, conv_w, conv_b, w_down):
    S = h.shape[1]
    u = h @ w_up
    up = jnp.pad(u, ((0, 0), (CONV_WIDTH - 1, 0), (0, 0)))
    uc = conv_b
    for i in range(CONV_WIDTH):
        uc = uc + up[:, i:i + S] * conv_w[i]
    gate, val = jnp.split(uc, 2, axis=-1)
    return (jax.nn.silu(gate) * val) @ w_down


def setup_inputs(seed: int = 0) -> dict:
    key = jax.random.key(seed)
    ks = jax.random.split(key, 20)
    D = D_MODEL
    f32 = jnp.float32
    nrm = lambda k, shape, s: jax.random.normal(k, shape, f32) * s
    x = nrm(ks[0], (BATCH, SEQ, D), 1.0)
    c = nrm(ks[1], (BATCH, D), 1.0)
    offset = jax.random.randint(ks[2], (BATCH, 1), 0, 1024, dtype=jnp.int32)
    positions = (offset + jnp.arange(SEQ, dtype=jnp.int32)[None, :]).astype(jnp.int32)
    return {
        "x": x,
        "c": c,
        "positions": positions,
        "w_ada": nrm(ks[3], (DEPTH, D, 6 * D), 0.5 * D ** -0.5),
        "b_ada": nrm(ks[4], (DEPTH, 6 * D), 0.01),
        "norm1_g": 1.0 + nrm(ks[5], (DEPTH, D), 0.01),
        "w_in": nrm(ks[6], (DEPTH, D, IN_COLS), D ** -0.5),
        "sb_norm_g": 1.0 + nrm(ks[7], (DEPTH, SB_WIDTH), 0.01),
        "dsa_norm_g": 1.0 + nrm(ks[8], (DEPTH, DSA_WIDTH), 0.01),
        "w_out": nrm(ks[9], (DEPTH, SB_WIDTH + DSA_WIDTH, D), (SB_WIDTH + DSA_WIDTH) ** -0.5),
        "norm2_g": 1.0 + nrm(ks[10], (DEPTH, D), 0.01),
        "w_up": nrm(ks[11], (DEPTH, D, 2 * D_FF), D ** -0.5),
        "conv_w": nrm(ks[12], (DEPTH, CONV_WIDTH, 2 * D_FF), CONV_WIDTH ** -0.5),
        "conv_b": nrm(ks[13], (DEPTH, 2 * D_FF), 0.01),
        "w_down": nrm(ks[14], (DEPTH, D_FF, D), D_FF ** -0.5),
        "final_norm_g": 1.0 + nrm(ks[15], (D,), 0.01),
    }


def reference(x, c, positions, w_ada, b_ada, norm1_g, w_in, sb_norm_g, dsa_norm_g,
              w_out, norm2_g, w_up, conv_w, conv_b, w_down, final_norm_g):
    B, S, D = x.shape
    idx_scale = (IDX_HEADS ** -0.5) * (IDX_DIM ** -0.5)
    for l in range(DEPTH):
        mod = jax.nn.silu(c) @ w_ada[l] + b_ada[l]
        sh1, sc1, g1, sh2, sc2, g2 = jnp.split(mod[:, None, :], 6, axis=-1)

        h = rmsnorm(x, norm1_g[l]) * (1.0 + sc1) + sh1
        proj = h @ w_in[l]
        q_sb, k_sb, v_sb, q_ds, k_ds, v_ds, q_ix, k_ix, w_ix = jnp.split(proj, SPLIT_POINTS, axis=-1)

        q_sb = q_sb.reshape(B, S, N_HEADS_SB, HEAD_DIM)
        k_sb = k_sb.reshape(B, S, N_HEADS_SB, HEAD_DIM)
        v_sb = v_sb.reshape(B, S, N_HEADS_SB, HEAD_DIM)
        o_sb = stick_breaking_attention(q_sb, k_sb, v_sb).reshape(B, S, SB_WIDTH)

        q_ds = rope_partial(q_ds.reshape(B, S, N_HEADS_DSA, HEAD_DIM), positions)
        k_ds = rope_partial(k_ds[:, :, None, :], positions)[:, :, 0, :]
        q_ix = rope_partial(q_ix.reshape(B, S, IDX_HEADS, IDX_DIM), positions)
        k_ix = rope_partial(k_ix[:, :, None, :], positions)[:, :, 0, :]
        o_ds = dsa_attention(q_ds, k_ds, v_ds, q_ix, k_ix, w_ix * idx_scale).reshape(B, S, DSA_WIDTH)

        merged = jnp.concatenate([rmsnorm(o_sb, sb_norm_g[l]), rmsnorm(o_ds, dsa_norm_g[l])], axis=-1)
        x = x + g1 * (merged @ w_out[l])

        h2 = rmsnorm(x, norm2_g[l]) * (1.0 + sc2) + sh2
        x = x + g2 * conv_ffn(h2, w_up[l], conv_w[l], conv_b[l], w_down[l])
    return rmsnorm(x, final_norm_g)
```

```python
import contextlib
import numpy as np
import concourse.bass as bass
import concourse.mybir as mybir
from concourse.bass_utils import run_bass_kernel_spmd

F32 = mybir.dt.float32
BF16 = mybir.dt.bfloat16
I32 = mybir.dt.int32
AF = mybir.ActivationFunctionType
ALU = mybir.AluOpType
AX = mybir.AxisListType

ENGS = ("pe", "act", "dve", "pool", "sp")


class Buf:
    __slots__ = ("name", "lw", "rs")

    def __init__(self, name):
        self.name = name
        self.lw = None
        self.rs = []


class Op:
    __slots__ = ("eng", "idx", "fn", "deps", "dma", "stream", "gen", "flag", "incidx")

    def __init__(self, eng, idx, fn, dma, stream):
        self.eng = eng; self.idx = idx; self.fn = fn; self.deps = []
        self.dma = dma; self.stream = stream; self.gen = 0
        self.flag = False; self.incidx = 0


class Em:
    def __init__(self, nc):
        self.nc = nc
        self.ops = {e: [] for e in ENGS}
        self.streams = {}
        self.last_dma = {}
        self.pending = {e: None for e in ENGS}
        self.pool_dmas = []

    def barrier(self):
        lasts = [self.ops[e][-1] for e in ENGS if self.ops[e]]
        lasts += list(self.last_dma.values())
        for e in ENGS:
            self.pending[e] = lasts

    def op(self, eng, fn, reads=(), writes=(), dma=False, stream=None):
        o = Op(eng, len(self.ops[eng]), fn, dma, stream)
        deps = []
        if self.pending[eng] is not None:
            deps.extend(self.pending[eng])
            self.pending[eng] = None
        if dma and eng == "pool":
            if len(self.pool_dmas) >= 4:
                deps.append(self.pool_dmas[-4])
            self.pool_dmas.append(o)
        for b in reads:
            if b.lw is not None:
                deps.append(b.lw)
        for b in writes:
            if b.lw is not None:
                deps.append(b.lw)
            deps.extend(b.rs)
        for b in reads:
            b.rs.append(o)
        for b in writes:
            b.lw = o
            b.rs = []
        if dma:
            g = self.streams.get(stream, 0) + 1
            self.streams[stream] = g
            o.gen = g
            self.last_dma[stream] = o
        best = {}
        for d in deps:
            if d is o:
                continue
            if d.dma:
                key = ("dma", d.stream)
                if key not in best or best[key].gen < d.gen:
                    best[key] = d
            else:
                if d.eng == eng and eng == "pe":
                    continue
                key = ("eng", d.eng)
                if key not in best or best[key].idx < d.idx:
                    best[key] = d
        o.deps = list(best.values())
        self.ops[eng].append(o)
        return o

    def emit(self, final_waits=()):
        nc = self.nc
        for e in ENGS:
            seen = {}
            for o in self.ops[e]:
                nd = []
                for d in o.deps:
                    key = ("dma", d.stream) if d.dma else ("eng", d.eng)
                    val = d.gen if d.dma else d.idx
                    if seen.get(key, -1) >= val:
                        continue
                    seen[key] = val
                    nd.append(d)
                    if not d.dma:
                        d.flag = True
                o.deps = nd
        for e in ENGS:
            c = 0
            for o in self.ops[e]:
                if o.flag and not o.dma:
                    c += 1
                    o.incidx = c
        with contextlib.ExitStack() as st:
            esem = {e: st.enter_context(nc.semaphore("s_" + e)) for e in ENGS}
            dsem = {s: st.enter_context(nc.semaphore("d_%d" % i)) for i, s in enumerate(self.streams)}
            block = st.enter_context(nc.Block())

            def run(e, eng):
                for o in self.ops[e]:
                    for d in o.deps:
                        if d.dma:
                            eng.wait_ge(dsem[d.stream], 16 * d.gen)
                        else:
                            eng.wait_ge(esem[d.eng], d.incidx)
                    ins = o.fn(eng)
                    if o.dma:
                        ins.then_inc(dsem[o.stream], 16)
                    elif o.flag:
                        ins.then_inc(esem[e], 1)
                if e == "sp":
                    for s in final_waits:
                        eng.wait_ge(dsem[s], 16 * self.streams[s])

            @block.tensor
            def _(eng):
                run("pe", eng)

            @block.scalar
            def _(eng):
                run("act", eng)

            @block.vector
            def _(eng):
                run("dve", eng)

            @block.gpsimd
            def _(eng):
                run("pool", eng)

            @block.sync
            def _(eng):
                run("sp", eng)


class Cfg:
    def __init__(self, D=2048, S=4096, DFF=5504, BS=114, NBLK=9, TB=3, NBIS=28, IH=16, ID=64,
                 TOPK_MAX=256, CPB=4):
        self.D, self.S, self.DFF = D, S, DFF
        self.HD = 128
        nh = D // 128
        self.NHS = nh // 2
        self.NHD = nh - self.NHS
        self.SBW = self.NHS * 128
        self.DSW = self.NHD * 128
        self.IH, self.ID = IH, ID
        self.TOPK = min(TOPK_MAX, S // 4)
        self.CPB = CPB
        self.OWN = S // CPB
        self.TQ = self.OWN + 2
        self.BS, self.NBLK, self.TB = BS, NBLK, TB
        assert BS * NBLK == self.TQ and NBLK % TB == 0 and BS <= 128
        self.NT = NBLK // TB
        self.TW = TB * BS
        assert self.TW <= 512
        self.KC = D // 128
        self.NKB = S // 128
        self.FC = DFF // 128
        assert DFF % 128 == 0 and S % 512 == 0 and self.OWN % 128 == 0
        self.NOB = self.OWN // 128
        self.NBIS = NBIS
        o = 0
        self.o_qsb = o; o += self.SBW
        self.o_ksb = o; o += self.SBW
        self.o_vsb = o; o += self.SBW
        self.o_qds = o; o += self.DSW
        self.o_kds = o; o += 128
        self.o_vds = o; o += 128
        self.o_qix = o; o += IH * ID
        self.o_kix = o; o += ID
        self.o_wix = o; o += IH
        self.INC = o
        self.NSP = S // 512
        self.idx_scale = (IH ** -0.5) * (ID ** -0.5)
        self.qk_scale = 128 ** -0.5
        self.stop = None


class _Stop(Exception):
    pass


TWO_PI = 6.283185307179586
PI = 3.141592653589793
NEGB = -30000.0


def build_program(cf):
    hold = {}
    try:
        return _build(cf, hold)
    except _Stop:
        hold["em"].emit(final_waits=[])
        return hold["nc"]


def _build(cf, hold):
    nc = bass.Bass("TRN2", target_bir_lowering=False)
    em = Em(nc)
    hold["nc"] = nc; hold["em"] = em
    D, S, KC, NKB, TQ, BS, NBLK, TB, NT, TW = cf.D, cf.S, cf.KC, cf.NKB, cf.TQ, cf.BS, cf.NBLK, cf.TB, cf.NT, cf.TW
    NHS, NHD, SBW, DSW, IH, ID, FC, DFF = cf.NHS, cf.NHD, cf.SBW, cf.DSW, cf.IH, cf.ID, cf.FC, cf.DFF
    NSP, NBIS, OWN, NOB = cf.NSP, cf.NBIS, cf.OWN, cf.NOB
    NM = NHS + NHD

    def din(name, shape, dt=F32):
        return nc.dram_tensor(name, list(shape), dt, kind="ExternalInput").ap()

    def dscr(name, shape, dt):
        return nc.dram_tensor(name, list(shape), dt, kind="Internal").ap()

    xkv = din("xkv", [S, D]); xq = din("xq", [TQ, D])
    posk = din("posk", [128, NKB], I32); posq = din("posq", [BS, NBLK], I32)
    qcol = din("qcol", [BS, NBLK]); qrel = din("qrel", [BS, NBLK * NSP]); qrow = din("qrow", [128, TQ])
    cb = din("cb", [128, KC])
    w_ada = din("w_ada", [D, 6 * D]); b_ada = din("b_ada", [1, 6 * D])
    w_in = din("w_in", [D, cf.INC]); w_out = din("w_out", [D, D])
    w_up = din("w_up", [D, 2 * DFF]); w_down = din("w_down", [DFF, D])
    n1g = din("n1g", [128, KC]); n2g = din("n2g", [128, KC])
    sbg = din("sbg", [128, NHS]); dsg = din("dsg", [128, NHD]); fng = din("fng", [128, D])
    convw = din("convw", [128, 3 * 2 * FC]); convb = din("convb", [128, 2 * FC])
    identf = din("identf", [128, 128]); cmat = din("cmat", [128, 5 * 128])
    iota = din("iota", [128, 512]); kcolc = din("kcolc", [128, 1]); pow2 = din("pow2", [128, NBIS + 1])
    invf = din("invf", [128, 16]); flag = din("flag", [128, 1])
    out = nc.dram_tensor("out", [OWN, D], F32, kind="ExternalOutput").ap()
    modrow = dscr("modrow", [1, 6 * D], F32)
    kTsb = dscr("kTsb", [NHS, 128, S], BF16)
    vsb = dscr("vsb", [NHS, 128, NKB * 128], BF16)
    xmid = dscr("xmid", [TQ, D], F32)
    yscr = dscr("yscr", [OWN, D], F32)
    h2Ts = dscr("h2Ts", [128, KC * TQ], BF16)

    def mm(o, lhsT, rhs, start, stop, r, w):
        em.op("pe", lambda e: e.matmul(o, lhsT=lhsT, rhs=rhs, start=start, stop=stop), r, w)

    def tr(o, in_, ident, r, w):
        em.op("pe", lambda e: e.transpose(o, in_, ident), r, w)

    def act(o, in_, func, r, w, **kw):
        em.op("act", lambda e: e.activation(out=o, in_=in_, func=func, **kw), r, w)

    def ts(eng, o, in0, s1, s2, op0, op1, r, w, accum_out=None):
        if op1 is None:
            em.op(eng, lambda e: e.tensor_scalar(out=o, in0=in0, scalar1=s1, scalar2=None, op0=op0), r, w)
        elif accum_out is None:
            em.op(eng, lambda e: e.tensor_scalar(out=o, in0=in0, scalar1=s1, scalar2=s2, op0=op0, op1=op1), r, w)
        else:
            em.op(eng, lambda e: e.tensor_scalar(out=o, in0=in0, scalar1=s1, scalar2=s2, op0=op0, op1=op1,
                                                 accum_out=accum_out), r, w)

    def tt(eng, o, in0, in1, op, r, w):
        em.op(eng, lambda e: e.tensor_tensor(out=o, in0=in0, in1=in1, op=op), r, w)

    def stt(o, in0, sc, in1, op0, op1, r, w):
        em.op("dve", lambda e: e.scalar_tensor_tensor(out=o, in0=in0, scalar=sc, in1=in1, op0=op0, op1=op1), r, w)

    def cp(eng, o, in_, r, w):
        if eng == "act":
            em.op("act", lambda e: e.copy(out=o, in_=in_), r, w)
        else:
            em.op(eng, lambda e: e.tensor_copy(out=o, in_=in_), r, w)

    def memset(eng, o, val, w):
        em.op(eng, lambda e: e.memset(o, val), (), w)

    def dma(q, o, in_, r, w, stream, slow=False):
        if slow:
            em.op(q, lambda e: e.dma_start(out=o, in_=in_, allow_slow_non_contiguous=True), r, w, dma=True, stream=stream)
        else:
            em.op(q, lambda e: e.dma_start(out=o, in_=in_), r, w, dma=True, stream=stream)

    def chk(tag):
        if cf.stop == tag:
            raise _Stop()

    with contextlib.ExitStack() as gst:
        def sbt(st, name, shape, dt):
            return st.enter_context(nc.sbuf_tensor("s_" + name, list(shape), dt))

        PS = gst.enter_context(nc.psum_tensor("PS", [128, 8, 512], F32))
        pb = [PS[:, i, :] for i in range(8)]
        bpb = [Buf("pb%d" % i) for i in range(8)]

        identf_t = sbt(gst, "identf", [128, 128], F32)
        cm_t = sbt(gst, "cmat", [128, 5, 128], BF16)
        iota_t = sbt(gst, "iota", [128, 512], F32)
        kcol_t = sbt(gst, "kcolc", [128, 1], F32)
        pow2_t = sbt(gst, "pow2", [128, NBIS + 1], F32)
        flag_t = sbt(gst, "flag", [128, 1], F32)
        vec_t = sbt(gst, "vecs", [128, 8, KC], F32)
        sbg_t = sbt(gst, "sbg", [128, NHS], F32)
        dsg_t = sbt(gst, "dsg", [128, NHD], F32)
        cvw_t = sbt(gst, "cvw", [128, 3, 2 * FC], F32)
        cvb_t = sbt(gst, "cvb", [128, 2 * FC], F32)
        qcol_t = sbt(gst, "qcol", [BS, NBLK], F32)
        qrel_t = sbt(gst, "qrel", [BS, NBLK, NSP], F32)
        cosK = sbt(gst, "cosK", [128, NKB, 16], F32); sinK = sbt(gst, "sinK", [128, NKB, 16], F32)
        cosQ = sbt(gst, "cosQ", [BS, NBLK, 16], F32); sinQ = sbt(gst, "sinQ", [BS, NBLK, 16], F32)
        kTds = sbt(gst, "kTds", [128, S], BF16)
        vds = sbt(gst, "vds", [128, NKB, 128], BF16)
        kTix = sbt(gst, "kTix", [128, S], BF16)
        bc = Buf("consts")
        b_vec = Buf("vecs"); b_rope = Buf("rope")
        b_kTds = Buf("kTds"); b_vds = Buf("vds"); b_kTix = Buf("kTix")

        for (t_, src, nm) in ((identf_t[:], identf[:, :], "c0"), (iota_t[:], iota[:, :], "c1"),
                              (kcol_t[:], kcolc[:, :], "c2"), (pow2_t[:], pow2[:, :], "c3"),
                              (flag_t[:], flag[:, :], "c4"), (sbg_t[:], sbg[:, :], "c5"), (dsg_t[:], dsg[:, :], "c6"),
                              (cvw_t[:], convw.rearrange("p (a f) -> p a f", a=3), "c7"), (cvb_t[:], convb[:, :], "c8"),
                              (qcol_t[:], qcol[:, :], "c9"),
                              (qrel_t[:], qrel.rearrange("p (a f) -> p a f", a=NBLK), "c10"),
                              (vec_t[:, 4, :], n1g[:, :], "c11"), (vec_t[:, 5, :], n2g[:, :], "c12")):
            dma("sp", t_, src, (), [bc], nm)
        dma("pool", cm_t[:], cmat.rearrange("p (a f) -> p a f", a=5), (), [bc], "c13")
        identb = cm_t[:, 0, :]; ntri = cm_t[:, 1, :]; nones = cm_t[:, 2, :]; umat = cm_t[:, 3, :]; onesb = cm_t[:, 4, :]

        def rope_tables(st, pos_ap, P, n, cos_t, sin_t, tag):
            pi_ = sbt(st, "pi" + tag, [P, n], I32)
            pf = sbt(st, "pf" + tag, [P, n], F32)
            ang = sbt(st, "ang" + tag, [P, n, 16], F32)
            ki = sbt(st, "ki" + tag, [P, n, 16], I32)
            kf = sbt(st, "kf" + tag, [P, n, 16], F32)
            tm = sbt(st, "tm" + tag, [P, n, 16], F32)
            ivf = sbt(st, "ivf" + tag, [P, 16], F32)
            b = Buf("ropetmp" + tag)
            dma("sp", pi_[:], pos_ap, (), [b], "rp" + tag)
            dma("sp", ivf[:], invf[0:P, :], (), [b], "rp" + tag)
            cp("dve", pf[:], pi_[:], [b], [b])
            for c in range(n):
                ts("dve", ang[:, c, :], ivf[:], pf[:, c:c + 1], None, ALU.mult, None, [b], [b])

            def reduce_sin(dst, shift):
                a2 = ang[:]
                if shift != 0.0:
                    ts("dve", tm[:], ang[:], shift, None, ALU.add, None, [b], [b])
                    a2 = tm[:]
                ts("dve", kf[:], a2, 1.0 / TWO_PI, None, ALU.mult, None, [b], [b])
                cp("dve", ki[:], kf[:], [b], [b])
                cp("dve", kf[:], ki[:], [b], [b])
                stt(tm[:], kf[:], -TWO_PI, a2, ALU.mult, ALU.add, [b], [b])
                ts("dve", kf[:], tm[:], PI, -TWO_PI, ALU.is_gt, ALU.mult, [b], [b])
                tt("dve", tm[:], tm[:], kf[:], ALU.add, [b], [b])
                ts("dve", kf[:], tm[:], -PI, TWO_PI, ALU.is_lt, ALU.mult, [b], [b])
                tt("dve", tm[:], tm[:], kf[:], ALU.add, [b], [b])
                act(dst, tm[:], AF.Sin, [b], [b_rope])

            reduce_sin(sin_t[:], 0.0)
            reduce_sin(cos_t[:], PI / 2)

        with contextlib.ExitStack() as st:
            rope_tables(st, posk[:, :], 128, NKB, cosK, sinK, "k")
            rope_tables(st, posq[:, :], BS, NBLK, cosQ, sinQ, "q")
        em.barrier()

        def rope_apply(x1, x2, cos_ap, sin_ap, tmp, P, h, r, w):
            tt("dve", tmp[:P, 0, :h], x1, cos_ap, ALU.mult, r, w)
            tt("dve", tmp[:P, 1, :h], x2, sin_ap, ALU.mult, r, w)
            tt("dve", tmp[:P, 2, :h], x2, cos_ap, ALU.mult, r, w)
            tt("dve", tmp[:P, 3, :h], x1, sin_ap, ALU.mult, r, w)
            tt("dve", x1, tmp[:P, 0, :h], tmp[:P, 1, :h], ALU.subtract, r, w)
            tt("dve", x2, tmp[:P, 2, :h], tmp[:P, 3, :h], ALU.add, r, w)

        nrm_ctr = [0]

        def norm_T(xt_ap, P, bx, dst_fn, bdst, G_ap, sh_ap, tmps, pbank):
            junk, ssq, bt = tmps
            act(junk[:P, :], xt_ap, AF.Square, [bx], [bt], accum_out=ssq[:P, 0:1])
            act(ssq[:P, 1:2], ssq[:P, 0:1], AF.Sqrt, [bt], [bt], scale=1.0 / D, bias=1e-6)
            em.op("dve", lambda e: e.reciprocal(out=ssq[:P, 2:3], in_=ssq[:P, 1:2]), [bt], [bt])
            ts("dve", xt_ap, xt_ap, ssq[:P, 2:3], None, ALU.mult, None, [bx, bt], [bx])
            xs = None
            for k0 in range(0, KC, 4):
                nj = min(4, KC - k0)
                pbi = pbank[nrm_ctr[0] % len(pbank)]
                nrm_ctr[0] += 1
                for j in range(nj):
                    tr(pb[pbi][:, j * 128:j * 128 + P], xt_ap[:, (k0 + j) * 128:(k0 + j + 1) * 128],
                       identf_t[:P, :P], [bx, bc], [bpb[pbi]])
                for j in range(nj):
                    k = k0 + j
                    if j % 2 == 0:
                        act(dst_fn(k), pb[pbi][:, j * 128:j * 128 + P], AF.Identity, [bpb[pbi], b_vec], [bdst],
                            scale=G_ap[:, k:k + 1], bias=sh_ap[:, k:k + 1])
                    else:
                        ts("dve", dst_fn(k), pb[pbi][:, j * 128:j * 128 + P], G_ap[:, k:k + 1], sh_ap[:, k:k + 1],
                           ALU.mult, ALU.add, [bpb[pbi], b_vec], [bdst])

        winv = w_in.rearrange("(k p) n -> p k n", p=128)
        G1 = vec_t[:, 0, :]; SH1 = vec_t[:, 1, :]; G2 = vec_t[:, 2, :]; SH2 = vec_t[:, 3, :]
        with contextlib.ExitStack() as st:
            MG = 256
            NG = (6 * D) // MG
            NG1 = (2 * D) // MG
            wg = [sbt(st, "wg%d" % i, [128, KC, MG], BF16) for i in range(2)]
            bwg = [Buf("wg%d" % i) for i in range(2)]
            cb_t = sbt(st, "cb", [128, KC], F32); cs_t = sbt(st, "cs", [128, KC], BF16)
            brow = [sbt(st, "brow%d" % i, [1, MG], F32) for i in range(2)]
            mrow = [sbt(st, "mrow%d" % i, [1, MG], F32) for i in range(2)]
            bbr = [Buf("brow%d" % i) for i in range(2)]; bmr = [Buf("mrow%d" % i) for i in range(2)]
            bcs = Buf("cs"); bmodg = [Buf("modrow%d" % i) for i in range(NG)]
            mrows = sbt(st, "mrows", [KC, 4, 128], F32); bmrows = Buf("mrows")
            dma("sp", cb_t[:], cb[:, :], (), [bcs], "cb")
            act(cs_t[:], cb_t[:], AF.Silu, [bcs], [bcs])
            wav = w_ada.rearrange("(k p) n -> p k n", p=128)

            def mod_load(g):
                s2 = g % 2
                dma("pool", wg[s2][:], wav[:, :, g * MG:(g + 1) * MG], (), [bwg[s2]], "wg%d" % s2)
                dma("sp", brow[s2][:], b_ada[0:1, g * MG:(g + 1) * MG], (), [bbr[s2]], "brow%d" % s2)

            def mod_compute(g, pbi):
                s2 = g % 2
                for k in range(KC):
                    mm(pb[pbi][0:1, 0:MG], cs_t[:, k:k + 1], wg[s2][:, k, :], k == 0, k == KC - 1,
                       [bcs, bwg[s2]], [bpb[pbi]])
                tt("dve", mrow[s2][:], pb[pbi][0:1, 0:MG], brow[s2][:], ALU.add, [bpb[pbi], bbr[s2]], [bmr[s2]])
                dma("sp", modrow[0:1, g * MG:(g + 1) * MG], mrow[s2][:], [bmr[s2]], [bmodg[g]], "modw%d" % s2)

            def mod_vec(i_, a_, slot, pbi):
                gs = [bmodg[g] for g in range(a_ * D // MG, (a_ + 1) * D // MG)]
                dma("sp", mrows[:, i_, :], modrow[0:1, a_ * D:(a_ + 1) * D].rearrange("o (k p) -> (o k) p", p=128),
                    gs, [bmrows], "mv")
                tr(pb[pbi][:, i_ * KC:(i_ + 1) * KC], mrows[:, i_, :], identf_t[:KC, :KC], [bmrows, bc], [bpb[pbi]])
                cp("dve", vec_t[:, slot, :], pb[pbi][:, i_ * KC:(i_ + 1) * KC], [bpb[pbi]], [b_vec])

            WAW = 2 * SBW + 320
            wA = sbt(st, "wA", [128, KC, WAW], BF16); bwA = Buf("wA")
            mod_load(0)
            if NG1 > 1:
                mod_load(1)
            for g in range(NG1):
                mod_compute(g, 7)
                if g + 2 < NG1:
                    mod_load(g + 2)
            c0 = 0
            for (src0, n) in ((cf.o_ksb, SBW), (cf.o_vsb, SBW), (cf.o_kds, 256), (cf.o_kix, 64)):
                for a in range(0, n, 512):
                    m = min(512, n - a)
                    dma("pool", wA[:, :, c0 + a:c0 + a + m], winv[:, :, src0 + a:src0 + a + m], (), [bwA], "wA")
                c0 += n
            mod_vec(0, 0, 1, 6)
            mod_vec(1, 1, 6, 6)
            stt(vec_t[:, 0, :], vec_t[:, 6, :], 1.0, vec_t[:, 4, :], ALU.add, ALU.mult, [b_vec, bc], [b_vec])
            gnext = [NG1]
            if gnext[0] < NG:
                mod_load(gnext[0])
            if gnext[0] + 1 < NG:
                mod_load(gnext[0] + 1)

            def mod_step():
                g = gnext[0]
                if g >= NG:
                    return
                mod_compute(g, 7)
                if g + 2 < NG:
                    mod_load(g + 2)
                gnext[0] += 1

            xt = [sbt(st, "xt%d" % i, [128, D], F32) for i in range(2)]; bxt = [Buf("xt%d" % i) for i in range(2)]
            junk = sbt(st, "junkA", [128, D], BF16); ssq = sbt(st, "ssqA", [128, 4], F32)
            bt = Buf("nrmtmpA")
            hTa = [sbt(st, "hTa%d" % i, [128, KC, 512], BF16) for i in range(2)]; bhT = [Buf("hTa%d" % i) for i in range(2)]
            vst = [sbt(st, "vst%d" % i, [128, SBW], BF16) for i in range(2)]; bvst = [Buf("vst%d" % i) for i in range(2)]
            kst = [sbt(st, "kst%d" % i, [128, 512], BF16) for i in range(2)]; bkst = [Buf("kst%d" % i) for i in range(2)]
            sm = sbt(st, "smA", [128, 384], F32); bsm = Buf("smA")
            rtmp = sbt(st, "rtmpA", [128, 4, 16], F32)
            vsbv = vsb.rearrange("h p x -> p h x")
            kctr = 0
            per_blk = -(-(NG - NG1) // NKB)

            def prep(c):
                s2 = c % 2; ti = (c // 4) % 2; cc = c % 4
                dma("sp", xt[s2][:], xkv[c * 128:(c + 1) * 128, :], (), [bxt[s2]], "xt%d" % s2)
                norm_T(xt[s2][:], 128, bxt[s2], lambda k, ti=ti, cc=cc: hTa[ti][:, k, cc * 128:(cc + 1) * 128],
                       bhT[ti], G1, SH1, (junk, ssq, bt), [0, 1])

            prep(0)
            for c in range(NKB):
                s2 = c % 2
                ti = (c // 4) % 2
                cc = c % 4
                if c + 1 < NKB:
                    prep(c + 1)
                for g in range(0, SBW, 512):
                    pbi = 2 + (g // 512) % 2
                    n = min(512, SBW - g)
                    for k in range(KC):
                        mm(pb[pbi][:, 0:n], hTa[ti][:, k, cc * 128:(cc + 1) * 128], wA[:, k, SBW + g:SBW + g + n],
                           k == 0, k == KC - 1, [bhT[ti], bwA], [bpb[pbi]])
                    cp("act", vst[s2][:, g:g + n], pb[pbi][:, 0:n], [bpb[pbi]], [bvst[s2]])
                dma("sp", vsbv[:, :, c * 128:(c + 1) * 128], vst[s2][:].rearrange("p (h d) -> p h d", h=NHS),
                    [bvst[s2]], (), "vsbw%d" % s2)
                for k in range(KC):
                    mm(pb[4][:, 0:320], hTa[ti][:, k, cc * 128:(cc + 1) * 128], wA[:, k, 2 * SBW:2 * SBW + 320],
                       k == 0, k == KC - 1, [bhT[ti], bwA], [bpb[4]])
                cp("dve", sm[:, 0:320], pb[4][:, 0:320], [bpb[4]], [bsm])
                cp("dve", vds[:, c, :], sm[:, 128:256], [bsm], [b_vds])
                rope_apply(sm[:, 0:16], sm[:, 16:32], cosK[:, c, :], sinK[:, c, :], rtmp, 128, 16, [bsm, b_rope], [bsm])
                rope_apply(sm[:, 256:264], sm[:, 264:272], cosK[:, c, 0:16:2], sinK[:, c, 0:16:2], rtmp, 128, 8,
                           [bsm, b_rope], [bsm])
                cp("dve", sm[:, 320:384], sm[:, 256:320], [bsm], [bsm])
                tr(pb[5][:, 0:128], sm[:, 0:128], identf_t[:], [bsm, bc], [bpb[5]])
                tr(pb[5][:, 128:256], sm[:, 256:384], identf_t[:], [bsm, bc], [bpb[5]])
                cp("act", kTds[:, c * 128:(c + 1) * 128], pb[5][:, 0:128], [bpb[5]], [b_kTds])
                cp("act", kTix[:, c * 128:(c + 1) * 128], pb[5][:, 128:256], [bpb[5]], [b_kTix])
                if cc == 3:
                    t0 = (c // 4) * 512
                    for h in range(NHS):
                        pbi = 2 + h % 2
                        for k in range(KC):
                            mm(pb[pbi][:, :], wA[:, k, h * 128:(h + 1) * 128], hTa[ti][:, k, :], k == 0, k == KC - 1,
                               [bhT[ti], bwA], [bpb[pbi]])
                        ks = kctr % 2; kctr += 1
                        cp("dve", kst[ks][:], pb[pbi][:, :], [bpb[pbi]], [bkst[ks]])
                        dma("sp", kTsb[h, :, t0:t0 + 512], kst[ks][:], [bkst[ks]], (), "ktw%d" % ks)
                for _ in range(per_blk):
                    mod_step()
            while gnext[0] < NG:
                mod_step()
            mod_vec(2, 3, 3, 6)
            mod_vec(3, 4, 7, 6)
            stt(vec_t[:, 2, :], vec_t[:, 7, :], 1.0, vec_t[:, 5, :], ALU.add, ALU.mult, [b_vec, bc], [b_vec])
        em.barrier()
        chk("A")
        wov = w_out.rearrange("(k p) n -> p k n", p=128)
        h2v = h2Ts.rearrange("p (k t) -> p k t", k=KC)
        U8 = mybir.dt.uint8
        with contextlib.ExitStack() as st:
            hTt = sbt(st, "hTt", [128, KC, TW], BF16); bhTt = Buf("hTt")
            xA = [sbt(st, "xA%d" % i, [BS, D], F32) for i in range(2)]; bxA = [Buf("xA%d" % i) for i in range(2)]
            junk = sbt(st, "junkT", [BS, D], BF16); ssq = sbt(st, "ssqT", [BS, 4], F32); bt = Buf("nrmtmpT")
            wq = [sbt(st, "wq%d" % i, [128, KC, 256], BF16) for i in range(2)]; bwq = [Buf("wq%d" % i) for i in range(2)]
            qTsb = sbt(st, "qTsb", [128, NHS, TW], BF16); bqsb = Buf("qTsb")
            qTds = sbt(st, "qTds", [128, NHD, TW], BF16); bqds = Buf("qTds")
            qTix = sbt(st, "qTix", [128, IH // 2, TW], BF16); bqix = Buf("qTix")
            wix = sbt(st, "wix", [BS, TB, IH], F32); bwix = Buf("wix")
            qtm = sbt(st, "qtm", [BS, 256], F32); bqtm = Buf("qtm")
            rtmp = sbt(st, "rtmpT", [BS, 4, 16], F32)
            Mb = sbt(st, "Mb", [BS, TB, S], BF16); bMb = [Buf("Mb%d" % i) for i in range(TB)]
            SCRB = max(S * 4 + 8192, NKB * TW * 2, TB * D * 4)
            scr = sbt(st, "scr", [128, SCRB], U8)
            kvh = sbt(st, "kvh", [128, 4 * S], U8)
            kTh = kvh[:, 0:2 * S].bitcast(BF16); bkTh = Buf("kTh")
            vh = kvh[:, 2 * S:4 * S].bitcast(BF16); bvh = Buf("vh")
            scoreb = [scr[:BS, 0:S * 4].bitcast(F32), kvh[:BS, 0:S * 4].bitcast(F32)]
            bscb = [Buf("score0"), Buf("score1")]
            relb = [scr[:BS, S * 4 + 4096 * i:S * 4 + 4096 * i + 2048].bitcast(BF16) for i in range(2)]
            brel = [Buf("rel%d" % i) for i in range(2)]
            biasT = [scr[:BS, S * 4 + 4096 * i + 2048:S * 4 + 4096 * (i + 1)].bitcast(F32) for i in range(2)]
            bbias = [Buf("biasT%d" % i) for i in range(2)]
            diagw = sbt(st, "diagw", [BS, IH, BS], BF16); bdiag = Buf("diagw")
            Yt = scr[:, 0:NKB * TW * 2].bitcast(BF16); bY = Buf("Yt")
            xqb = [scr[:BS, D * 4 * i:D * 4 * (i + 1)].bitcast(F32) for i in range(TB)]; bxq = [Buf("xqb%d" % i) for i in range(TB)]
            bis = [sbt(st, "bis%d" % i, [BS, 8 + NSP], F32) for i in range(2)]; bbis = [Buf("bis%d" % i) for i in range(2)]
            dtab = [sbt(st, "dtab%d" % i, [BS, NBIS + 1], F32) for i in range(2)]
            ndtab = [sbt(st, "ndtab%d" % i, [BS, NBIS + 1], F32) for i in range(2)]
            mrg = sbt(st, "mrg", [128, NM, TW], BF16); bmrg = [Buf("mrg%d" % i) for i in range(NM)]
            ytmp = sbt(st, "ytmp", [128, TW], F32); bytmp = Buf("ytmp")
            qrow_t = sbt(st, "qrow", [128, TW], F32); bqrow = Buf("qrow")
            ebuf = [sbt(st, "ebuf%d" % i, [128, TW], F32) for i in range(2)]; beb = [Buf("ebuf%d" % i) for i in range(2)]
            spb = [sbt(st, "spb%d" % i, [128, TW], BF16) for i in range(2)]; bspb = [Buf("spb%d" % i) for i in range(2)]
            Ab = [sbt(st, "Ab%d" % i, [128, TW], BF16) for i in range(2)]; bAb = [Buf("Ab%d" % i) for i in range(2)]
            wb = [sbt(st, "wb%d" % i, [128, TW], BF16) for i in range(2)]; bwb = [Buf("wb%d" % i) for i in range(2)]
            pbuf = [sbt(st, "pbuf%d" % i, [128, TW], BF16) for i in range(2)]; bpbuf = [Buf("pbuf%d" % i) for i in range(2)]
            rden = sbt(st, "rden", [128, TW], F32); brden = Buf("rden")
            g1t = [sbt(st, "g1t%d" % i, [BS, 256], F32) for i in range(2)]; bg1 = [Buf("g1t%d" % i) for i in range(2)]
            gsq = sbt(st, "gsq", [128, TW], BF16); bgsq = Buf("gsq")
            grs = sbt(st, "grs", [128, 2, TW], F32); bgrs = Buf("grs")
            h2st = sbt(st, "h2st", [128, KC, BS], BF16); bh2st = Buf("h2st")
            wqctr = [0]

            def wq_load(src_v, col0, n):
                s = wqctr[0] % 2; wqctr[0] += 1
                dma("pool", wq[s][:, :, 0:n], src_v[:, :, col0:col0 + n], (), [bwq[s]], "wq%d" % s)
                return s

            for t in range(NT):
                tc0 = t * TW
                for bi in range(TB):
                    blk = t * TB + bi
                    xa = blk % 2
                    dma("sp", xA[xa][:], xq[blk * BS:(blk + 1) * BS, :], (), [bxA[xa]], "xA%d" % xa)
                    norm_T(xA[xa][:], BS, bxA[xa], lambda k, bi=bi: hTt[:, k, bi * BS:(bi + 1) * BS], bhTt,
                           G1, SH1, (junk, ssq, bt), [0, 1])
                dma("sp", qrow_t[:], qrow[:, tc0:tc0 + TW], (), [bqrow], "qrow")
                chk("T1")
                for g in range(0, SBW, 256):
                    s = wq_load(winv, cf.o_qsb + g, 256)
                    for hh in range(2):
                        h = g // 128 + hh
                        pbi = 2 + h % 2
                        for k in range(KC):
                            mm(pb[pbi][:, 0:TW], wq[s][:, k, hh * 128:(hh + 1) * 128], hTt[:, k, :], k == 0, k == KC - 1,
                               [bwq[s], bhTt], [bpb[pbi]])
                        act(qTsb[:, h, :], pb[pbi][:, 0:TW], AF.Copy, [bpb[pbi]], [bqsb], scale=cf.qk_scale)
                for g in range(0, DSW, 256):
                    s = wq_load(winv, cf.o_qds + g, 256)
                    for bi in range(TB):
                        blk = t * TB + bi
                        for k in range(KC):
                            mm(pb[4][:BS, 0:256], hTt[:, k, bi * BS:(bi + 1) * BS], wq[s][:, k, :], k == 0, k == KC - 1,
                               [bwq[s], bhTt], [bpb[4]])
                        cp("act", qtm[:, :], pb[4][:BS, 0:256], [bpb[4]], [bqtm])
                        for hh in range(2):
                            o_ = hh * 128
                            rope_apply(qtm[:, o_:o_ + 16], qtm[:, o_ + 16:o_ + 32], cosQ[:, blk, :], sinQ[:, blk, :],
                                       rtmp, BS, 16, [bqtm, b_rope], [bqtm])
                        for hh in range(2):
                            tr(pb[5][:, hh * 128:hh * 128 + BS], qtm[:, hh * 128:(hh + 1) * 128], identf_t[:BS, :BS],
                               [bqtm, bc], [bpb[5]])
                        for hh in range(2):
                            h = g // 128 + hh
                            act(qTds[:, h, bi * BS:(bi + 1) * BS], pb[5][:, hh * 128:hh * 128 + BS], AF.Copy,
                                [bpb[5]], [bqds], scale=cf.qk_scale)
                for g in range(0, IH * ID, 256):
                    s = wq_load(winv, cf.o_qix + g, 256)
                    for bi in range(TB):
                        blk = t * TB + bi
                        for k in range(KC):
                            mm(pb[4][:BS, 0:256], hTt[:, k, bi * BS:(bi + 1) * BS], wq[s][:, k, :], k == 0, k == KC - 1,
                               [bwq[s], bhTt], [bpb[4]])
                        cp("act", qtm[:, :], pb[4][:BS, 0:256], [bpb[4]], [bqtm])
                        for hh in range(256 // ID):
                            o_ = hh * ID
                            rope_apply(qtm[:, o_:o_ + 8], qtm[:, o_ + 8:o_ + 16], cosQ[:, blk, 0:16:2], sinQ[:, blk, 0:16:2],
                                       rtmp, BS, 8, [bqtm, b_rope], [bqtm])
                        for pp in range(2):
                            tr(pb[5][:, pp * 128:pp * 128 + BS], qtm[:, pp * 128:(pp + 1) * 128], identf_t[:BS, :BS],
                               [bqtm, bc], [bpb[5]])
                        for pp in range(2):
                            cp("act", qTix[:, g // 128 + pp, bi * BS:(bi + 1) * BS], pb[5][:, pp * 128:pp * 128 + BS],
                               [bpb[5]], [bqix])
                s = wq_load(winv, cf.o_wix, IH)
                for bi in range(TB):
                    for k in range(KC):
                        mm(pb[4][:BS, 0:IH], hTt[:, k, bi * BS:(bi + 1) * BS], wq[s][:, k, 0:IH], k == 0, k == KC - 1,
                           [bwq[s], bhTt], [bpb[4]])
                    act(wix[:, bi, :], pb[4][:BS, 0:IH], AF.Copy, [bpb[4]], [bwix], scale=cf.idx_scale)

                chk("T2")
                def idx_block(bi):
                    blk = t * TB + bi
                    sb_ = bi % 2; sc = scoreb[sb_]; bs_ = bscb[sb_]; B_ = bis[sb_]; bB = bbis[sb_]
                    for h in range(IH):
                        ts("pool", diagw[:, h, :], identb[:BS, :BS], wix[:, bi, h:h + 1], None, ALU.mult, None,
                           [bc, bwix], [bdiag])
                    gctr = 0; spctr = 0
                    for sp_ in range(0, S, 1024):
                        nsp = min(1024, S - sp_); nb = nsp // 512
                        ab = 4 + 2 * (spctr % 2); spctr += 1
                        for h in range(IH):
                            pg = (gctr % 2) * 2; gctr += 1
                            po = (h % 2) * 64
                            for q_ in range(nb):
                                mm(pb[pg + q_][:BS, :], qTix[po:po + 64, h // 2, bi * BS:(bi + 1) * BS],
                                   kTix[po:po + 64, sp_ + q_ * 512:sp_ + (q_ + 1) * 512], True, True,
                                   [bqix, b_kTix], [bpb[pg + q_]])
                            rs = h % 2
                            em.op("dve", lambda e, rs=rs, pg=pg, nb=nb, nsp=nsp: e.tensor_scalar(
                                out=relb[rs][:, 0:nsp].rearrange("p (a f) -> p a f", a=nb), in0=PS[:BS, pg:pg + nb, :],
                                scalar1=0.0, scalar2=None, op0=ALU.max),
                                [bpb[pg + q_] for q_ in range(nb)], [brel[rs]])
                            for q_ in range(nb):
                                mm(pb[ab + q_][:BS, :], diagw[:, h, :], relb[rs][:, q_ * 512:(q_ + 1) * 512], h == 0, h == IH - 1,
                                   [bdiag, brel[rs]], [bpb[ab + q_]])
                        for q_ in range(nb):
                            kt = sp_ // 512 + q_
                            em.op("dve", lambda e, kt=kt, ab=ab, q_=q_, B_=B_: e.tensor_reduce(
                                out=B_[:, 8 + kt:9 + kt], in_=pb[ab + q_][:BS, :], axis=AX.X, op=ALU.min),
                                [bpb[ab + q_]], [bB])
                            ts("dve", biasT[q_ % 2], iota_t[:BS, :], qrel_t[:, blk, kt:kt + 1], -1e30, ALU.is_gt, ALU.mult,
                               [bc], [bbias[q_ % 2]])
                            tt("dve", sc[:, kt * 512:(kt + 1) * 512], pb[ab + q_][:BS, :], biasT[q_ % 2], ALU.add,
                               [bpb[ab + q_], bbias[q_ % 2]], [bs_])
                    em.op("dve", lambda e: e.tensor_reduce(out=B_[:, 0:1], in_=sc, axis=AX.X, op=ALU.max), [bs_], [bB])
                    em.op("dve", lambda e: e.tensor_reduce(out=B_[:, 1:2], in_=B_[:, 8:8 + NSP], axis=AX.X, op=ALU.min), [bB], [bB])
                    stt(B_[:, 6:7], B_[:, 0:1], 2.0, B_[:, 1:2], ALU.add, ALU.subtract, [bB], [bB])
                    ts("dve", dtab[sb_][:], pow2_t[:BS, :], B_[:, 6:7], None, ALU.mult, None, [bB, bc], [bB])
                    ts("dve", ndtab[sb_][:], dtab[sb_][:], -1.0, None, ALU.mult, None, [bB], [bB])
                    ts("dve", B_[:, 2:3], B_[:, 1:2], -1.0, 1.0, ALU.mult, ALU.add, [bB], [bB])
                    tt("dve", B_[:, 2:3], B_[:, 2:3], dtab[sb_][:, 0:1], ALU.subtract, [bB], [bB])

                def bisect(bi):
                    sb_ = bi % 2; sc = scoreb[sb_]; bs_ = bscb[sb_]; B_ = bis[sb_]; bB = bbis[sb_]
                    for r_ in range(NBIS):
                        act(Mb[:, bi, :], sc, AF.Sign, [bs_, bB], [bMb[bi], bB], bias=B_[:, 2:3], accum_out=B_[:, 3:4])
                        act(B_[:, 4:5], B_[:, 3:4], AF.Sign, [bB], [bB], bias=float(S - 2 * cf.TOPK) + 0.5)
                        act(B_[:, 2:3], B_[:, 4:5], AF.Identity, [bB], [bB], scale=ndtab[sb_][:, r_ + 1:r_ + 2], bias=B_[:, 2:3])

                def finalize(bi):
                    sb_ = bi % 2; sc = scoreb[sb_]; bs_ = bscb[sb_]; B_ = bis[sb_]; bB = bbis[sb_]
                    stt(B_[:, 5:6], B_[:, 2:3], -1.0, dtab[sb_][:, NBIS:NBIS + 1], ALU.mult, ALU.subtract, [bB], [bB])
                    ts("dve", Mb[:, bi, :], sc, B_[:, 5:6], NEGB, ALU.is_le, ALU.mult, [bs_, bB], [bMb[bi]])

                idx_block(0)
                for bi in range(TB):
                    bisect(bi)
                    if bi + 1 < TB:
                        idx_block(bi + 1)
                    finalize(bi)

                chk("T3")
                for h in range(NHD):
                    def S_step(c):
                        pz = c % 2
                        mm(pb[pz][:, 0:TW], kTds[:, c * 128:(c + 1) * 128], qTds[:, h, :], True, False,
                           [b_kTds, bqds], [bpb[pz]])
                        for bi in range(TB):
                            mm(pb[pz][:, bi * BS:(bi + 1) * BS], Mb[:, bi, c * 128:(c + 1) * 128], identb[:BS, :BS],
                               False, bi == TB - 1, [bMb[bi], bc], [bpb[pz]])
                        act(pbuf[pz][:], pb[pz][:, 0:TW], AF.Exp, [bpb[pz]], [bpbuf[pz]])

                    def PV_step(c):
                        pz = c % 2
                        mm(pb[2][:, 0:TW], vds[:, c, :], pbuf[pz][:], c == 0, c == NKB - 1, [b_vds, bpbuf[pz]], [bpb[2]])
                        mm(pb[3][:, 0:TW], onesb, pbuf[pz][:], c == 0, c == NKB - 1, [bc, bpbuf[pz]], [bpb[3]])

                    S_step(0)
                    for c in range(NKB):
                        if c + 1 < NKB:
                            S_step(c + 1)
                        PV_step(c)
                    em.op("dve", lambda e: e.reciprocal(out=rden[:], in_=pb[3][:, 0:TW]), [bpb[3]], [brden])
                    tt("dve", mrg[:, NHS + h, :], pb[2][:, 0:TW], rden[:], ALU.mult, [bpb[2], brden], [bmrg[NHS + h]])

                chk("T4")
                em.barrier()
                for c in range(NKB):
                    ts("dve", ytmp[:], qrow_t[:], float(-128 * c), 0.0, ALU.add, ALU.max, [bqrow], [bytmp])
                    ts("dve", Yt[:, c * TW:(c + 1) * TW], ytmp[:], kcol_t[:, 0:1], None, ALU.is_equal, None, [bytmp, bc], [bY])
                for h in range(NHS):
                    dma("sp", kTh, kTsb[h, :, :], (), [bkTh], "kTh")
                    dma("sp", vh, vsb[h, :, :], (), [bvh], "vh")
                    order = list(range(NKB - 1, -1, -1))

                    def Z_step(i):
                        c = order[i]; pz = 4 + i % 2
                        mm(pb[pz][:, 0:TW], kTh[:, c * 128:(c + 1) * 128], qTsb[:, h, :], True, False,
                           [bkTh, bqsb], [bpb[pz]])
                        mm(pb[pz][:, 0:TW], umat, Yt[:, c * TW:(c + 1) * TW], False, True, [bc, bY], [bpb[pz]])
                        act(ebuf[i % 2][:], pb[pz][:, 0:TW], AF.Exp, [bpb[pz]], [beb[i % 2]])
                        act(spb[i % 2][:], ebuf[i % 2][:], AF.Ln, [beb[i % 2]], [bspb[i % 2]], bias=1.0)

                    def X_step(i):
                        c = order[i]; px = 6 + i % 2
                        mm(pb[px][:, 0:TW], kTh[:, c * 128:(c + 1) * 128], qTsb[:, h, :], True, False,
                           [bkTh, bqsb], [bpb[px]])
                        mm(pb[px][:, 0:TW], umat, Yt[:, c * TW:(c + 1) * TW], False, False, [bc, bY], [bpb[px]])
                        if i > 0:
                            mm(pb[px][:, 0:TW], nones, Ab[i % 2][:], False, False, [bc, bAb[i % 2]], [bpb[px]])
                        mm(pb[px][:, 0:TW], ntri, spb[i % 2][:], False, True, [bc, bspb[i % 2]], [bpb[px]])
                        if i == 0:
                            cp("pool", Ab[1][:], spb[0][:], [bspb[0]], [bAb[1]])
                        elif i + 1 < NKB:
                            tt("pool", Ab[(i + 1) % 2][:], Ab[i % 2][:], spb[i % 2][:], ALU.add,
                               [bAb[i % 2], bspb[i % 2]], [bAb[(i + 1) % 2]])
                        act(wb[i % 2][:], pb[px][:, 0:TW], AF.Exp, [bpb[px]], [bwb[i % 2]])

                    def PVs(i):
                        c = order[i]
                        mm(pb[3][:, 0:TW], vh[:, c * 128:(c + 1) * 128], wb[i % 2][:], i == 0, i == NKB - 1, [bvh, bwb[i % 2]], [bpb[3]])

                    Z_step(0)
                    for i in range(NKB):
                        if i + 1 < NKB:
                            Z_step(i + 1)
                        X_step(i)
                        if i > 0:
                            PVs(i - 1)
                    PVs(NKB - 1)
                    cp("dve", mrg[:, h, :], pb[3][:, 0:TW], [bpb[3]], [bmrg[h]])

                chk("T5")
                for gi, (h0, nh_, g_t, W_) in enumerate(((0, NHS, sbg_t, SBW), (NHS, NHD, dsg_t, DSW))):
                    for hh in range(nh_):
                        act(gsq[:], mrg[:, h0 + hh, :], AF.Square, [bmrg[h0 + hh]], [bgsq])
                        mm(pb[0][:, 0:TW], onesb, gsq[:], hh == 0, hh == nh_ - 1, [bc, bgsq], [bpb[0]])
                    act(grs[:, 0, :], pb[0][:, 0:TW], AF.Sqrt, [bpb[0]], [bgrs], scale=1.0 / W_, bias=1e-6)
                    em.op("dve", lambda e: e.reciprocal(out=grs[:, 1, :], in_=grs[:, 0, :]), [bgrs], [bgrs])
                    for hh in range(nh_):
                        stt(mrg[:, h0 + hh, :], mrg[:, h0 + hh, :], g_t[:, hh:hh + 1], grs[:, 1, :], ALU.mult, ALU.mult,
                            [bmrg[h0 + hh], bgrs, bc], [bmrg[h0 + hh]])

                chk("T6")
                em.barrier()
                for bi in range(TB):
                    blk = t * TB + bi
                    dma("sp", xqb[bi], xq[blk * BS:(blk + 1) * BS, :], (), [bxq[bi]], "xqb%d" % bi)
                for gi, g in enumerate(range(0, D, 256)):
                    s = wq_load(wov, g, 256)
                    gs = gi % 2
                    dma("sp", g1t[gs][:], modrow[0:1, 2 * D + g:2 * D + g + 256].partition_broadcast(BS), (), [bg1[gs]], "g1t%d" % gs)
                    for bi in range(TB):
                        pbi = bi % 2
                        for k in range(NM):
                            mm(pb[pbi][:BS, 0:256], mrg[:, k, bi * BS:(bi + 1) * BS], wq[s][:, k, :], k == 0, k == NM - 1,
                               bmrg + [bwq[s]], [bpb[pbi]])
                        tt("dve", qtm[:, :], pb[pbi][:BS, 0:256], g1t[gs][:], ALU.mult, [bpb[pbi], bg1[gs]], [bqtm])
                        tt("dve", xqb[bi][:, g:g + 256], xqb[bi][:, g:g + 256], qtm[:, :], ALU.add, [bqtm, bxq[bi]], [bxq[bi]])
                for bi in range(TB):
                    blk = t * TB + bi
                    dma("sp", xmid[blk * BS:(blk + 1) * BS, :], xqb[bi], [bxq[bi]], (), "xmidw%d" % bi)
                    norm_T(xqb[bi], BS, bxq[bi], lambda k: h2st[:, k, :], bh2st, G2, SH2, (junk, ssq, bt), [2, 3])
                    dma("sp", h2v[:, :, blk * BS:(blk + 1) * BS], h2st[:], [bh2st], (), "h2w")
                em.barrier()
        em.barrier()

        chk("T")
        wuv = w_up.rearrange("(k p) n -> p k n", p=128)
        wdv = w_down.rearrange("(k p) n -> p k n", p=128)
        with contextlib.ExitStack() as st:
            mT = sbt(st, "mT", [128, FC, OWN], BF16); bmT = Buf("mT")
            h2T = sbt(st, "h2T", [128, KC, TQ], BF16); b_h2T = Buf("h2T")
            dma("sp", h2T[:], h2Ts.rearrange("p (k t) -> p k t", k=KC), (), [b_h2T], "h2l")
            with contextlib.ExitStack() as st2:
                wu = [sbt(st2, "wu%d" % i, [128, KC, 256], BF16) for i in range(3)]; bwu = [Buf("wu%d" % i) for i in range(3)]
                usb = [sbt(st2, "usb%d" % i, [128, TQ], F32) for i in range(2)]; busb = [Buf("usb%d" % i) for i in range(2)]
                ucg = sbt(st2, "ucg", [128, OWN], F32); bucg = Buf("ucg")
                ucv = sbt(st2, "ucv", [128, OWN], F32); bucv = Buf("ucv")
                for f in range(FC):
                    s = f % 3
                    dma("pool", wu[s][:, :, 0:128], wuv[:, :, f * 128:(f + 1) * 128], (), [bwu[s]], "wu%d" % s)
                    dma("pool", wu[s][:, :, 128:256], wuv[:, :, DFF + f * 128:DFF + (f + 1) * 128], (), [bwu[s]], "wu%d" % s)
                    for half in range(2):
                        ci = f + half * FC
                        ub = usb[half]; bub = busb[half]
                        for t in range(NT):
                            pbi = (2 * t + half) % 4
                            for k in range(KC):
                                mm(pb[pbi][:, 0:TW], wu[s][:, k, half * 128:(half + 1) * 128], h2T[:, k, t * TW:(t + 1) * TW],
                                   k == 0, k == KC - 1, [bwu[s], b_h2T], [bpb[pbi]])
                            cp("act", ub[:, t * TW:(t + 1) * TW], pb[pbi][:, 0:TW], [bpb[pbi]], [bub])
                        ts("dve", ub[:, 0:2], ub[:, 0:2], flag_t[:, 0:1], None, ALU.mult, None, [bub, bc], [bub])
                        uc = ucg if half == 0 else ucv
                        buc = bucg if half == 0 else bucv
                        act(uc[:], ub[:, 2:TQ], AF.Identity, [bub, bc], [buc], scale=cvw_t[:, 2, ci:ci + 1], bias=cvb_t[:, ci:ci + 1])
                        stt(uc[:], ub[:, 1:TQ - 1], cvw_t[:, 1, ci:ci + 1], uc[:], ALU.mult, ALU.add, [bub, bc, buc], [buc])
                        stt(uc[:], ub[:, 0:TQ - 2], cvw_t[:, 0, ci:ci + 1], uc[:], ALU.mult, ALU.add, [bub, bc, buc], [buc])
                    act(ucg[:], ucg[:], AF.Silu, [bucg], [bucg])
                    tt("dve", mT[:, f, :], ucg[:], ucv[:], ALU.mult, [bucg, bucv], [bmT])
            em.barrier()
            with contextlib.ExitStack() as st2:
                wd = [sbt(st2, "wd%d" % i, [128, FC, 256], BF16) for i in range(2)]; bwd = [Buf("wd%d" % i) for i in range(2)]
                yst = [sbt(st2, "yst%d" % i, [128, 256], F32) for i in range(2)]; byst = [Buf("yst%d" % i) for i in range(2)]
                yc = 0
                for gi, g in enumerate(range(0, D, 256)):
                    s = gi % 2
                    for f0 in range(0, FC, 16):
                        f1 = min(FC, f0 + 16)
                        dma("pool", wd[s][:, f0:f1, :], wdv[:, f0:f1, g:g + 256], (), [bwd[s]], "wd%d" % s)
                    for tb in range(NOB):
                        pbi = tb % 2
                        for f in range(FC):
                            mm(pb[pbi][:, 0:256], mT[:, f, tb * 128:(tb + 1) * 128], wd[s][:, f, :], f == 0, f == FC - 1,
                               [bmT, bwd[s]], [bpb[pbi]])
                        ys = yc % 2; yc += 1
                        cp("act", yst[ys][:], pb[pbi][:, 0:256], [bpb[pbi]], [byst[ys]])
                        dma("sp", yscr[tb * 128:(tb + 1) * 128, g:g + 256], yst[ys][:], [byst[ys]], (), "yscrw%d" % ys)
        em.barrier()
        with contextlib.ExitStack() as st:
            g2bc = sbt(st, "g2bc", [128, D], F32); fngt = sbt(st, "fngt", [128, D], F32); bgg = Buf("g2fng")
            xm = [sbt(st, "xm%d" % i, [128, D], F32) for i in range(2)]; bxm = [Buf("xm%d" % i) for i in range(2)]
            yy = [sbt(st, "yy%d" % i, [128, D], F32) for i in range(2)]; byy = [Buf("yy%d" % i) for i in range(2)]
            junk = sbt(st, "junkF", [128, D], BF16); ssq = sbt(st, "ssqF", [128, 4], F32); bt = Buf("nrmF")
            dma("sp", g2bc[:], modrow[0:1, 5 * D:6 * D].partition_broadcast(128), (), [bgg], "g2bc")
            dma("sp", fngt[:], fng[:, :], (), [bgg], "g2bc")
            for tb in range(NOB):
                s = tb % 2
                dma("sp", xm[s][:], xmid[2 + tb * 128:2 + (tb + 1) * 128, :], (), [bxm[s]], "xm%d" % s)
                dma("sp", yy[s][:], yscr[tb * 128:(tb + 1) * 128, :], (), [byy[s]], "yy%d" % s)
                tt("dve", yy[s][:], yy[s][:], g2bc[:], ALU.mult, [byy[s], bgg], [byy[s]])
                tt("dve", xm[s][:], xm[s][:], yy[s][:], ALU.add, [byy[s], bxm[s]], [bxm[s]])
                act(junk[:], xm[s][:], AF.Square, [bxm[s]], [bt], accum_out=ssq[:, 0:1])
                act(ssq[:, 1:2], ssq[:, 0:1], AF.Sqrt, [bt], [bt], scale=1.0 / D, bias=1e-6)
                em.op("dve", lambda e: e.reciprocal(out=ssq[:, 2:3], in_=ssq[:, 1:2]), [bt], [bt])
                stt(yy[s][:], xm[s][:], ssq[:, 2:3], fngt[:], ALU.mult, ALU.mult, [bxm[s], bt, bgg], [byy[s]])
                dma("sp", out[tb * 128:(tb + 1) * 128, :], yy[s][:], [byy[s]], (), "outw%d" % s)
        em.emit(final_waits=["outw0", "outw1"] if NOB > 1 else ["outw0"])
    return nc


def host_inputs(cf, inp, core):
    D, S, KC, NKB, TQ, BS, NBLK, NSP, FC = cf.D, cf.S, cf.KC, cf.NKB, cf.TQ, cf.BS, cf.NBLK, cf.NSP, cf.FC
    b = core // cf.CPB; j = core % cf.CPB
    f32 = np.float32
    x = np.asarray(inp["x"], f32); pos = np.asarray(inp["positions"], np.int32)
    t0 = j * cf.OWN - 2
    tok = np.arange(t0, t0 + TQ)
    tokc = np.maximum(tok, 0)
    m = {}
    m["xkv"] = np.ascontiguousarray(x[b])
    m["xq"] = np.ascontiguousarray(x[b][tokc])
    m["posk"] = np.ascontiguousarray(pos[b].reshape(NKB, 128).T)
    m["posq"] = np.ascontiguousarray(pos[b][tokc].reshape(NBLK, BS).T)
    qc = tokc.astype(f32).reshape(NBLK, BS).T
    m["qcol"] = np.ascontiguousarray(qc)
    qr = qc[:, :, None] - (512.0 * np.arange(NSP, dtype=f32))[None, None, :]
    m["qrel"] = np.ascontiguousarray(qr.reshape(BS, NBLK * NSP).astype(f32))
    m["qrow"] = np.ascontiguousarray(np.broadcast_to(tokc.astype(f32)[None, :], (128, TQ)))
    pk = lambda v: np.ascontiguousarray(np.asarray(v, f32).reshape(-1, 128).T)
    m["cb"] = pk(np.asarray(inp["c"], f32)[b])
    m["w_ada"] = np.ascontiguousarray(np.asarray(inp["w_ada"], f32)[0])
    m["b_ada"] = np.ascontiguousarray(np.asarray(inp["b_ada"], f32)[0][None, :])
    m["w_in"] = np.ascontiguousarray(np.asarray(inp["w_in"], f32)[0])
    m["w_out"] = np.ascontiguousarray(np.asarray(inp["w_out"], f32)[0])
    m["w_up"] = np.ascontiguousarray(np.asarray(inp["w_up"], f32)[0])
    m["w_down"] = np.ascontiguousarray(np.asarray(inp["w_down"], f32)[0])
    m["n1g"] = pk(np.asarray(inp["norm1_g"])[0]); m["n2g"] = pk(np.asarray(inp["norm2_g"])[0])
    m["sbg"] = pk(np.asarray(inp["sb_norm_g"])[0]); m["dsg"] = pk(np.asarray(inp["dsa_norm_g"])[0])
    m["fng"] = np.ascontiguousarray(np.broadcast_to(np.asarray(inp["final_norm_g"], f32)[None, :], (128, D)))
    cw = np.asarray(inp["conv_w"], f32)[0]
    m["convw"] = np.ascontiguousarray(np.stack([pk(cw[i]) for i in range(3)], axis=1).reshape(128, 3 * 2 * FC))
    m["convb"] = pk(np.asarray(inp["conv_b"], f32)[0])
    m["identf"] = np.eye(128, dtype=f32)
    jj = np.arange(128)[:, None]; ss = np.arange(128)[None, :]
    cm = np.stack([np.eye(128, dtype=f32), -(jj >= ss).astype(f32), -np.ones((128, 128), f32),
                   NEGB * (ss >= jj).astype(f32), np.ones((128, 128), f32)], axis=1)
    m["cmat"] = np.ascontiguousarray(cm.reshape(128, 5 * 128))
    m["iota"] = np.ascontiguousarray(np.broadcast_to(np.arange(512, dtype=f32)[None, :], (128, 512)))
    m["kcolc"] = np.arange(128, dtype=f32)[:, None].copy()
    m["pow2"] = np.ascontiguousarray(np.broadcast_to((0.5 ** np.arange(1, cf.NBIS + 2)).astype(f32)[None, :], (128, cf.NBIS + 1)))
    ivf = (np.float32(500000.0) ** (-(np.arange(16, dtype=f32) / np.float32(16)))).astype(f32)
    m["invf"] = np.ascontiguousarray(np.broadcast_to(ivf[None, :], (128, 16)))
    m["flag"] = np.full((128, 1), 0.0 if j == 0 else 1.0, f32)
    return m


_CACHE = {}


def kernel(x, c, positions, w_ada, b_ada, norm1_g, w_in, sb_norm_g, dsa_norm_g, w_out, norm2_g,
           w_up, conv_w, conv_b, w_down, final_norm_g):
    inp = dict(x=x, c=c, positions=positions, w_ada=w_ada, b_ada=b_ada, norm1_g=norm1_g, w_in=w_in,
               sb_norm_g=sb_norm_g, dsa_norm_g=dsa_norm_g, w_out=w_out, norm2_g=norm2_g, w_up=w_up,
               conv_w=conv_w, conv_b=conv_b, w_down=w_down, final_norm_g=final_norm_g)
    cf = Cfg()
    nc = build_program(cf)
    in_maps = [host_inputs(cf, inp, core) for core in range(8)]
    res = run_bass_kernel_spmd(nc, in_maps, core_ids=list(range(8)))
    outp = np.zeros((2, cf.S, cf.D), np.float32)
    for core in range(8):
        b = core // cf.CPB; j = core % cf.CPB
        outp[b, j * cf.OWN:(j + 1) * cf.OWN, :] = np.asarray(res.results[core]["out"], np.float32)
    return outp
```

```python
import contextlib
import numpy as np
import concourse.bass as bass
import concourse.mybir as mybir
from concourse.bass_utils import run_bass_kernel_spmd

F32 = mybir.dt.float32
BF16 = mybir.dt.bfloat16
I32 = mybir.dt.int32
AF = mybir.ActivationFunctionType
ALU = mybir.AluOpType
AX = mybir.AxisListType

ENGS = ("pe", "act", "dve", "pool", "sp")


class Buf:
    __slots__ = ("name", "lw", "rs")

    def __init__(self, name):
        self.name = name
        self.lw = None
        self.rs = []


class Op:
    __slots__ = ("eng", "idx", "fn", "deps", "dma", "stream", "gen", "flag", "incidx")

    def __init__(self, eng, idx, fn, dma, stream):
        self.eng = eng; self.idx = idx; self.fn = fn; self.deps = []
        self.dma = dma; self.stream = stream; self.gen = 0
        self.flag = False; self.incidx = 0


class Em:
    def __init__(self, nc):
        self.nc = nc
        self.ops = {e: [] for e in ENGS}
        self.streams = {}
        self.last_dma = {}
        self.pending = {e: None for e in ENGS}
        self.pool_dmas = []

    def barrier(self):
        lasts = [self.ops[e][-1] for e in ENGS if self.ops[e]]
        lasts += list(self.last_dma.values())
        for e in ENGS:
            self.pending[e] = lasts

    def op(self, eng, fn, reads=(), writes=(), dma=False, stream=None):
        o = Op(eng, len(self.ops[eng]), fn, dma, stream)
        deps = []
        if self.pending[eng] is not None:
            deps.extend(self.pending[eng])
            self.pending[eng] = None
        if dma and eng == "pool":
            if len(self.pool_dmas) >= 4:
                deps.append(self.pool_dmas[-4])
            self.pool_dmas.append(o)
        for b in reads:
            if b.lw is not None:
                deps.append(b.lw)
        for b in writes:
            if b.lw is not None:
                deps.append(b.lw)
            deps.extend(b.rs)
        for b in reads:
            b.rs.append(o)
        for b in writes:
            b.lw = o
            b.rs = []
        if dma:
            g = self.streams.get(stream, 0) + 1
            self.streams[stream] = g
            o.gen = g
            self.last_dma[stream] = o
        best = {}
        for d in deps:
            if d is o:
                continue
            if d.dma:
                key = ("dma", d.stream)
                if key not in best or best[key].gen < d.gen:
                    best[key] = d
            else:
                if d.eng == eng and eng == "pe":
                    continue
                key = ("eng", d.eng)
                if key not in best or best[key].idx < d.idx:
                    best[key] = d
        o.deps = list(best.values())
        self.ops[eng].append(o)
        return o

    def emit(self, final_waits=()):
        nc = self.nc
        for e in ENGS:
            seen = {}
            for o in self.ops[e]:
                nd = []
                for d in o.deps:
                    key = ("dma", d.stream) if d.dma else ("eng", d.eng)
                    val = d.gen if d.dma else d.idx
                    if seen.get(key, -1) >= val:
                        continue
                    seen[key] = val
                    nd.append(d)
                    if not d.dma:
                        d.flag = True
                o.deps = nd
        for e in ENGS:
            c = 0
            for o in self.ops[e]:
                if o.flag and not o.dma:
                    c += 1
                    o.incidx = c
        with contextlib.ExitStack() as st:
            esem = {e: st.enter_context(nc.semaphore("s_" + e)) for e in ENGS}
            dsem = {s: st.enter_context(nc.semaphore("d_%d" % i)) for i, s in enumerate(self.streams)}
            block = st.enter_context(nc.Block())

            def run(e, eng):
                for o in self.ops[e]:
                    for d in o.deps:
                        if d.dma:
                            eng.wait_ge(dsem[d.stream], 16 * d.gen)
                        else:
                            eng.wait_ge(esem[d.eng], d.incidx)
                    ins = o.fn(eng)
                    if o.dma:
                        ins.then_inc(dsem[o.stream], 16)
                    elif o.flag:
                        ins.then_inc(esem[e], 1)
                if e == "sp":
                    for s in final_waits:
                        eng.wait_ge(dsem[s], 16 * self.streams[s])

            @block.tensor
            def _(eng):
                run("pe", eng)

            @block.scalar
            def _(eng):
                run("act", eng)

            @block.vector
            def _(eng):
                run("dve", eng)

            @block.gpsimd
            def _(eng):
                run("pool", eng)

            @block.sync
            def _(eng):
                run("sp", eng)


class Cfg:
    def __init__(self, D=2048, S=4096, DFF=5504, BS=114, NBLK=9, TB=3, NBIS=26, IH=16, ID=64,
                 TOPK_MAX=256, CPB=4):
        self.D, self.S, self.DFF = D, S, DFF
        self.HD = 128
        nh = D // 128
        self.NHS = nh // 2
        self.NHD = nh - self.NHS
        self.SBW = self.NHS * 128
        self.DSW = self.NHD * 128
        self.IH, self.ID = IH, ID
        self.TOPK = min(TOPK_MAX, S // 4)
        self.CPB = CPB
        self.OWN = S // CPB
        self.TQ = self.OWN + 2
        self.BS, self.NBLK, self.TB = BS, NBLK, TB
        assert BS * NBLK == self.TQ and NBLK % TB == 0 and BS <= 128
        self.NT = NBLK // TB
        self.TW = TB * BS
        assert self.TW <= 512
        self.KC = D // 128
        self.NKB = S // 128
        self.FC = DFF // 128
        assert DFF % 128 == 0 and S % 512 == 0 and self.OWN % 128 == 0
        self.NOB = self.OWN // 128
        self.NBIS = NBIS
        o = 0
        self.o_qsb = o; o += self.SBW
        self.o_ksb = o; o += self.SBW
        self.o_vsb = o; o += self.SBW
        self.o_qds = o; o += self.DSW
        self.o_kds = o; o += 128
        self.o_vds = o; o += 128
        self.o_qix = o; o += IH * ID
        self.o_kix = o; o += ID
        self.o_wix = o; o += IH
        self.INC = o
        self.NSP = S // 512
        self.idx_scale = (IH ** -0.5) * (ID ** -0.5)
        self.qk_scale = 128 ** -0.5
        self.stop = None


class _Stop(Exception):
    pass


TWO_PI = 6.283185307179586
PI = 3.141592653589793
NEGB = -30000.0


def build_program(cf):
    hold = {}
    try:
        return _build(cf, hold)
    except _Stop:
        hold["em"].emit(final_waits=[])
        return hold["nc"]


def _build(cf, hold):
    nc = bass.Bass("TRN2", target_bir_lowering=False)
    em = Em(nc)
    hold["nc"] = nc; hold["em"] = em
    D, S, KC, NKB, TQ, BS, NBLK, TB, NT, TW = cf.D, cf.S, cf.KC, cf.NKB, cf.TQ, cf.BS, cf.NBLK, cf.TB, cf.NT, cf.TW
    NHS, NHD, SBW, DSW, IH, ID, FC, DFF = cf.NHS, cf.NHD, cf.SBW, cf.DSW, cf.IH, cf.ID, cf.FC, cf.DFF
    NSP, NBIS, OWN, NOB = cf.NSP, cf.NBIS, cf.OWN, cf.NOB
    NM = NHS + NHD

    def din(name, shape, dt=F32):
        return nc.dram_tensor(name, list(shape), dt, kind="ExternalInput").ap()

    def dscr(name, shape, dt):
        return nc.dram_tensor(name, list(shape), dt, kind="Internal").ap()

    xkv = din("xkv", [S, D]); xq = din("xq", [TQ, D])
    posk = din("posk", [128, NKB], I32); posq = din("posq", [BS, NBLK], I32)
    qcol = din("qcol", [BS, NBLK]); qrel = din("qrel", [BS, NBLK * NSP]); qrow = din("qrow", [128, TQ])
    cb = din("cb", [128, KC])
    w_ada = din("w_ada", [D, 6 * D]); b_ada = din("b_ada", [1, 6 * D])
    w_in = din("w_in", [D, cf.INC]); w_out = din("w_out", [D, D])
    w_up = din("w_up", [D, 2 * DFF]); w_down = din("w_down", [DFF, D])
    n1g = din("n1g", [128, KC]); n2g = din("n2g", [128, KC])
    sbg = din("sbg", [128, NHS]); dsg = din("dsg", [128, NHD]); fng = din("fng", [128, D])
    convw = din("convw", [128, 3 * 2 * FC]); convb = din("convb", [128, 2 * FC])
    identf = din("identf", [128, 128]); cmat = din("cmat", [128, 5 * 128])
    iota = din("iota", [128, 512]); kcolc = din("kcolc", [128, 1]); pow2 = din("pow2", [128, NBIS + 1])
    invf = din("invf", [128, 16]); flag = din("flag", [128, 1])
    out = nc.dram_tensor("out", [OWN, D], F32, kind="ExternalOutput").ap()
    modrow = dscr("modrow", [1, 6 * D], F32)
    kTsb = dscr("kTsb", [NHS, 128, S], BF16)
    vsb = dscr("vsb", [NHS, 128, NKB * 128], BF16)
    xmid = dscr("xmid", [TQ, D], F32)
    yscr = dscr("yscr", [OWN, D], F32)
    h2Ts = dscr("h2Ts", [128, KC * TQ], BF16)

    def mm(o, lhsT, rhs, start, stop, r, w):
        em.op("pe", lambda e: e.matmul(o, lhsT=lhsT, rhs=rhs, start=start, stop=stop), r, w)

    def tr(o, in_, ident, r, w):
        em.op("pe", lambda e: e.transpose(o, in_, ident), r, w)

    def act(o, in_, func, r, w, **kw):
        em.op("act", lambda e: e.activation(out=o, in_=in_, func=func, **kw), r, w)

    def ts(eng, o, in0, s1, s2, op0, op1, r, w, accum_out=None):
        if op1 is None:
            em.op(eng, lambda e: e.tensor_scalar(out=o, in0=in0, scalar1=s1, scalar2=None, op0=op0), r, w)
        elif accum_out is None:
            em.op(eng, lambda e: e.tensor_scalar(out=o, in0=in0, scalar1=s1, scalar2=s2, op0=op0, op1=op1), r, w)
        else:
            em.op(eng, lambda e: e.tensor_scalar(out=o, in0=in0, scalar1=s1, scalar2=s2, op0=op0, op1=op1,
                                                 accum_out=accum_out), r, w)

    def tt(eng, o, in0, in1, op, r, w):
        em.op(eng, lambda e: e.tensor_tensor(out=o, in0=in0, in1=in1, op=op), r, w)

    def stt(o, in0, sc, in1, op0, op1, r, w):
        em.op("dve", lambda e: e.scalar_tensor_tensor(out=o, in0=in0, scalar=sc, in1=in1, op0=op0, op1=op1), r, w)

    def cp(eng, o, in_, r, w):
        if eng == "act":
            em.op("act", lambda e: e.copy(out=o, in_=in_), r, w)
        else:
            em.op(eng, lambda e: e.tensor_copy(out=o, in_=in_), r, w)

    def memset(eng, o, val, w):
        em.op(eng, lambda e: e.memset(o, val), (), w)

    def dma(q, o, in_, r, w, stream, slow=False):
        if slow:
            em.op(q, lambda e: e.dma_start(out=o, in_=in_, allow_slow_non_contiguous=True), r, w, dma=True, stream=stream)
        else:
            em.op(q, lambda e: e.dma_start(out=o, in_=in_), r, w, dma=True, stream=stream)

    def chk(tag):
        if cf.stop == tag:
            raise _Stop()

    with contextlib.ExitStack() as gst:
        def sbt(st, name, shape, dt):
            return st.enter_context(nc.sbuf_tensor("s_" + name, list(shape), dt))

        PS = gst.enter_context(nc.psum_tensor("PS", [128, 8, 512], F32))
        pb = [PS[:, i, :] for i in range(8)]
        bpb = [Buf("pb%d" % i) for i in range(8)]

        identf_t = sbt(gst, "identf", [128, 128], F32)
        cm_t = sbt(gst, "cmat", [128, 5, 128], BF16)
        iota_t = sbt(gst, "iota", [128, 512], F32)
        kcol_t = sbt(gst, "kcolc", [128, 1], F32)
        pow2_t = sbt(gst, "pow2", [128, NBIS + 1], F32)
        flag_t = sbt(gst, "flag", [128, 1], F32)
        vec_t = sbt(gst, "vecs", [128, 8, KC], F32)
        sbg_t = sbt(gst, "sbg", [128, NHS], F32)
        dsg_t = sbt(gst, "dsg", [128, NHD], F32)
        cvw_t = sbt(gst, "cvw", [128, 3, 2 * FC], F32)
        cvb_t = sbt(gst, "cvb", [128, 2 * FC], F32)
        qcol_t = sbt(gst, "qcol", [BS, NBLK], F32)
        qrel_t = sbt(gst, "qrel", [BS, NBLK, NSP], F32)
        cosK = sbt(gst, "cosK", [128, NKB, 16], F32); sinK = sbt(gst, "sinK", [128, NKB, 16], F32)
        cosQ = sbt(gst, "cosQ", [BS, NBLK, 16], F32); sinQ = sbt(gst, "sinQ", [BS, NBLK, 16], F32)
        kTds = sbt(gst, "kTds", [128, S], BF16)
        vds = sbt(gst, "vds", [128, NKB, 128], BF16)
        kTix = sbt(gst, "kTix", [128, S], BF16)
        bc = Buf("consts")
        b_vec = Buf("vecs"); b_rope = Buf("rope")
        b_kTds = Buf("kTds"); b_vds = Buf("vds"); b_kTix = Buf("kTix")

        for (t_, src, nm) in ((identf_t[:], identf[:, :], "c0"), (iota_t[:], iota[:, :], "c1"),
                              (kcol_t[:], kcolc[:, :], "c2"), (pow2_t[:], pow2[:, :], "c3"),
                              (flag_t[:], flag[:, :], "c4"), (sbg_t[:], sbg[:, :], "c5"), (dsg_t[:], dsg[:, :], "c6"),
                              (cvw_t[:], convw.rearrange("p (a f) -> p a f", a=3), "c7"), (cvb_t[:], convb[:, :], "c8"),
                              (qcol_t[:], qcol[:, :], "c9"),
                              (qrel_t[:], qrel.rearrange("p (a f) -> p a f", a=NBLK), "c10"),
                              (vec_t[:, 4, :], n1g[:, :], "c11"), (vec_t[:, 5, :], n2g[:, :], "c12")):
            dma("sp", t_, src, (), [bc], nm)
        dma("pool", cm_t[:], cmat.rearrange("p (a f) -> p a f", a=5), (), [bc], "c13")
        identb = cm_t[:, 0, :]; ntri = cm_t[:, 1, :]; nones = cm_t[:, 2, :]; umat = cm_t[:, 3, :]; onesb = cm_t[:, 4, :]

        def rope_tables(st, pos_ap, P, n, cos_t, sin_t, tag):
            pi_ = sbt(st, "pi" + tag, [P, n], I32)
            pf = sbt(st, "pf" + tag, [P, n], F32)
            ang = sbt(st, "ang" + tag, [P, n, 16], F32)
            ki = sbt(st, "ki" + tag, [P, n, 16], I32)
            kf = sbt(st, "kf" + tag, [P, n, 16], F32)
            tm = sbt(st, "tm" + tag, [P, n, 16], F32)
            ivf = sbt(st, "ivf" + tag, [P, 16], F32)
            b = Buf("ropetmp" + tag)
            dma("sp", pi_[:], pos_ap, (), [b], "rp" + tag)
            dma("sp", ivf[:], invf[0:P, :], (), [b], "rp" + tag)
            cp("dve", pf[:], pi_[:], [b], [b])
            for c in range(n):
                ts("dve", ang[:, c, :], ivf[:], pf[:, c:c + 1], None, ALU.mult, None, [b], [b])

            def reduce_sin(dst, shift):
                a2 = ang[:]
                if shift != 0.0:
                    ts("dve", tm[:], ang[:], shift, None, ALU.add, None, [b], [b])
                    a2 = tm[:]
                ts("dve", kf[:], a2, 1.0 / TWO_PI, None, ALU.mult, None, [b], [b])
                cp("dve", ki[:], kf[:], [b], [b])
                cp("dve", kf[:], ki[:], [b], [b])
                stt(tm[:], kf[:], -TWO_PI, a2, ALU.mult, ALU.add, [b], [b])
                ts("dve", kf[:], tm[:], PI, -TWO_PI, ALU.is_gt, ALU.mult, [b], [b])
                tt("dve", tm[:], tm[:], kf[:], ALU.add, [b], [b])
                ts("dve", kf[:], tm[:], -PI, TWO_PI, ALU.is_lt, ALU.mult, [b], [b])
                tt("dve", tm[:], tm[:], kf[:], ALU.add, [b], [b])
                act(dst, tm[:], AF.Sin, [b], [b_rope])

            reduce_sin(sin_t[:], 0.0)
            reduce_sin(cos_t[:], PI / 2)

        with contextlib.ExitStack() as st:
            rope_tables(st, posk[:, :], 128, NKB, cosK, sinK, "k")
            rope_tables(st, posq[:, :], BS, NBLK, cosQ, sinQ, "q")
        em.barrier()

        def rope_apply(x1, x2, cos_ap, sin_ap, tmp, P, h, r, w):
            tt("dve", tmp[:P, 0, :h], x1, cos_ap, ALU.mult, r, w)
            tt("dve", tmp[:P, 1, :h], x2, sin_ap, ALU.mult, r, w)
            tt("dve", tmp[:P, 2, :h], x2, cos_ap, ALU.mult, r, w)
            tt("dve", tmp[:P, 3, :h], x1, sin_ap, ALU.mult, r, w)
            tt("dve", x1, tmp[:P, 0, :h], tmp[:P, 1, :h], ALU.subtract, r, w)
            tt("dve", x2, tmp[:P, 2, :h], tmp[:P, 3, :h], ALU.add, r, w)

        nrm_ctr = [0]

        def norm_T(xt_ap, P, bx, dst_fn, bdst, G_ap, sh_ap, tmps, pbank):
            junk, ssq, bt = tmps
            act(junk[:P, :], xt_ap, AF.Square, [bx], [bt], accum_out=ssq[:P, 0:1])
            act(ssq[:P, 1:2], ssq[:P, 0:1], AF.Sqrt, [bt], [bt], scale=1.0 / D, bias=1e-6)
            em.op("dve", lambda e: e.reciprocal(out=ssq[:P, 2:3], in_=ssq[:P, 1:2]), [bt], [bt])
            ts("dve", xt_ap, xt_ap, ssq[:P, 2:3], None, ALU.mult, None, [bx, bt], [bx])
            xs = None
            for k0 in range(0, KC, 4):
                nj = min(4, KC - k0)
                pbi = pbank[nrm_ctr[0] % len(pbank)]
                nrm_ctr[0] += 1
                for j in range(nj):
                    tr(pb[pbi][:, j * 128:j * 128 + P], xt_ap[:, (k0 + j) * 128:(k0 + j + 1) * 128],
                       identf_t[:P, :P], [bx, bc], [bpb[pbi]])
                for j in range(nj):
                    k = k0 + j
                    if j % 2 == 0:
                        act(dst_fn(k), pb[pbi][:, j * 128:j * 128 + P], AF.Identity, [bpb[pbi], b_vec], [bdst],
                            scale=G_ap[:, k:k + 1], bias=sh_ap[:, k:k + 1])
                    else:
                        ts("dve", dst_fn(k), pb[pbi][:, j * 128:j * 128 + P], G_ap[:, k:k + 1], sh_ap[:, k:k + 1],
                           ALU.mult, ALU.add, [bpb[pbi], b_vec], [bdst])

        winv = w_in.rearrange("(k p) n -> p k n", p=128)
        G1 = vec_t[:, 0, :]; SH1 = vec_t[:, 1, :]; G2 = vec_t[:, 2, :]; SH2 = vec_t[:, 3, :]
        with contextlib.ExitStack() as st:
            MG = 256
            NG = (6 * D) // MG
            NG1 = (2 * D) // MG
            wg = [sbt(st, "wg%d" % i, [128, KC, MG], BF16) for i in range(2)]
            bwg = [Buf("wg%d" % i) for i in range(2)]
            cb_t = sbt(st, "cb", [128, KC], F32); cs_t = sbt(st, "cs", [128, KC], BF16)
            brow = [sbt(st, "brow%d" % i, [1, MG], F32) for i in range(2)]
            mrow = [sbt(st, "mrow%d" % i, [1, MG], F32) for i in range(2)]
            bbr = [Buf("brow%d" % i) for i in range(2)]; bmr = [Buf("mrow%d" % i) for i in range(2)]
            bcs = Buf("cs"); bmodg = [Buf("modrow%d" % i) for i in range(NG)]
            mrows = sbt(st, "mrows", [KC, 4, 128], F32); bmrows = Buf("mrows")
            dma("sp", cb_t[:], cb[:, :], (), [bcs], "cb")
            act(cs_t[:], cb_t[:], AF.Silu, [bcs], [bcs])
            wav = w_ada.rearrange("(k p) n -> p k n", p=128)

            def mod_load(g):
                s2 = g % 2
                dma("pool", wg[s2][:], wav[:, :, g * MG:(g + 1) * MG], (), [bwg[s2]], "wg%d" % s2)
                dma("sp", brow[s2][:], b_ada[0:1, g * MG:(g + 1) * MG], (), [bbr[s2]], "brow%d" % s2)

            def mod_compute(g, pbi):
                s2 = g % 2
                for k in range(KC):
                    mm(pb[pbi][0:1, 0:MG], cs_t[:, k:k + 1], wg[s2][:, k, :], k == 0, k == KC - 1,
                       [bcs, bwg[s2]], [bpb[pbi]])
                tt("dve", mrow[s2][:], pb[pbi][0:1, 0:MG], brow[s2][:], ALU.add, [bpb[pbi], bbr[s2]], [bmr[s2]])
                dma("sp", modrow[0:1, g * MG:(g + 1) * MG], mrow[s2][:], [bmr[s2]], [bmodg[g]], "modw%d" % s2)

            def mod_vec(i_, a_, slot, pbi):
                gs = [bmodg[g] for g in range(a_ * D // MG, (a_ + 1) * D // MG)]
                dma("sp", mrows[:, i_, :], modrow[0:1, a_ * D:(a_ + 1) * D].rearrange("o (k p) -> (o k) p", p=128),
                    gs, [bmrows], "mv")
                tr(pb[pbi][:, i_ * KC:(i_ + 1) * KC], mrows[:, i_, :], identf_t[:KC, :KC], [bmrows, bc], [bpb[pbi]])
                cp("dve", vec_t[:, slot, :], pb[pbi][:, i_ * KC:(i_ + 1) * KC], [bpb[pbi]], [b_vec])

            WAW = 2 * SBW + 320
            wA = sbt(st, "wA", [128, KC, WAW], BF16); bwA = Buf("wA")
            mod_load(0)
            if NG1 > 1:
                mod_load(1)
            for g in range(NG1):
                mod_compute(g, 7)
                if g + 2 < NG1:
                    mod_load(g + 2)
            c0 = 0
            for (src0, n) in ((cf.o_ksb, SBW), (cf.o_vsb, SBW), (cf.o_kds, 256), (cf.o_kix, 64)):
                for a in range(0, n, 512):
                    m = min(512, n - a)
                    dma("pool", wA[:, :, c0 + a:c0 + a + m], winv[:, :, src0 + a:src0 + a + m], (), [bwA], "wA")
                c0 += n
            mod_vec(0, 0, 1, 6)
            mod_vec(1, 1, 6, 6)
            stt(vec_t[:, 0, :], vec_t[:, 6, :], 1.0, vec_t[:, 4, :], ALU.add, ALU.mult, [b_vec, bc], [b_vec])
            gnext = [NG1]
            if gnext[0] < NG:
                mod_load(gnext[0])
            if gnext[0] + 1 < NG:
                mod_load(gnext[0] + 1)

            def mod_step():
                g = gnext[0]
                if g >= NG:
                    return
                mod_compute(g, 7)
                if g + 2 < NG:
                    mod_load(g + 2)
                gnext[0] += 1

            xt = [sbt(st, "xt%d" % i, [128, D], F32) for i in range(3)]; bxt = [Buf("xt%d" % i) for i in range(3)]
            junk = sbt(st, "junkA", [128, D], BF16); ssq = sbt(st, "ssqA", [128, 4], F32)
            bt = Buf("nrmtmpA")
            hTa = [sbt(st, "hTa%d" % i, [128, KC, 512], BF16) for i in range(2)]; bhT = [Buf("hTa%d" % i) for i in range(2)]
            vst = [sbt(st, "vst%d" % i, [128, SBW], BF16) for i in range(2)]; bvst = [Buf("vst%d" % i) for i in range(2)]
            kst = [sbt(st, "kst%d" % i, [128, 512], BF16) for i in range(2)]; bkst = [Buf("kst%d" % i) for i in range(2)]
            sm = [sbt(st, "smA%d" % i, [128, 384], F32) for i in range(2)]; bsm = [Buf("smA%d" % i) for i in range(2)]
            rtmp = sbt(st, "rtmpA", [128, 4, 16], F32)
            vsbv = vsb.rearrange("h p x -> p h x")
            kctr = 0
            per_blk = -(-(NG - NG1) // NKB)

            def prep(c):
                s3 = c % 3; ti = (c // 4) % 2; cc = c % 4
                dma("sp", xt[s3][:], xkv[c * 128:(c + 1) * 128, :], (), [bxt[s3]], "xt%d" % s3)
                norm_T(xt[s3][:], 128, bxt[s3], lambda k, ti=ti, cc=cc: hTa[ti][:, k, cc * 128:(cc + 1) * 128],
                       bhT[ti], G1, SH1, (junk, ssq, bt), [0, 1, 6])

            def ktr(c):
                tr(pb[5][:, 0:128], sm[c % 2][:, 0:128], identf_t[:], [bsm[c % 2], bc], [bpb[5]])
                tr(pb[5][:, 128:256], sm[c % 2][:, 256:384], identf_t[:], [bsm[c % 2], bc], [bpb[5]])
                cp("act", kTds[:, c * 128:(c + 1) * 128], pb[5][:, 0:128], [bpb[5]], [b_kTds])
                cp("act", kTix[:, c * 128:(c + 1) * 128], pb[5][:, 128:256], [bpb[5]], [b_kTix])

            prep(0)
            if NKB > 1:
                prep(1)
            for c in range(NKB):
                s2 = c % 2
                ti = (c // 4) % 2
                cc = c % 4
                if c + 2 < NKB:
                    prep(c + 2)
                for g in range(0, SBW, 512):
                    pbi = 2 + (g // 512) % 2
                    n = min(512, SBW - g)
                    for k in range(KC):
                        mm(pb[pbi][:, 0:n], hTa[ti][:, k, cc * 128:(cc + 1) * 128], wA[:, k, SBW + g:SBW + g + n],
                           k == 0, k == KC - 1, [bhT[ti], bwA], [bpb[pbi]])
                    cp("act", vst[s2][:, g:g + n], pb[pbi][:, 0:n], [bpb[pbi]], [bvst[s2]])
                dma("sp", vsbv[:, :, c * 128:(c + 1) * 128], vst[s2][:].rearrange("p (h d) -> p h d", h=NHS),
                    [bvst[s2]], (), "vsbw%d" % s2)
                for k in range(KC):
                    mm(pb[4][:, 0:320], hTa[ti][:, k, cc * 128:(cc + 1) * 128], wA[:, k, 2 * SBW:2 * SBW + 320],
                       k == 0, k == KC - 1, [bhT[ti], bwA], [bpb[4]])
                smc = sm[c % 2]; bsmc = bsm[c % 2]
                cp("act", smc[:, 0:320], pb[4][:, 0:320], [bpb[4]], [bsmc])
                cp("pool", vds[:, c, :], smc[:, 128:256], [bsmc], [b_vds])
                rope_apply(smc[:, 0:16], smc[:, 16:32], cosK[:, c, :], sinK[:, c, :], rtmp, 128, 16, [bsmc, b_rope], [bsmc])
                rope_apply(smc[:, 256:264], smc[:, 264:272], cosK[:, c, 0:16:2], sinK[:, c, 0:16:2], rtmp, 128, 8,
                           [bsmc, b_rope], [bsmc])
                cp("dve", smc[:, 320:384], smc[:, 256:320], [bsmc], [bsmc])
                if c > 0:
                    ktr(c - 1)
                if cc == 3:
                    t0 = (c // 4) * 512
                    for h in range(NHS):
                        pbi = 2 + h % 2
                        for k in range(KC):
                            mm(pb[pbi][:, :], wA[:, k, h * 128:(h + 1) * 128], hTa[ti][:, k, :], k == 0, k == KC - 1,
                               [bhT[ti], bwA], [bpb[pbi]])
                        ks = kctr % 2; kctr += 1
                        cp("dve", kst[ks][:], pb[pbi][:, :], [bpb[pbi]], [bkst[ks]])
                        dma("sp", kTsb[h, :, t0:t0 + 512], kst[ks][:], [bkst[ks]], (), "ktw%d" % ks)
                for _ in range(per_blk):
                    mod_step()
            ktr(NKB - 1)
            while gnext[0] < NG:
                mod_step()
            mod_vec(2, 3, 3, 6)
            mod_vec(3, 4, 7, 6)
            stt(vec_t[:, 2, :], vec_t[:, 7, :], 1.0, vec_t[:, 5, :], ALU.add, ALU.mult, [b_vec, bc], [b_vec])
        em.barrier()
        chk("A")
        wov = w_out.rearrange("(k p) n -> p k n", p=128)
        h2v = h2Ts.rearrange("p (k t) -> p k t", k=KC)
        U8 = mybir.dt.uint8
        with contextlib.ExitStack() as st:
            hTt = sbt(st, "hTt", [128, KC, TW], BF16); bhTt = Buf("hTt")
            xA = [sbt(st, "xA%d" % i, [BS, D], F32) for i in range(2)]; bxA = [Buf("xA%d" % i) for i in range(2)]
            junk = sbt(st, "junkT", [BS, D], BF16); ssq = sbt(st, "ssqT", [BS, 4], F32); bt = Buf("nrmtmpT")
            wq = [sbt(st, "wq%d" % i, [128, KC, 256], BF16) for i in range(2)]; bwq = [Buf("wq%d" % i) for i in range(2)]
            qTsb = sbt(st, "qTsb", [128, NHS, TW], BF16); bqsb = Buf("qTsb")
            qTds = sbt(st, "qTds", [128, NHD, TW], BF16); bqds = Buf("qTds")
            qTix = sbt(st, "qTix", [128, IH // 2, TW], BF16); bqix = Buf("qTix")
            wix = sbt(st, "wix", [BS, TB, IH], F32); bwix = Buf("wix")
            qtm = sbt(st, "qtm", [BS, 256], F32); bqtm = Buf("qtm")
            rtmp = sbt(st, "rtmpT", [BS, 4, 16], F32)
            Mb = sbt(st, "Mb", [BS, TB, S], BF16); bMb = [Buf("Mb%d" % i) for i in range(TB)]
            SCRB = max(S * 4 + 8192, NKB * TW * 2, TB * D * 4)
            scr = sbt(st, "scr", [128, SCRB], U8)
            kvh = sbt(st, "kvh", [128, 4 * S], U8)
            kTh = kvh[:, 0:2 * S].bitcast(BF16); bkTh = Buf("kTh")
            vh = kvh[:, 2 * S:4 * S].bitcast(BF16); bvh = Buf("vh")
            scoreb = [scr[:BS, 0:S * 4].bitcast(F32), kvh[:BS, 0:S * 4].bitcast(F32)]
            bscb = [Buf("score0"), Buf("score1")]
            relb = [scr[:BS, S * 4 + 4096 * i:S * 4 + 4096 * i + 2048].bitcast(BF16) for i in range(2)]
            brel = [Buf("rel%d" % i) for i in range(2)]
            biasT = [scr[:BS, S * 4 + 4096 * i + 2048:S * 4 + 4096 * (i + 1)].bitcast(F32) for i in range(2)]
            bbias = [Buf("biasT%d" % i) for i in range(2)]
            diagw = sbt(st, "diagw", [BS, IH, BS], BF16); bdiag = Buf("diagw")
            Yt = scr[:, 0:NKB * TW * 2].bitcast(BF16); bY = Buf("Yt")
            xqb = [scr[:BS, D * 4 * i:D * 4 * (i + 1)].bitcast(F32) for i in range(TB)]; bxq = [Buf("xqb%d" % i) for i in range(TB)]
            bis = [sbt(st, "bis%d" % i, [BS, 8 + NSP], F32) for i in range(2)]; bbis = [Buf("bis%d" % i) for i in range(2)]
            dtab = [sbt(st, "dtab%d" % i, [BS, NBIS + 1], F32) for i in range(2)]
            ndtab = [sbt(st, "ndtab%d" % i, [BS, NBIS + 1], F32) for i in range(2)]
            mrg = sbt(st, "mrg", [128, NM, TW], BF16); bmrg = [Buf("mrg%d" % i) for i in range(NM)]
            ytmp = sbt(st, "ytmp", [128, TW], F32); bytmp = Buf("ytmp")
            qrow_t = sbt(st, "qrow", [128, TW], F32); bqrow = Buf("qrow")
            ebuf = [sbt(st, "ebuf%d" % i, [128, TW], F32) for i in range(2)]; beb = [Buf("ebuf%d" % i) for i in range(2)]
            spb = [sbt(st, "spb%d" % i, [128, TW], BF16) for i in range(2)]; bspb = [Buf("spb%d" % i) for i in range(2)]
            Ab = [sbt(st, "Ab%d" % i, [128, TW], BF16) for i in range(2)]; bAb = [Buf("Ab%d" % i) for i in range(2)]
            wb = [sbt(st, "wb%d" % i, [128, TW], BF16) for i in range(2)]; bwb = [Buf("wb%d" % i) for i in range(2)]
            pbuf = [sbt(st, "pbuf%d" % i, [128, TW], BF16) for i in range(2)]; bpbuf = [Buf("pbuf%d" % i) for i in range(2)]
            rden = sbt(st, "rden", [128, TW], F32); brden = Buf("rden")
            g1t = [sbt(st, "g1t%d" % i, [BS, 256], F32) for i in range(2)]; bg1 = [Buf("g1t%d" % i) for i in range(2)]
            gsq = sbt(st, "gsq", [128, TW], BF16); bgsq = Buf("gsq")
            grs = sbt(st, "grs", [128, 2, TW], F32); bgrs = Buf("grs")
            h2st = sbt(st, "h2st", [128, KC, BS], BF16); bh2st = Buf("h2st")
            wqctr = [0]

            def wq_load(src_v, col0, n):
                s = wqctr[0] % 2; wqctr[0] += 1
                dma("pool", wq[s][:, :, 0:n], src_v[:, :, col0:col0 + n], (), [bwq[s]], "wq%d" % s)
                return s

            for t in range(NT):
                tc0 = t * TW
                for bi in range(TB):
                    blk = t * TB + bi
                    xa = blk % 2
                    dma("sp", xA[xa][:], xq[blk * BS:(blk + 1) * BS, :], (), [bxA[xa]], "xA%d" % xa)
                    norm_T(xA[xa][:], BS, bxA[xa], lambda k, bi=bi: hTt[:, k, bi * BS:(bi + 1) * BS], bhTt,
                           G1, SH1, (junk, ssq, bt), [0, 1])
                dma("sp", qrow_t[:], qrow[:, tc0:tc0 + TW], (), [bqrow], "qrow")
                chk("T1")
                for g in range(0, SBW, 256):
                    s = wq_load(winv, cf.o_qsb + g, 256)
                    for hh in range(2):
                        h = g // 128 + hh
                        pbi = 2 + h % 2
                        for k in range(KC):
                            mm(pb[pbi][:, 0:TW], wq[s][:, k, hh * 128:(hh + 1) * 128], hTt[:, k, :], k == 0, k == KC - 1,
                               [bwq[s], bhTt], [bpb[pbi]])
                        act(qTsb[:, h, :], pb[pbi][:, 0:TW], AF.Copy, [bpb[pbi]], [bqsb], scale=cf.qk_scale)
                for g in range(0, DSW, 256):
                    s = wq_load(winv, cf.o_qds + g, 256)
                    for bi in range(TB):
                        blk = t * TB + bi
                        for k in range(KC):
                            mm(pb[4][:BS, 0:256], hTt[:, k, bi * BS:(bi + 1) * BS], wq[s][:, k, :], k == 0, k == KC - 1,
                               [bwq[s], bhTt], [bpb[4]])
                        cp("act", qtm[:, :], pb[4][:BS, 0:256], [bpb[4]], [bqtm])
                        for hh in range(2):
                            o_ = hh * 128
                            rope_apply(qtm[:, o_:o_ + 16], qtm[:, o_ + 16:o_ + 32], cosQ[:, blk, :], sinQ[:, blk, :],
                                       rtmp, BS, 16, [bqtm, b_rope], [bqtm])
                        for hh in range(2):
                            tr(pb[5][:, hh * 128:hh * 128 + BS], qtm[:, hh * 128:(hh + 1) * 128], identf_t[:BS, :BS],
                               [bqtm, bc], [bpb[5]])
                        for hh in range(2):
                            h = g // 128 + hh
                            act(qTds[:, h, bi * BS:(bi + 1) * BS], pb[5][:, hh * 128:hh * 128 + BS], AF.Copy,
                                [bpb[5]], [bqds], scale=cf.qk_scale)
                for g in range(0, IH * ID, 256):
                    s = wq_load(winv, cf.o_qix + g, 256)
                    for bi in range(TB):
                        blk = t * TB + bi
                        for k in range(KC):
                            mm(pb[4][:BS, 0:256], hTt[:, k, bi * BS:(bi + 1) * BS], wq[s][:, k, :], k == 0, k == KC - 1,
                               [bwq[s], bhTt], [bpb[4]])
                        cp("act", qtm[:, :], pb[4][:BS, 0:256], [bpb[4]], [bqtm])
                        for hh in range(256 // ID):
                            o_ = hh * ID
                            rope_apply(qtm[:, o_:o_ + 8], qtm[:, o_ + 8:o_ + 16], cosQ[:, blk, 0:16:2], sinQ[:, blk, 0:16:2],
                                       rtmp, BS, 8, [bqtm, b_rope], [bqtm])
                        for pp in range(2):
                            tr(pb[5][:, pp * 128:pp * 128 + BS], qtm[:, pp * 128:(pp + 1) * 128], identf_t[:BS, :BS],
                               [bqtm, bc], [bpb[5]])
                        for pp in range(2):
                            cp("act", qTix[:, g // 128 + pp, bi * BS:(bi + 1) * BS], pb[5][:, pp * 128:pp * 128 + BS],
                               [bpb[5]], [bqix])
                s = wq_load(winv, cf.o_wix, IH)
                for bi in range(TB):
                    for k in range(KC):
                        mm(pb[4][:BS, 0:IH], hTt[:, k, bi * BS:(bi + 1) * BS], wq[s][:, k, 0:IH], k == 0, k == KC - 1,
                           [bwq[s], bhTt], [bpb[4]])
                    act(wix[:, bi, :], pb[4][:BS, 0:IH], AF.Copy, [bpb[4]], [bwix], scale=cf.idx_scale)

                chk("T2")
                def idx_block(bi):
                    blk = t * TB + bi
                    sb_ = bi % 2; sc = scoreb[sb_]; bs_ = bscb[sb_]; B_ = bis[sb_]; bB = bbis[sb_]
                    for h in range(IH):
                        ts("pool", diagw[:, h, :], identb[:BS, :BS], wix[:, bi, h:h + 1], None, ALU.mult, None,
                           [bc, bwix], [bdiag])
                    gctr = 0; spctr = 0
                    for sp_ in range(0, S, 1024):
                        nsp = min(1024, S - sp_); nb = nsp // 512
                        ab = 4 + 2 * (spctr % 2); spctr += 1
                        def idx_mm(h, pg):
                            po = (h % 2) * 64
                            for q_ in range(nb):
                                mm(pb[pg + q_][:BS, :], qTix[po:po + 64, h // 2, bi * BS:(bi + 1) * BS],
                                   kTix[po:po + 64, sp_ + q_ * 512:sp_ + (q_ + 1) * 512], True, True,
                                   [bqix, b_kTix], [bpb[pg + q_]])

                        pgs = []
                        for h in range(IH):
                            pgs.append((gctr % 2) * 2); gctr += 1
                        idx_mm(0, pgs[0])
                        for h in range(IH):
                            pg = pgs[h]
                            if h + 1 < IH:
                                idx_mm(h + 1, pgs[h + 1])
                            rs = h % 2
                            em.op("dve", lambda e, rs=rs, pg=pg, nb=nb, nsp=nsp: e.tensor_scalar(
                                out=relb[rs][:, 0:nsp].rearrange("p (a f) -> p a f", a=nb), in0=PS[:BS, pg:pg + nb, :],
                                scalar1=0.0, scalar2=None, op0=ALU.max),
                                [bpb[pg + q_] for q_ in range(nb)], [brel[rs]])
                            for q_ in range(nb):
                                mm(pb[ab + q_][:BS, :], diagw[:, h, :], relb[rs][:, q_ * 512:(q_ + 1) * 512], h == 0, h == IH - 1,
                                   [bdiag, brel[rs]], [bpb[ab + q_]])
                        for q_ in range(nb):
                            kt = sp_ // 512 + q_
                            em.op("dve", lambda e, kt=kt, ab=ab, q_=q_, B_=B_: e.tensor_reduce(
                                out=B_[:, 8 + kt:9 + kt], in_=pb[ab + q_][:BS, :], axis=AX.X, op=ALU.min),
                                [bpb[ab + q_]], [bB])
                            ts("dve", biasT[q_ % 2], iota_t[:BS, :], qrel_t[:, blk, kt:kt + 1], -1e30, ALU.is_gt, ALU.mult,
                               [bc], [bbias[q_ % 2]])
                            tt("dve", sc[:, kt * 512:(kt + 1) * 512], pb[ab + q_][:BS, :], biasT[q_ % 2], ALU.add,
                               [bpb[ab + q_], bbias[q_ % 2]], [bs_])
                    em.op("dve", lambda e: e.tensor_reduce(out=B_[:, 0:1], in_=sc, axis=AX.X, op=ALU.max), [bs_], [bB])
                    em.op("dve", lambda e: e.tensor_reduce(out=B_[:, 1:2], in_=B_[:, 8:8 + NSP], axis=AX.X, op=ALU.min), [bB], [bB])
                    stt(B_[:, 6:7], B_[:, 0:1], 2.0, B_[:, 1:2], ALU.add, ALU.subtract, [bB], [bB])
                    ts("dve", dtab[sb_][:], pow2_t[:BS, :], B_[:, 6:7], None, ALU.mult, None, [bB, bc], [bB])
                    ts("dve", ndtab[sb_][:], dtab[sb_][:], -1.0, None, ALU.mult, None, [bB], [bB])
                    ts("dve", B_[:, 2:3], B_[:, 1:2], -1.0, 1.0, ALU.mult, ALU.add, [bB], [bB])
                    tt("dve", B_[:, 2:3], B_[:, 2:3], dtab[sb_][:, 0:1], ALU.subtract, [bB], [bB])

                def bisect(bi):
                    sb_ = bi % 2; sc = scoreb[sb_]; bs_ = bscb[sb_]; B_ = bis[sb_]; bB = bbis[sb_]
                    for r_ in range(NBIS):
                        act(Mb[:, bi, :], sc, AF.Sign, [bs_, bB], [bMb[bi], bB], bias=B_[:, 2:3], accum_out=B_[:, 3:4])
                        act(B_[:, 4:5], B_[:, 3:4], AF.Sign, [bB], [bB], bias=float(S - 2 * cf.TOPK) + 0.5)
                        act(B_[:, 2:3], B_[:, 4:5], AF.Identity, [bB], [bB], scale=ndtab[sb_][:, r_ + 1:r_ + 2], bias=B_[:, 2:3])

                def finalize(bi):
                    sb_ = bi % 2; sc = scoreb[sb_]; bs_ = bscb[sb_]; B_ = bis[sb_]; bB = bbis[sb_]
                    stt(B_[:, 5:6], B_[:, 2:3], -1.0, dtab[sb_][:, NBIS:NBIS + 1], ALU.mult, ALU.subtract, [bB], [bB])
                    ts("dve", Mb[:, bi, :], sc, B_[:, 5:6], NEGB, ALU.is_le, ALU.mult, [bs_, bB], [bMb[bi]])

                idx_block(0)
                for bi in range(TB):
                    bisect(bi)
                    if bi + 1 < TB:
                        idx_block(bi + 1)
                    finalize(bi)

                chk("T3")
                for h in range(NHD):
                    def S_step(c):
                        pz = c % 2
                        mm(pb[pz][:, 0:TW], kTds[:, c * 128:(c + 1) * 128], qTds[:, h, :], True, False,
                           [b_kTds, bqds], [bpb[pz]])
                        for bi in range(TB):
                            mm(pb[pz][:, bi * BS:(bi + 1) * BS], Mb[:, bi, c * 128:(c + 1) * 128], identb[:BS, :BS],
                               False, bi == TB - 1, [bMb[bi], bc], [bpb[pz]])
                        act(pbuf[pz][:], pb[pz][:, 0:TW], AF.Exp, [bpb[pz]], [bpbuf[pz]])

                    def PV_step(c):
                        pz = c % 2
                        mm(pb[2][:, 0:TW], vds[:, c, :], pbuf[pz][:], c == 0, c == NKB - 1, [b_vds, bpbuf[pz]], [bpb[2]])
                        mm(pb[3][:, 0:TW], onesb, pbuf[pz][:], c == 0, c == NKB - 1, [bc, bpbuf[pz]], [bpb[3]])

                    S_step(0)
                    for c in range(NKB):
                        if c + 1 < NKB:
                            S_step(c + 1)
                        PV_step(c)
                    em.op("dve", lambda e: e.reciprocal(out=rden[:], in_=pb[3][:, 0:TW]), [bpb[3]], [brden])
                    tt("dve", mrg[:, NHS + h, :], pb[2][:, 0:TW], rden[:], ALU.mult, [bpb[2], brden], [bmrg[NHS + h]])

                chk("T4")
                em.barrier()
                for c in range(NKB):
                    ts("dve", ytmp[:], qrow_t[:], float(-128 * c), 0.0, ALU.add, ALU.max, [bqrow], [bytmp])
                    ts("dve", Yt[:, c * TW:(c + 1) * TW], ytmp[:], kcol_t[:, 0:1], None, ALU.is_equal, None, [bytmp, bc], [bY])
                for h in range(NHS):
                    dma("sp", kTh, kTsb[h, :, :], (), [bkTh], "kTh")
                    dma("sp", vh, vsb[h, :, :], (), [bvh], "vh")
                    order = list(range(NKB - 1, -1, -1))

                    def Z_step(i):
                        c = order[i]; pz = 4 + i % 2
                        mm(pb[pz][:, 0:TW], kTh[:, c * 128:(c + 1) * 128], qTsb[:, h, :], True, False,
                           [bkTh, bqsb], [bpb[pz]])
                        mm(pb[pz][:, 0:TW], umat, Yt[:, c * TW:(c + 1) * TW], False, True, [bc, bY], [bpb[pz]])
                        act(ebuf[i % 2][:], pb[pz][:, 0:TW], AF.Exp, [bpb[pz]], [beb[i % 2]])
                        act(spb[i % 2][:], ebuf[i % 2][:], AF.Ln, [beb[i % 2]], [bspb[i % 2]], bias=1.0)

                    def X_step(i):
                        c = order[i]; px = 6 + i % 2
                        mm(pb[px][:, 0:TW], kTh[:, c * 128:(c + 1) * 128], qTsb[:, h, :], True, False,
                           [bkTh, bqsb], [bpb[px]])
                        mm(pb[px][:, 0:TW], umat, Yt[:, c * TW:(c + 1) * TW], False, False, [bc, bY], [bpb[px]])
                        if i > 0:
                            mm(pb[px][:, 0:TW], nones, Ab[i % 2][:], False, False, [bc, bAb[i % 2]], [bpb[px]])
                        mm(pb[px][:, 0:TW], ntri, spb[i % 2][:], False, True, [bc, bspb[i % 2]], [bpb[px]])
                        if i == 0:
                            cp("pool", Ab[1][:], spb[0][:], [bspb[0]], [bAb[1]])
                        elif i + 1 < NKB:
                            tt("pool", Ab[(i + 1) % 2][:], Ab[i % 2][:], spb[i % 2][:], ALU.add,
                               [bAb[i % 2], bspb[i % 2]], [bAb[(i + 1) % 2]])
                        act(wb[i % 2][:], pb[px][:, 0:TW], AF.Exp, [bpb[px]], [bwb[i % 2]])

                    def PVs(i):
                        c = order[i]
                        mm(pb[3][:, 0:TW], vh[:, c * 128:(c + 1) * 128], wb[i % 2][:], i == 0, i == NKB - 1, [bvh, bwb[i % 2]], [bpb[3]])

                    Z_step(0)
                    for i in range(NKB):
                        if i + 1 < NKB:
                            Z_step(i + 1)
                        X_step(i)
                        if i > 0:
                            PVs(i - 1)
                    PVs(NKB - 1)
                    cp("dve", mrg[:, h, :], pb[3][:, 0:TW], [bpb[3]], [bmrg[h]])

                chk("T5")
                for gi, (h0, nh_, g_t, W_) in enumerate(((0, NHS, sbg_t, SBW), (NHS, NHD, dsg_t, DSW))):
                    for hh in range(nh_):
                        act(gsq[:], mrg[:, h0 + hh, :], AF.Square, [bmrg[h0 + hh]], [bgsq])
                        mm(pb[0][:, 0:TW], onesb, gsq[:], hh == 0, hh == nh_ - 1, [bc, bgsq], [bpb[0]])
                    act(grs[:, 0, :], pb[0][:, 0:TW], AF.Sqrt, [bpb[0]], [bgrs], scale=1.0 / W_, bias=1e-6)
                    em.op("dve", lambda e: e.reciprocal(out=grs[:, 1, :], in_=grs[:, 0, :]), [bgrs], [bgrs])
                    for hh in range(nh_):
                        stt(mrg[:, h0 + hh, :], mrg[:, h0 + hh, :], g_t[:, hh:hh + 1], grs[:, 1, :], ALU.mult, ALU.mult,
                            [bmrg[h0 + hh], bgrs, bc], [bmrg[h0 + hh]])

                chk("T6")
                em.barrier()
                for bi in range(TB):
                    blk = t * TB + bi
                    dma("sp", xqb[bi], xq[blk * BS:(blk + 1) * BS, :], (), [bxq[bi]], "xqb%d" % bi)
                for gi, g in enumerate(range(0, D, 256)):
                    s = wq_load(wov, g, 256)
                    gs = gi % 2
                    dma("sp", g1t[gs][:], modrow[0:1, 2 * D + g:2 * D + g + 256].partition_broadcast(BS), (), [bg1[gs]], "g1t%d" % gs)
                    for bi in range(TB):
                        pbi = bi % 2
                        for k in range(NM):
                            mm(pb[pbi][:BS, 0:256], mrg[:, k, bi * BS:(bi + 1) * BS], wq[s][:, k, :], k == 0, k == NM - 1,
                               bmrg + [bwq[s]], [bpb[pbi]])
                        tt("dve", qtm[:, :], pb[pbi][:BS, 0:256], g1t[gs][:], ALU.mult, [bpb[pbi], bg1[gs]], [bqtm])
                        tt("dve", xqb[bi][:, g:g + 256], xqb[bi][:, g:g + 256], qtm[:, :], ALU.add, [bqtm, bxq[bi]], [bxq[bi]])
                for bi in range(TB):
                    blk = t * TB + bi
                    dma("sp", xmid[blk * BS:(blk + 1) * BS, :], xqb[bi], [bxq[bi]], (), "xmidw%d" % bi)
                    norm_T(xqb[bi], BS, bxq[bi], lambda k: h2st[:, k, :], bh2st, G2, SH2, (junk, ssq, bt), [2, 3])
                    dma("sp", h2v[:, :, blk * BS:(blk + 1) * BS], h2st[:], [bh2st], (), "h2w")
                em.barrier()
        em.barrier()

        chk("T")
        wuv = w_up.rearrange("(k p) n -> p k n", p=128)
        wdv = w_down.rearrange("(k p) n -> p k n", p=128)
        with contextlib.ExitStack() as st:
            mT = sbt(st, "mT", [128, FC, OWN], BF16); bmT = Buf("mT")
            h2T = sbt(st, "h2T", [128, KC, TQ], BF16); b_h2T = Buf("h2T")
            dma("sp", h2T[:], h2Ts.rearrange("p (k t) -> p k t", k=KC), (), [b_h2T], "h2l")
            with contextlib.ExitStack() as st2:
                wu = [sbt(st2, "wu%d" % i, [128, KC, 256], BF16) for i in range(3)]; bwu = [Buf("wu%d" % i) for i in range(3)]
                usb = [sbt(st2, "usb%d" % i, [128, TQ], F32) for i in range(2)]; busb = [Buf("usb%d" % i) for i in range(2)]
                ucg = sbt(st2, "ucg", [128, OWN], F32); bucg = Buf("ucg")
                ucv = sbt(st2, "ucv", [128, OWN], F32); bucv = Buf("ucv")
                for f in range(FC):
                    s = f % 3
                    dma("pool", wu[s][:, :, 0:128], wuv[:, :, f * 128:(f + 1) * 128], (), [bwu[s]], "wu%d" % s)
                    dma("pool", wu[s][:, :, 128:256], wuv[:, :, DFF + f * 128:DFF + (f + 1) * 128], (), [bwu[s]], "wu%d" % s)
                    for half in range(2):
                        ci = f + half * FC
                        ub = usb[half]; bub = busb[half]
                        for t in range(NT):
                            pbi = (2 * t + half) % 4
                            for k in range(KC):
                                mm(pb[pbi][:, 0:TW], wu[s][:, k, half * 128:(half + 1) * 128], h2T[:, k, t * TW:(t + 1) * TW],
                                   k == 0, k == KC - 1, [bwu[s], b_h2T], [bpb[pbi]])
                            cp("act", ub[:, t * TW:(t + 1) * TW], pb[pbi][:, 0:TW], [bpb[pbi]], [bub])
                        ts("dve", ub[:, 0:2], ub[:, 0:2], flag_t[:, 0:1], None, ALU.mult, None, [bub, bc], [bub])
                        uc = ucg if half == 0 else ucv
                        buc = bucg if half == 0 else bucv
                        act(uc[:], ub[:, 2:TQ], AF.Identity, [bub, bc], [buc], scale=cvw_t[:, 2, ci:ci + 1], bias=cvb_t[:, ci:ci + 1])
                        stt(uc[:], ub[:, 1:TQ - 1], cvw_t[:, 1, ci:ci + 1], uc[:], ALU.mult, ALU.add, [bub, bc, buc], [buc])
                        stt(uc[:], ub[:, 0:TQ - 2], cvw_t[:, 0, ci:ci + 1], uc[:], ALU.mult, ALU.add, [bub, bc, buc], [buc])
                    act(ucg[:], ucg[:], AF.Silu, [bucg], [bucg])
                    tt("dve", mT[:, f, :], ucg[:], ucv[:], ALU.mult, [bucg, bucv], [bmT])
            em.barrier()
            with contextlib.ExitStack() as st2:
                wd = [sbt(st2, "wd%d" % i, [128, FC, 256], BF16) for i in range(2)]; bwd = [Buf("wd%d" % i) for i in range(2)]
                yst = [sbt(st2, "yst%d" % i, [128, 256], F32) for i in range(2)]; byst = [Buf("yst%d" % i) for i in range(2)]
                yc = 0
                for gi, g in enumerate(range(0, D, 256)):
                    s = gi % 2
                    for f0 in range(0, FC, 16):
                        f1 = min(FC, f0 + 16)
                        dma("pool", wd[s][:, f0:f1, :], wdv[:, f0:f1, g:g + 256], (), [bwd[s]], "wd%d" % s)
                    for tb in range(NOB):
                        pbi = tb % 2
                        for f in range(FC):
                            mm(pb[pbi][:, 0:256], mT[:, f, tb * 128:(tb + 1) * 128], wd[s][:, f, :], f == 0, f == FC - 1,
                               [bmT, bwd[s]], [bpb[pbi]])
                        ys = yc % 2; yc += 1
                        cp("act", yst[ys][:], pb[pbi][:, 0:256], [bpb[pbi]], [byst[ys]])
                        dma("sp", yscr[tb * 128:(tb + 1) * 128, g:g + 256], yst[ys][:], [byst[ys]], (), "yscrw%d" % ys)
        em.barrier()
        with contextlib.ExitStack() as st:
            g2bc = sbt(st, "g2bc", [128, D], F32); fngt = sbt(st, "fngt", [128, D], F32); bgg = Buf("g2fng")
            xm = [sbt(st, "xm%d" % i, [128, D], F32) for i in range(2)]; bxm = [Buf("xm%d" % i) for i in range(2)]
            yy = [sbt(st, "yy%d" % i, [128, D], F32) for i in range(2)]; byy = [Buf("yy%d" % i) for i in range(2)]
            junk = sbt(st, "junkF", [128, D], BF16); ssq = sbt(st, "ssqF", [128, 4], F32); bt = Buf("nrmF")
            dma("sp", g2bc[:], modrow[0:1, 5 * D:6 * D].partition_broadcast(128), (), [bgg], "g2bc")
            dma("sp", fngt[:], fng[:, :], (), [bgg], "g2bc")
            for tb in range(NOB):
                s = tb % 2
                dma("sp", xm[s][:], xmid[2 + tb * 128:2 + (tb + 1) * 128, :], (), [bxm[s]], "xm%d" % s)
                dma("sp", yy[s][:], yscr[tb * 128:(tb + 1) * 128, :], (), [byy[s]], "yy%d" % s)
                tt("dve", yy[s][:], yy[s][:], g2bc[:], ALU.mult, [byy[s], bgg], [byy[s]])
                tt("dve", xm[s][:], xm[s][:], yy[s][:], ALU.add, [byy[s], bxm[s]], [bxm[s]])
                act(junk[:], xm[s][:], AF.Square, [bxm[s]], [bt], accum_out=ssq[:, 0:1])
                act(ssq[:, 1:2], ssq[:, 0:1], AF.Sqrt, [bt], [bt], scale=1.0 / D, bias=1e-6)
                em.op("dve", lambda e: e.reciprocal(out=ssq[:, 2:3], in_=ssq[:, 1:2]), [bt], [bt])
                stt(yy[s][:], xm[s][:], ssq[:, 2:3], fngt[:], ALU.mult, ALU.mult, [bxm[s], bt, bgg], [byy[s]])
                dma("sp", out[tb * 128:(tb + 1) * 128, :], yy[s][:], [byy[s]], (), "outw%d" % s)
        em.emit(final_waits=["outw0", "outw1"] if NOB > 1 else ["outw0"])
    return nc


def host_inputs(cf, inp, core):
    D, S, KC, NKB, TQ, BS, NBLK, NSP, FC = cf.D, cf.S, cf.KC, cf.NKB, cf.TQ, cf.BS, cf.NBLK, cf.NSP, cf.FC
    b = core // cf.CPB; j = core % cf.CPB
    f32 = np.float32
    x = np.asarray(inp["x"], f32); pos = np.asarray(inp["positions"], np.int32)
    t0 = j * cf.OWN - 2
    tok = np.arange(t0, t0 + TQ)
    tokc = np.maximum(tok, 0)
    m = {}
    m["xkv"] = np.ascontiguousarray(x[b])
    m["xq"] = np.ascontiguousarray(x[b][tokc])
    m["posk"] = np.ascontiguousarray(pos[b].reshape(NKB, 128).T)
    m["posq"] = np.ascontiguousarray(pos[b][tokc].reshape(NBLK, BS).T)
    qc = tokc.astype(f32).reshape(NBLK, BS).T
    m["qcol"] = np.ascontiguousarray(qc)
    qr = qc[:, :, None] - (512.0 * np.arange(NSP, dtype=f32))[None, None, :]
    m["qrel"] = np.ascontiguousarray(qr.reshape(BS, NBLK * NSP).astype(f32))
    m["qrow"] = np.ascontiguousarray(np.broadcast_to(tokc.astype(f32)[None, :], (128, TQ)))
    pk = lambda v: np.ascontiguousarray(np.asarray(v, f32).reshape(-1, 128).T)
    m["cb"] = pk(np.asarray(inp["c"], f32)[b])
    m["w_ada"] = np.ascontiguousarray(np.asarray(inp["w_ada"], f32)[0])
    m["b_ada"] = np.ascontiguousarray(np.asarray(inp["b_ada"], f32)[0][None, :])
    m["w_in"] = np.ascontiguousarray(np.asarray(inp["w_in"], f32)[0])
    m["w_out"] = np.ascontiguousarray(np.asarray(inp["w_out"], f32)[0])
    m["w_up"] = np.ascontiguousarray(np.asarray(inp["w_up"], f32)[0])
    m["w_down"] = np.ascontiguousarray(np.asarray(inp["w_down"], f32)[0])
    m["n1g"] = pk(np.asarray(inp["norm1_g"])[0]); m["n2g"] = pk(np.asarray(inp["norm2_g"])[0])
    m["sbg"] = pk(np.asarray(inp["sb_norm_g"])[0]); m["dsg"] = pk(np.asarray(inp["dsa_norm_g"])[0])
    m["fng"] = np.ascontiguousarray(np.broadcast_to(np.asarray(inp["final_norm_g"], f32)[None, :], (128, D)))
    cw = np.asarray(inp["conv_w"], f32)[0]
    m["convw"] = np.ascontiguousarray(np.stack([pk(cw[i]) for i in range(3)], axis=1).reshape(128, 3 * 2 * FC))
    m["convb"] = pk(np.asarray(inp["conv_b"], f32)[0])
    m["identf"] = np.eye(128, dtype=f32)
    jj = np.arange(128)[:, None]; ss = np.arange(128)[None, :]
    cm = np.stack([np.eye(128, dtype=f32), -(jj >= ss).astype(f32), -np.ones((128, 128), f32),
                   NEGB * (ss >= jj).astype(f32), np.ones((128, 128), f32)], axis=1)
    m["cmat"] = np.ascontiguousarray(cm.reshape(128, 5 * 128))
    m["iota"] = np.ascontiguousarray(np.broadcast_to(np.arange(512, dtype=f32)[None, :], (128, 512)))
    m["kcolc"] = np.arange(128, dtype=f32)[:, None].copy()
    m["pow2"] = np.ascontiguousarray(np.broadcast_to((0.5 ** np.arange(1, cf.NBIS + 2)).astype(f32)[None, :], (128, cf.NBIS + 1)))
    ivf = (np.float32(500000.0) ** (-(np.arange(16, dtype=f32) / np.float32(16)))).astype(f32)
    m["invf"] = np.ascontiguousarray(np.broadcast_to(ivf[None, :], (128, 16)))
    m["flag"] = np.full((128, 1), 0.0 if j == 0 else 1.0, f32)
    return m


_CACHE = {}


def kernel(x, c, positions, w_ada, b_ada, norm1_g, w_in, sb_norm_g, dsa_norm_g, w_out, norm2_g,
           w_up, conv_w, conv_b, w_down, final_norm_g):
    inp = dict(x=x, c=c, positions=positions, w_ada=w_ada, b_ada=b_ada, norm1_g=norm1_g, w_in=w_in,
               sb_norm_g=sb_norm_g, dsa_norm_g=dsa_norm_g, w_out=w_out, norm2_g=norm2_g, w_up=w_up,
               conv_w=conv_w, conv_b=conv_b, w_down=w_down, final_norm_g=final_norm_g)
    cf = Cfg()
    nc = build_program(cf)
    in_maps = [host_inputs(cf, inp, core) for core in range(8)]
    res = run_bass_kernel_spmd(nc, in_maps, core_ids=list(range(8)))
    outp = np.zeros((2, cf.S, cf.D), np.float32)
    for core in range(8):
        b = core // cf.CPB; j = core % cf.CPB
        outp[b, j * cf.OWN:(j + 1) * cf.OWN, :] = np.asarray(res.results[core]["out"], np.float32)
    return outp
```

```python
import contextlib
import numpy as np
import concourse.bass as bass
import concourse.mybir as mybir
from concourse.bass_utils import run_bass_kernel_spmd

F32 = mybir.dt.float32
BF16 = mybir.dt.bfloat16
I32 = mybir.dt.int32
AF = mybir.ActivationFunctionType
ALU = mybir.AluOpType
AX = mybir.AxisListType

ENGS = ("pe", "act", "dve", "pool", "sp")


class Buf:
    __slots__ = ("name", "lw", "rs")

    def __init__(self, name):
        self.name = name
        self.lw = None
        self.rs = []


class Op:
    __slots__ = ("eng", "idx", "fn", "deps", "dma", "stream", "gen", "flag", "incidx")

    def __init__(self, eng, idx, fn, dma, stream):
        self.eng = eng; self.idx = idx; self.fn = fn; self.deps = []
        self.dma = dma; self.stream = stream; self.gen = 0
        self.flag = False; self.incidx = 0


class Em:
    def __init__(self, nc):
        self.nc = nc
        self.ops = {e: [] for e in ENGS}
        self.streams = {}
        self.last_dma = {}
        self.pending = {e: None for e in ENGS}
        self.pool_dmas = []

    def barrier(self):
        lasts = [self.ops[e][-1] for e in ENGS if self.ops[e]]
        lasts += list(self.last_dma.values())
        for e in ENGS:
            self.pending[e] = lasts

    def op(self, eng, fn, reads=(), writes=(), dma=False, stream=None):
        o = Op(eng, len(self.ops[eng]), fn, dma, stream)
        deps = []
        if self.pending[eng] is not None:
            deps.extend(self.pending[eng])
            self.pending[eng] = None
        if dma and eng == "pool":
            if len(self.pool_dmas) >= 4:
                deps.append(self.pool_dmas[-4])
            self.pool_dmas.append(o)
        for b in reads:
            if b.lw is not None:
                deps.append(b.lw)
        for b in writes:
            if b.lw is not None:
                deps.append(b.lw)
            deps.extend(b.rs)
        for b in reads:
            b.rs.append(o)
        for b in writes:
            b.lw = o
            b.rs = []
        if dma:
            g = self.streams.get(stream, 0) + 1
            self.streams[stream] = g
            o.gen = g
            self.last_dma[stream] = o
        best = {}
        for d in deps:
            if d is o:
                continue
            if d.dma:
                key = ("dma", d.stream)
                if key not in best or best[key].gen < d.gen:
                    best[key] = d
            else:
                if d.eng == eng and eng == "pe":
                    continue
                key = ("eng", d.eng)
                if key not in best or best[key].idx < d.idx:
                    best[key] = d
        o.deps = list(best.values())
        self.ops[eng].append(o)
        return o

    def emit(self, final_waits=()):
        nc = self.nc
        for e in ENGS:
            seen = {}
            for o in self.ops[e]:
                nd = []
                for d in o.deps:
                    key = ("dma", d.stream) if d.dma else ("eng", d.eng)
                    val = d.gen if d.dma else d.idx
                    if seen.get(key, -1) >= val:
                        continue
                    seen[key] = val
                    nd.append(d)
                    if not d.dma:
                        d.flag = True
                o.deps = nd
        for e in ENGS:
            c = 0
            for o in self.ops[e]:
                if o.flag and not o.dma:
                    c += 1
                    o.incidx = c
        with contextlib.ExitStack() as st:
            esem = {e: st.enter_context(nc.semaphore("s_" + e)) for e in ENGS}
            dsem = {s: st.enter_context(nc.semaphore("d_%d" % i)) for i, s in enumerate(self.streams)}
            block = st.enter_context(nc.Block())

            def run(e, eng):
                for o in self.ops[e]:
                    for d in o.deps:
                        if d.dma:
                            eng.wait_ge(dsem[d.stream], 16 * d.gen)
                        else:
                            eng.wait_ge(esem[d.eng], d.incidx)
                    ins = o.fn(eng)
                    if o.dma:
                        ins.then_inc(dsem[o.stream], 16)
                    elif o.flag:
                        ins.then_inc(esem[e], 1)
                if e == "sp":
                    for s in final_waits:
                        eng.wait_ge(dsem[s], 16 * self.streams[s])

            @block.tensor
            def _(eng):
                run("pe", eng)

            @block.scalar
            def _(eng):
                run("act", eng)

            @block.vector
            def _(eng):
                run("dve", eng)

            @block.gpsimd
            def _(eng):
                run("pool", eng)

            @block.sync
            def _(eng):
                run("sp", eng)


class Cfg:
    def __init__(self, D=2048, S=4096, DFF=5504, BS=114, NBLK=9, TB=3, NBIS=26, IH=16, ID=64,
                 TOPK_MAX=256, CPB=4):
        self.D, self.S, self.DFF = D, S, DFF
        self.HD = 128
        nh = D // 128
        self.NHS = nh // 2
        self.NHD = nh - self.NHS
        self.SBW = self.NHS * 128
        self.DSW = self.NHD * 128
        self.IH, self.ID = IH, ID
        self.TOPK = min(TOPK_MAX, S // 4)
        self.CPB = CPB
        self.OWN = S // CPB
        self.TQ = self.OWN + 2
        self.BS, self.NBLK, self.TB = BS, NBLK, TB
        assert BS * NBLK == self.TQ and NBLK % TB == 0 and BS <= 128
        self.NT = NBLK // TB
        self.TW = TB * BS
        assert self.TW <= 512
        self.KC = D // 128
        self.NKB = S // 128
        self.FC = DFF // 128
        assert DFF % 128 == 0 and S % 512 == 0 and self.OWN % 128 == 0
        self.NOB = self.OWN // 128
        self.NBIS = NBIS
        o = 0
        self.o_qsb = o; o += self.SBW
        self.o_ksb = o; o += self.SBW
        self.o_vsb = o; o += self.SBW
        self.o_qds = o; o += self.DSW
        self.o_kds = o; o += 128
        self.o_vds = o; o += 128
        self.o_qix = o; o += IH * ID
        self.o_kix = o; o += ID
        self.o_wix = o; o += IH
        self.INC = o
        self.NSP = S // 512
        self.idx_scale = (IH ** -0.5) * (ID ** -0.5)
        self.qk_scale = 128 ** -0.5
        self.stop = None


class _Stop(Exception):
    pass


TWO_PI = 6.283185307179586
PI = 3.141592653589793
NEGB = -30000.0


def build_program(cf):
    hold = {}
    try:
        return _build(cf, hold)
    except _Stop:
        hold["em"].emit(final_waits=[])
        return hold["nc"]


def _build(cf, hold):
    nc = bass.Bass("TRN2", target_bir_lowering=False)
    em = Em(nc)
    hold["nc"] = nc; hold["em"] = em
    D, S, KC, NKB, TQ, BS, NBLK, TB, NT, TW = cf.D, cf.S, cf.KC, cf.NKB, cf.TQ, cf.BS, cf.NBLK, cf.TB, cf.NT, cf.TW
    NHS, NHD, SBW, DSW, IH, ID, FC, DFF = cf.NHS, cf.NHD, cf.SBW, cf.DSW, cf.IH, cf.ID, cf.FC, cf.DFF
    NSP, NBIS, OWN, NOB = cf.NSP, cf.NBIS, cf.OWN, cf.NOB
    NM = NHS + NHD

    def din(name, shape, dt=F32):
        return nc.dram_tensor(name, list(shape), dt, kind="ExternalInput").ap()

    def dscr(name, shape, dt):
        return nc.dram_tensor(name, list(shape), dt, kind="Internal").ap()

    xkv = din("xkv", [S, D]); xq = din("xq", [TQ, D])
    posk = din("posk", [128, NKB], I32); posq = din("posq", [BS, NBLK], I32)
    qcol = din("qcol", [BS, NBLK]); qrel = din("qrel", [BS, NBLK * NSP]); qrow = din("qrow", [128, TQ])
    cb = din("cb", [128, KC])
    w_ada = din("w_ada", [D, 6 * D]); b_ada = din("b_ada", [1, 6 * D])
    w_in = din("w_in", [D, cf.INC]); w_out = din("w_out", [D, D])
    w_up = din("w_up", [D, 2 * DFF]); w_down = din("w_down", [DFF, D])
    n1g = din("n1g", [128, KC]); n2g = din("n2g", [128, KC])
    sbg = din("sbg", [128, NHS]); dsg = din("dsg", [128, NHD]); fng = din("fng", [128, D])
    convw = din("convw", [128, 3 * 2 * FC]); convb = din("convb", [128, 2 * FC])
    identf = din("identf", [128, 128]); cmat = din("cmat", [128, 5 * 128])
    iota = din("iota", [128, 512]); kcolc = din("kcolc", [128, 1]); pow2 = din("pow2", [128, NBIS + 1])
    invf = din("invf", [128, 16]); flag = din("flag", [128, 1])
    out = nc.dram_tensor("out", [OWN, D], F32, kind="ExternalOutput").ap()
    modrow = dscr("modrow", [1, 6 * D], F32)
    kTsb = dscr("kTsb", [NHS, 128, S], BF16)
    vsb = dscr("vsb", [NHS, 128, NKB * 128], BF16)
    xmid = dscr("xmid", [TQ, D], F32)
    yscr = dscr("yscr", [OWN, D], F32)
    h2Ts = dscr("h2Ts", [128, KC * TQ], BF16)

    def mm(o, lhsT, rhs, start, stop, r, w):
        em.op("pe", lambda e: e.matmul(o, lhsT=lhsT, rhs=rhs, start=start, stop=stop), r, w)

    def tr(o, in_, ident, r, w):
        em.op("pe", lambda e: e.transpose(o, in_, ident), r, w)

    def act(o, in_, func, r, w, **kw):
        em.op("act", lambda e: e.activation(out=o, in_=in_, func=func, **kw), r, w)

    def ts(eng, o, in0, s1, s2, op0, op1, r, w, accum_out=None):
        if op1 is None:
            em.op(eng, lambda e: e.tensor_scalar(out=o, in0=in0, scalar1=s1, scalar2=None, op0=op0), r, w)
        elif accum_out is None:
            em.op(eng, lambda e: e.tensor_scalar(out=o, in0=in0, scalar1=s1, scalar2=s2, op0=op0, op1=op1), r, w)
        else:
            em.op(eng, lambda e: e.tensor_scalar(out=o, in0=in0, scalar1=s1, scalar2=s2, op0=op0, op1=op1,
                                                 accum_out=accum_out), r, w)

    def tt(eng, o, in0, in1, op, r, w):
        em.op(eng, lambda e: e.tensor_tensor(out=o, in0=in0, in1=in1, op=op), r, w)

    def stt(o, in0, sc, in1, op0, op1, r, w):
        em.op("dve", lambda e: e.scalar_tensor_tensor(out=o, in0=in0, scalar=sc, in1=in1, op0=op0, op1=op1), r, w)

    def cp(eng, o, in_, r, w):
        if eng == "act":
            em.op("act", lambda e: e.copy(out=o, in_=in_), r, w)
        else:
            em.op(eng, lambda e: e.tensor_copy(out=o, in_=in_), r, w)

    def memset(eng, o, val, w):
        em.op(eng, lambda e: e.memset(o, val), (), w)

    def dma(q, o, in_, r, w, stream, slow=False):
        if slow:
            em.op(q, lambda e: e.dma_start(out=o, in_=in_, allow_slow_non_contiguous=True), r, w, dma=True, stream=stream)
        else:
            em.op(q, lambda e: e.dma_start(out=o, in_=in_), r, w, dma=True, stream=stream)

    def chk(tag):
        if cf.stop == tag:
            raise _Stop()

    with contextlib.ExitStack() as gst:
        def sbt(st, name, shape, dt):
            return st.enter_context(nc.sbuf_tensor("s_" + name, list(shape), dt))

        PS = gst.enter_context(nc.psum_tensor("PS", [128, 8, 512], F32))
        pb = [PS[:, i, :] for i in range(8)]
        bpb = [Buf("pb%d" % i) for i in range(8)]

        identf_t = sbt(gst, "identf", [128, 128], F32)
        cm_t = sbt(gst, "cmat", [128, 5, 128], BF16)
        iota_t = sbt(gst, "iota", [128, 512], F32)
        kcol_t = sbt(gst, "kcolc", [128, 1], F32)
        pow2_t = sbt(gst, "pow2", [128, NBIS + 1], F32)
        flag_t = sbt(gst, "flag", [128, 1], F32)
        vec_t = sbt(gst, "vecs", [128, 8, KC], F32)
        sbg_t = sbt(gst, "sbg", [128, NHS], F32)
        dsg_t = sbt(gst, "dsg", [128, NHD], F32)
        cvw_t = sbt(gst, "cvw", [128, 3, 2 * FC], F32)
        cvb_t = sbt(gst, "cvb", [128, 2 * FC], F32)
        qcol_t = sbt(gst, "qcol", [BS, NBLK], F32)
        qrel_t = sbt(gst, "qrel", [BS, NBLK, NSP], F32)
        cosK = sbt(gst, "cosK", [128, NKB, 16], F32); sinK = sbt(gst, "sinK", [128, NKB, 16], F32)
        cosQ = sbt(gst, "cosQ", [BS, NBLK, 16], F32); sinQ = sbt(gst, "sinQ", [BS, NBLK, 16], F32)
        kTds = sbt(gst, "kTds", [128, S], BF16)
        vds = sbt(gst, "vds", [128, NKB, 128], BF16)
        kTix = sbt(gst, "kTix", [128, S], BF16)
        bc = Buf("consts")
        b_vec = Buf("vecs"); b_rope = Buf("rope")
        b_kTds = Buf("kTds"); b_vds = Buf("vds"); b_kTix = Buf("kTix")

        for (t_, src, nm) in ((identf_t[:], identf[:, :], "c0"), (iota_t[:], iota[:, :], "c1"),
                              (kcol_t[:], kcolc[:, :], "c2"), (pow2_t[:], pow2[:, :], "c3"),
                              (flag_t[:], flag[:, :], "c4"), (sbg_t[:], sbg[:, :], "c5"), (dsg_t[:], dsg[:, :], "c6"),
                              (cvw_t[:], convw.rearrange("p (a f) -> p a f", a=3), "c7"), (cvb_t[:], convb[:, :], "c8"),
                              (qcol_t[:], qcol[:, :], "c9"),
                              (qrel_t[:], qrel.rearrange("p (a f) -> p a f", a=NBLK), "c10"),
                              (vec_t[:, 4, :], n1g[:, :], "c11"), (vec_t[:, 5, :], n2g[:, :], "c12")):
            dma("sp", t_, src, (), [bc], nm)
        dma("pool", cm_t[:], cmat.rearrange("p (a f) -> p a f", a=5), (), [bc], "c13")
        identb = cm_t[:, 0, :]; ntri = cm_t[:, 1, :]; nones = cm_t[:, 2, :]; umat = cm_t[:, 3, :]; onesb = cm_t[:, 4, :]

        def rope_tables(st, pos_ap, P, n, cos_t, sin_t, tag):
            pi_ = sbt(st, "pi" + tag, [P, n], I32)
            pf = sbt(st, "pf" + tag, [P, n], F32)
            ang = sbt(st, "ang" + tag, [P, n, 16], F32)
            ki = sbt(st, "ki" + tag, [P, n, 16], I32)
            kf = sbt(st, "kf" + tag, [P, n, 16], F32)
            tm = sbt(st, "tm" + tag, [P, n, 16], F32)
            ivf = sbt(st, "ivf" + tag, [P, 16], F32)
            b = Buf("ropetmp" + tag)
            dma("sp", pi_[:], pos_ap, (), [b], "rp" + tag)
            dma("sp", ivf[:], invf[0:P, :], (), [b], "rp" + tag)
            cp("dve", pf[:], pi_[:], [b], [b])
            for c in range(n):
                ts("dve", ang[:, c, :], ivf[:], pf[:, c:c + 1], None, ALU.mult, None, [b], [b])

            def reduce_sin(dst, shift):
                a2 = ang[:]
                if shift != 0.0:
                    ts("dve", tm[:], ang[:], shift, None, ALU.add, None, [b], [b])
                    a2 = tm[:]
                ts("dve", kf[:], a2, 1.0 / TWO_PI, None, ALU.mult, None, [b], [b])
                cp("dve", ki[:], kf[:], [b], [b])
                cp("dve", kf[:], ki[:], [b], [b])
                stt(tm[:], kf[:], -TWO_PI, a2, ALU.mult, ALU.add, [b], [b])
                ts("dve", kf[:], tm[:], PI, -TWO_PI, ALU.is_gt, ALU.mult, [b], [b])
                tt("dve", tm[:], tm[:], kf[:], ALU.add, [b], [b])
                ts("dve", kf[:], tm[:], -PI, TWO_PI, ALU.is_lt, ALU.mult, [b], [b])
                tt("dve", tm[:], tm[:], kf[:], ALU.add, [b], [b])
                act(dst, tm[:], AF.Sin, [b], [b_rope])

            reduce_sin(sin_t[:], 0.0)
            reduce_sin(cos_t[:], PI / 2)

        with contextlib.ExitStack() as st:
            rope_tables(st, posk[:, :], 128, NKB, cosK, sinK, "k")
            rope_tables(st, posq[:, :], BS, NBLK, cosQ, sinQ, "q")
        em.barrier()

        def rope_apply(x1, x2, cos_ap, sin_ap, tmp, P, h, r, w):
            tt("dve", tmp[:P, 0, :h], x1, cos_ap, ALU.mult, r, w)
            tt("dve", tmp[:P, 1, :h], x2, sin_ap, ALU.mult, r, w)
            tt("dve", tmp[:P, 2, :h], x2, cos_ap, ALU.mult, r, w)
            tt("dve", tmp[:P, 3, :h], x1, sin_ap, ALU.mult, r, w)
            tt("dve", x1, tmp[:P, 0, :h], tmp[:P, 1, :h], ALU.subtract, r, w)
            tt("dve", x2, tmp[:P, 2, :h], tmp[:P, 3, :h], ALU.add, r, w)

        nrm_ctr = [0]

        def norm_T(xt_ap, P, bx, dst_fn, bdst, G_ap, sh_ap, tmps, pbank):
            junk, ssq, bt = tmps
            act(junk[:P, :], xt_ap, AF.Square, [bx], [bt], accum_out=ssq[:P, 0:1])
            act(ssq[:P, 1:2], ssq[:P, 0:1], AF.Sqrt, [bt], [bt], scale=1.0 / D, bias=1e-6)
            em.op("dve", lambda e: e.reciprocal(out=ssq[:P, 2:3], in_=ssq[:P, 1:2]), [bt], [bt])
            ts("dve", xt_ap, xt_ap, ssq[:P, 2:3], None, ALU.mult, None, [bx, bt], [bx])
            xs = None
            for k0 in range(0, KC, 4):
                nj = min(4, KC - k0)
                pbi = pbank[nrm_ctr[0] % len(pbank)]
                nrm_ctr[0] += 1
                for j in range(nj):
                    tr(pb[pbi][:, j * 128:j * 128 + P], xt_ap[:, (k0 + j) * 128:(k0 + j + 1) * 128],
                       identf_t[:P, :P], [bx, bc], [bpb[pbi]])
                for j in range(nj):
                    k = k0 + j
                    if j % 2 == 0:
                        act(dst_fn(k), pb[pbi][:, j * 128:j * 128 + P], AF.Identity, [bpb[pbi], b_vec], [bdst],
                            scale=G_ap[:, k:k + 1], bias=sh_ap[:, k:k + 1])
                    else:
                        ts("dve", dst_fn(k), pb[pbi][:, j * 128:j * 128 + P], G_ap[:, k:k + 1], sh_ap[:, k:k + 1],
                           ALU.mult, ALU.add, [bpb[pbi], b_vec], [bdst])

        winv = w_in.rearrange("(k p) n -> p k n", p=128)
        G1 = vec_t[:, 0, :]; SH1 = vec_t[:, 1, :]; G2 = vec_t[:, 2, :]; SH2 = vec_t[:, 3, :]
        with contextlib.ExitStack() as st:
            MG = 256
            NG = (6 * D) // MG
            NG1 = (2 * D) // MG
            wg = [sbt(st, "wg%d" % i, [128, KC, MG], BF16) for i in range(2)]
            bwg = [Buf("wg%d" % i) for i in range(2)]
            cb_t = sbt(st, "cb", [128, KC], F32); cs_t = sbt(st, "cs", [128, KC], BF16)
            brow = [sbt(st, "brow%d" % i, [1, MG], F32) for i in range(2)]
            mrow = [sbt(st, "mrow%d" % i, [1, MG], F32) for i in range(2)]
            bbr = [Buf("brow%d" % i) for i in range(2)]; bmr = [Buf("mrow%d" % i) for i in range(2)]
            bcs = Buf("cs"); bmodg = [Buf("modrow%d" % i) for i in range(NG)]
            mrows = sbt(st, "mrows", [KC, 4, 128], F32); bmrows = Buf("mrows")
            dma("sp", cb_t[:], cb[:, :], (), [bcs], "cb")
            act(cs_t[:], cb_t[:], AF.Silu, [bcs], [bcs])
            wav = w_ada.rearrange("(k p) n -> p k n", p=128)

            def mod_load(g):
                s2 = g % 2
                dma("pool", wg[s2][:], wav[:, :, g * MG:(g + 1) * MG], (), [bwg[s2]], "wg%d" % s2)
                dma("sp", brow[s2][:], b_ada[0:1, g * MG:(g + 1) * MG], (), [bbr[s2]], "brow%d" % s2)

            def mod_compute(g, pbi):
                s2 = g % 2
                for k in range(KC):
                    mm(pb[pbi][0:1, 0:MG], cs_t[:, k:k + 1], wg[s2][:, k, :], k == 0, k == KC - 1,
                       [bcs, bwg[s2]], [bpb[pbi]])
                tt("dve", mrow[s2][:], pb[pbi][0:1, 0:MG], brow[s2][:], ALU.add, [bpb[pbi], bbr[s2]], [bmr[s2]])
                dma("pool", modrow[0:1, g * MG:(g + 1) * MG], mrow[s2][:], [bmr[s2]], [bmodg[g]], "modw%d" % s2)

            def mod_vec(i_, a_, slot, pbi):
                gs = [bmodg[g] for g in range(a_ * D // MG, (a_ + 1) * D // MG)]
                dma("sp", mrows[:, i_, :], modrow[0:1, a_ * D:(a_ + 1) * D].rearrange("o (k p) -> (o k) p", p=128),
                    gs, [bmrows], "mv")
                tr(pb[pbi][:, i_ * KC:(i_ + 1) * KC], mrows[:, i_, :], identf_t[:KC, :KC], [bmrows, bc], [bpb[pbi]])
                cp("dve", vec_t[:, slot, :], pb[pbi][:, i_ * KC:(i_ + 1) * KC], [bpb[pbi]], [b_vec])

            WAW = 2 * SBW + 320
            wA = sbt(st, "wA", [128, KC, WAW], BF16); bwA = Buf("wA")
            mod_load(0)
            if NG1 > 1:
                mod_load(1)
            for g in range(NG1):
                mod_compute(g, 7)
                if g + 2 < NG1:
                    mod_load(g + 2)
            c0 = 0
            for (src0, n) in ((cf.o_ksb, SBW), (cf.o_vsb, SBW), (cf.o_kds, 256), (cf.o_kix, 64)):
                for a in range(0, n, 512):
                    m = min(512, n - a)
                    dma("pool", wA[:, :, c0 + a:c0 + a + m], winv[:, :, src0 + a:src0 + a + m], (), [bwA], "wA")
                c0 += n
            mod_vec(0, 0, 1, 6)
            mod_vec(1, 1, 6, 6)
            stt(vec_t[:, 0, :], vec_t[:, 6, :], 1.0, vec_t[:, 4, :], ALU.add, ALU.mult, [b_vec, bc], [b_vec])
            gnext = [NG1]
            if gnext[0] < NG:
                mod_load(gnext[0])
            if gnext[0] + 1 < NG:
                mod_load(gnext[0] + 1)

            def mod_step():
                g = gnext[0]
                if g >= NG:
                    return
                mod_compute(g, 7)
                if g + 2 < NG:
                    mod_load(g + 2)
                gnext[0] += 1

            xt = [sbt(st, "xt%d" % i, [128, D], F32) for i in range(3)]; bxt = [Buf("xt%d" % i) for i in range(3)]
            junk = sbt(st, "junkA", [128, D], BF16); ssq = sbt(st, "ssqA", [128, 4], F32)
            bt = Buf("nrmtmpA")
            hTa = [sbt(st, "hTa%d" % i, [128, KC, 512], BF16) for i in range(2)]; bhT = [Buf("hTa%d" % i) for i in range(2)]
            vst = [sbt(st, "vst%d" % i, [128, SBW], BF16) for i in range(2)]; bvst = [Buf("vst%d" % i) for i in range(2)]
            kst = [sbt(st, "kst%d" % i, [128, 512], BF16) for i in range(2)]; bkst = [Buf("kst%d" % i) for i in range(2)]
            sm = [sbt(st, "smA%d" % i, [128, 384], F32) for i in range(2)]; bsm = [Buf("smA%d" % i) for i in range(2)]
            rtmp = sbt(st, "rtmpA", [128, 4, 16], F32)
            vsbv = vsb.rearrange("h p x -> p h x")
            kctr = 0
            per_blk = -(-(NG - NG1) // NKB)

            def prep(c):
                s3 = c % 3; ti = (c // 4) % 2; cc = c % 4
                dma("sp", xt[s3][:], xkv[c * 128:(c + 1) * 128, :], (), [bxt[s3]], "xt%d" % s3)
                norm_T(xt[s3][:], 128, bxt[s3], lambda k, ti=ti, cc=cc: hTa[ti][:, k, cc * 128:(cc + 1) * 128],
                       bhT[ti], G1, SH1, (junk, ssq, bt), [0, 1, 6])

            def ktr(c):
                tr(pb[5][:, 0:128], sm[c % 2][:, 0:128], identf_t[:], [bsm[c % 2], bc], [bpb[5]])
                tr(pb[5][:, 128:256], sm[c % 2][:, 256:384], identf_t[:], [bsm[c % 2], bc], [bpb[5]])
                cp("act", kTds[:, c * 128:(c + 1) * 128], pb[5][:, 0:128], [bpb[5]], [b_kTds])
                cp("act", kTix[:, c * 128:(c + 1) * 128], pb[5][:, 128:256], [bpb[5]], [b_kTix])

            prep(0)
            if NKB > 1:
                prep(1)
            for c in range(NKB):
                s2 = c % 2
                ti = (c // 4) % 2
                cc = c % 4
                if c + 2 < NKB:
                    prep(c + 2)
                for g in range(0, SBW, 512):
                    pbi = 2 + (g // 512) % 2
                    n = min(512, SBW - g)
                    for k in range(KC):
                        mm(pb[pbi][:, 0:n], hTa[ti][:, k, cc * 128:(cc + 1) * 128], wA[:, k, SBW + g:SBW + g + n],
                           k == 0, k == KC - 1, [bhT[ti], bwA], [bpb[pbi]])
                    cp("act", vst[s2][:, g:g + n], pb[pbi][:, 0:n], [bpb[pbi]], [bvst[s2]])
                dma("act", vsbv[:, :, c * 128:(c + 1) * 128], vst[s2][:].rearrange("p (h d) -> p h d", h=NHS),
                    [bvst[s2]], (), "vsbw%d" % s2)
                for k in range(KC):
                    mm(pb[4][:, 0:320], hTa[ti][:, k, cc * 128:(cc + 1) * 128], wA[:, k, 2 * SBW:2 * SBW + 320],
                       k == 0, k == KC - 1, [bhT[ti], bwA], [bpb[4]])
                smc = sm[c % 2]; bsmc = bsm[c % 2]
                cp("act", smc[:, 0:320], pb[4][:, 0:320], [bpb[4]], [bsmc])
                cp("pool", vds[:, c, :], smc[:, 128:256], [bsmc], [b_vds])
                rope_apply(smc[:, 0:16], smc[:, 16:32], cosK[:, c, :], sinK[:, c, :], rtmp, 128, 16, [bsmc, b_rope], [bsmc])
                rope_apply(smc[:, 256:264], smc[:, 264:272], cosK[:, c, 0:16:2], sinK[:, c, 0:16:2], rtmp, 128, 8,
                           [bsmc, b_rope], [bsmc])
                cp("dve", smc[:, 320:384], smc[:, 256:320], [bsmc], [bsmc])
                if c > 0:
                    ktr(c - 1)
                if cc == 3:
                    t0 = (c // 4) * 512
                    for h in range(NHS):
                        pbi = 2 + h % 2
                        for k in range(KC):
                            mm(pb[pbi][:, :], wA[:, k, h * 128:(h + 1) * 128], hTa[ti][:, k, :], k == 0, k == KC - 1,
                               [bhT[ti], bwA], [bpb[pbi]])
                        ks = kctr % 2; kctr += 1
                        cp("act", kst[ks][:], pb[pbi][:, :], [bpb[pbi]], [bkst[ks]])
                        dma("act", kTsb[h, :, t0:t0 + 512], kst[ks][:], [bkst[ks]], (), "ktw%d" % ks)
                for _ in range(per_blk):
                    mod_step()
            ktr(NKB - 1)
            while gnext[0] < NG:
                mod_step()
            mod_vec(2, 3, 3, 6)
            mod_vec(3, 4, 7, 6)
            stt(vec_t[:, 2, :], vec_t[:, 7, :], 1.0, vec_t[:, 5, :], ALU.add, ALU.mult, [b_vec, bc], [b_vec])
        em.barrier()
        chk("A")
        wov = w_out.rearrange("(k p) n -> p k n", p=128)
        h2v = h2Ts.rearrange("p (k t) -> p k t", k=KC)
        U8 = mybir.dt.uint8
        with contextlib.ExitStack() as st:
            hTt = sbt(st, "hTt", [128, KC, TW], BF16); bhTt = Buf("hTt")
            xA = [sbt(st, "xA%d" % i, [BS, D], F32) for i in range(2)]; bxA = [Buf("xA%d" % i) for i in range(2)]
            junk = sbt(st, "junkT", [BS, D], BF16); ssq = sbt(st, "ssqT", [BS, 4], F32); bt = Buf("nrmtmpT")
            wq = [sbt(st, "wq%d" % i, [128, KC, 256], BF16) for i in range(2)]; bwq = [Buf("wq%d" % i) for i in range(2)]
            qTsb = sbt(st, "qTsb", [128, NHS, TW], BF16); bqsb = Buf("qTsb")
            qTds = sbt(st, "qTds", [128, NHD, TW], BF16); bqds = Buf("qTds")
            qTix = sbt(st, "qTix", [128, IH // 2, TW], BF16); bqix = Buf("qTix")
            wix = sbt(st, "wix", [BS, TB, IH], F32); bwix = Buf("wix")
            qtm = sbt(st, "qtm", [BS, 256], F32); bqtm = Buf("qtm")
            rtmp = sbt(st, "rtmpT", [BS, 4, 16], F32)
            Mb = sbt(st, "Mb", [BS, TB, S], BF16); bMb = [Buf("Mb%d" % i) for i in range(TB)]
            SCRB = max(S * 4 + 8192, NKB * TW * 2, TB * D * 4)
            scr = sbt(st, "scr", [128, SCRB], U8)
            kvh = sbt(st, "kvh", [128, 4 * S], U8)
            kTh = kvh[:, 0:2 * S].bitcast(BF16); bkTh = Buf("kTh")
            vh = kvh[:, 2 * S:4 * S].bitcast(BF16); bvh = Buf("vh")
            scoreb = [scr[:BS, 0:S * 4].bitcast(F32), kvh[:BS, 0:S * 4].bitcast(F32)]
            bscb = [Buf("score0"), Buf("score1")]
            relb = [scr[:BS, S * 4 + 4096 * i:S * 4 + 4096 * i + 2048].bitcast(BF16) for i in range(2)]
            brel = [Buf("rel%d" % i) for i in range(2)]
            biasT = [scr[:BS, S * 4 + 4096 * i + 2048:S * 4 + 4096 * (i + 1)].bitcast(F32) for i in range(2)]
            bbias = [Buf("biasT%d" % i) for i in range(2)]
            diagw = sbt(st, "diagw", [BS, IH, BS], BF16); bdiag = Buf("diagw")
            Yt = scr[:, 0:NKB * TW * 2].bitcast(BF16); bY = Buf("Yt")
            xqb = [scr[:BS, D * 4 * i:D * 4 * (i + 1)].bitcast(F32) for i in range(TB)]; bxq = [Buf("xqb%d" % i) for i in range(TB)]
            bis = [sbt(st, "bis%d" % i, [BS, 8 + NSP], F32) for i in range(2)]; bbis = [Buf("bis%d" % i) for i in range(2)]
            dtab = [sbt(st, "dtab%d" % i, [BS, NBIS + 1], F32) for i in range(2)]
            ndtab = [sbt(st, "ndtab%d" % i, [BS, NBIS + 1], F32) for i in range(2)]
            mrg = sbt(st, "mrg", [128, NM, TW], BF16); bmrg = [Buf("mrg%d" % i) for i in range(NM)]
            ytmp = sbt(st, "ytmp", [128, TW], F32); bytmp = Buf("ytmp")
            qrow_t = sbt(st, "qrow", [128, TW], F32); bqrow = Buf("qrow")
            ebuf = [sbt(st, "ebuf%d" % i, [128, TW], F32) for i in range(2)]; beb = [Buf("ebuf%d" % i) for i in range(2)]
            spb = [sbt(st, "spb%d" % i, [128, TW], BF16) for i in range(2)]; bspb = [Buf("spb%d" % i) for i in range(2)]
            Ab = [sbt(st, "Ab%d" % i, [128, TW], BF16) for i in range(2)]; bAb = [Buf("Ab%d" % i) for i in range(2)]
            wb = [sbt(st, "wb%d" % i, [128, TW], BF16) for i in range(2)]; bwb = [Buf("wb%d" % i) for i in range(2)]
            pbuf = [sbt(st, "pbuf%d" % i, [128, TW], BF16) for i in range(2)]; bpbuf = [Buf("pbuf%d" % i) for i in range(2)]
            rden = sbt(st, "rden", [128, TW], F32); brden = Buf("rden")
            g1t = [sbt(st, "g1t%d" % i, [BS, 256], F32) for i in range(2)]; bg1 = [Buf("g1t%d" % i) for i in range(2)]
            gsq = sbt(st, "gsq", [128, TW], BF16); bgsq = Buf("gsq")
            grs = sbt(st, "grs", [128, 2, TW], F32); bgrs = Buf("grs")
            h2st = sbt(st, "h2st", [128, KC, BS], BF16); bh2st = Buf("h2st")
            wqctr = [0]

            def wq_load(src_v, col0, n):
                s = wqctr[0] % 2; wqctr[0] += 1
                dma("pool", wq[s][:, :, 0:n], src_v[:, :, col0:col0 + n], (), [bwq[s]], "wq%d" % s)
                return s

            for t in range(NT):
                tc0 = t * TW
                for bi in range(TB):
                    blk = t * TB + bi
                    xa = blk % 2
                    dma("sp", xA[xa][:], xq[blk * BS:(blk + 1) * BS, :], (), [bxA[xa]], "xA%d" % xa)
                    norm_T(xA[xa][:], BS, bxA[xa], lambda k, bi=bi: hTt[:, k, bi * BS:(bi + 1) * BS], bhTt,
                           G1, SH1, (junk, ssq, bt), [0, 1])
                dma("sp", qrow_t[:], qrow[:, tc0:tc0 + TW], (), [bqrow], "qrow")
                chk("T1")
                for g in range(0, SBW, 256):
                    s = wq_load(winv, cf.o_qsb + g, 256)
                    for hh in range(2):
                        h = g // 128 + hh
                        pbi = 2 + h % 2
                        for k in range(KC):
                            mm(pb[pbi][:, 0:TW], wq[s][:, k, hh * 128:(hh + 1) * 128], hTt[:, k, :], k == 0, k == KC - 1,
                               [bwq[s], bhTt], [bpb[pbi]])
                        act(qTsb[:, h, :], pb[pbi][:, 0:TW], AF.Copy, [bpb[pbi]], [bqsb], scale=cf.qk_scale)
                for g in range(0, DSW, 256):
                    s = wq_load(winv, cf.o_qds + g, 256)
                    for bi in range(TB):
                        blk = t * TB + bi
                        for k in range(KC):
                            mm(pb[4][:BS, 0:256], hTt[:, k, bi * BS:(bi + 1) * BS], wq[s][:, k, :], k == 0, k == KC - 1,
                               [bwq[s], bhTt], [bpb[4]])
                        cp("act", qtm[:, :], pb[4][:BS, 0:256], [bpb[4]], [bqtm])
                        for hh in range(2):
                            o_ = hh * 128
                            rope_apply(qtm[:, o_:o_ + 16], qtm[:, o_ + 16:o_ + 32], cosQ[:, blk, :], sinQ[:, blk, :],
                                       rtmp, BS, 16, [bqtm, b_rope], [bqtm])
                        for hh in range(2):
                            tr(pb[5][:, hh * 128:hh * 128 + BS], qtm[:, hh * 128:(hh + 1) * 128], identf_t[:BS, :BS],
                               [bqtm, bc], [bpb[5]])
                        for hh in range(2):
                            h = g // 128 + hh
                            act(qTds[:, h, bi * BS:(bi + 1) * BS], pb[5][:, hh * 128:hh * 128 + BS], AF.Copy,
                                [bpb[5]], [bqds], scale=cf.qk_scale)
                for g in range(0, IH * ID, 256):
                    s = wq_load(winv, cf.o_qix + g, 256)
                    for bi in range(TB):
                        blk = t * TB + bi
                        for k in range(KC):
                            mm(pb[4][:BS, 0:256], hTt[:, k, bi * BS:(bi + 1) * BS], wq[s][:, k, :], k == 0, k == KC - 1,
                               [bwq[s], bhTt], [bpb[4]])
                        cp("act", qtm[:, :], pb[4][:BS, 0:256], [bpb[4]], [bqtm])
                        for hh in range(256 // ID):
                            o_ = hh * ID
                            rope_apply(qtm[:, o_:o_ + 8], qtm[:, o_ + 8:o_ + 16], cosQ[:, blk, 0:16:2], sinQ[:, blk, 0:16:2],
                                       rtmp, BS, 8, [bqtm, b_rope], [bqtm])
                        for pp in range(2):
                            tr(pb[5][:, pp * 128:pp * 128 + BS], qtm[:, pp * 128:(pp + 1) * 128], identf_t[:BS, :BS],
                               [bqtm, bc], [bpb[5]])
                        for pp in range(2):
                            cp("act", qTix[:, g // 128 + pp, bi * BS:(bi + 1) * BS], pb[5][:, pp * 128:pp * 128 + BS],
                               [bpb[5]], [bqix])
                s = wq_load(winv, cf.o_wix, IH)
                for bi in range(TB):
                    for k in range(KC):
                        mm(pb[4][:BS, 0:IH], hTt[:, k, bi * BS:(bi + 1) * BS], wq[s][:, k, 0:IH], k == 0, k == KC - 1,
                           [bwq[s], bhTt], [bpb[4]])
                    act(wix[:, bi, :], pb[4][:BS, 0:IH], AF.Copy, [bpb[4]], [bwix], scale=cf.idx_scale)

                chk("T2")
                def idx_block(bi):
                    blk = t * TB + bi
                    sb_ = bi % 2; sc = scoreb[sb_]; bs_ = bscb[sb_]; B_ = bis[sb_]; bB = bbis[sb_]
                    for h in range(IH):
                        ts("pool", diagw[:, h, :], identb[:BS, :BS], wix[:, bi, h:h + 1], None, ALU.mult, None,
                           [bc, bwix], [bdiag])
                    gctr = 0; spctr = 0
                    for sp_ in range(0, S, 1024):
                        nsp = min(1024, S - sp_); nb = nsp // 512
                        ab = 4 + 2 * (spctr % 2); spctr += 1
                        def idx_mm(h, pg):
                            po = (h % 2) * 64
                            for q_ in range(nb):
                                mm(pb[pg + q_][:BS, :], qTix[po:po + 64, h // 2, bi * BS:(bi + 1) * BS],
                                   kTix[po:po + 64, sp_ + q_ * 512:sp_ + (q_ + 1) * 512], True, True,
                                   [bqix, b_kTix], [bpb[pg + q_]])

                        pgs = []
                        for h in range(IH):
                            pgs.append((gctr % 2) * 2); gctr += 1
                        idx_mm(0, pgs[0])
                        for h in range(IH):
                            pg = pgs[h]
                            if h + 1 < IH:
                                idx_mm(h + 1, pgs[h + 1])
                            rs = h % 2
                            em.op("dve", lambda e, rs=rs, pg=pg, nb=nb, nsp=nsp: e.tensor_scalar(
                                out=relb[rs][:, 0:nsp].rearrange("p (a f) -> p a f", a=nb), in0=PS[:BS, pg:pg + nb, :],
                                scalar1=0.0, scalar2=None, op0=ALU.max),
                                [bpb[pg + q_] for q_ in range(nb)], [brel[rs]])
                            for q_ in range(nb):
                                mm(pb[ab + q_][:BS, :], diagw[:, h, :], relb[rs][:, q_ * 512:(q_ + 1) * 512], h == 0, h == IH - 1,
                                   [bdiag, brel[rs]], [bpb[ab + q_]])
                        for q_ in range(nb):
                            kt = sp_ // 512 + q_
                            em.op("dve", lambda e, kt=kt, ab=ab, q_=q_, B_=B_: e.tensor_reduce(
                                out=B_[:, 8 + kt:9 + kt], in_=pb[ab + q_][:BS, :], axis=AX.X, op=ALU.min),
                                [bpb[ab + q_]], [bB])
                            ts("dve", biasT[q_ % 2], iota_t[:BS, :], qrel_t[:, blk, kt:kt + 1], -1e30, ALU.is_gt, ALU.mult,
                               [bc], [bbias[q_ % 2]])
                            tt("dve", sc[:, kt * 512:(kt + 1) * 512], pb[ab + q_][:BS, :], biasT[q_ % 2], ALU.add,
                               [bpb[ab + q_], bbias[q_ % 2]], [bs_])
                    em.op("dve", lambda e: e.tensor_reduce(out=B_[:, 0:1], in_=sc, axis=AX.X, op=ALU.max), [bs_], [bB])
                    em.op("dve", lambda e: e.tensor_reduce(out=B_[:, 1:2], in_=B_[:, 8:8 + NSP], axis=AX.X, op=ALU.min), [bB], [bB])
                    stt(B_[:, 6:7], B_[:, 0:1], 2.0, B_[:, 1:2], ALU.add, ALU.subtract, [bB], [bB])
                    ts("dve", dtab[sb_][:], pow2_t[:BS, :], B_[:, 6:7], None, ALU.mult, None, [bB, bc], [bB])
                    ts("dve", ndtab[sb_][:], dtab[sb_][:], -1.0, None, ALU.mult, None, [bB], [bB])
                    ts("dve", B_[:, 2:3], B_[:, 1:2], -1.0, 1.0, ALU.mult, ALU.add, [bB], [bB])
                    tt("dve", B_[:, 2:3], B_[:, 2:3], dtab[sb_][:, 0:1], ALU.subtract, [bB], [bB])

                def bisect(bi):
                    sb_ = bi % 2; sc = scoreb[sb_]; bs_ = bscb[sb_]; B_ = bis[sb_]; bB = bbis[sb_]
                    for r_ in range(NBIS):
                        act(Mb[:, bi, :], sc, AF.Sign, [bs_, bB], [bMb[bi], bB], bias=B_[:, 2:3], accum_out=B_[:, 3:4])
                        act(B_[:, 4:5], B_[:, 3:4], AF.Sign, [bB], [bB], bias=float(S - 2 * cf.TOPK) + 0.5)
                        act(B_[:, 2:3], B_[:, 4:5], AF.Identity, [bB], [bB], scale=ndtab[sb_][:, r_ + 1:r_ + 2], bias=B_[:, 2:3])

                def finalize(bi):
                    sb_ = bi % 2; sc = scoreb[sb_]; bs_ = bscb[sb_]; B_ = bis[sb_]; bB = bbis[sb_]
                    stt(B_[:, 5:6], B_[:, 2:3], -1.0, dtab[sb_][:, NBIS:NBIS + 1], ALU.mult, ALU.subtract, [bB], [bB])
                    ts("dve", Mb[:, bi, :], sc, B_[:, 5:6], NEGB, ALU.is_le, ALU.mult, [bs_, bB], [bMb[bi]])

                idx_block(0)
                for bi in range(TB):
                    bisect(bi)
                    if bi + 1 < TB:
                        idx_block(bi + 1)
                    finalize(bi)

                chk("T3")
                for h in range(NHD):
                    def S_step(c):
                        pz = c % 2
                        mm(pb[pz][:, 0:TW], kTds[:, c * 128:(c + 1) * 128], qTds[:, h, :], True, False,
                           [b_kTds, bqds], [bpb[pz]])
                        for bi in range(TB):
                            mm(pb[pz][:, bi * BS:(bi + 1) * BS], Mb[:, bi, c * 128:(c + 1) * 128], identb[:BS, :BS],
                               False, bi == TB - 1, [bMb[bi], bc], [bpb[pz]])
                        act(pbuf[pz][:], pb[pz][:, 0:TW], AF.Exp, [bpb[pz]], [bpbuf[pz]])

                    def PV_step(c):
                        pz = c % 2
                        mm(pb[2][:, 0:TW], vds[:, c, :], pbuf[pz][:], c == 0, c == NKB - 1, [b_vds, bpbuf[pz]], [bpb[2]])
                        mm(pb[3][:, 0:TW], onesb, pbuf[pz][:], c == 0, c == NKB - 1, [bc, bpbuf[pz]], [bpb[3]])

                    S_step(0)
                    for c in range(NKB):
                        if c + 1 < NKB:
                            S_step(c + 1)
                        PV_step(c)
                    em.op("dve", lambda e: e.reciprocal(out=rden[:], in_=pb[3][:, 0:TW]), [bpb[3]], [brden])
                    tt("dve", mrg[:, NHS + h, :], pb[2][:, 0:TW], rden[:], ALU.mult, [bpb[2], brden], [bmrg[NHS + h]])

                chk("T4")
                em.barrier()
                for c in range(NKB):
                    ts("dve", ytmp[:], qrow_t[:], float(-128 * c), 0.0, ALU.add, ALU.max, [bqrow], [bytmp])
                    ts("dve", Yt[:, c * TW:(c + 1) * TW], ytmp[:], kcol_t[:, 0:1], None, ALU.is_equal, None, [bytmp, bc], [bY])
                for h in range(NHS):
                    dma("sp", kTh, kTsb[h, :, :], (), [bkTh], "kTh")
                    dma("sp", vh, vsb[h, :, :], (), [bvh], "vh")
                    order = list(range(NKB - 1, -1, -1))

                    def Z_step(i):
                        c = order[i]; pz = 4 + i % 2
                        mm(pb[pz][:, 0:TW], kTh[:, c * 128:(c + 1) * 128], qTsb[:, h, :], True, False,
                           [bkTh, bqsb], [bpb[pz]])
                        mm(pb[pz][:, 0:TW], umat, Yt[:, c * TW:(c + 1) * TW], False, True, [bc, bY], [bpb[pz]])
                        act(ebuf[i % 2][:], pb[pz][:, 0:TW], AF.Exp, [bpb[pz]], [beb[i % 2]])
                        act(spb[i % 2][:], ebuf[i % 2][:], AF.Ln, [beb[i % 2]], [bspb[i % 2]], bias=1.0)

                    def X_step(i):
                        c = order[i]; px = 6 + i % 2
                        mm(pb[px][:, 0:TW], kTh[:, c * 128:(c + 1) * 128], qTsb[:, h, :], True, False,
                           [bkTh, bqsb], [bpb[px]])
                        mm(pb[px][:, 0:TW], umat, Yt[:, c * TW:(c + 1) * TW], False, False, [bc, bY], [bpb[px]])
                        if i > 0:
                            mm(pb[px][:, 0:TW], nones, Ab[i % 2][:], False, False, [bc, bAb[i % 2]], [bpb[px]])
                        mm(pb[px][:, 0:TW], ntri, spb[i % 2][:], False, True, [bc, bspb[i % 2]], [bpb[px]])
                        if i == 0:
                            cp("pool", Ab[1][:], spb[0][:], [bspb[0]], [bAb[1]])
                        elif i + 1 < NKB:
                            tt("pool", Ab[(i + 1) % 2][:], Ab[i % 2][:], spb[i % 2][:], ALU.add,
                               [bAb[i % 2], bspb[i % 2]], [bAb[(i + 1) % 2]])
                        act(wb[i % 2][:], pb[px][:, 0:TW], AF.Exp, [bpb[px]], [bwb[i % 2]])

                    def PVs(i):
                        c = order[i]
                        mm(pb[3][:, 0:TW], vh[:, c * 128:(c + 1) * 128], wb[i % 2][:], i == 0, i == NKB - 1, [bvh, bwb[i % 2]], [bpb[3]])

                    Z_step(0)
                    for i in range(NKB):
                        if i + 1 < NKB:
                            Z_step(i + 1)
                        X_step(i)
                        if i > 0:
                            PVs(i - 1)
                    PVs(NKB - 1)
                    cp("dve", mrg[:, h, :], pb[3][:, 0:TW], [bpb[3]], [bmrg[h]])

                chk("T5")
                for gi, (h0, nh_, g_t, W_) in enumerate(((0, NHS, sbg_t, SBW), (NHS, NHD, dsg_t, DSW))):
                    for hh in range(nh_):
                        act(gsq[:], mrg[:, h0 + hh, :], AF.Square, [bmrg[h0 + hh]], [bgsq])
                        mm(pb[0][:, 0:TW], onesb, gsq[:], hh == 0, hh == nh_ - 1, [bc, bgsq], [bpb[0]])
                    act(grs[:, 0, :], pb[0][:, 0:TW], AF.Sqrt, [bpb[0]], [bgrs], scale=1.0 / W_, bias=1e-6)
                    em.op("dve", lambda e: e.reciprocal(out=grs[:, 1, :], in_=grs[:, 0, :]), [bgrs], [bgrs])
                    for hh in range(nh_):
                        stt(mrg[:, h0 + hh, :], mrg[:, h0 + hh, :], g_t[:, hh:hh + 1], grs[:, 1, :], ALU.mult, ALU.mult,
                            [bmrg[h0 + hh], bgrs, bc], [bmrg[h0 + hh]])

                chk("T6")
                em.barrier()
                for bi in range(TB):
                    blk = t * TB + bi
                    dma("sp", xqb[bi], xq[blk * BS:(blk + 1) * BS, :], (), [bxq[bi]], "xqb%d" % bi)
                for gi, g in enumerate(range(0, D, 256)):
                    s = wq_load(wov, g, 256)
                    gs = gi % 2
                    dma("sp", g1t[gs][:], modrow[0:1, 2 * D + g:2 * D + g + 256].partition_broadcast(BS), (), [bg1[gs]], "g1t%d" % gs)
                    for bi in range(TB):
                        pbi = bi % 2
                        for k in range(NM):
                            mm(pb[pbi][:BS, 0:256], mrg[:, k, bi * BS:(bi + 1) * BS], wq[s][:, k, :], k == 0, k == NM - 1,
                               bmrg + [bwq[s]], [bpb[pbi]])
                        tt("dve", qtm[:, :], pb[pbi][:BS, 0:256], g1t[gs][:], ALU.mult, [bpb[pbi], bg1[gs]], [bqtm])
                        tt("dve", xqb[bi][:, g:g + 256], xqb[bi][:, g:g + 256], qtm[:, :], ALU.add, [bqtm, bxq[bi]], [bxq[bi]])
                for bi in range(TB):
                    blk = t * TB + bi
                    dma("pool", xmid[blk * BS:(blk + 1) * BS, :], xqb[bi], [bxq[bi]], (), "xmidw%d" % bi)
                    norm_T(xqb[bi], BS, bxq[bi], lambda k: h2st[:, k, :], bh2st, G2, SH2, (junk, ssq, bt), [2, 3])
                    dma("act", h2v[:, :, blk * BS:(blk + 1) * BS], h2st[:], [bh2st], (), "h2w")
                em.barrier()
        em.barrier()

        chk("T")
        wuv = w_up.rearrange("(k p) n -> p k n", p=128)
        wdv = w_down.rearrange("(k p) n -> p k n", p=128)
        with contextlib.ExitStack() as st:
            mT = sbt(st, "mT", [128, FC, OWN], BF16); bmT = Buf("mT")
            h2T = sbt(st, "h2T", [128, KC, TQ], BF16); b_h2T = Buf("h2T")
            dma("sp", h2T[:], h2Ts.rearrange("p (k t) -> p k t", k=KC), (), [b_h2T], "h2l")
            with contextlib.ExitStack() as st2:
                wu = [sbt(st2, "wu%d" % i, [128, KC, 256], BF16) for i in range(3)]; bwu = [Buf("wu%d" % i) for i in range(3)]
                usb = [sbt(st2, "usb%d" % i, [128, TQ], F32) for i in range(2)]; busb = [Buf("usb%d" % i) for i in range(2)]
                ucg = sbt(st2, "ucg", [128, OWN], F32); bucg = Buf("ucg")
                ucv = sbt(st2, "ucv", [128, OWN], F32); bucv = Buf("ucv")
                for f in range(FC):
                    s = f % 3
                    dma("pool", wu[s][:, :, 0:128], wuv[:, :, f * 128:(f + 1) * 128], (), [bwu[s]], "wu%d" % s)
                    dma("pool", wu[s][:, :, 128:256], wuv[:, :, DFF + f * 128:DFF + (f + 1) * 128], (), [bwu[s]], "wu%d" % s)
                    for half in range(2):
                        ci = f + half * FC
                        ub = usb[half]; bub = busb[half]
                        for t in range(NT):
                            pbi = (2 * t + half) % 4
                            for k in range(KC):
                                mm(pb[pbi][:, 0:TW], wu[s][:, k, half * 128:(half + 1) * 128], h2T[:, k, t * TW:(t + 1) * TW],
                                   k == 0, k == KC - 1, [bwu[s], b_h2T], [bpb[pbi]])
                            cp("act", ub[:, t * TW:(t + 1) * TW], pb[pbi][:, 0:TW], [bpb[pbi]], [bub])
                        ts("dve", ub[:, 0:2], ub[:, 0:2], flag_t[:, 0:1], None, ALU.mult, None, [bub, bc], [bub])
                        uc = ucg if half == 0 else ucv
                        buc = bucg if half == 0 else bucv
                        act(uc[:], ub[:, 2:TQ], AF.Identity, [bub, bc], [buc], scale=cvw_t[:, 2, ci:ci + 1], bias=cvb_t[:, ci:ci + 1])
                        stt(uc[:], ub[:, 1:TQ - 1], cvw_t[:, 1, ci:ci + 1], uc[:], ALU.mult, ALU.add, [bub, bc, buc], [buc])
                        stt(uc[:], ub[:, 0:TQ - 2], cvw_t[:, 0, ci:ci + 1], uc[:], ALU.mult, ALU.add, [bub, bc, buc], [buc])
                    act(ucg[:], ucg[:], AF.Silu, [bucg], [bucg])
                    tt("dve", mT[:, f, :], ucg[:], ucv[:], ALU.mult, [bucg, bucv], [bmT])
            em.barrier()
            with contextlib.ExitStack() as st2:
                wd = [sbt(st2, "wd%d" % i, [128, FC, 256], BF16) for i in range(2)]; bwd = [Buf("wd%d" % i) for i in range(2)]
                yst = [sbt(st2, "yst%d" % i, [128, 256], F32) for i in range(2)]; byst = [Buf("yst%d" % i) for i in range(2)]
                yc = 0
                for gi, g in enumerate(range(0, D, 256)):
                    s = gi % 2
                    for f0 in range(0, FC, 16):
                        f1 = min(FC, f0 + 16)
                        dma("pool", wd[s][:, f0:f1, :], wdv[:, f0:f1, g:g + 256], (), [bwd[s]], "wd%d" % s)
                    for tb in range(NOB):
                        pbi = tb % 2
                        for f in range(FC):
                            mm(pb[pbi][:, 0:256], mT[:, f, tb * 128:(tb + 1) * 128], wd[s][:, f, :], f == 0, f == FC - 1,
                               [bmT, bwd[s]], [bpb[pbi]])
                        ys = yc % 2; yc += 1
                        cp("act", yst[ys][:], pb[pbi][:, 0:256], [bpb[pbi]], [byst[ys]])
                        dma("act", yscr[tb * 128:(tb + 1) * 128, g:g + 256], yst[ys][:], [byst[ys]], (), "yscrw%d" % ys)
        em.barrier()
        with contextlib.ExitStack() as st:
            g2bc = sbt(st, "g2bc", [128, D], F32); fngt = sbt(st, "fngt", [128, D], F32); bgg = Buf("g2fng")
            xm = [sbt(st, "xm%d" % i, [128, D], F32) for i in range(2)]; bxm = [Buf("xm%d" % i) for i in range(2)]
            yy = [sbt(st, "yy%d" % i, [128, D], F32) for i in range(2)]; byy = [Buf("yy%d" % i) for i in range(2)]
            junk = sbt(st, "junkF", [128, D], BF16); ssq = sbt(st, "ssqF", [128, 4], F32); bt = Buf("nrmF")
            dma("sp", g2bc[:], modrow[0:1, 5 * D:6 * D].partition_broadcast(128), (), [bgg], "g2bc")
            dma("sp", fngt[:], fng[:, :], (), [bgg], "g2bc")
            for tb in range(NOB):
                s = tb % 2
                dma("sp", xm[s][:], xmid[2 + tb * 128:2 + (tb + 1) * 128, :], (), [bxm[s]], "xm%d" % s)
                dma("sp", yy[s][:], yscr[tb * 128:(tb + 1) * 128, :], (), [byy[s]], "yy%d" % s)
                tt("dve", yy[s][:], yy[s][:], g2bc[:], ALU.mult, [byy[s], bgg], [byy[s]])
                tt("dve", xm[s][:], xm[s][:], yy[s][:], ALU.add, [byy[s], bxm[s]], [bxm[s]])
                act(junk[:], xm[s][:], AF.Square, [bxm[s]], [bt], accum_out=ssq[:, 0:1])
                act(ssq[:, 1:2], ssq[:, 0:1], AF.Sqrt, [bt], [bt], scale=1.0 / D, bias=1e-6)
                em.op("dve", lambda e: e.reciprocal(out=ssq[:, 2:3], in_=ssq[:, 1:2]), [bt], [bt])
                stt(yy[s][:], xm[s][:], ssq[:, 2:3], fngt[:], ALU.mult, ALU.mult, [bxm[s], bt, bgg], [byy[s]])
                dma("pool", out[tb * 128:(tb + 1) * 128, :], yy[s][:], [byy[s]], (), "outw%d" % s)
        em.emit(final_waits=["outw0", "outw1"] if NOB > 1 else ["outw0"])
    return nc


def host_inputs(cf, inp, core):
    D, S, KC, NKB, TQ, BS, NBLK, NSP, FC = cf.D, cf.S, cf.KC, cf.NKB, cf.TQ, cf.BS, cf.NBLK, cf.NSP, cf.FC
    b = core // cf.CPB; j = core % cf.CPB
    f32 = np.float32
    x = np.asarray(inp["x"], f32); pos = np.asarray(inp["positions"], np.int32)
    t0 = j * cf.OWN - 2
    tok = np.arange(t0, t0 + TQ)
    tokc = np.maximum(tok, 0)
    m = {}
    m["xkv"] = np.ascontiguousarray(x[b])
    m["xq"] = np.ascontiguousarray(x[b][tokc])
    m["posk"] = np.ascontiguousarray(pos[b].reshape(NKB, 128).T)
    m["posq"] = np.ascontiguousarray(pos[b][tokc].reshape(NBLK, BS).T)
    qc = tokc.astype(f32).reshape(NBLK, BS).T
    m["qcol"] = np.ascontiguousarray(qc)
    qr = qc[:, :, None] - (512.0 * np.arange(NSP, dtype=f32))[None, None, :]
    m["qrel"] = np.ascontiguousarray(qr.reshape(BS, NBLK * NSP).astype(f32))
    m["qrow"] = np.ascontiguousarray(np.broadcast_to(tokc.astype(f32)[None, :], (128, TQ)))
    pk = lambda v: np.ascontiguousarray(np.asarray(v, f32).reshape(-1, 128).T)
    m["cb"] = pk(np.asarray(inp["c"], f32)[b])
    m["w_ada"] = np.ascontiguousarray(np.asarray(inp["w_ada"], f32)[0])
    m["b_ada"] = np.ascontiguousarray(np.asarray(inp["b_ada"], f32)[0][None, :])
    m["w_in"] = np.ascontiguousarray(np.asarray(inp["w_in"], f32)[0])
    m["w_out"] = np.ascontiguousarray(np.asarray(inp["w_out"], f32)[0])
    m["w_up"] = np.ascontiguousarray(np.asarray(inp["w_up"], f32)[0])
    m["w_down"] = np.ascontiguousarray(np.asarray(inp["w_down"], f32)[0])
    m["n1g"] = pk(np.asarray(inp["norm1_g"])[0]); m["n2g"] = pk(np.asarray(inp["norm2_g"])[0])
    m["sbg"] = pk(np.asarray(inp["sb_norm_g"])[0]); m["dsg"] = pk(np.asarray(inp["dsa_norm_g"])[0])
    m["fng"] = np.ascontiguousarray(np.broadcast_to(np.asarray(inp["final_norm_g"], f32)[None, :], (128, D)))
    cw = np.asarray(inp["conv_w"], f32)[0]
    m["convw"] = np.ascontiguousarray(np.stack([pk(cw[i]) for i in range(3)], axis=1).reshape(128, 3 * 2 * FC))
    m["convb"] = pk(np.asarray(inp["conv_b"], f32)[0])
    m["identf"] = np.eye(128, dtype=f32)
    jj = np.arange(128)[:, None]; ss = np.arange(128)[None, :]
    cm = np.stack([np.eye(128, dtype=f32), -(jj >= ss).astype(f32), -np.ones((128, 128), f32),
                   NEGB * (ss >= jj).astype(f32), np.ones((128, 128), f32)], axis=1)
    m["cmat"] = np.ascontiguousarray(cm.reshape(128, 5 * 128))
    m["iota"] = np.ascontiguousarray(np.broadcast_to(np.arange(512, dtype=f32)[None, :], (128, 512)))
    m["kcolc"] = np.arange(128, dtype=f32)[:, None].copy()
    m["pow2"] = np.ascontiguousarray(np.broadcast_to((0.5 ** np.arange(1, cf.NBIS + 2)).astype(f32)[None, :], (128, cf.NBIS + 1)))
    ivf = (np.float32(500000.0) ** (-(np.arange(16, dtype=f32) / np.float32(16)))).astype(f32)
    m["invf"] = np.ascontiguousarray(np.broadcast_to(ivf[None, :], (128, 16)))
    m["flag"] = np.full((128, 1), 0.0 if j == 0 else 1.0, f32)
    return m


_CACHE = {}


def kernel(x, c, positions, w_ada, b_ada, norm1_g, w_in, sb_norm_g, dsa_norm_g, w_out, norm2_g,
           w_up, conv_w, conv_b, w_down, final_norm_g):
    inp = dict(x=x, c=c, positions=positions, w_ada=w_ada, b_ada=b_ada, norm1_g=norm1_g, w_in=w_in,
               sb_norm_g=sb_norm_g, dsa_norm_g=dsa_norm_g, w_out=w_out, norm2_g=norm2_g, w_up=w_up,
               conv_w=conv_w, conv_b=conv_b, w_down=w_down, final_norm_g=final_norm_g)
    cf = Cfg()
    nc = build_program(cf)
    in_maps = [host_inputs(cf, inp, core) for core in range(8)]
    res = run_bass_kernel_spmd(nc, in_maps, core_ids=list(range(8)))
    outp = np.zeros((2, cf.S, cf.D), np.float32)
    for core in range(8):
        b = core // cf.CPB; j = core % cf.CPB
        outp[b, j * cf.OWN:(j + 1) * cf.OWN, :] = np.asarray(res.results[core]["out"], np.float32)
    return outp
```

```python
import contextlib
import numpy as np
import concourse.bass as bass
import concourse.mybir as mybir
from concourse.bass_utils import run_bass_kernel_spmd

F32 = mybir.dt.float32
BF16 = mybir.dt.bfloat16
I32 = mybir.dt.int32
AF = mybir.ActivationFunctionType
ALU = mybir.AluOpType
AX = mybir.AxisListType

ENGS = ("pe", "act", "dve", "pool", "sp")


class Buf:
    __slots__ = ("name", "lw", "rs")

    def __init__(self, name):
        self.name = name
        self.lw = None
        self.rs = []


class Op:
    __slots__ = ("eng", "idx", "fn", "deps", "dma", "stream", "gen", "flag", "incidx")

    def __init__(self, eng, idx, fn, dma, stream):
        self.eng = eng; self.idx = idx; self.fn = fn; self.deps = []
        self.dma = dma; self.stream = stream; self.gen = 0
        self.flag = False; self.incidx = 0


class Em:
    def __init__(self, nc):
        self.nc = nc
        self.ops = {e: [] for e in ENGS}
        self.streams = {}
        self.last_dma = {}
        self.pending = {e: None for e in ENGS}
        self.pool_dmas = []

    def barrier(self):
        lasts = [self.ops[e][-1] for e in ENGS if self.ops[e]]
        lasts += list(self.last_dma.values())
        for e in ENGS:
            self.pending[e] = lasts

    def op(self, eng, fn, reads=(), writes=(), dma=False, stream=None):
        o = Op(eng, len(self.ops[eng]), fn, dma, stream)
        deps = []
        if self.pending[eng] is not None:
            deps.extend(self.pending[eng])
            self.pending[eng] = None
        if dma and eng == "pool":
            if len(self.pool_dmas) >= 4:
                deps.append(self.pool_dmas[-4])
            self.pool_dmas.append(o)
        for b in reads:
            if b.lw is not None:
                deps.append(b.lw)
        for b in writes:
            if b.lw is not None:
                deps.append(b.lw)
            deps.extend(b.rs)
        for b in reads:
            b.rs.append(o)
        for b in writes:
            b.lw = o
            b.rs = []
        if dma:
            g = self.streams.get(stream, 0) + 1
            self.streams[stream] = g
            o.gen = g
            self.last_dma[stream] = o
        best = {}
        for d in deps:
            if d is o:
                continue
            if d.dma:
                key = ("dma", d.stream)
                if key not in best or best[key].gen < d.gen:
                    best[key] = d
            else:
                if d.eng == eng and eng == "pe":
                    continue
                key = ("eng", d.eng)
                if key not in best or best[key].idx < d.idx:
                    best[key] = d
        o.deps = list(best.values())
        self.ops[eng].append(o)
        return o

    def emit(self, final_waits=()):
        nc = self.nc
        for e in ENGS:
            seen = {}
            for o in self.ops[e]:
                nd = []
                for d in o.deps:
                    key = ("dma", d.stream) if d.dma else ("eng", d.eng)
                    val = d.gen if d.dma else d.idx
                    if seen.get(key, -1) >= val:
                        continue
                    seen[key] = val
                    nd.append(d)
                    if not d.dma:
                        d.flag = True
                o.deps = nd
        for e in ENGS:
            c = 0
            for o in self.ops[e]:
                if o.flag and not o.dma:
                    c += 1
                    o.incidx = c
        with contextlib.ExitStack() as st:
            esem = {e: st.enter_context(nc.semaphore("s_" + e)) for e in ENGS}
            dsem = {s: st.enter_context(nc.semaphore("d_%d" % i)) for i, s in enumerate(self.streams)}
            block = st.enter_context(nc.Block())

            def run(e, eng):
                for o in self.ops[e]:
                    for d in o.deps:
                        if d.dma:
                            eng.wait_ge(dsem[d.stream], 16 * d.gen)
                        else:
                            eng.wait_ge(esem[d.eng], d.incidx)
                    ins = o.fn(eng)
                    if o.dma:
                        ins.then_inc(dsem[o.stream], 16)
                    elif o.flag:
                        ins.then_inc(esem[e], 1)
                if e == "sp":
                    for s in final_waits:
                        eng.wait_ge(dsem[s], 16 * self.streams[s])

            @block.tensor
            def _(eng):
                run("pe", eng)

            @block.scalar
            def _(eng):
                run("act", eng)

            @block.vector
            def _(eng):
                run("dve", eng)

            @block.gpsimd
            def _(eng):
                run("pool", eng)

            @block.sync
            def _(eng):
                run("sp", eng)


class Cfg:
    def __init__(self, D=2048, S=4096, DFF=5504, BS=114, NBLK=9, TB=3, NBIS=26, IH=16, ID=64,
                 TOPK_MAX=256, CPB=4):
        self.D, self.S, self.DFF = D, S, DFF
        self.HD = 128
        nh = D // 128
        self.NHS = nh // 2
        self.NHD = nh - self.NHS
        self.SBW = self.NHS * 128
        self.DSW = self.NHD * 128
        self.IH, self.ID = IH, ID
        self.TOPK = min(TOPK_MAX, S // 4)
        self.CPB = CPB
        self.OWN = S // CPB
        self.TQ = self.OWN + 2
        self.BS, self.NBLK, self.TB = BS, NBLK, TB
        assert BS * NBLK == self.TQ and NBLK % TB == 0 and BS <= 128
        self.NT = NBLK // TB
        self.TW = TB * BS
        assert self.TW <= 512
        self.KC = D // 128
        self.NKB = S // 128
        self.FC = DFF // 128
        assert DFF % 128 == 0 and S % 512 == 0 and self.OWN % 128 == 0
        self.NOB = self.OWN // 128
        self.NBIS = NBIS
        o = 0
        self.o_qsb = o; o += self.SBW
        self.o_ksb = o; o += self.SBW
        self.o_vsb = o; o += self.SBW
        self.o_qds = o; o += self.DSW
        self.o_kds = o; o += 128
        self.o_vds = o; o += 128
        self.o_qix = o; o += IH * ID
        self.o_kix = o; o += ID
        self.o_wix = o; o += IH
        self.INC = o
        self.NSP = S // 512
        self.idx_scale = (IH ** -0.5) * (ID ** -0.5)
        self.qk_scale = 128 ** -0.5
        self.stop = None


class _Stop(Exception):
    pass


TWO_PI = 6.283185307179586
PI = 3.141592653589793
NEGB = -30000.0


def build_program(cf):
    hold = {}
    try:
        return _build(cf, hold)
    except _Stop:
        hold["em"].emit(final_waits=[])
        return hold["nc"]


def _build(cf, hold):
    nc = bass.Bass("TRN2", target_bir_lowering=False)
    em = Em(nc)
    hold["nc"] = nc; hold["em"] = em
    D, S, KC, NKB, TQ, BS, NBLK, TB, NT, TW = cf.D, cf.S, cf.KC, cf.NKB, cf.TQ, cf.BS, cf.NBLK, cf.TB, cf.NT, cf.TW
    NHS, NHD, SBW, DSW, IH, ID, FC, DFF = cf.NHS, cf.NHD, cf.SBW, cf.DSW, cf.IH, cf.ID, cf.FC, cf.DFF
    NSP, NBIS, OWN, NOB = cf.NSP, cf.NBIS, cf.OWN, cf.NOB
    NM = NHS + NHD

    def din(name, shape, dt=F32):
        return nc.dram_tensor(name, list(shape), dt, kind="ExternalInput").ap()

    def dscr(name, shape, dt):
        return nc.dram_tensor(name, list(shape), dt, kind="Internal").ap()

    xkv = din("xkv", [S, D]); xq = din("xq", [TQ, D])
    posk = din("posk", [128, NKB], I32); posq = din("posq", [BS, NBLK], I32)
    qcol = din("qcol", [BS, NBLK]); qrel = din("qrel", [BS, NBLK * NSP]); qrow = din("qrow", [128, TQ])
    cb = din("cb", [128, KC])
    w_ada = din("w_ada", [D, 6 * D]); b_ada = din("b_ada", [1, 6 * D])
    w_in = din("w_in", [D, cf.INC]); w_out = din("w_out", [D, D])
    w_up = din("w_up", [D, 2 * DFF]); w_down = din("w_down", [DFF, D])
    n1g = din("n1g", [128, KC]); n2g = din("n2g", [128, KC])
    sbg = din("sbg", [128, NHS]); dsg = din("dsg", [128, NHD]); fng = din("fng", [128, D])
    convw = din("convw", [128, 3 * 2 * FC]); convb = din("convb", [128, 2 * FC])
    identf = din("identf", [128, 128]); cmat = din("cmat", [128, 5 * 128])
    iota = din("iota", [128, 512]); kcolc = din("kcolc", [128, 1]); pow2 = din("pow2", [128, NBIS + 1])
    invf = din("invf", [128, 16]); flag = din("flag", [128, 1])
    out = nc.dram_tensor("out", [OWN, D], F32, kind="ExternalOutput").ap()
    modrow = dscr("modrow", [1, 6 * D], F32)
    kTsb = dscr("kTsb", [NHS, 128, S], BF16)
    vsb = dscr("vsb", [NHS, 128, NKB * 128], BF16)
    xmid = dscr("xmid", [TQ, D], F32)
    yscr = dscr("yscr", [OWN, D], F32)
    h2Ts = dscr("h2Ts", [128, KC * TQ], BF16)

    def mm(o, lhsT, rhs, start, stop, r, w):
        em.op("pe", lambda e: e.matmul(o, lhsT=lhsT, rhs=rhs, start=start, stop=stop), r, w)

    def tr(o, in_, ident, r, w):
        em.op("pe", lambda e: e.transpose(o, in_, ident), r, w)

    def act(o, in_, func, r, w, **kw):
        em.op("act", lambda e: e.activation(out=o, in_=in_, func=func, **kw), r, w)

    def ts(eng, o, in0, s1, s2, op0, op1, r, w, accum_out=None):
        if op1 is None:
            em.op(eng, lambda e: e.tensor_scalar(out=o, in0=in0, scalar1=s1, scalar2=None, op0=op0), r, w)
        elif accum_out is None:
            em.op(eng, lambda e: e.tensor_scalar(out=o, in0=in0, scalar1=s1, scalar2=s2, op0=op0, op1=op1), r, w)
        else:
            em.op(eng, lambda e: e.tensor_scalar(out=o, in0=in0, scalar1=s1, scalar2=s2, op0=op0, op1=op1,
                                                 accum_out=accum_out), r, w)

    def tt(eng, o, in0, in1, op, r, w):
        em.op(eng, lambda e: e.tensor_tensor(out=o, in0=in0, in1=in1, op=op), r, w)

    def stt(o, in0, sc, in1, op0, op1, r, w):
        em.op("dve", lambda e: e.scalar_tensor_tensor(out=o, in0=in0, scalar=sc, in1=in1, op0=op0, op1=op1), r, w)

    def cp(eng, o, in_, r, w):
        if eng == "act":
            em.op("act", lambda e: e.copy(out=o, in_=in_), r, w)
        else:
            em.op(eng, lambda e: e.tensor_copy(out=o, in_=in_), r, w)

    def memset(eng, o, val, w):
        em.op(eng, lambda e: e.memset(o, val), (), w)

    def dma(q, o, in_, r, w, stream, slow=False):
        if slow:
            em.op(q, lambda e: e.dma_start(out=o, in_=in_, allow_slow_non_contiguous=True), r, w, dma=True, stream=stream)
        else:
            em.op(q, lambda e: e.dma_start(out=o, in_=in_), r, w, dma=True, stream=stream)

    def chk(tag):
        if cf.stop == tag:
            raise _Stop()

    with contextlib.ExitStack() as gst:
        def sbt(st, name, shape, dt):
            return st.enter_context(nc.sbuf_tensor("s_" + name, list(shape), dt))

        PS = gst.enter_context(nc.psum_tensor("PS", [128, 8, 512], F32))
        pb = [PS[:, i, :] for i in range(8)]
        bpb = [Buf("pb%d" % i) for i in range(8)]

        identf_t = sbt(gst, "identf", [128, 128], F32)
        cm_t = sbt(gst, "cmat", [128, 5, 128], BF16)
        iota_t = sbt(gst, "iota", [128, 512], F32)
        kcol_t = sbt(gst, "kcolc", [128, 1], F32)
        pow2_t = sbt(gst, "pow2", [128, NBIS + 1], F32)
        flag_t = sbt(gst, "flag", [128, 1], F32)
        vec_t = sbt(gst, "vecs", [128, 8, KC], F32)
        sbg_t = sbt(gst, "sbg", [128, NHS], F32)
        dsg_t = sbt(gst, "dsg", [128, NHD], F32)
        cvw_t = sbt(gst, "cvw", [128, 3, 2 * FC], F32)
        cvb_t = sbt(gst, "cvb", [128, 2 * FC], F32)
        qcol_t = sbt(gst, "qcol", [BS, NBLK], F32)
        qrel_t = sbt(gst, "qrel", [BS, NBLK, NSP], F32)
        cosK = sbt(gst, "cosK", [128, NKB, 16], F32); sinK = sbt(gst, "sinK", [128, NKB, 16], F32)
        cosQ = sbt(gst, "cosQ", [BS, NBLK, 16], F32); sinQ = sbt(gst, "sinQ", [BS, NBLK, 16], F32)
        kTds = sbt(gst, "kTds", [128, S], BF16)
        vds = sbt(gst, "vds", [128, NKB, 128], BF16)
        kTix = sbt(gst, "kTix", [128, S], BF16)
        bc = Buf("consts")
        b_vec = Buf("vecs"); b_rope = Buf("rope")
        b_kTds = Buf("kTds"); b_vds = Buf("vds"); b_kTix = Buf("kTix")

        for (t_, src, nm) in ((identf_t[:], identf[:, :], "c0"), (iota_t[:], iota[:, :], "c1"),
                              (kcol_t[:], kcolc[:, :], "c2"), (pow2_t[:], pow2[:, :], "c3"),
                              (flag_t[:], flag[:, :], "c4"), (sbg_t[:], sbg[:, :], "c5"), (dsg_t[:], dsg[:, :], "c6"),
                              (cvw_t[:], convw.rearrange("p (a f) -> p a f", a=3), "c7"), (cvb_t[:], convb[:, :], "c8"),
                              (qcol_t[:], qcol[:, :], "c9"),
                              (qrel_t[:], qrel.rearrange("p (a f) -> p a f", a=NBLK), "c10"),
                              (vec_t[:, 4, :], n1g[:, :], "c11"), (vec_t[:, 5, :], n2g[:, :], "c12")):
            dma("sp", t_, src, (), [bc], nm)
        dma("pool", cm_t[:], cmat.rearrange("p (a f) -> p a f", a=5), (), [bc], "c13")
        identb = cm_t[:, 0, :]; ntri = cm_t[:, 1, :]; nones = cm_t[:, 2, :]; umat = cm_t[:, 3, :]; onesb = cm_t[:, 4, :]

        def rope_tables(st, pos_ap, P, n, cos_t, sin_t, tag):
            pi_ = sbt(st, "pi" + tag, [P, n], I32)
            pf = sbt(st, "pf" + tag, [P, n], F32)
            ang = sbt(st, "ang" + tag, [P, n, 16], F32)
            ki = sbt(st, "ki" + tag, [P, n, 16], I32)
            kf = sbt(st, "kf" + tag, [P, n, 16], F32)
            tm = sbt(st, "tm" + tag, [P, n, 16], F32)
            ivf = sbt(st, "ivf" + tag, [P, 16], F32)
            b = Buf("ropetmp" + tag)
            dma("sp", pi_[:], pos_ap, (), [b], "rp" + tag)
            dma("sp", ivf[:], invf[0:P, :], (), [b], "rp" + tag)
            cp("dve", pf[:], pi_[:], [b], [b])
            for c in range(n):
                ts("dve", ang[:, c, :], ivf[:], pf[:, c:c + 1], None, ALU.mult, None, [b], [b])

            def reduce_sin(dst, shift):
                a2 = ang[:]
                if shift != 0.0:
                    ts("dve", tm[:], ang[:], shift, None, ALU.add, None, [b], [b])
                    a2 = tm[:]
                ts("dve", kf[:], a2, 1.0 / TWO_PI, None, ALU.mult, None, [b], [b])
                cp("dve", ki[:], kf[:], [b], [b])
                cp("dve", kf[:], ki[:], [b], [b])
                stt(tm[:], kf[:], -TWO_PI, a2, ALU.mult, ALU.add, [b], [b])
                ts("dve", kf[:], tm[:], PI, -TWO_PI, ALU.is_gt, ALU.mult, [b], [b])
                tt("dve", tm[:], tm[:], kf[:], ALU.add, [b], [b])
                ts("dve", kf[:], tm[:], -PI, TWO_PI, ALU.is_lt, ALU.mult, [b], [b])
                tt("dve", tm[:], tm[:], kf[:], ALU.add, [b], [b])
                act(dst, tm[:], AF.Sin, [b], [b_rope])

            reduce_sin(sin_t[:], 0.0)
            reduce_sin(cos_t[:], PI / 2)

        with contextlib.ExitStack() as st:
            rope_tables(st, posk[:, :], 128, NKB, cosK, sinK, "k")
            rope_tables(st, posq[:, :], BS, NBLK, cosQ, sinQ, "q")
        em.barrier()

        rope_bufs = {}

        def rope_apply(x1, x2, cos_ap, sin_ap, tmp, P, h, r, w):
            bt_ = rope_bufs.setdefault(id(tmp), Buf("ropetmp"))
            r = list(r) + [bt_]; w = list(w) + [bt_]
            tt("dve", tmp[:P, 0, :h], x1, cos_ap, ALU.mult, r, w)
            tt("dve", tmp[:P, 1, :h], x2, sin_ap, ALU.mult, r, w)
            tt("dve", tmp[:P, 2, :h], x2, cos_ap, ALU.mult, r, w)
            tt("dve", tmp[:P, 3, :h], x1, sin_ap, ALU.mult, r, w)
            tt("dve", x1, tmp[:P, 0, :h], tmp[:P, 1, :h], ALU.subtract, r, w)
            tt("dve", x2, tmp[:P, 2, :h], tmp[:P, 3, :h], ALU.add, r, w)

        nrm_ctr = [0]

        def norm_T(xt_ap, P, bx, dst_fn, bdst, G_ap, sh_ap, tmps, pbank, xs_out=None, bxs=None):
            junk, ssq, bt = tmps
            act(junk[:P, :], xt_ap, AF.Square, [bx], [bt], accum_out=ssq[:P, 0:1])
            act(ssq[:P, 1:2], ssq[:P, 0:1], AF.Sqrt, [bt], [bt], scale=1.0 / D, bias=1e-6)
            em.op("dve", lambda e: e.reciprocal(out=ssq[:P, 2:3], in_=ssq[:P, 1:2]), [bt], [bt])
            if xs_out is None:
                ts("dve", xt_ap, xt_ap, ssq[:P, 2:3], None, ALU.mult, None, [bx, bt], [bx])
            else:
                ts("dve", xs_out, xt_ap, ssq[:P, 2:3], None, ALU.mult, None, [bx, bt], [bxs])
                xt_ap = xs_out; bx = bxs
            for k0 in range(0, KC, 4):
                nj = min(4, KC - k0)
                pbi = pbank[nrm_ctr[0] % len(pbank)]
                nrm_ctr[0] += 1
                for j in range(nj):
                    tr(pb[pbi][:, j * 128:j * 128 + P], xt_ap[:, (k0 + j) * 128:(k0 + j + 1) * 128],
                       identf_t[:P, :P], [bx, bc], [bpb[pbi]])
                for j in range(nj):
                    k = k0 + j
                    if j % 2 == 0:
                        act(dst_fn(k), pb[pbi][:, j * 128:j * 128 + P], AF.Identity, [bpb[pbi], b_vec], [bdst],
                            scale=G_ap[:, k:k + 1], bias=sh_ap[:, k:k + 1])
                    else:
                        ts("dve", dst_fn(k), pb[pbi][:, j * 128:j * 128 + P], G_ap[:, k:k + 1], sh_ap[:, k:k + 1],
                           ALU.mult, ALU.add, [bpb[pbi], b_vec], [bdst])

        winv = w_in.rearrange("(k p) n -> p k n", p=128)
        G1 = vec_t[:, 0, :]; SH1 = vec_t[:, 1, :]; G2 = vec_t[:, 2, :]; SH2 = vec_t[:, 3, :]
        with contextlib.ExitStack() as st:
            MG = 256
            NG = (6 * D) // MG
            NG1 = (2 * D) // MG
            wg = [sbt(st, "wg%d" % i, [128, KC, MG], BF16) for i in range(2)]
            bwg = [Buf("wg%d" % i) for i in range(2)]
            cb_t = sbt(st, "cb", [128, KC], F32); cs_t = sbt(st, "cs", [128, KC], BF16)
            brow = [sbt(st, "brow%d" % i, [1, MG], F32) for i in range(2)]
            mrow = [sbt(st, "mrow%d" % i, [1, MG], F32) for i in range(2)]
            bbr = [Buf("brow%d" % i) for i in range(2)]; bmr = [Buf("mrow%d" % i) for i in range(2)]
            bcs = Buf("cs"); bmodg = [Buf("modrow%d" % i) for i in range(NG)]
            mrows = sbt(st, "mrows", [KC, 4, 128], F32); bmrows = Buf("mrows")
            dma("sp", cb_t[:], cb[:, :], (), [bcs], "cb")
            act(cs_t[:], cb_t[:], AF.Silu, [bcs], [bcs])
            wav = w_ada.rearrange("(k p) n -> p k n", p=128)

            def mod_load(g):
                s2 = g % 2
                dma("pool", wg[s2][:], wav[:, :, g * MG:(g + 1) * MG], (), [bwg[s2]], "wg%d" % s2)
                dma("pool", brow[s2][:], b_ada[0:1, g * MG:(g + 1) * MG], (), [bbr[s2]], "brow%d" % s2)

            def mod_compute(g, pbi):
                s2 = g % 2
                for k in range(KC):
                    mm(pb[pbi][0:1, 0:MG], cs_t[:, k:k + 1], wg[s2][:, k, :], k == 0, k == KC - 1,
                       [bcs, bwg[s2]], [bpb[pbi]])
                tt("dve", mrow[s2][:], pb[pbi][0:1, 0:MG], brow[s2][:], ALU.add, [bpb[pbi], bbr[s2]], [bmr[s2]])
                dma("pool", modrow[0:1, g * MG:(g + 1) * MG], mrow[s2][:], [bmr[s2]], [bmodg[g]], "modw%d" % s2)

            def mod_vec(i_, a_, slot, pbi):
                gs = [bmodg[g] for g in range(a_ * D // MG, (a_ + 1) * D // MG)]
                dma("sp", mrows[:, i_, :], modrow[0:1, a_ * D:(a_ + 1) * D].rearrange("o (k p) -> (o k) p", p=128),
                    gs, [bmrows], "mv")
                tr(pb[pbi][:, i_ * KC:(i_ + 1) * KC], mrows[:, i_, :], identf_t[:KC, :KC], [bmrows, bc], [bpb[pbi]])
                cp("dve", vec_t[:, slot, :], pb[pbi][:, i_ * KC:(i_ + 1) * KC], [bpb[pbi]], [b_vec])

            WAW = 2 * SBW + 320
            wA = sbt(st, "wA", [128, KC, WAW], BF16); bwA = Buf("wA")
            mod_load(0)
            if NG1 > 1:
                mod_load(1)
            for g in range(NG1):
                mod_compute(g, 7)
                if g + 2 < NG1:
                    mod_load(g + 2)
            c0 = 0
            for (src0, n) in ((cf.o_ksb, SBW), (cf.o_vsb, SBW), (cf.o_kds, 256), (cf.o_kix, 64)):
                for a in range(0, n, 512):
                    m = min(512, n - a)
                    dma("pool", wA[:, :, c0 + a:c0 + a + m], winv[:, :, src0 + a:src0 + a + m], (), [bwA], "wA")
                c0 += n
            mod_vec(0, 0, 1, 6)
            mod_vec(1, 1, 6, 6)
            stt(vec_t[:, 0, :], vec_t[:, 6, :], 1.0, vec_t[:, 4, :], ALU.add, ALU.mult, [b_vec, bc], [b_vec])
            gnext = [NG1]
            if gnext[0] < NG:
                mod_load(gnext[0])
            if gnext[0] + 1 < NG:
                mod_load(gnext[0] + 1)

            def mod_step():
                g = gnext[0]
                if g >= NG:
                    return
                mod_compute(g, 7)
                if g + 2 < NG:
                    mod_load(g + 2)
                gnext[0] += 1

            xt = [sbt(st, "xt%d" % i, [128, D], F32) for i in range(3)]; bxt = [Buf("xt%d" % i) for i in range(3)]
            junk = sbt(st, "junkA", [128, D], BF16); ssq = sbt(st, "ssqA", [128, 4], F32)
            bt = Buf("nrmtmpA")
            hTa = [sbt(st, "hTa%d" % i, [128, KC, 512], BF16) for i in range(2)]; bhT = [Buf("hTa%d" % i) for i in range(2)]
            vst = [sbt(st, "vst%d" % i, [128, SBW], BF16) for i in range(2)]; bvst = [Buf("vst%d" % i) for i in range(2)]
            kst = [sbt(st, "kst%d" % i, [128, 512], BF16) for i in range(2)]; bkst = [Buf("kst%d" % i) for i in range(2)]
            sm = [sbt(st, "smA%d" % i, [128, 384], F32) for i in range(2)]; bsm = [Buf("smA%d" % i) for i in range(2)]
            rtmp = sbt(st, "rtmpA", [128, 4, 16], F32)
            vsbv = vsb.rearrange("h p x -> p h x")
            kctr = 0
            per_blk = -(-(NG - NG1) // NKB)

            def prep(c):
                s3 = c % 3; ti = (c // 4) % 2; cc = c % 4
                dma("sp", xt[s3][:], xkv[c * 128:(c + 1) * 128, :], (), [bxt[s3]], "xt%d" % s3)
                norm_T(xt[s3][:], 128, bxt[s3], lambda k, ti=ti, cc=cc: hTa[ti][:, k, cc * 128:(cc + 1) * 128],
                       bhT[ti], G1, SH1, (junk, ssq, bt), [0, 1, 6])

            def ktr(c):
                tr(pb[5][:, 0:128], sm[c % 2][:, 0:128], identf_t[:], [bsm[c % 2], bc], [bpb[5]])
                tr(pb[5][:, 128:256], sm[c % 2][:, 256:384], identf_t[:], [bsm[c % 2], bc], [bpb[5]])
                cp("act", kTds[:, c * 128:(c + 1) * 128], pb[5][:, 0:128], [bpb[5]], [b_kTds])
                cp("act", kTix[:, c * 128:(c + 1) * 128], pb[5][:, 128:256], [bpb[5]], [b_kTix])

            prep(0)
            if NKB > 1:
                prep(1)
            for c in range(NKB):
                s2 = c % 2
                ti = (c // 4) % 2
                cc = c % 4
                for g in range(0, SBW, 512):
                    pbi = 2 + (g // 512) % 2
                    n = min(512, SBW - g)
                    for k in range(KC):
                        mm(pb[pbi][:, 0:n], hTa[ti][:, k, cc * 128:(cc + 1) * 128], wA[:, k, SBW + g:SBW + g + n],
                           k == 0, k == KC - 1, [bhT[ti], bwA], [bpb[pbi]])
                    cp("act", vst[s2][:, g:g + n], pb[pbi][:, 0:n], [bpb[pbi]], [bvst[s2]])
                dma("act", vsbv[:, :, c * 128:(c + 1) * 128], vst[s2][:].rearrange("p (h d) -> p h d", h=NHS),
                    [bvst[s2]], (), "vsbw%d" % s2)
                for k in range(KC):
                    mm(pb[4][:, 0:320], hTa[ti][:, k, cc * 128:(cc + 1) * 128], wA[:, k, 2 * SBW:2 * SBW + 320],
                       k == 0, k == KC - 1, [bhT[ti], bwA], [bpb[4]])
                smc = sm[c % 2]; bsmc = bsm[c % 2]
                cp("act", smc[:, 0:320], pb[4][:, 0:320], [bpb[4]], [bsmc])
                cp("pool", vds[:, c, :], smc[:, 128:256], [bsmc], [b_vds])
                rope_apply(smc[:, 0:16], smc[:, 16:32], cosK[:, c, :], sinK[:, c, :], rtmp, 128, 16, [bsmc, b_rope], [bsmc])
                rope_apply(smc[:, 256:264], smc[:, 264:272], cosK[:, c, 0:16:2], sinK[:, c, 0:16:2], rtmp, 128, 8,
                           [bsmc, b_rope], [bsmc])
                cp("dve", smc[:, 320:384], smc[:, 256:320], [bsmc], [bsmc])
                if c > 0:
                    ktr(c - 1)
                if cc == 3:
                    t0 = (c // 4) * 512
                    for h in range(NHS):
                        pbi = 2 + h % 2
                        for k in range(KC):
                            mm(pb[pbi][:, :], wA[:, k, h * 128:(h + 1) * 128], hTa[ti][:, k, :], k == 0, k == KC - 1,
                               [bhT[ti], bwA], [bpb[pbi]])
                        ks = kctr % 2; kctr += 1
                        cp("act", kst[ks][:], pb[pbi][:, :], [bpb[pbi]], [bkst[ks]])
                        dma("act", kTsb[h, :, t0:t0 + 512], kst[ks][:], [bkst[ks]], (), "ktw%d" % ks)
                for _ in range(per_blk):
                    mod_step()
                if c + 2 < NKB:
                    prep(c + 2)
            ktr(NKB - 1)
            while gnext[0] < NG:
                mod_step()
            mod_vec(2, 3, 3, 6)
            mod_vec(3, 4, 7, 6)
            stt(vec_t[:, 2, :], vec_t[:, 7, :], 1.0, vec_t[:, 5, :], ALU.add, ALU.mult, [b_vec, bc], [b_vec])
        em.barrier()
        chk("A")
        wov = w_out.rearrange("(k p) n -> p k n", p=128)
        h2v = h2Ts.rearrange("p (k t) -> p k t", k=KC)
        U8 = mybir.dt.uint8
        with contextlib.ExitStack() as st:
            hTt = sbt(st, "hTt", [128, KC, TW], BF16); bhTt = Buf("hTt")
            xA = [sbt(st, "xA%d" % i, [BS, D], F32) for i in range(2)]; bxA = [Buf("xA%d" % i) for i in range(2)]
            junk = sbt(st, "junkT", [BS, D], BF16); ssq = sbt(st, "ssqT", [BS, 4], F32); bt = Buf("nrmtmpT")
            wq = [sbt(st, "wq%d" % i, [128, KC, 256], BF16) for i in range(2)]; bwq = [Buf("wq%d" % i) for i in range(2)]
            qTsb = sbt(st, "qTsb", [128, NHS, TW], BF16); bqsb = Buf("qTsb")
            qTds = sbt(st, "qTds", [128, NHD, TW], BF16); bqds = Buf("qTds")
            qTix = sbt(st, "qTix", [128, IH // 2, TW], BF16); bqix = Buf("qTix")
            wix = sbt(st, "wix", [BS, TB, IH], F32); bwix = Buf("wix")
            qtm = sbt(st, "qtm", [BS, 256], F32); bqtm = Buf("qtm")
            qtm2 = [sbt(st, "qtm2_%d" % i, [BS, 256], F32) for i in range(2)]; bqtm2 = [Buf("qtm2_%d" % i) for i in range(2)]
            rtmp = sbt(st, "rtmpT", [BS, 4, 16], F32)
            Mb = sbt(st, "Mb", [BS, TB, S], BF16); bMb = [Buf("Mb%d" % i) for i in range(TB)]
            SCRB = max(S * 4 + 8192, NKB * TW * 2, TB * D * 4)
            scr = sbt(st, "scr", [128, SCRB], U8)
            kvh = sbt(st, "kvh", [128, 4 * S], U8)
            kTh = kvh[:, 0:2 * S].bitcast(BF16); bkTh = Buf("kTh")
            vh = kvh[:, 2 * S:4 * S].bitcast(BF16); bvh = Buf("vh")
            scoreb = [scr[:BS, 0:S * 4].bitcast(F32), kvh[:BS, 0:S * 4].bitcast(F32)]
            bscb = [Buf("score0"), Buf("score1")]
            relb = [scr[:BS, S * 4 + 4096 * i:S * 4 + 4096 * i + 2048].bitcast(BF16) for i in range(2)]
            brel = [Buf("rel%d" % i) for i in range(2)]
            biasT = [scr[:BS, S * 4 + 4096 * i + 2048:S * 4 + 4096 * (i + 1)].bitcast(F32) for i in range(2)]
            bbias = [Buf("biasT%d" % i) for i in range(2)]
            diagw = sbt(st, "diagw", [BS, IH, BS], BF16); bdiag = Buf("diagw")
            Yt = scr[:, 0:NKB * TW * 2].bitcast(BF16); bY = Buf("Yt")
            xqb = [scr[:BS, D * 4 * i:D * 4 * (i + 1)].bitcast(F32) for i in range(TB)]; bxq = [Buf("xqb%d" % i) for i in range(TB)]
            bis = [sbt(st, "bis%d" % i, [BS, 8 + NSP], F32) for i in range(2)]; bbis = [Buf("bis%d" % i) for i in range(2)]
            dtab = [sbt(st, "dtab%d" % i, [BS, NBIS + 1], F32) for i in range(2)]
            ndtab = [sbt(st, "ndtab%d" % i, [BS, NBIS + 1], F32) for i in range(2)]
            mrg = sbt(st, "mrg", [128, NM, TW], BF16); bmrg = [Buf("mrg%d" % i) for i in range(NM)]
            ytmp = sbt(st, "ytmp", [128, TW], F32); bytmp = Buf("ytmp")
            qrow_t = sbt(st, "qrow", [128, TW], F32); bqrow = Buf("qrow")
            ebuf = [sbt(st, "ebuf%d" % i, [128, TW], F32) for i in range(2)]; beb = [Buf("ebuf%d" % i) for i in range(2)]
            spb = [sbt(st, "spb%d" % i, [128, TW], BF16) for i in range(2)]; bspb = [Buf("spb%d" % i) for i in range(2)]
            Ab = [sbt(st, "Ab%d" % i, [128, TW], BF16) for i in range(2)]; bAb = [Buf("Ab%d" % i) for i in range(2)]
            wb = [sbt(st, "wb%d" % i, [128, TW], BF16) for i in range(2)]; bwb = [Buf("wb%d" % i) for i in range(2)]
            pbuf = [sbt(st, "pbuf%d" % i, [128, TW], BF16) for i in range(2)]; bpbuf = [Buf("pbuf%d" % i) for i in range(2)]
            rden = sbt(st, "rden", [128, TW], F32); brden = Buf("rden")
            g1t = [sbt(st, "g1t%d" % i, [BS, 256], F32) for i in range(2)]; bg1 = [Buf("g1t%d" % i) for i in range(2)]
            gsq = sbt(st, "gsq", [128, TW], BF16); bgsq = Buf("gsq")
            grs = sbt(st, "grs", [128, 2, TW], F32); bgrs = Buf("grs")
            h2st = sbt(st, "h2st", [128, KC, BS], BF16); bh2st = Buf("h2st")
            wqctr = [0]

            def wq_load(src_v, col0, n):
                s = wqctr[0] % 2; wqctr[0] += 1
                dma("pool", wq[s][:, :, 0:n], src_v[:, :, col0:col0 + n], (), [bwq[s]], "wq%d" % s)
                return s

            for t in range(NT):
                tc0 = t * TW
                for bi in range(TB):
                    blk = t * TB + bi
                    xa = blk % 2
                    dma("sp", xA[xa][:], xq[blk * BS:(blk + 1) * BS, :], (), [bxA[xa]], "xA%d" % xa)
                    norm_T(xA[xa][:], BS, bxA[xa], lambda k, bi=bi: hTt[:, k, bi * BS:(bi + 1) * BS], bhTt,
                           G1, SH1, (junk, ssq, bt), [0, 1])
                dma("sp", qrow_t[:], qrow[:, tc0:tc0 + TW], (), [bqrow], "qrow")
                chk("T1")
                for g in range(0, SBW, 256):
                    s = wq_load(winv, cf.o_qsb + g, 256)
                    for hh in range(2):
                        h = g // 128 + hh
                        pbi = 2 + h % 2
                        for k in range(KC):
                            mm(pb[pbi][:, 0:TW], wq[s][:, k, hh * 128:(hh + 1) * 128], hTt[:, k, :], k == 0, k == KC - 1,
                               [bwq[s], bhTt], [bpb[pbi]])
                        act(qTsb[:, h, :], pb[pbi][:, 0:TW], AF.Copy, [bpb[pbi]], [bqsb], scale=cf.qk_scale)
                qsteps = [("ds", g) for g in range(0, DSW, 256)] + [("ix", g) for g in range(0, IH * ID, 256)]
                pend = []
                sctr = [0]

                def q_mm(kind, s, bi):
                    blk = t * TB + bi
                    sl = sctr[0] % 2; sctr[0] += 1
                    pm = 4 if sl == 0 else 6
                    for k in range(KC):
                        mm(pb[pm][:BS, 0:256], hTt[:, k, bi * BS:(bi + 1) * BS], wq[s][:, k, :], k == 0, k == KC - 1,
                           [bwq[s], bhTt], [bpb[pm]])
                    cp("act", qtm2[sl][:, :], pb[pm][:BS, 0:256], [bpb[pm]], [bqtm2[sl]])
                    if kind == "ds":
                        for hh in range(2):
                            o_ = hh * 128
                            rope_apply(qtm2[sl][:, o_:o_ + 16], qtm2[sl][:, o_ + 16:o_ + 32], cosQ[:, blk, :], sinQ[:, blk, :],
                                       rtmp, BS, 16, [bqtm2[sl], b_rope], [bqtm2[sl]])
                    else:
                        for hh in range(256 // ID):
                            o_ = hh * ID
                            rope_apply(qtm2[sl][:, o_:o_ + 8], qtm2[sl][:, o_ + 8:o_ + 16], cosQ[:, blk, 0:16:2],
                                       sinQ[:, blk, 0:16:2], rtmp, BS, 8, [bqtm2[sl], b_rope], [bqtm2[sl]])
                    return sl

                def q_tr(kind, g, bi, sl):
                    pt = 5 if sl == 0 else 7
                    for pp in range(2):
                        tr(pb[pt][:, pp * 128:pp * 128 + BS], qtm2[sl][:, pp * 128:(pp + 1) * 128], identf_t[:BS, :BS],
                           [bqtm2[sl], bc], [bpb[pt]])
                    for pp in range(2):
                        if kind == "ds":
                            act(qTds[:, g // 128 + pp, bi * BS:(bi + 1) * BS], pb[pt][:, pp * 128:pp * 128 + BS], AF.Copy,
                                [bpb[pt]], [bqds], scale=cf.qk_scale)
                        else:
                            cp("act", qTix[:, g // 128 + pp, bi * BS:(bi + 1) * BS], pb[pt][:, pp * 128:pp * 128 + BS],
                               [bpb[pt]], [bqix])

                for (kind, g) in qsteps:
                    s = wq_load(winv, (cf.o_qds if kind == "ds" else cf.o_qix) + g, 256)
                    for bi in range(TB):
                        sl = q_mm(kind, s, bi)
                        if pend:
                            q_tr(*pend.pop())
                        pend.append((kind, g, bi, sl))
                if pend:
                    q_tr(*pend.pop())
                s = wq_load(winv, cf.o_wix, IH)
                for bi in range(TB):
                    for k in range(KC):
                        mm(pb[4][:BS, 0:IH], hTt[:, k, bi * BS:(bi + 1) * BS], wq[s][:, k, 0:IH], k == 0, k == KC - 1,
                           [bwq[s], bhTt], [bpb[4]])
                    act(wix[:, bi, :], pb[4][:BS, 0:IH], AF.Copy, [bpb[4]], [bwix], scale=cf.idx_scale)

                chk("T2")
                def idx_block(bi):
                    blk = t * TB + bi
                    sb_ = bi % 2; sc = scoreb[sb_]; bs_ = bscb[sb_]; B_ = bis[sb_]; bB = bbis[sb_]
                    for h in range(IH):
                        ts("pool", diagw[:, h, :], identb[:BS, :BS], wix[:, bi, h:h + 1], None, ALU.mult, None,
                           [bc, bwix], [bdiag])
                    gctr = 0; spctr = 0
                    for sp_ in range(0, S, 1024):
                        nsp = min(1024, S - sp_); nb = nsp // 512
                        ab = 4 + 2 * (spctr % 2); spctr += 1
                        def idx_mm(h, pg):
                            po = (h % 2) * 64
                            for q_ in range(nb):
                                mm(pb[pg + q_][:BS, :], qTix[po:po + 64, h // 2, bi * BS:(bi + 1) * BS],
                                   kTix[po:po + 64, sp_ + q_ * 512:sp_ + (q_ + 1) * 512], True, True,
                                   [bqix, b_kTix], [bpb[pg + q_]])

                        pgs = []
                        for h in range(IH):
                            pgs.append((gctr % 2) * 2); gctr += 1
                        idx_mm(0, pgs[0])
                        for h in range(IH):
                            pg = pgs[h]
                            if h + 1 < IH:
                                idx_mm(h + 1, pgs[h + 1])
                            rs = h % 2
                            em.op("dve", lambda e, rs=rs, pg=pg, nb=nb, nsp=nsp: e.tensor_scalar(
                                out=relb[rs][:, 0:nsp].rearrange("p (a f) -> p a f", a=nb), in0=PS[:BS, pg:pg + nb, :],
                                scalar1=0.0, scalar2=None, op0=ALU.max),
                                [bpb[pg + q_] for q_ in range(nb)], [brel[rs]])
                            for q_ in range(nb):
                                mm(pb[ab + q_][:BS, :], diagw[:, h, :], relb[rs][:, q_ * 512:(q_ + 1) * 512], h == 0, h == IH - 1,
                                   [bdiag, brel[rs]], [bpb[ab + q_]])
                        for q_ in range(nb):
                            kt = sp_ // 512 + q_
                            em.op("dve", lambda e, kt=kt, ab=ab, q_=q_, B_=B_: e.tensor_reduce(
                                out=B_[:, 8 + kt:9 + kt], in_=pb[ab + q_][:BS, :], axis=AX.X, op=ALU.min),
                                [bpb[ab + q_]], [bB])
                            ts("dve", biasT[q_ % 2], iota_t[:BS, :], qrel_t[:, blk, kt:kt + 1], -1e30, ALU.is_gt, ALU.mult,
                               [bc], [bbias[q_ % 2]])
                            tt("dve", sc[:, kt * 512:(kt + 1) * 512], pb[ab + q_][:BS, :], biasT[q_ % 2], ALU.add,
                               [bpb[ab + q_], bbias[q_ % 2]], [bs_])
                    em.op("dve", lambda e: e.tensor_reduce(out=B_[:, 0:1], in_=sc, axis=AX.X, op=ALU.max), [bs_], [bB])
                    em.op("dve", lambda e: e.tensor_reduce(out=B_[:, 1:2], in_=B_[:, 8:8 + NSP], axis=AX.X, op=ALU.min), [bB], [bB])
                    stt(B_[:, 6:7], B_[:, 0:1], 2.0, B_[:, 1:2], ALU.add, ALU.subtract, [bB], [bB])
                    ts("dve", dtab[sb_][:], pow2_t[:BS, :], B_[:, 6:7], None, ALU.mult, None, [bB, bc], [bB])
                    ts("dve", ndtab[sb_][:], dtab[sb_][:], -1.0, None, ALU.mult, None, [bB], [bB])
                    ts("dve", B_[:, 2:3], B_[:, 1:2], -1.0, 1.0, ALU.mult, ALU.add, [bB], [bB])
                    tt("dve", B_[:, 2:3], B_[:, 2:3], dtab[sb_][:, 0:1], ALU.subtract, [bB], [bB])

                def bisect(bi):
                    sb_ = bi % 2; sc = scoreb[sb_]; bs_ = bscb[sb_]; B_ = bis[sb_]; bB = bbis[sb_]
                    for r_ in range(NBIS):
                        act(Mb[:, bi, :], sc, AF.Sign, [bs_, bB], [bMb[bi], bB], bias=B_[:, 2:3], accum_out=B_[:, 3:4])
                        act(B_[:, 4:5], B_[:, 3:4], AF.Sign, [bB], [bB], bias=float(S - 2 * cf.TOPK) + 0.5)
                        act(B_[:, 2:3], B_[:, 4:5], AF.Identity, [bB], [bB], scale=ndtab[sb_][:, r_ + 1:r_ + 2], bias=B_[:, 2:3])

                def finalize(bi):
                    sb_ = bi % 2; sc = scoreb[sb_]; bs_ = bscb[sb_]; B_ = bis[sb_]; bB = bbis[sb_]
                    stt(B_[:, 5:6], B_[:, 2:3], -1.0, dtab[sb_][:, NBIS:NBIS + 1], ALU.mult, ALU.subtract, [bB], [bB])
                    ts("dve", Mb[:, bi, :], sc, B_[:, 5:6], NEGB, ALU.is_le, ALU.mult, [bs_, bB], [bMb[bi]])

                idx_block(0)
                for bi in range(TB):
                    bisect(bi)
                    if bi + 1 < TB:
                        idx_block(bi + 1)
                    finalize(bi)

                chk("T3")
                for h in range(NHD):
                    def S_step(c):
                        pz = c % 2
                        mm(pb[pz][:, 0:TW], kTds[:, c * 128:(c + 1) * 128], qTds[:, h, :], True, False,
                           [b_kTds, bqds], [bpb[pz]])
                        for bi in range(TB):
                            mm(pb[pz][:, bi * BS:(bi + 1) * BS], Mb[:, bi, c * 128:(c + 1) * 128], identb[:BS, :BS],
                               False, bi == TB - 1, [bMb[bi], bc], [bpb[pz]])
                        act(pbuf[pz][:], pb[pz][:, 0:TW], AF.Exp, [bpb[pz]], [bpbuf[pz]])

                    def PV_step(c):
                        pz = c % 2
                        mm(pb[2][:, 0:TW], vds[:, c, :], pbuf[pz][:], c == 0, c == NKB - 1, [b_vds, bpbuf[pz]], [bpb[2]])
                        mm(pb[3][:, 0:TW], onesb, pbuf[pz][:], c == 0, c == NKB - 1, [bc, bpbuf[pz]], [bpb[3]])

                    S_step(0)
                    for c in range(NKB):
                        if c + 1 < NKB:
                            S_step(c + 1)
                        PV_step(c)
                    em.op("dve", lambda e: e.reciprocal(out=rden[:], in_=pb[3][:, 0:TW]), [bpb[3]], [brden])
                    tt("dve", mrg[:, NHS + h, :], pb[2][:, 0:TW], rden[:], ALU.mult, [bpb[2], brden], [bmrg[NHS + h]])

                chk("T4")
                em.barrier()
                for c in range(NKB):
                    ts("dve", ytmp[:], qrow_t[:], float(-128 * c), 0.0, ALU.add, ALU.max, [bqrow], [bytmp])
                    ts("dve", Yt[:, c * TW:(c + 1) * TW], ytmp[:], kcol_t[:, 0:1], None, ALU.is_equal, None, [bytmp, bc], [bY])
                for h in range(NHS):
                    dma("sp", kTh, kTsb[h, :, :], (), [bkTh], "kTh")
                    dma("sp", vh, vsb[h, :, :], (), [bvh], "vh")
                    order = list(range(NKB - 1, -1, -1))

                    def Z_step(i):
                        c = order[i]; pz = 4 + i % 2
                        mm(pb[pz][:, 0:TW], kTh[:, c * 128:(c + 1) * 128], qTsb[:, h, :], True, False,
                           [bkTh, bqsb], [bpb[pz]])
                        mm(pb[pz][:, 0:TW], umat, Yt[:, c * TW:(c + 1) * TW], False, True, [bc, bY], [bpb[pz]])
                        act(ebuf[i % 2][:], pb[pz][:, 0:TW], AF.Exp, [bpb[pz]], [beb[i % 2]])
                        act(spb[i % 2][:], ebuf[i % 2][:], AF.Ln, [beb[i % 2]], [bspb[i % 2]], bias=1.0)

                    def X_step(i):
                        c = order[i]; px = 6 + i % 2
                        mm(pb[px][:, 0:TW], kTh[:, c * 128:(c + 1) * 128], qTsb[:, h, :], True, False,
                           [bkTh, bqsb], [bpb[px]])
                        mm(pb[px][:, 0:TW], umat, Yt[:, c * TW:(c + 1) * TW], False, False, [bc, bY], [bpb[px]])
                        if i > 0:
                            mm(pb[px][:, 0:TW], nones, Ab[i % 2][:], False, False, [bc, bAb[i % 2]], [bpb[px]])
                        mm(pb[px][:, 0:TW], ntri, spb[i % 2][:], False, True, [bc, bspb[i % 2]], [bpb[px]])
                        if i == 0:
                            cp("pool", Ab[1][:], spb[0][:], [bspb[0]], [bAb[1]])
                        elif i + 1 < NKB:
                            tt("pool", Ab[(i + 1) % 2][:], Ab[i % 2][:], spb[i % 2][:], ALU.add,
                               [bAb[i % 2], bspb[i % 2]], [bAb[(i + 1) % 2]])
                        act(wb[i % 2][:], pb[px][:, 0:TW], AF.Exp, [bpb[px]], [bwb[i % 2]])

                    def PVs(i):
                        c = order[i]
                        mm(pb[3][:, 0:TW], vh[:, c * 128:(c + 1) * 128], wb[i % 2][:], i == 0, i == NKB - 1, [bvh, bwb[i % 2]], [bpb[3]])

                    Z_step(0)
                    for i in range(NKB):
                        if i + 1 < NKB:
                            Z_step(i + 1)
                        X_step(i)
                        if i > 0:
                            PVs(i - 1)
                    PVs(NKB - 1)
                    cp("dve", mrg[:, h, :], pb[3][:, 0:TW], [bpb[3]], [bmrg[h]])

                chk("T5")
                for gi, (h0, nh_, g_t, W_) in enumerate(((0, NHS, sbg_t, SBW), (NHS, NHD, dsg_t, DSW))):
                    for hh in range(nh_):
                        act(gsq[:], mrg[:, h0 + hh, :], AF.Square, [bmrg[h0 + hh]], [bgsq])
                        mm(pb[0][:, 0:TW], onesb, gsq[:], hh == 0, hh == nh_ - 1, [bc, bgsq], [bpb[0]])
                    act(grs[:, 0, :], pb[0][:, 0:TW], AF.Sqrt, [bpb[0]], [bgrs], scale=1.0 / W_, bias=1e-6)
                    em.op("dve", lambda e: e.reciprocal(out=grs[:, 1, :], in_=grs[:, 0, :]), [bgrs], [bgrs])
                    for hh in range(nh_):
                        stt(mrg[:, h0 + hh, :], mrg[:, h0 + hh, :], g_t[:, hh:hh + 1], grs[:, 1, :], ALU.mult, ALU.mult,
                            [bmrg[h0 + hh], bgrs, bc], [bmrg[h0 + hh]])

                chk("T6")
                em.barrier()
                for bi in range(TB):
                    blk = t * TB + bi
                    dma("sp", xqb[bi], xq[blk * BS:(blk + 1) * BS, :], (), [bxq[bi]], "xqb%d" % bi)
                for gi, g in enumerate(range(0, D, 256)):
                    s = wq_load(wov, g, 256)
                    gs = gi % 2
                    dma("sp", g1t[gs][:], modrow[0:1, 2 * D + g:2 * D + g + 256].partition_broadcast(BS), (), [bg1[gs]], "g1t%d" % gs)
                    for bi in range(TB):
                        pbi = bi % 2
                        for k in range(NM):
                            mm(pb[pbi][:BS, 0:256], mrg[:, k, bi * BS:(bi + 1) * BS], wq[s][:, k, :], k == 0, k == NM - 1,
                               bmrg + [bwq[s]], [bpb[pbi]])
                        tt("dve", qtm[:, :], pb[pbi][:BS, 0:256], g1t[gs][:], ALU.mult, [bpb[pbi], bg1[gs]], [bqtm])
                        tt("dve", xqb[bi][:, g:g + 256], xqb[bi][:, g:g + 256], qtm[:, :], ALU.add, [bqtm, bxq[bi]], [bxq[bi]])
                for bi in range(TB):
                    blk = t * TB + bi
                    dma("pool", xmid[blk * BS:(blk + 1) * BS, :], xqb[bi], [bxq[bi]], (), "xmidw%d" % bi)
                    norm_T(xqb[bi], BS, bxq[bi], lambda k: h2st[:, k, :], bh2st, G2, SH2, (junk, ssq, bt), [2, 3],
                           xs_out=xA[blk % 2][:], bxs=bxA[blk % 2])
                    dma("act", h2v[:, :, blk * BS:(blk + 1) * BS], h2st[:], [bh2st], (), "h2w")
                em.barrier()
        em.barrier()

        chk("T")
        wuv = w_up.rearrange("(k p) n -> p k n", p=128)
        wdv = w_down.rearrange("(k p) n -> p k n", p=128)
        with contextlib.ExitStack() as st:
            mT = sbt(st, "mT", [128, FC, OWN], BF16); bmT = Buf("mT")
            h2T = sbt(st, "h2T", [128, KC, TQ], BF16); b_h2T = Buf("h2T")
            dma("sp", h2T[:], h2Ts.rearrange("p (k t) -> p k t", k=KC), (), [b_h2T], "h2l")
            with contextlib.ExitStack() as st2:
                wu = [sbt(st2, "wu%d" % i, [128, KC, 256], BF16) for i in range(3)]; bwu = [Buf("wu%d" % i) for i in range(3)]
                usb = [sbt(st2, "usb%d" % i, [128, TQ], F32) for i in range(2)]; busb = [Buf("usb%d" % i) for i in range(2)]
                ucg = sbt(st2, "ucg", [128, OWN], F32); bucg = Buf("ucg")
                ucv = sbt(st2, "ucv", [128, OWN], F32); bucv = Buf("ucv")
                for f in range(FC):
                    s = f % 3
                    dma("pool", wu[s][:, :, 0:128], wuv[:, :, f * 128:(f + 1) * 128], (), [bwu[s]], "wu%d" % s)
                    dma("pool", wu[s][:, :, 128:256], wuv[:, :, DFF + f * 128:DFF + (f + 1) * 128], (), [bwu[s]], "wu%d" % s)
                    for half in range(2):
                        ci = f + half * FC
                        ub = usb[half]; bub = busb[half]
                        for t in range(NT):
                            pbi = (2 * t + half) % 4
                            for k in range(KC):
                                mm(pb[pbi][:, 0:TW], wu[s][:, k, half * 128:(half + 1) * 128], h2T[:, k, t * TW:(t + 1) * TW],
                                   k == 0, k == KC - 1, [bwu[s], b_h2T], [bpb[pbi]])
                            cp("act", ub[:, t * TW:(t + 1) * TW], pb[pbi][:, 0:TW], [bpb[pbi]], [bub])
                        ts("dve", ub[:, 0:2], ub[:, 0:2], flag_t[:, 0:1], None, ALU.mult, None, [bub, bc], [bub])
                        uc = ucg if half == 0 else ucv
                        buc = bucg if half == 0 else bucv
                        act(uc[:], ub[:, 2:TQ], AF.Identity, [bub, bc], [buc], scale=cvw_t[:, 2, ci:ci + 1], bias=cvb_t[:, ci:ci + 1])
                        stt(uc[:], ub[:, 1:TQ - 1], cvw_t[:, 1, ci:ci + 1], uc[:], ALU.mult, ALU.add, [bub, bc, buc], [buc])
                        stt(uc[:], ub[:, 0:TQ - 2], cvw_t[:, 0, ci:ci + 1], uc[:], ALU.mult, ALU.add, [bub, bc, buc], [buc])
                    act(ucg[:], ucg[:], AF.Silu, [bucg], [bucg])
                    tt("dve", mT[:, f, :], ucg[:], ucv[:], ALU.mult, [bucg, bucv], [bmT])
            em.barrier()
            with contextlib.ExitStack() as st2:
                wd = [sbt(st2, "wd%d" % i, [128, FC, 256], BF16) for i in range(2)]; bwd = [Buf("wd%d" % i) for i in range(2)]
                yst = [sbt(st2, "yst%d" % i, [128, 256], F32) for i in range(2)]; byst = [Buf("yst%d" % i) for i in range(2)]
                yc = 0
                for gi, g in enumerate(range(0, D, 256)):
                    s = gi % 2
                    for f0 in range(0, FC, 16):
                        f1 = min(FC, f0 + 16)
                        dma("pool", wd[s][:, f0:f1, :], wdv[:, f0:f1, g:g + 256], (), [bwd[s]], "wd%d" % s)
                    for tb in range(NOB):
                        pbi = tb % 2
                        for f in range(FC):
                            mm(pb[pbi][:, 0:256], mT[:, f, tb * 128:(tb + 1) * 128], wd[s][:, f, :], f == 0, f == FC - 1,
                               [bmT, bwd[s]], [bpb[pbi]])
                        ys = yc % 2; yc += 1
                        cp("act", yst[ys][:], pb[pbi][:, 0:256], [bpb[pbi]], [byst[ys]])
                        dma("act", yscr[tb * 128:(tb + 1) * 128, g:g + 256], yst[ys][:], [byst[ys]], (), "yscrw%d" % ys)
        em.barrier()
        with contextlib.ExitStack() as st:
            g2bc = sbt(st, "g2bc", [128, D], F32); fngt = sbt(st, "fngt", [128, D], F32); bgg = Buf("g2fng")
            xm = [sbt(st, "xm%d" % i, [128, D], F32) for i in range(2)]; bxm = [Buf("xm%d" % i) for i in range(2)]
            yy = [sbt(st, "yy%d" % i, [128, D], F32) for i in range(2)]; byy = [Buf("yy%d" % i) for i in range(2)]
            junk = sbt(st, "junkF", [128, D], BF16); ssq = sbt(st, "ssqF", [128, 4], F32); bt = Buf("nrmF")
            dma("sp", g2bc[:], modrow[0:1, 5 * D:6 * D].partition_broadcast(128), (), [bgg], "g2bc")
            dma("sp", fngt[:], fng[:, :], (), [bgg], "g2bc")
            for tb in range(NOB):
                s = tb % 2
                dma("sp", xm[s][:], xmid[2 + tb * 128:2 + (tb + 1) * 128, :], (), [bxm[s]], "xm%d" % s)
                dma("sp", yy[s][:], yscr[tb * 128:(tb + 1) * 128, :], (), [byy[s]], "yy%d" % s)
                tt("dve", yy[s][:], yy[s][:], g2bc[:], ALU.mult, [byy[s], bgg], [byy[s]])
                tt("dve", xm[s][:], xm[s][:], yy[s][:], ALU.add, [byy[s], bxm[s]], [bxm[s]])
                act(junk[:], xm[s][:], AF.Square, [bxm[s]], [bt], accum_out=ssq[:, 0:1])
                act(ssq[:, 1:2], ssq[:, 0:1], AF.Sqrt, [bt], [bt], scale=1.0 / D, bias=1e-6)
                em.op("dve", lambda e: e.reciprocal(out=ssq[:, 2:3], in_=ssq[:, 1:2]), [bt], [bt])
                stt(yy[s][:], xm[s][:], ssq[:, 2:3], fngt[:], ALU.mult, ALU.mult, [bxm[s], bt, bgg], [byy[s]])
                dma("pool", out[tb * 128:(tb + 1) * 128, :], yy[s][:], [byy[s]], (), "outw%d" % s)
        em.emit(final_waits=["outw0", "outw1"] if NOB > 1 else ["outw0"])
    return nc


def host_inputs(cf, inp, core):
    D, S, KC, NKB, TQ, BS, NBLK, NSP, FC = cf.D, cf.S, cf.KC, cf.NKB, cf.TQ, cf.BS, cf.NBLK, cf.NSP, cf.FC
    b = core // cf.CPB; j = core % cf.CPB
    f32 = np.float32
    x = np.asarray(inp["x"], f32); pos = np.asarray(inp["positions"], np.int32)
    t0 = j * cf.OWN - 2
    tok = np.arange(t0, t0 + TQ)
    tokc = np.maximum(tok, 0)
    m = {}
    m["xkv"] = np.ascontiguousarray(x[b])
    m["xq"] = np.ascontiguousarray(x[b][tokc])
    m["posk"] = np.ascontiguousarray(pos[b].reshape(NKB, 128).T)
    m["posq"] = np.ascontiguousarray(pos[b][tokc].reshape(NBLK, BS).T)
    qc = tokc.astype(f32).reshape(NBLK, BS).T
    m["qcol"] = np.ascontiguousarray(qc)
    qr = qc[:, :, None] - (512.0 * np.arange(NSP, dtype=f32))[None, None, :]
    m["qrel"] = np.ascontiguousarray(qr.reshape(BS, NBLK * NSP).astype(f32))
    m["qrow"] = np.ascontiguousarray(np.broadcast_to(tokc.astype(f32)[None, :], (128, TQ)))
    pk = lambda v: np.ascontiguousarray(np.asarray(v, f32).reshape(-1, 128).T)
    m["cb"] = pk(np.asarray(inp["c"], f32)[b])
    m["w_ada"] = np.ascontiguousarray(np.asarray(inp["w_ada"], f32)[0])
    m["b_ada"] = np.ascontiguousarray(np.asarray(inp["b_ada"], f32)[0][None, :])
    m["w_in"] = np.ascontiguousarray(np.asarray(inp["w_in"], f32)[0])
    m["w_out"] = np.ascontiguousarray(np.asarray(inp["w_out"], f32)[0])
    m["w_up"] = np.ascontiguousarray(np.asarray(inp["w_up"], f32)[0])
    m["w_down"] = np.ascontiguousarray(np.asarray(inp["w_down"], f32)[0])
    m["n1g"] = pk(np.asarray(inp["norm1_g"])[0]); m["n2g"] = pk(np.asarray(inp["norm2_g"])[0])
    m["sbg"] = pk(np.asarray(inp["sb_norm_g"])[0]); m["dsg"] = pk(np.asarray(inp["dsa_norm_g"])[0])
    m["fng"] = np.ascontiguousarray(np.broadcast_to(np.asarray(inp["final_norm_g"], f32)[None, :], (128, D)))
    cw = np.asarray(inp["conv_w"], f32)[0]
    m["convw"] = np.ascontiguousarray(np.stack([pk(cw[i]) for i in range(3)], axis=1).reshape(128, 3 * 2 * FC))
    m["convb"] = pk(np.asarray(inp["conv_b"], f32)[0])
    m["identf"] = np.eye(128, dtype=f32)
    jj = np.arange(128)[:, None]; ss = np.arange(128)[None, :]
    cm = np.stack([np.eye(128, dtype=f32), -(jj >= ss).astype(f32), -np.ones((128, 128), f32),
                   NEGB * (ss >= jj).astype(f32), np.ones((128, 128), f32)], axis=1)
    m["cmat"] = np.ascontiguousarray(cm.reshape(128, 5 * 128))
    m["iota"] = np.ascontiguousarray(np.broadcast_to(np.arange(512, dtype=f32)[None, :], (128, 512)))
    m["kcolc"] = np.arange(128, dtype=f32)[:, None].copy()
    m["pow2"] = np.ascontiguousarray(np.broadcast_to((0.5 ** np.arange(1, cf.NBIS + 2)).astype(f32)[None, :], (128, cf.NBIS + 1)))
    ivf = (np.float32(500000.0) ** (-(np.arange(16, dtype=f32) / np.float32(16)))).astype(f32)
    m["invf"] = np.ascontiguousarray(np.broadcast_to(ivf[None, :], (128, 16)))
    m["flag"] = np.full((128, 1), 0.0 if j == 0 else 1.0, f32)
    return m


_CACHE = {}


def kernel(x, c, positions, w_ada, b_ada, norm1_g, w_in, sb_norm_g, dsa_norm_g, w_out, norm2_g,
           w_up, conv_w, conv_b, w_down, final_norm_g):
    inp = dict(x=x, c=c, positions=positions, w_ada=w_ada, b_ada=b_ada, norm1_g=norm1_g, w_in=w_in,
               sb_norm_g=sb_norm_g, dsa_norm_g=dsa_norm_g, w_out=w_out, norm2_g=norm2_g, w_up=w_up,
               conv_w=conv_w, conv_b=conv_b, w_down=w_down, final_norm_g=final_norm_g)
    cf = Cfg()
    nc = build_program(cf)
    in_maps = [host_inputs(cf, inp, core) for core in range(8)]
    res = run_bass_kernel_spmd(nc, in_maps, core_ids=list(range(8)))
    outp = np.zeros((2, cf.S, cf.D), np.float32)
    for core in range(8):
        b = core // cf.CPB; j = core % cf.CPB
        outp[b, j * cf.OWN:(j + 1) * cf.OWN, :] = np.asarray(res.results[core]["out"], np.float32)
    return outp
```

```python
import contextlib
import numpy as np
import concourse.bass as bass
import concourse.mybir as mybir
from concourse.bass_utils import run_bass_kernel_spmd

F32 = mybir.dt.float32
BF16 = mybir.dt.bfloat16
I32 = mybir.dt.int32
AF = mybir.ActivationFunctionType
ALU = mybir.AluOpType
AX = mybir.AxisListType

ENGS = ("pe", "act", "dve", "pool", "sp")


class Buf:
    __slots__ = ("name", "lw", "rs")

    def __init__(self, name):
        self.name = name
        self.lw = None
        self.rs = []


class Op:
    __slots__ = ("eng", "idx", "fn", "deps", "dma", "stream", "gen", "flag", "incidx")

    def __init__(self, eng, idx, fn, dma, stream):
        self.eng = eng; self.idx = idx; self.fn = fn; self.deps = []
        self.dma = dma; self.stream = stream; self.gen = 0
        self.flag = False; self.incidx = 0


class Em:
    def __init__(self, nc):
        self.nc = nc
        self.ops = {e: [] for e in ENGS}
        self.streams = {}
        self.last_dma = {}
        self.pending = {e: None for e in ENGS}
        self.pool_dmas = []

    def barrier(self):
        lasts = [self.ops[e][-1] for e in ENGS if self.ops[e]]
        lasts += list(self.last_dma.values())
        for e in ENGS:
            self.pending[e] = lasts

    def op(self, eng, fn, reads=(), writes=(), dma=False, stream=None):
        o = Op(eng, len(self.ops[eng]), fn, dma, stream)
        deps = []
        if self.pending[eng] is not None:
            deps.extend(self.pending[eng])
            self.pending[eng] = None
        if dma and eng == "pool":
            if len(self.pool_dmas) >= 4:
                deps.append(self.pool_dmas[-4])
            self.pool_dmas.append(o)
        for b in reads:
            if b.lw is not None:
                deps.append(b.lw)
        for b in writes:
            if b.lw is not None:
                deps.append(b.lw)
            deps.extend(b.rs)
        for b in reads:
            b.rs.append(o)
        for b in writes:
            b.lw = o
            b.rs = []
        if dma:
            g = self.streams.get(stream, 0) + 1
            self.streams[stream] = g
            o.gen = g
            self.last_dma[stream] = o
        best = {}
        for d in deps:
            if d is o:
                continue
            if d.dma:
                key = ("dma", d.stream)
                if key not in best or best[key].gen < d.gen:
                    best[key] = d
            else:
                if d.eng == eng and eng == "pe":
                    continue
                key = ("eng", d.eng)
                if key not in best or best[key].idx < d.idx:
                    best[key] = d
        o.deps = list(best.values())
        self.ops[eng].append(o)
        return o

    def emit(self, final_waits=()):
        nc = self.nc
        for e in ENGS:
            seen = {}
            for o in self.ops[e]:
                nd = []
                for d in o.deps:
                    key = ("dma", d.stream) if d.dma else ("eng", d.eng)
                    val = d.gen if d.dma else d.idx
                    if seen.get(key, -1) >= val:
                        continue
                    seen[key] = val
                    nd.append(d)
                    if not d.dma:
                        d.flag = True
                o.deps = nd
        for e in ENGS:
            c = 0
            for o in self.ops[e]:
                if o.flag and not o.dma:
                    c += 1
                    o.incidx = c
        with contextlib.ExitStack() as st:
            esem = {e: st.enter_context(nc.semaphore("s_" + e)) for e in ENGS}
            dsem = {s: st.enter_context(nc.semaphore("d_%d" % i)) for i, s in enumerate(self.streams)}
            block = st.enter_context(nc.Block())

            def run(e, eng):
                for o in self.ops[e]:
                    for d in o.deps:
                        if d.dma:
                            eng.wait_ge(dsem[d.stream], 16 * d.gen)
                        else:
                            eng.wait_ge(esem[d.eng], d.incidx)
                    ins = o.fn(eng)
                    if o.dma:
                        ins.then_inc(dsem[o.stream], 16)
                    elif o.flag:
                        ins.then_inc(esem[e], 1)
                if e == "sp":
                    for s in final_waits:
                        eng.wait_ge(dsem[s], 16 * self.streams[s])

            @block.tensor
            def _(eng):
                run("pe", eng)

            @block.scalar
            def _(eng):
                run("act", eng)

            @block.vector
            def _(eng):
                run("dve", eng)

            @block.gpsimd
            def _(eng):
                run("pool", eng)

            @block.sync
            def _(eng):
                run("sp", eng)


class Cfg:
    def __init__(self, D=2048, S=4096, DFF=5504, BS=86, TB=3, NSEG=4, NBIS=26, IH=16, ID=64,
                 TOPK_MAX=256, CPB=4):
        self.D, self.S, self.DFF = D, S, DFF
        self.HD = 128
        nh = D // 128
        self.NHS = nh // 2
        self.NHD = nh - self.NHS
        self.SBW = self.NHS * 128
        self.DSW = self.NHD * 128
        self.IH, self.ID = IH, ID
        self.TOPK = min(TOPK_MAX, S // 4)
        self.CPB = CPB
        self.OWN = S // CPB
        self.NSEG = NSEG
        self.SEG = self.OWN // NSEG
        self.TW = self.SEG + 2
        self.BS, self.TB = BS, TB
        assert BS * TB == self.TW and BS <= 128 and self.TW <= 512
        self.NT = NSEG
        self.NBLK = NSEG * TB
        self.TQ = NSEG * self.TW
        self.KC = D // 128
        self.NKB = S // 128
        self.FC = DFF // 128
        assert DFF % 128 == 0 and S % 512 == 0 and self.SEG % 128 == 0
        self.NOB = self.OWN // 128
        self.BPS = self.SEG // 128
        self.NBIS = NBIS
        o = 0
        self.o_qsb = o; o += self.SBW
        self.o_ksb = o; o += self.SBW
        self.o_vsb = o; o += self.SBW
        self.o_qds = o; o += self.DSW
        self.o_kds = o; o += 128
        self.o_vds = o; o += 128
        self.o_qix = o; o += IH * ID
        self.o_kix = o; o += ID
        self.o_wix = o; o += IH
        self.INC = o
        self.NSP = S // 512
        self.idx_scale = (IH ** -0.5) * (ID ** -0.5)
        self.qk_scale = 128 ** -0.5
        self.stop = None

    def tile_keys(self, t):
        k = (self.CPB * t + self.CPB) * self.SEG
        assert k % 512 == 0 and k <= self.S
        return k

    def seg_tokens(self, j, t):
        gt = self.CPB * t + j
        return gt * self.SEG


class _Stop(Exception):
    pass


TWO_PI = 6.283185307179586
PI = 3.141592653589793
NEGB = -30000.0


def build_program(cf):
    hold = {}
    try:
        return _build(cf, hold)
    except _Stop:
        hold["em"].emit(final_waits=[])
        return hold["nc"]


def _build(cf, hold):
    nc = bass.Bass("TRN2", target_bir_lowering=False)
    em = Em(nc)
    hold["nc"] = nc; hold["em"] = em
    D, S, KC, NKB, TQ, BS, NBLK, TB, NT, TW = cf.D, cf.S, cf.KC, cf.NKB, cf.TQ, cf.BS, cf.NBLK, cf.TB, cf.NT, cf.TW
    NHS, NHD, SBW, DSW, IH, ID, FC, DFF = cf.NHS, cf.NHD, cf.SBW, cf.DSW, cf.IH, cf.ID, cf.FC, cf.DFF
    NSP, NBIS, OWN, NOB = cf.NSP, cf.NBIS, cf.OWN, cf.NOB
    NM = NHS + NHD

    def din(name, shape, dt=F32):
        return nc.dram_tensor(name, list(shape), dt, kind="ExternalInput").ap()

    def dscr(name, shape, dt):
        return nc.dram_tensor(name, list(shape), dt, kind="Internal").ap()

    xkv = din("xkv", [S, D]); xq = din("xq", [TQ, D])
    posk = din("posk", [128, NKB], I32); posq = din("posq", [BS, NBLK], I32)
    qcol = din("qcol", [BS, NBLK]); qrel = din("qrel", [BS, NBLK * NSP]); qrow = din("qrow", [128, TQ])
    cb = din("cb", [128, KC])
    w_ada = din("w_ada", [D, 6 * D]); b_ada = din("b_ada", [1, 6 * D])
    w_in = din("w_in", [D, cf.INC]); w_out = din("w_out", [D, D])
    w_up = din("w_up", [D, 2 * DFF]); w_down = din("w_down", [DFF, D])
    n1g = din("n1g", [128, KC]); n2g = din("n2g", [128, KC])
    sbg = din("sbg", [128, NHS]); dsg = din("dsg", [128, NHD]); fng = din("fng", [128, D])
    convw = din("convw", [128, 3 * 2 * FC]); convb = din("convb", [128, 2 * FC])
    identf = din("identf", [128, 128]); cmat = din("cmat", [128, 5 * 128])
    iota = din("iota", [128, 512]); kcolc = din("kcolc", [128, 1]); pow2 = din("pow2", [128, NBIS + 1])
    invf = din("invf", [128, 16]); flag = din("flag", [128, 1])
    out = nc.dram_tensor("out", [OWN, D], F32, kind="ExternalOutput").ap()
    modrow = dscr("modrow", [1, 6 * D], F32)
    kTsb = dscr("kTsb", [NHS, 128, S], BF16)
    vsb = dscr("vsb", [NHS, 128, NKB * 128], BF16)
    xmid = dscr("xmid", [TQ, D], F32)
    yscr = dscr("yscr", [OWN, D], F32)
    h2Ts = dscr("h2Ts", [128, KC * TQ], BF16)

    def mm(o, lhsT, rhs, start, stop, r, w):
        em.op("pe", lambda e: e.matmul(o, lhsT=lhsT, rhs=rhs, start=start, stop=stop), r, w)

    def tr(o, in_, ident, r, w):
        em.op("pe", lambda e: e.transpose(o, in_, ident), r, w)

    def act(o, in_, func, r, w, **kw):
        em.op("act", lambda e: e.activation(out=o, in_=in_, func=func, **kw), r, w)

    def ts(eng, o, in0, s1, s2, op0, op1, r, w, accum_out=None):
        if op1 is None:
            em.op(eng, lambda e: e.tensor_scalar(out=o, in0=in0, scalar1=s1, scalar2=None, op0=op0), r, w)
        elif accum_out is None:
            em.op(eng, lambda e: e.tensor_scalar(out=o, in0=in0, scalar1=s1, scalar2=s2, op0=op0, op1=op1), r, w)
        else:
            em.op(eng, lambda e: e.tensor_scalar(out=o, in0=in0, scalar1=s1, scalar2=s2, op0=op0, op1=op1,
                                                 accum_out=accum_out), r, w)

    def tt(eng, o, in0, in1, op, r, w):
        em.op(eng, lambda e: e.tensor_tensor(out=o, in0=in0, in1=in1, op=op), r, w)

    def stt(o, in0, sc, in1, op0, op1, r, w):
        em.op("dve", lambda e: e.scalar_tensor_tensor(out=o, in0=in0, scalar=sc, in1=in1, op0=op0, op1=op1), r, w)

    def cp(eng, o, in_, r, w):
        if eng == "act":
            em.op("act", lambda e: e.copy(out=o, in_=in_), r, w)
        else:
            em.op(eng, lambda e: e.tensor_copy(out=o, in_=in_), r, w)

    def memset(eng, o, val, w):
        em.op(eng, lambda e: e.memset(o, val), (), w)

    def dma(q, o, in_, r, w, stream, slow=False):
        if slow:
            em.op(q, lambda e: e.dma_start(out=o, in_=in_, allow_slow_non_contiguous=True), r, w, dma=True, stream=stream)
        else:
            em.op(q, lambda e: e.dma_start(out=o, in_=in_), r, w, dma=True, stream=stream)

    def chk(tag):
        if cf.stop == tag:
            raise _Stop()

    with contextlib.ExitStack() as gst:
        def sbt(st, name, shape, dt):
            return st.enter_context(nc.sbuf_tensor("s_" + name, list(shape), dt))

        PS = gst.enter_context(nc.psum_tensor("PS", [128, 8, 512], F32))
        pb = [PS[:, i, :] for i in range(8)]
        bpb = [Buf("pb%d" % i) for i in range(8)]

        identf_t = sbt(gst, "identf", [128, 128], F32)
        cm_t = sbt(gst, "cmat", [128, 5, 128], BF16)
        iota_t = sbt(gst, "iota", [128, 512], F32)
        kcol_t = sbt(gst, "kcolc", [128, 1], F32)
        pow2_t = sbt(gst, "pow2", [128, NBIS + 1], F32)
        flag_t = sbt(gst, "flag", [128, 1], F32)
        vec_t = sbt(gst, "vecs", [128, 8, KC], F32)
        sbg_t = sbt(gst, "sbg", [128, NHS], F32)
        dsg_t = sbt(gst, "dsg", [128, NHD], F32)
        cvw_t = sbt(gst, "cvw", [128, 3, 2 * FC], F32)
        cvb_t = sbt(gst, "cvb", [128, 2 * FC], F32)
        qcol_t = sbt(gst, "qcol", [BS, NBLK], F32)
        qrel_t = sbt(gst, "qrel", [BS, NBLK, NSP], F32)
        cosK = sbt(gst, "cosK", [128, NKB, 16], F32); sinK = sbt(gst, "sinK", [128, NKB, 16], F32)
        cosQ = sbt(gst, "cosQ", [BS, NBLK, 16], F32); sinQ = sbt(gst, "sinQ", [BS, NBLK, 16], F32)
        kTds = sbt(gst, "kTds", [128, S], BF16)
        vds = sbt(gst, "vds", [128, NKB, 128], BF16)
        kTix = sbt(gst, "kTix", [128, S], BF16)
        bc = Buf("consts")
        b_vec = Buf("vecs"); b_rope = Buf("rope")
        b_kTds = Buf("kTds"); b_vds = Buf("vds"); b_kTix = Buf("kTix")

        for (t_, src, nm) in ((identf_t[:], identf[:, :], "c0"), (iota_t[:], iota[:, :], "c1"),
                              (kcol_t[:], kcolc[:, :], "c2"), (pow2_t[:], pow2[:, :], "c3"),
                              (flag_t[:], flag[:, :], "c4"), (sbg_t[:], sbg[:, :], "c5"), (dsg_t[:], dsg[:, :], "c6"),
                              (cvw_t[:], convw.rearrange("p (a f) -> p a f", a=3), "c7"), (cvb_t[:], convb[:, :], "c8"),
                              (qcol_t[:], qcol[:, :], "c9"),
                              (qrel_t[:], qrel.rearrange("p (a f) -> p a f", a=NBLK), "c10"),
                              (vec_t[:, 4, :], n1g[:, :], "c11"), (vec_t[:, 5, :], n2g[:, :], "c12")):
            dma("sp", t_, src, (), [bc], nm)
        dma("pool", cm_t[:], cmat.rearrange("p (a f) -> p a f", a=5), (), [bc], "c13")
        identb = cm_t[:, 0, :]; ntri = cm_t[:, 1, :]; nones = cm_t[:, 2, :]; umat = cm_t[:, 3, :]; onesb = cm_t[:, 4, :]

        def rope_tables(st, pos_ap, P, n, cos_t, sin_t, tag):
            pi_ = sbt(st, "pi" + tag, [P, n], I32)
            pf = sbt(st, "pf" + tag, [P, n], F32)
            ang = sbt(st, "ang" + tag, [P, n, 16], F32)
            ki = sbt(st, "ki" + tag, [P, n, 16], I32)
            kf = sbt(st, "kf" + tag, [P, n, 16], F32)
            tm = sbt(st, "tm" + tag, [P, n, 16], F32)
            ivf = sbt(st, "ivf" + tag, [P, 16], F32)
            b = Buf("ropetmp" + tag)
            dma("sp", pi_[:], pos_ap, (), [b], "rp" + tag)
            dma("sp", ivf[:], invf[0:P, :], (), [b], "rp" + tag)
            cp("dve", pf[:], pi_[:], [b], [b])
            for c in range(n):
                ts("dve", ang[:, c, :], ivf[:], pf[:, c:c + 1], None, ALU.mult, None, [b], [b])

            def reduce_sin(dst, shift):
                a2 = ang[:]
                if shift != 0.0:
                    ts("dve", tm[:], ang[:], shift, None, ALU.add, None, [b], [b])
                    a2 = tm[:]
                ts("dve", kf[:], a2, 1.0 / TWO_PI, None, ALU.mult, None, [b], [b])
                cp("dve", ki[:], kf[:], [b], [b])
                cp("dve", kf[:], ki[:], [b], [b])
                stt(tm[:], kf[:], -TWO_PI, a2, ALU.mult, ALU.add, [b], [b])
                ts("dve", kf[:], tm[:], PI, -TWO_PI, ALU.is_gt, ALU.mult, [b], [b])
                tt("dve", tm[:], tm[:], kf[:], ALU.add, [b], [b])
                ts("dve", kf[:], tm[:], -PI, TWO_PI, ALU.is_lt, ALU.mult, [b], [b])
                tt("dve", tm[:], tm[:], kf[:], ALU.add, [b], [b])
                act(dst, tm[:], AF.Sin, [b], [b_rope])

            reduce_sin(sin_t[:], 0.0)
            reduce_sin(cos_t[:], PI / 2)

        with contextlib.ExitStack() as st:
            rope_tables(st, posk[:, :], 128, NKB, cosK, sinK, "k")
            rope_tables(st, posq[:, :], BS, NBLK, cosQ, sinQ, "q")
        em.barrier()

        rope_bufs = {}

        def rope_apply(x1, x2, cos_ap, sin_ap, tmp, P, h, r, w):
            bt_ = rope_bufs.setdefault(id(tmp), Buf("ropetmp"))
            r = list(r) + [bt_]; w = list(w) + [bt_]
            tt("dve", tmp[:P, 0, :h], x1, cos_ap, ALU.mult, r, w)
            tt("dve", tmp[:P, 1, :h], x2, sin_ap, ALU.mult, r, w)
            tt("dve", tmp[:P, 2, :h], x2, cos_ap, ALU.mult, r, w)
            tt("dve", tmp[:P, 3, :h], x1, sin_ap, ALU.mult, r, w)
            tt("dve", x1, tmp[:P, 0, :h], tmp[:P, 1, :h], ALU.subtract, r, w)
            tt("dve", x2, tmp[:P, 2, :h], tmp[:P, 3, :h], ALU.add, r, w)

        nrm_ctr = [0]

        def norm_T(xt_ap, P, bx, dst_fn, bdst, G_ap, sh_ap, tmps, pbank, xs_out=None, bxs=None):
            junk, ssq, bt = tmps
            act(junk[:P, :], xt_ap, AF.Square, [bx], [bt], accum_out=ssq[:P, 0:1])
            act(ssq[:P, 1:2], ssq[:P, 0:1], AF.Sqrt, [bt], [bt], scale=1.0 / D, bias=1e-6)
            em.op("dve", lambda e: e.reciprocal(out=ssq[:P, 2:3], in_=ssq[:P, 1:2]), [bt], [bt])
            if xs_out is None:
                ts("dve", xt_ap, xt_ap, ssq[:P, 2:3], None, ALU.mult, None, [bx, bt], [bx])
            else:
                ts("dve", xs_out, xt_ap, ssq[:P, 2:3], None, ALU.mult, None, [bx, bt], [bxs])
                xt_ap = xs_out; bx = bxs
            for k0 in range(0, KC, 4):
                nj = min(4, KC - k0)
                pbi = pbank[nrm_ctr[0] % len(pbank)]
                nrm_ctr[0] += 1
                for j in range(nj):
                    tr(pb[pbi][:, j * 128:j * 128 + P], xt_ap[:, (k0 + j) * 128:(k0 + j + 1) * 128],
                       identf_t[:P, :P], [bx, bc], [bpb[pbi]])
                for j in range(nj):
                    k = k0 + j
                    if j % 2 == 0:
                        act(dst_fn(k), pb[pbi][:, j * 128:j * 128 + P], AF.Identity, [bpb[pbi], b_vec], [bdst],
                            scale=G_ap[:, k:k + 1], bias=sh_ap[:, k:k + 1])
                    else:
                        ts("dve", dst_fn(k), pb[pbi][:, j * 128:j * 128 + P], G_ap[:, k:k + 1], sh_ap[:, k:k + 1],
                           ALU.mult, ALU.add, [bpb[pbi], b_vec], [bdst])

        winv = w_in.rearrange("(k p) n -> p k n", p=128)
        G1 = vec_t[:, 0, :]; SH1 = vec_t[:, 1, :]; G2 = vec_t[:, 2, :]; SH2 = vec_t[:, 3, :]
        with contextlib.ExitStack() as st:
            MG = 256
            NG = (6 * D) // MG
            NG1 = (2 * D) // MG
            wg = [sbt(st, "wg%d" % i, [128, KC, MG], BF16) for i in range(2)]
            bwg = [Buf("wg%d" % i) for i in range(2)]
            cb_t = sbt(st, "cb", [128, KC], F32); cs_t = sbt(st, "cs", [128, KC], BF16)
            brow = [sbt(st, "brow%d" % i, [1, MG], F32) for i in range(2)]
            mrow = [sbt(st, "mrow%d" % i, [1, MG], F32) for i in range(2)]
            bbr = [Buf("brow%d" % i) for i in range(2)]; bmr = [Buf("mrow%d" % i) for i in range(2)]
            bcs = Buf("cs"); bmodg = [Buf("modrow%d" % i) for i in range(NG)]
            mrows = sbt(st, "mrows", [KC, 4, 128], F32); bmrows = Buf("mrows")
            dma("sp", cb_t[:], cb[:, :], (), [bcs], "cb")
            act(cs_t[:], cb_t[:], AF.Silu, [bcs], [bcs])
            wav = w_ada.rearrange("(k p) n -> p k n", p=128)

            def mod_load(g):
                s2 = g % 2
                dma("pool", wg[s2][:], wav[:, :, g * MG:(g + 1) * MG], (), [bwg[s2]], "wg%d" % s2)
                dma("pool", brow[s2][:], b_ada[0:1, g * MG:(g + 1) * MG], (), [bbr[s2]], "brow%d" % s2)

            def mod_compute(g, pbi):
                s2 = g % 2
                for k in range(KC):
                    mm(pb[pbi][0:1, 0:MG], cs_t[:, k:k + 1], wg[s2][:, k, :], k == 0, k == KC - 1,
                       [bcs, bwg[s2]], [bpb[pbi]])
                tt("dve", mrow[s2][:], pb[pbi][0:1, 0:MG], brow[s2][:], ALU.add, [bpb[pbi], bbr[s2]], [bmr[s2]])
                dma("pool", modrow[0:1, g * MG:(g + 1) * MG], mrow[s2][:], [bmr[s2]], [bmodg[g]], "modw%d" % s2)

            def mod_vec(i_, a_, slot, pbi):
                gs = [bmodg[g] for g in range(a_ * D // MG, (a_ + 1) * D // MG)]
                dma("sp", mrows[:, i_, :], modrow[0:1, a_ * D:(a_ + 1) * D].rearrange("o (k p) -> (o k) p", p=128),
                    gs, [bmrows], "mv")
                tr(pb[pbi][:, i_ * KC:(i_ + 1) * KC], mrows[:, i_, :], identf_t[:KC, :KC], [bmrows, bc], [bpb[pbi]])
                cp("dve", vec_t[:, slot, :], pb[pbi][:, i_ * KC:(i_ + 1) * KC], [bpb[pbi]], [b_vec])

            WAW = 2 * SBW + 320
            wA = sbt(st, "wA", [128, KC, WAW], BF16); bwA = Buf("wA")
            mod_load(0)
            if NG1 > 1:
                mod_load(1)
            for g in range(NG1):
                mod_compute(g, 7)
                if g + 2 < NG1:
                    mod_load(g + 2)
            c0 = 0
            for (src0, n) in ((cf.o_ksb, SBW), (cf.o_vsb, SBW), (cf.o_kds, 256), (cf.o_kix, 64)):
                for a in range(0, n, 512):
                    m = min(512, n - a)
                    dma("pool", wA[:, :, c0 + a:c0 + a + m], winv[:, :, src0 + a:src0 + a + m], (), [bwA], "wA")
                c0 += n
            mod_vec(0, 0, 1, 6)
            mod_vec(1, 1, 6, 6)
            stt(vec_t[:, 0, :], vec_t[:, 6, :], 1.0, vec_t[:, 4, :], ALU.add, ALU.mult, [b_vec, bc], [b_vec])
            gnext = [NG1]
            if gnext[0] < NG:
                mod_load(gnext[0])
            if gnext[0] + 1 < NG:
                mod_load(gnext[0] + 1)

            def mod_step():
                g = gnext[0]
                if g >= NG:
                    return
                mod_compute(g, 7)
                if g + 2 < NG:
                    mod_load(g + 2)
                gnext[0] += 1

            xt = [sbt(st, "xt%d" % i, [128, D], F32) for i in range(3)]; bxt = [Buf("xt%d" % i) for i in range(3)]
            junk = sbt(st, "junkA", [128, D], BF16); ssq = sbt(st, "ssqA", [128, 4], F32)
            bt = Buf("nrmtmpA")
            hTa = [sbt(st, "hTa%d" % i, [128, KC, 512], BF16) for i in range(2)]; bhT = [Buf("hTa%d" % i) for i in range(2)]
            vst = [sbt(st, "vst%d" % i, [128, SBW], BF16) for i in range(2)]; bvst = [Buf("vst%d" % i) for i in range(2)]
            kst = [sbt(st, "kst%d" % i, [128, 512], BF16) for i in range(2)]; bkst = [Buf("kst%d" % i) for i in range(2)]
            sm = [sbt(st, "smA%d" % i, [128, 384], F32) for i in range(2)]; bsm = [Buf("smA%d" % i) for i in range(2)]
            rtmp = sbt(st, "rtmpA", [128, 4, 16], F32)
            vsbv = vsb.rearrange("h p x -> p h x")
            kctr = 0
            per_blk = -(-(NG - NG1) // NKB)

            def prep(c):
                s3 = c % 3; ti = (c // 4) % 2; cc = c % 4
                dma("sp", xt[s3][:], xkv[c * 128:(c + 1) * 128, :], (), [bxt[s3]], "xt%d" % s3)
                norm_T(xt[s3][:], 128, bxt[s3], lambda k, ti=ti, cc=cc: hTa[ti][:, k, cc * 128:(cc + 1) * 128],
                       bhT[ti], G1, SH1, (junk, ssq, bt), [0, 1, 6])

            def ktr(c):
                tr(pb[5][:, 0:128], sm[c % 2][:, 0:128], identf_t[:], [bsm[c % 2], bc], [bpb[5]])
                tr(pb[5][:, 128:256], sm[c % 2][:, 256:384], identf_t[:], [bsm[c % 2], bc], [bpb[5]])
                cp("act", kTds[:, c * 128:(c + 1) * 128], pb[5][:, 0:128], [bpb[5]], [b_kTds])
                cp("act", kTix[:, c * 128:(c + 1) * 128], pb[5][:, 128:256], [bpb[5]], [b_kTix])

            prep(0)
            if NKB > 1:
                prep(1)
            for c in range(NKB):
                s2 = c % 2
                ti = (c // 4) % 2
                cc = c % 4
                for g in range(0, SBW, 512):
                    pbi = 2 + (g // 512) % 2
                    n = min(512, SBW - g)
                    for k in range(KC):
                        mm(pb[pbi][:, 0:n], hTa[ti][:, k, cc * 128:(cc + 1) * 128], wA[:, k, SBW + g:SBW + g + n],
                           k == 0, k == KC - 1, [bhT[ti], bwA], [bpb[pbi]])
                    cp("act", vst[s2][:, g:g + n], pb[pbi][:, 0:n], [bpb[pbi]], [bvst[s2]])
                dma("act", vsbv[:, :, c * 128:(c + 1) * 128], vst[s2][:].rearrange("p (h d) -> p h d", h=NHS),
                    [bvst[s2]], (), "vsbw%d" % s2)
                for k in range(KC):
                    mm(pb[4][:, 0:320], hTa[ti][:, k, cc * 128:(cc + 1) * 128], wA[:, k, 2 * SBW:2 * SBW + 320],
                       k == 0, k == KC - 1, [bhT[ti], bwA], [bpb[4]])
                smc = sm[c % 2]; bsmc = bsm[c % 2]
                cp("act", smc[:, 0:320], pb[4][:, 0:320], [bpb[4]], [bsmc])
                cp("pool", vds[:, c, :], smc[:, 128:256], [bsmc], [b_vds])
                rope_apply(smc[:, 0:16], smc[:, 16:32], cosK[:, c, :], sinK[:, c, :], rtmp, 128, 16, [bsmc, b_rope], [bsmc])
                rope_apply(smc[:, 256:264], smc[:, 264:272], cosK[:, c, 0:16:2], sinK[:, c, 0:16:2], rtmp, 128, 8,
                           [bsmc, b_rope], [bsmc])
                cp("dve", smc[:, 320:384], smc[:, 256:320], [bsmc], [bsmc])
                if c > 0:
                    ktr(c - 1)
                if cc == 3:
                    t0 = (c // 4) * 512
                    for h in range(NHS):
                        pbi = 2 + h % 2
                        for k in range(KC):
                            mm(pb[pbi][:, :], wA[:, k, h * 128:(h + 1) * 128], hTa[ti][:, k, :], k == 0, k == KC - 1,
                               [bhT[ti], bwA], [bpb[pbi]])
                        ks = kctr % 2; kctr += 1
                        cp("act", kst[ks][:], pb[pbi][:, :], [bpb[pbi]], [bkst[ks]])
                        dma("act", kTsb[h, :, t0:t0 + 512], kst[ks][:], [bkst[ks]], (), "ktw%d" % ks)
                for _ in range(per_blk):
                    mod_step()
                if c + 2 < NKB:
                    prep(c + 2)
            ktr(NKB - 1)
            while gnext[0] < NG:
                mod_step()
            mod_vec(2, 3, 3, 6)
            mod_vec(3, 4, 7, 6)
            stt(vec_t[:, 2, :], vec_t[:, 7, :], 1.0, vec_t[:, 5, :], ALU.add, ALU.mult, [b_vec, bc], [b_vec])
        em.barrier()
        chk("A")
        wov = w_out.rearrange("(k p) n -> p k n", p=128)
        h2v = h2Ts.rearrange("p (k t) -> p k t", k=KC)
        U8 = mybir.dt.uint8
        with contextlib.ExitStack() as st:
            hTt = sbt(st, "hTt", [128, KC, TW], BF16); bhTt = Buf("hTt")
            xA = [sbt(st, "xA%d" % i, [BS, D], F32) for i in range(2)]; bxA = [Buf("xA%d" % i) for i in range(2)]
            junk = sbt(st, "junkT", [BS, D], BF16); ssq = sbt(st, "ssqT", [BS, 4], F32); bt = Buf("nrmtmpT")
            wq = [sbt(st, "wq%d" % i, [128, KC, 256], BF16) for i in range(2)]; bwq = [Buf("wq%d" % i) for i in range(2)]
            qTsb = sbt(st, "qTsb", [128, NHS, TW], BF16); bqsb = Buf("qTsb")
            qTds = sbt(st, "qTds", [128, NHD, TW], BF16); bqds = Buf("qTds")
            qTix = sbt(st, "qTix", [128, IH // 2, TW], BF16); bqix = Buf("qTix")
            wix = sbt(st, "wix", [BS, TB, IH], F32); bwix = Buf("wix")
            qtm = sbt(st, "qtm", [BS, 256], F32); bqtm = Buf("qtm")
            qtm2 = [sbt(st, "qtm2_%d" % i, [BS, 256], F32) for i in range(2)]; bqtm2 = [Buf("qtm2_%d" % i) for i in range(2)]
            rtmp = sbt(st, "rtmpT", [BS, 4, 16], F32)
            Mb = sbt(st, "Mb", [BS, TB, S], BF16); bMb = [Buf("Mb%d" % i) for i in range(TB)]
            SCRB = max(S * 4 + 8192, NKB * TW * 2, TB * D * 4)
            scr = sbt(st, "scr", [128, SCRB], U8)
            kvh = sbt(st, "kvh", [128, 4 * S], U8)
            kTh = kvh[:, 0:2 * S].bitcast(BF16); bkTh = Buf("kTh")
            vh = kvh[:, 2 * S:4 * S].bitcast(BF16); bvh = Buf("vh")
            scoreb = [scr[:BS, 0:S * 4].bitcast(F32), kvh[:BS, 0:S * 4].bitcast(F32)]
            bscb = [Buf("score0"), Buf("score1")]
            relb = [scr[:BS, S * 4 + 4096 * i:S * 4 + 4096 * i + 2048].bitcast(BF16) for i in range(2)]
            brel = [Buf("rel%d" % i) for i in range(2)]
            biasT = [scr[:BS, S * 4 + 4096 * i + 2048:S * 4 + 4096 * (i + 1)].bitcast(F32) for i in range(2)]
            bbias = [Buf("biasT%d" % i) for i in range(2)]
            diagw = sbt(st, "diagw", [BS, IH, BS], BF16); bdiag = Buf("diagw")
            Yt = scr[:, 0:NKB * TW * 2].bitcast(BF16); bY = Buf("Yt")
            xqb = [scr[:BS, D * 4 * i:D * 4 * (i + 1)].bitcast(F32) for i in range(TB)]; bxq = [Buf("xqb%d" % i) for i in range(TB)]
            bis = [sbt(st, "bis%d" % i, [BS, 8 + NSP], F32) for i in range(2)]; bbis = [Buf("bis%d" % i) for i in range(2)]
            dtab = [sbt(st, "dtab%d" % i, [BS, NBIS + 1], F32) for i in range(2)]
            ndtab = [sbt(st, "ndtab%d" % i, [BS, NBIS + 1], F32) for i in range(2)]
            mrg = sbt(st, "mrg", [128, NM, TW], BF16); bmrg = [Buf("mrg%d" % i) for i in range(NM)]
            ytmp = sbt(st, "ytmp", [128, TW], F32); bytmp = Buf("ytmp")
            qrow_t = sbt(st, "qrow", [128, TW], F32); bqrow = Buf("qrow")
            ebuf = [sbt(st, "ebuf%d" % i, [128, TW], F32) for i in range(2)]; beb = [Buf("ebuf%d" % i) for i in range(2)]
            spb = [sbt(st, "spb%d" % i, [128, TW], BF16) for i in range(2)]; bspb = [Buf("spb%d" % i) for i in range(2)]
            Ab = [sbt(st, "Ab%d" % i, [128, TW], BF16) for i in range(2)]; bAb = [Buf("Ab%d" % i) for i in range(2)]
            wb = [sbt(st, "wb%d" % i, [128, TW], BF16) for i in range(2)]; bwb = [Buf("wb%d" % i) for i in range(2)]
            pbuf = [sbt(st, "pbuf%d" % i, [128, TW], BF16) for i in range(2)]; bpbuf = [Buf("pbuf%d" % i) for i in range(2)]
            rden = sbt(st, "rden", [128, TW], F32); brden = Buf("rden")
            g1t = [sbt(st, "g1t%d" % i, [BS, 256], F32) for i in range(2)]; bg1 = [Buf("g1t%d" % i) for i in range(2)]
            gsq = sbt(st, "gsq", [128, TW], BF16); bgsq = Buf("gsq")
            grs = sbt(st, "grs", [128, 2, TW], F32); bgrs = Buf("grs")
            h2st = sbt(st, "h2st", [128, KC, BS], BF16); bh2st = Buf("h2st")
            wqctr = [0]

            def wq_load(src_v, col0, n):
                s = wqctr[0] % 2; wqctr[0] += 1
                dma("pool", wq[s][:, :, 0:n], src_v[:, :, col0:col0 + n], (), [bwq[s]], "wq%d" % s)
                return s

            for t in range(NT):
                tc0 = t * TW
                St = cf.tile_keys(t); NKBt = St // 128; NSPt = St // 512
                for bi in range(TB):
                    blk = t * TB + bi
                    xa = blk % 2
                    dma("sp", xA[xa][:], xq[blk * BS:(blk + 1) * BS, :], (), [bxA[xa]], "xA%d" % xa)
                    norm_T(xA[xa][:], BS, bxA[xa], lambda k, bi=bi: hTt[:, k, bi * BS:(bi + 1) * BS], bhTt,
                           G1, SH1, (junk, ssq, bt), [0, 1])
                dma("sp", qrow_t[:], qrow[:, tc0:tc0 + TW], (), [bqrow], "qrow")
                chk("T1")
                for g in range(0, SBW, 256):
                    s = wq_load(winv, cf.o_qsb + g, 256)
                    for hh in range(2):
                        h = g // 128 + hh
                        pbi = 2 + h % 2
                        for k in range(KC):
                            mm(pb[pbi][:, 0:TW], wq[s][:, k, hh * 128:(hh + 1) * 128], hTt[:, k, :], k == 0, k == KC - 1,
                               [bwq[s], bhTt], [bpb[pbi]])
                        act(qTsb[:, h, :], pb[pbi][:, 0:TW], AF.Copy, [bpb[pbi]], [bqsb], scale=cf.qk_scale)
                qsteps = [("ds", g) for g in range(0, DSW, 256)] + [("ix", g) for g in range(0, IH * ID, 256)]
                pend = []
                sctr = [0]

                def q_mm(kind, s, bi):
                    blk = t * TB + bi
                    sl = sctr[0] % 2; sctr[0] += 1
                    pm = 4 if sl == 0 else 6
                    for k in range(KC):
                        mm(pb[pm][:BS, 0:256], hTt[:, k, bi * BS:(bi + 1) * BS], wq[s][:, k, :], k == 0, k == KC - 1,
                           [bwq[s], bhTt], [bpb[pm]])
                    cp("act", qtm2[sl][:, :], pb[pm][:BS, 0:256], [bpb[pm]], [bqtm2[sl]])
                    if kind == "ds":
                        for hh in range(2):
                            o_ = hh * 128
                            rope_apply(qtm2[sl][:, o_:o_ + 16], qtm2[sl][:, o_ + 16:o_ + 32], cosQ[:, blk, :], sinQ[:, blk, :],
                                       rtmp, BS, 16, [bqtm2[sl], b_rope], [bqtm2[sl]])
                    else:
                        for hh in range(256 // ID):
                            o_ = hh * ID
                            rope_apply(qtm2[sl][:, o_:o_ + 8], qtm2[sl][:, o_ + 8:o_ + 16], cosQ[:, blk, 0:16:2],
                                       sinQ[:, blk, 0:16:2], rtmp, BS, 8, [bqtm2[sl], b_rope], [bqtm2[sl]])
                    return sl

                def q_tr(kind, g, bi, sl):
                    pt = 5 if sl == 0 else 7
                    for pp in range(2):
                        tr(pb[pt][:, pp * 128:pp * 128 + BS], qtm2[sl][:, pp * 128:(pp + 1) * 128], identf_t[:BS, :BS],
                           [bqtm2[sl], bc], [bpb[pt]])
                    for pp in range(2):
                        if kind == "ds":
                            act(qTds[:, g // 128 + pp, bi * BS:(bi + 1) * BS], pb[pt][:, pp * 128:pp * 128 + BS], AF.Copy,
                                [bpb[pt]], [bqds], scale=cf.qk_scale)
                        else:
                            cp("act", qTix[:, g // 128 + pp, bi * BS:(bi + 1) * BS], pb[pt][:, pp * 128:pp * 128 + BS],
                               [bpb[pt]], [bqix])

                for (kind, g) in qsteps:
                    s = wq_load(winv, (cf.o_qds if kind == "ds" else cf.o_qix) + g, 256)
                    for bi in range(TB):
                        sl = q_mm(kind, s, bi)
                        if pend:
                            q_tr(*pend.pop())
                        pend.append((kind, g, bi, sl))
                if pend:
                    q_tr(*pend.pop())
                s = wq_load(winv, cf.o_wix, IH)
                for bi in range(TB):
                    for k in range(KC):
                        mm(pb[4][:BS, 0:IH], hTt[:, k, bi * BS:(bi + 1) * BS], wq[s][:, k, 0:IH], k == 0, k == KC - 1,
                           [bwq[s], bhTt], [bpb[4]])
                    act(wix[:, bi, :], pb[4][:BS, 0:IH], AF.Copy, [bpb[4]], [bwix], scale=cf.idx_scale)

                chk("T2")
                def idx_block(bi):
                    blk = t * TB + bi
                    sb_ = bi % 2; sc = scoreb[sb_]; bs_ = bscb[sb_]; B_ = bis[sb_]; bB = bbis[sb_]
                    for h in range(IH):
                        ts("pool", diagw[:, h, :], identb[:BS, :BS], wix[:, bi, h:h + 1], None, ALU.mult, None,
                           [bc, bwix], [bdiag])
                    gctr = 0; spctr = 0
                    for sp_ in range(0, St, 1024):
                        nsp = min(1024, St - sp_); nb = nsp // 512
                        ab = 4 + 2 * (spctr % 2); spctr += 1
                        def idx_mm(h, pg):
                            po = (h % 2) * 64
                            for q_ in range(nb):
                                mm(pb[pg + q_][:BS, :], qTix[po:po + 64, h // 2, bi * BS:(bi + 1) * BS],
                                   kTix[po:po + 64, sp_ + q_ * 512:sp_ + (q_ + 1) * 512], True, True,
                                   [bqix, b_kTix], [bpb[pg + q_]])

                        pgs = []
                        for h in range(IH):
                            pgs.append((gctr % 2) * 2); gctr += 1
                        idx_mm(0, pgs[0])
                        for h in range(IH):
                            pg = pgs[h]
                            if h + 1 < IH:
                                idx_mm(h + 1, pgs[h + 1])
                            rs = h % 2
                            em.op("dve", lambda e, rs=rs, pg=pg, nb=nb, nsp=nsp: e.tensor_scalar(
                                out=relb[rs][:, 0:nsp].rearrange("p (a f) -> p a f", a=nb), in0=PS[:BS, pg:pg + nb, :],
                                scalar1=0.0, scalar2=None, op0=ALU.max),
                                [bpb[pg + q_] for q_ in range(nb)], [brel[rs]])
                            for q_ in range(nb):
                                mm(pb[ab + q_][:BS, :], diagw[:, h, :], relb[rs][:, q_ * 512:(q_ + 1) * 512], h == 0, h == IH - 1,
                                   [bdiag, brel[rs]], [bpb[ab + q_]])
                        for q_ in range(nb):
                            kt = sp_ // 512 + q_
                            em.op("dve", lambda e, kt=kt, ab=ab, q_=q_, B_=B_: e.tensor_reduce(
                                out=B_[:, 8 + kt:9 + kt], in_=pb[ab + q_][:BS, :], axis=AX.X, op=ALU.min),
                                [bpb[ab + q_]], [bB])
                            ts("dve", biasT[q_ % 2], iota_t[:BS, :], qrel_t[:, blk, kt:kt + 1], -1e30, ALU.is_gt, ALU.mult,
                               [bc], [bbias[q_ % 2]])
                            tt("dve", sc[:, kt * 512:(kt + 1) * 512], pb[ab + q_][:BS, :], biasT[q_ % 2], ALU.add,
                               [bpb[ab + q_], bbias[q_ % 2]], [bs_])
                    em.op("dve", lambda e, St=St, sc=sc, B_=B_: e.tensor_reduce(out=B_[:, 0:1], in_=sc[:, 0:St], axis=AX.X, op=ALU.max), [bs_], [bB])
                    em.op("dve", lambda e, NSPt=NSPt, B_=B_: e.tensor_reduce(out=B_[:, 1:2], in_=B_[:, 8:8 + NSPt], axis=AX.X, op=ALU.min), [bB], [bB])
                    stt(B_[:, 6:7], B_[:, 0:1], 2.0, B_[:, 1:2], ALU.add, ALU.subtract, [bB], [bB])
                    ts("dve", dtab[sb_][:], pow2_t[:BS, :], B_[:, 6:7], None, ALU.mult, None, [bB, bc], [bB])
                    ts("dve", ndtab[sb_][:], dtab[sb_][:], -1.0, None, ALU.mult, None, [bB], [bB])
                    ts("dve", B_[:, 2:3], B_[:, 1:2], -1.0, 1.0, ALU.mult, ALU.add, [bB], [bB])
                    tt("dve", B_[:, 2:3], B_[:, 2:3], dtab[sb_][:, 0:1], ALU.subtract, [bB], [bB])

                def bisect(bi):
                    sb_ = bi % 2; sc = scoreb[sb_]; bs_ = bscb[sb_]; B_ = bis[sb_]; bB = bbis[sb_]
                    for r_ in range(NBIS):
                        act(Mb[:, bi, 0:St], sc[:, 0:St], AF.Sign, [bs_, bB], [bMb[bi], bB], bias=B_[:, 2:3], accum_out=B_[:, 3:4])
                        act(B_[:, 4:5], B_[:, 3:4], AF.Sign, [bB], [bB], bias=float(St - 2 * cf.TOPK) + 0.5)
                        act(B_[:, 2:3], B_[:, 4:5], AF.Identity, [bB], [bB], scale=ndtab[sb_][:, r_ + 1:r_ + 2], bias=B_[:, 2:3])

                def finalize(bi):
                    sb_ = bi % 2; sc = scoreb[sb_]; bs_ = bscb[sb_]; B_ = bis[sb_]; bB = bbis[sb_]
                    stt(B_[:, 5:6], B_[:, 2:3], -1.0, dtab[sb_][:, NBIS:NBIS + 1], ALU.mult, ALU.subtract, [bB], [bB])
                    ts("dve", Mb[:, bi, 0:St], sc[:, 0:St], B_[:, 5:6], NEGB, ALU.is_le, ALU.mult, [bs_, bB], [bMb[bi]])

                idx_block(0)
                for bi in range(TB):
                    bisect(bi)
                    if bi + 1 < TB:
                        idx_block(bi + 1)
                    finalize(bi)

                chk("T3")
                for h in range(NHD):
                    def S_step(c):
                        pz = c % 2
                        mm(pb[pz][:, 0:TW], kTds[:, c * 128:(c + 1) * 128], qTds[:, h, :], True, False,
                           [b_kTds, bqds], [bpb[pz]])
                        for bi in range(TB):
                            mm(pb[pz][:, bi * BS:(bi + 1) * BS], Mb[:, bi, c * 128:(c + 1) * 128], identb[:BS, :BS],
                               False, bi == TB - 1, [bMb[bi], bc], [bpb[pz]])
                        act(pbuf[pz][:], pb[pz][:, 0:TW], AF.Exp, [bpb[pz]], [bpbuf[pz]])

                    def PV_step(c):
                        pz = c % 2
                        mm(pb[2][:, 0:TW], vds[:, c, :], pbuf[pz][:], c == 0, c == NKBt - 1, [b_vds, bpbuf[pz]], [bpb[2]])
                        mm(pb[3][:, 0:TW], onesb, pbuf[pz][:], c == 0, c == NKBt - 1, [bc, bpbuf[pz]], [bpb[3]])

                    S_step(0)
                    for c in range(NKBt):
                        if c + 1 < NKBt:
                            S_step(c + 1)
                        PV_step(c)
                    em.op("dve", lambda e: e.reciprocal(out=rden[:], in_=pb[3][:, 0:TW]), [bpb[3]], [brden])
                    tt("dve", mrg[:, NHS + h, :], pb[2][:, 0:TW], rden[:], ALU.mult, [bpb[2], brden], [bmrg[NHS + h]])

                chk("T4")
                em.barrier()
                for c in range(NKBt):
                    ts("dve", ytmp[:], qrow_t[:], float(-128 * c), 0.0, ALU.add, ALU.max, [bqrow], [bytmp])
                    ts("dve", Yt[:, c * TW:(c + 1) * TW], ytmp[:], kcol_t[:, 0:1], None, ALU.is_equal, None, [bytmp, bc], [bY])
                for h in range(NHS):
                    dma("sp", kTh[:, 0:St], kTsb[h, :, 0:St], (), [bkTh], "kTh")
                    dma("sp", vh[:, 0:St], vsb[h, :, 0:St], (), [bvh], "vh")
                    order = list(range(NKBt - 1, -1, -1))

                    def Z_step(i):
                        c = order[i]; pz = 4 + i % 2
                        mm(pb[pz][:, 0:TW], kTh[:, c * 128:(c + 1) * 128], qTsb[:, h, :], True, False,
                           [bkTh, bqsb], [bpb[pz]])
                        mm(pb[pz][:, 0:TW], umat, Yt[:, c * TW:(c + 1) * TW], False, True, [bc, bY], [bpb[pz]])
                        act(ebuf[i % 2][:], pb[pz][:, 0:TW], AF.Exp, [bpb[pz]], [beb[i % 2]])
                        act(spb[i % 2][:], ebuf[i % 2][:], AF.Ln, [beb[i % 2]], [bspb[i % 2]], bias=1.0)

                    def X_step(i):
                        c = order[i]; px = 6 + i % 2
                        mm(pb[px][:, 0:TW], kTh[:, c * 128:(c + 1) * 128], qTsb[:, h, :], True, False,
                           [bkTh, bqsb], [bpb[px]])
                        mm(pb[px][:, 0:TW], umat, Yt[:, c * TW:(c + 1) * TW], False, False, [bc, bY], [bpb[px]])
                        if i > 0:
                            mm(pb[px][:, 0:TW], nones, Ab[i % 2][:], False, False, [bc, bAb[i % 2]], [bpb[px]])
                        mm(pb[px][:, 0:TW], ntri, spb[i % 2][:], False, True, [bc, bspb[i % 2]], [bpb[px]])
                        if i == 0:
                            cp("pool", Ab[1][:], spb[0][:], [bspb[0]], [bAb[1]])
                        elif i + 1 < NKBt:
                            tt("pool", Ab[(i + 1) % 2][:], Ab[i % 2][:], spb[i % 2][:], ALU.add,
                               [bAb[i % 2], bspb[i % 2]], [bAb[(i + 1) % 2]])
                        act(wb[i % 2][:], pb[px][:, 0:TW], AF.Exp, [bpb[px]], [bwb[i % 2]])

                    def PVs(i):
                        c = order[i]
                        mm(pb[3][:, 0:TW], vh[:, c * 128:(c + 1) * 128], wb[i % 2][:], i == 0, i == NKBt - 1, [bvh, bwb[i % 2]], [bpb[3]])

                    Z_step(0)
                    for i in range(NKBt):
                        if i + 1 < NKBt:
                            Z_step(i + 1)
                        X_step(i)
                        if i > 0:
                            PVs(i - 1)
                    PVs(NKBt - 1)
                    cp("dve", mrg[:, h, :], pb[3][:, 0:TW], [bpb[3]], [bmrg[h]])

                chk("T5")
                for gi, (h0, nh_, g_t, W_) in enumerate(((0, NHS, sbg_t, SBW), (NHS, NHD, dsg_t, DSW))):
                    for hh in range(nh_):
                        act(gsq[:], mrg[:, h0 + hh, :], AF.Square, [bmrg[h0 + hh]], [bgsq])
                        mm(pb[0][:, 0:TW], onesb, gsq[:], hh == 0, hh == nh_ - 1, [bc, bgsq], [bpb[0]])
                    act(grs[:, 0, :], pb[0][:, 0:TW], AF.Sqrt, [bpb[0]], [bgrs], scale=1.0 / W_, bias=1e-6)
                    em.op("dve", lambda e: e.reciprocal(out=grs[:, 1, :], in_=grs[:, 0, :]), [bgrs], [bgrs])
                    for hh in range(nh_):
                        stt(mrg[:, h0 + hh, :], mrg[:, h0 + hh, :], g_t[:, hh:hh + 1], grs[:, 1, :], ALU.mult, ALU.mult,
                            [bmrg[h0 + hh], bgrs, bc], [bmrg[h0 + hh]])

                chk("T6")
                em.barrier()
                for bi in range(TB):
                    blk = t * TB + bi
                    dma("sp", xqb[bi], xq[blk * BS:(blk + 1) * BS, :], (), [bxq[bi]], "xqb%d" % bi)
                for gi, g in enumerate(range(0, D, 256)):
                    s = wq_load(wov, g, 256)
                    gs = gi % 2
                    dma("sp", g1t[gs][:], modrow[0:1, 2 * D + g:2 * D + g + 256].partition_broadcast(BS), (), [bg1[gs]], "g1t%d" % gs)
                    for bi in range(TB):
                        pbi = bi % 2
                        for k in range(NM):
                            mm(pb[pbi][:BS, 0:256], mrg[:, k, bi * BS:(bi + 1) * BS], wq[s][:, k, :], k == 0, k == NM - 1,
                               bmrg + [bwq[s]], [bpb[pbi]])
                        tt("dve", qtm[:, :], pb[pbi][:BS, 0:256], g1t[gs][:], ALU.mult, [bpb[pbi], bg1[gs]], [bqtm])
                        tt("dve", xqb[bi][:, g:g + 256], xqb[bi][:, g:g + 256], qtm[:, :], ALU.add, [bqtm, bxq[bi]], [bxq[bi]])
                for bi in range(TB):
                    blk = t * TB + bi
                    dma("pool", xmid[blk * BS:(blk + 1) * BS, :], xqb[bi], [bxq[bi]], (), "xmidw%d" % bi)
                    norm_T(xqb[bi], BS, bxq[bi], lambda k: h2st[:, k, :], bh2st, G2, SH2, (junk, ssq, bt), [2, 3],
                           xs_out=xA[blk % 2][:], bxs=bxA[blk % 2])
                    dma("act", h2v[:, :, blk * BS:(blk + 1) * BS], h2st[:], [bh2st], (), "h2w")
                em.barrier()
        em.barrier()

        chk("T")
        wuv = w_up.rearrange("(k p) n -> p k n", p=128)
        wdv = w_down.rearrange("(k p) n -> p k n", p=128)
        with contextlib.ExitStack() as st:
            mT = sbt(st, "mT", [128, FC, OWN], BF16); bmT = Buf("mT")
            h2T = sbt(st, "h2T", [128, KC, TQ], BF16); b_h2T = Buf("h2T")
            dma("sp", h2T[:], h2Ts.rearrange("p (k t) -> p k t", k=KC), (), [b_h2T], "h2l")
            with contextlib.ExitStack() as st2:
                wu = [sbt(st2, "wu%d" % i, [128, KC, 256], BF16) for i in range(3)]; bwu = [Buf("wu%d" % i) for i in range(3)]
                usb = [sbt(st2, "usb%d" % i, [128, TQ], F32) for i in range(2)]; busb = [Buf("usb%d" % i) for i in range(2)]
                ucg = sbt(st2, "ucg", [128, OWN], F32); bucg = Buf("ucg")
                ucv = sbt(st2, "ucv", [128, OWN], F32); bucv = Buf("ucv")
                for f in range(FC):
                    s = f % 3
                    dma("pool", wu[s][:, :, 0:128], wuv[:, :, f * 128:(f + 1) * 128], (), [bwu[s]], "wu%d" % s)
                    dma("pool", wu[s][:, :, 128:256], wuv[:, :, DFF + f * 128:DFF + (f + 1) * 128], (), [bwu[s]], "wu%d" % s)
                    for half in range(2):
                        ci = f + half * FC
                        ub = usb[half]; bub = busb[half]
                        for t in range(NT):
                            pbi = (2 * t + half) % 4
                            for k in range(KC):
                                mm(pb[pbi][:, 0:TW], wu[s][:, k, half * 128:(half + 1) * 128], h2T[:, k, t * TW:(t + 1) * TW],
                                   k == 0, k == KC - 1, [bwu[s], b_h2T], [bpb[pbi]])
                            cp("act", ub[:, t * TW:(t + 1) * TW], pb[pbi][:, 0:TW], [bpb[pbi]], [bub])
                        ts("dve", ub[:, 0:2], ub[:, 0:2], flag_t[:, 0:1], None, ALU.mult, None, [bub, bc], [bub])
                        uc = ucg if half == 0 else ucv
                        buc = bucg if half == 0 else bucv
                        ubv = ub[:].rearrange("p (s w) -> p s w", s=NT)
                        uc3 = uc[:].rearrange("p (s w) -> p s w", s=NT)
                        act(uc3, ubv[:, :, 2:TW], AF.Identity, [bub, bc], [buc], scale=cvw_t[:, 2, ci:ci + 1], bias=cvb_t[:, ci:ci + 1])
                        stt(uc3, ubv[:, :, 1:TW - 1], cvw_t[:, 1, ci:ci + 1], uc3, ALU.mult, ALU.add, [bub, bc, buc], [buc])
                        stt(uc3, ubv[:, :, 0:TW - 2], cvw_t[:, 0, ci:ci + 1], uc3, ALU.mult, ALU.add, [bub, bc, buc], [buc])
                    act(ucg[:], ucg[:], AF.Silu, [bucg], [bucg])
                    tt("dve", mT[:, f, :], ucg[:], ucv[:], ALU.mult, [bucg, bucv], [bmT])
            em.barrier()
            with contextlib.ExitStack() as st2:
                wd = [sbt(st2, "wd%d" % i, [128, FC, 256], BF16) for i in range(2)]; bwd = [Buf("wd%d" % i) for i in range(2)]
                yst = [sbt(st2, "yst%d" % i, [128, 256], F32) for i in range(2)]; byst = [Buf("yst%d" % i) for i in range(2)]
                yc = 0
                for gi, g in enumerate(range(0, D, 256)):
                    s = gi % 2
                    for f0 in range(0, FC, 16):
                        f1 = min(FC, f0 + 16)
                        dma("pool", wd[s][:, f0:f1, :], wdv[:, f0:f1, g:g + 256], (), [bwd[s]], "wd%d" % s)
                    for tb in range(NOB):
                        pbi = tb % 2
                        for f in range(FC):
                            mm(pb[pbi][:, 0:256], mT[:, f, tb * 128:(tb + 1) * 128], wd[s][:, f, :], f == 0, f == FC - 1,
                               [bmT, bwd[s]], [bpb[pbi]])
                        ys = yc % 2; yc += 1
                        cp("act", yst[ys][:], pb[pbi][:, 0:256], [bpb[pbi]], [byst[ys]])
                        dma("act", yscr[tb * 128:(tb + 1) * 128, g:g + 256], yst[ys][:], [byst[ys]], (), "yscrw%d" % ys)
        em.barrier()
        with contextlib.ExitStack() as st:
            g2bc = sbt(st, "g2bc", [128, D], F32); fngt = sbt(st, "fngt", [128, D], F32); bgg = Buf("g2fng")
            xm = [sbt(st, "xm%d" % i, [128, D], F32) for i in range(2)]; bxm = [Buf("xm%d" % i) for i in range(2)]
            yy = [sbt(st, "yy%d" % i, [128, D], F32) for i in range(2)]; byy = [Buf("yy%d" % i) for i in range(2)]
            junk = sbt(st, "junkF", [128, D], BF16); ssq = sbt(st, "ssqF", [128, 4], F32); bt = Buf("nrmF")
            dma("sp", g2bc[:], modrow[0:1, 5 * D:6 * D].partition_broadcast(128), (), [bgg], "g2bc")
            dma("sp", fngt[:], fng[:, :], (), [bgg], "g2bc")
            for tb in range(NOB):
                s = tb % 2
                r0_ = (tb // cf.BPS) * TW + 2 + (tb % cf.BPS) * 128
                dma("sp", xm[s][:], xmid[r0_:r0_ + 128, :], (), [bxm[s]], "xm%d" % s)
                dma("sp", yy[s][:], yscr[tb * 128:(tb + 1) * 128, :], (), [byy[s]], "yy%d" % s)
                tt("dve", yy[s][:], yy[s][:], g2bc[:], ALU.mult, [byy[s], bgg], [byy[s]])
                tt("dve", xm[s][:], xm[s][:], yy[s][:], ALU.add, [byy[s], bxm[s]], [bxm[s]])
                act(junk[:], xm[s][:], AF.Square, [bxm[s]], [bt], accum_out=ssq[:, 0:1])
                act(ssq[:, 1:2], ssq[:, 0:1], AF.Sqrt, [bt], [bt], scale=1.0 / D, bias=1e-6)
                em.op("dve", lambda e: e.reciprocal(out=ssq[:, 2:3], in_=ssq[:, 1:2]), [bt], [bt])
                stt(yy[s][:], xm[s][:], ssq[:, 2:3], fngt[:], ALU.mult, ALU.mult, [bxm[s], bt, bgg], [byy[s]])
                dma("pool", out[tb * 128:(tb + 1) * 128, :], yy[s][:], [byy[s]], (), "outw%d" % s)
        em.emit(final_waits=["outw0", "outw1"] if NOB > 1 else ["outw0"])
    return nc


def host_inputs(cf, inp, core):
    D, S, KC, NKB, TQ, BS, NBLK, NSP, FC = cf.D, cf.S, cf.KC, cf.NKB, cf.TQ, cf.BS, cf.NBLK, cf.NSP, cf.FC
    b = core // cf.CPB; j = core % cf.CPB
    f32 = np.float32
    x = np.asarray(inp["x"], f32); pos = np.asarray(inp["positions"], np.int32)
    tok = np.concatenate([np.arange(cf.seg_tokens(j, t) - 2, cf.seg_tokens(j, t) + cf.SEG) for t in range(cf.NT)])
    assert tok.shape[0] == TQ
    tokc = np.maximum(tok, 0)
    m = {}
    m["xkv"] = np.ascontiguousarray(x[b])
    m["xq"] = np.ascontiguousarray(x[b][tokc])
    m["posk"] = np.ascontiguousarray(pos[b].reshape(NKB, 128).T)
    m["posq"] = np.ascontiguousarray(pos[b][tokc].reshape(NBLK, BS).T)
    qc = tokc.astype(f32).reshape(NBLK, BS).T
    m["qcol"] = np.ascontiguousarray(qc)
    qr = qc[:, :, None] - (512.0 * np.arange(NSP, dtype=f32))[None, None, :]
    m["qrel"] = np.ascontiguousarray(qr.reshape(BS, NBLK * NSP).astype(f32))
    m["qrow"] = np.ascontiguousarray(np.broadcast_to(tokc.astype(f32)[None, :], (128, TQ)))
    pk = lambda v: np.ascontiguousarray(np.asarray(v, f32).reshape(-1, 128).T)
    m["cb"] = pk(np.asarray(inp["c"], f32)[b])
    m["w_ada"] = np.ascontiguousarray(np.asarray(inp["w_ada"], f32)[0])
    m["b_ada"] = np.ascontiguousarray(np.asarray(inp["b_ada"], f32)[0][None, :])
    m["w_in"] = np.ascontiguousarray(np.asarray(inp["w_in"], f32)[0])
    m["w_out"] = np.ascontiguousarray(np.asarray(inp["w_out"], f32)[0])
    m["w_up"] = np.ascontiguousarray(np.asarray(inp["w_up"], f32)[0])
    m["w_down"] = np.ascontiguousarray(np.asarray(inp["w_down"], f32)[0])
    m["n1g"] = pk(np.asarray(inp["norm1_g"])[0]); m["n2g"] = pk(np.asarray(inp["norm2_g"])[0])
    m["sbg"] = pk(np.asarray(inp["sb_norm_g"])[0]); m["dsg"] = pk(np.asarray(inp["dsa_norm_g"])[0])
    m["fng"] = np.ascontiguousarray(np.broadcast_to(np.asarray(inp["final_norm_g"], f32)[None, :], (128, D)))
    cw = np.asarray(inp["conv_w"], f32)[0]
    m["convw"] = np.ascontiguousarray(np.stack([pk(cw[i]) for i in range(3)], axis=1).reshape(128, 3 * 2 * FC))
    m["convb"] = pk(np.asarray(inp["conv_b"], f32)[0])
    m["identf"] = np.eye(128, dtype=f32)
    jj = np.arange(128)[:, None]; ss = np.arange(128)[None, :]
    cm = np.stack([np.eye(128, dtype=f32), -(jj >= ss).astype(f32), -np.ones((128, 128), f32),
                   NEGB * (ss >= jj).astype(f32), np.ones((128, 128), f32)], axis=1)
    m["cmat"] = np.ascontiguousarray(cm.reshape(128, 5 * 128))
    m["iota"] = np.ascontiguousarray(np.broadcast_to(np.arange(512, dtype=f32)[None, :], (128, 512)))
    m["kcolc"] = np.arange(128, dtype=f32)[:, None].copy()
    m["pow2"] = np.ascontiguousarray(np.broadcast_to((0.5 ** np.arange(1, cf.NBIS + 2)).astype(f32)[None, :], (128, cf.NBIS + 1)))
    ivf = (np.float32(500000.0) ** (-(np.arange(16, dtype=f32) / np.float32(16)))).astype(f32)
    m["invf"] = np.ascontiguousarray(np.broadcast_to(ivf[None, :], (128, 16)))
    m["flag"] = np.full((128, 1), 0.0 if j == 0 else 1.0, f32)
    return m


_CACHE = {}


def kernel(x, c, positions, w_ada, b_ada, norm1_g, w_in, sb_norm_g, dsa_norm_g, w_out, norm2_g,
           w_up, conv_w, conv_b, w_down, final_norm_g):
    inp = dict(x=x, c=c, positions=positions, w_ada=w_ada, b_ada=b_ada, norm1_g=norm1_g, w_in=w_in,
               sb_norm_g=sb_norm_g, dsa_norm_g=dsa_norm_g, w_out=w_out, norm2_g=norm2_g, w_up=w_up,
               conv_w=conv_w, conv_b=conv_b, w_down=w_down, final_norm_g=final_norm_g)
    cf = Cfg()
    nc = build_program(cf)
    in_maps = [host_inputs(cf, inp, core) for core in range(8)]
    res = run_bass_kernel_spmd(nc, in_maps, core_ids=list(range(8)))
    outp = np.zeros((2, cf.S, cf.D), np.float32)
    for core in range(8):
        b = core // cf.CPB; j = core % cf.CPB
        o_ = np.asarray(res.results[core]["out"], np.float32)
        for t in range(cf.NT):
            g0 = cf.seg_tokens(j, t)
            outp[b, g0:g0 + cf.SEG, :] = o_[t * cf.SEG:(t + 1) * cf.SEG]
    return outp
```

```python
import contextlib
import numpy as np
import concourse.bass as bass
import concourse.mybir as mybir
from concourse.bass_utils import run_bass_kernel_spmd

F32 = mybir.dt.float32
BF16 = mybir.dt.bfloat16
I32 = mybir.dt.int32
AF = mybir.ActivationFunctionType
ALU = mybir.AluOpType
AX = mybir.AxisListType

ENGS = ("pe", "act", "dve", "pool", "sp")


class Buf:
    __slots__ = ("name", "lw", "rs")

    def __init__(self, name):
        self.name = name
        self.lw = None
        self.rs = []


class Op:
    __slots__ = ("eng", "idx", "fn", "deps", "dma", "stream", "gen", "flag", "incidx")

    def __init__(self, eng, idx, fn, dma, stream):
        self.eng = eng; self.idx = idx; self.fn = fn; self.deps = []
        self.dma = dma; self.stream = stream; self.gen = 0
        self.flag = False; self.incidx = 0


class Em:
    def __init__(self, nc):
        self.nc = nc
        self.ops = {e: [] for e in ENGS}
        self.streams = {}
        self.last_dma = {}
        self.pending = {e: None for e in ENGS}
        self.pool_dmas = []

    def barrier(self):
        lasts = [self.ops[e][-1] for e in ENGS if self.ops[e]]
        lasts += list(self.last_dma.values())
        for e in ENGS:
            self.pending[e] = lasts

    def op(self, eng, fn, reads=(), writes=(), dma=False, stream=None):
        o = Op(eng, len(self.ops[eng]), fn, dma, stream)
        deps = []
        if self.pending[eng] is not None:
            deps.extend(self.pending[eng])
            self.pending[eng] = None
        if dma and eng == "pool":
            if len(self.pool_dmas) >= 4:
                deps.append(self.pool_dmas[-4])
            self.pool_dmas.append(o)
        for b in reads:
            if b.lw is not None:
                deps.append(b.lw)
        for b in writes:
            if b.lw is not None:
                deps.append(b.lw)
            deps.extend(b.rs)
        for b in reads:
            b.rs.append(o)
        for b in writes:
            b.lw = o
            b.rs = []
        if dma:
            g = self.streams.get(stream, 0) + 1
            self.streams[stream] = g
            o.gen = g
            self.last_dma[stream] = o
        best = {}
        for d in deps:
            if d is o:
                continue
            if d.dma:
                key = ("dma", d.stream)
                if key not in best or best[key].gen < d.gen:
                    best[key] = d
            else:
                if d.eng == eng and eng == "pe":
                    continue
                key = ("eng", d.eng)
                if key not in best or best[key].idx < d.idx:
                    best[key] = d
        o.deps = list(best.values())
        self.ops[eng].append(o)
        return o

    def emit(self, final_waits=()):
        nc = self.nc
        for e in ENGS:
            seen = {}
            for o in self.ops[e]:
                nd = []
                for d in o.deps:
                    key = ("dma", d.stream) if d.dma else ("eng", d.eng)
                    val = d.gen if d.dma else d.idx
                    if seen.get(key, -1) >= val:
                        continue
                    seen[key] = val
                    nd.append(d)
                    if not d.dma:
                        d.flag = True
                o.deps = nd
        for e in ENGS:
            c = 0
            for o in self.ops[e]:
                if o.flag and not o.dma:
                    c += 1
                    o.incidx = c
        with contextlib.ExitStack() as st:
            esem = {e: st.enter_context(nc.semaphore("s_" + e)) for e in ENGS}
            dsem = {s: st.enter_context(nc.semaphore("d_%d" % i)) for i, s in enumerate(self.streams)}
            block = st.enter_context(nc.Block())

            def run(e, eng):
                for o in self.ops[e]:
                    for d in o.deps:
                        if d.dma:
                            eng.wait_ge(dsem[d.stream], 16 * d.gen)
                        else:
                            eng.wait_ge(esem[d.eng], d.incidx)
                    ins = o.fn(eng)
                    if o.dma:
                        ins.then_inc(dsem[o.stream], 16)
                    elif o.flag:
                        ins.then_inc(esem[e], 1)
                if e == "sp":
                    for s in final_waits:
                        eng.wait_ge(dsem[s], 16 * self.streams[s])

            @block.tensor
            def _(eng):
                run("pe", eng)

            @block.scalar
            def _(eng):
                run("act", eng)

            @block.vector
            def _(eng):
                run("dve", eng)

            @block.gpsimd
            def _(eng):
                run("pool", eng)

            @block.sync
            def _(eng):
                run("sp", eng)


class Cfg:
    def __init__(self, D=2048, S=4096, DFF=5504, BS=86, TB=3, NSEG=4, NBIS=24, IH=16, ID=64,
                 TOPK_MAX=256, CPB=4):
        self.D, self.S, self.DFF = D, S, DFF
        self.HD = 128
        nh = D // 128
        self.NHS = nh // 2
        self.NHD = nh - self.NHS
        self.SBW = self.NHS * 128
        self.DSW = self.NHD * 128
        self.IH, self.ID = IH, ID
        self.TOPK = min(TOPK_MAX, S // 4)
        self.CPB = CPB
        self.OWN = S // CPB
        self.NSEG = NSEG
        self.SEG = self.OWN // NSEG
        self.TW = self.SEG + 2
        self.BS, self.TB = BS, TB
        assert BS * TB == self.TW and BS <= 128 and self.TW <= 512
        self.NT = NSEG
        self.NBLK = NSEG * TB
        self.TQ = NSEG * self.TW
        self.KC = D // 128
        self.NKB = S // 128
        self.FC = DFF // 128
        assert DFF % 128 == 0 and S % 512 == 0 and self.SEG % 128 == 0
        self.NOB = self.OWN // 128
        self.BPS = self.SEG // 128
        self.NBIS = NBIS
        o = 0
        self.o_qsb = o; o += self.SBW
        self.o_ksb = o; o += self.SBW
        self.o_vsb = o; o += self.SBW
        self.o_qds = o; o += self.DSW
        self.o_kds = o; o += 128
        self.o_vds = o; o += 128
        self.o_qix = o; o += IH * ID
        self.o_kix = o; o += ID
        self.o_wix = o; o += IH
        self.INC = o
        self.NSP = S // 512
        self.idx_scale = (IH ** -0.5) * (ID ** -0.5)
        self.qk_scale = 128 ** -0.5
        self.stop = None

    def tile_keys(self, t):
        k = (self.CPB * t + self.CPB) * self.SEG
        assert k % 512 == 0 and k <= self.S
        return k

    def seg_tokens(self, j, t):
        gt = self.CPB * t + j
        return gt * self.SEG


class _Stop(Exception):
    pass


TWO_PI = 6.283185307179586
PI = 3.141592653589793
NEGB = -30000.0


def build_program(cf):
    hold = {}
    try:
        return _build(cf, hold)
    except _Stop:
        hold["em"].emit(final_waits=[])
        return hold["nc"]


def _build(cf, hold):
    nc = bass.Bass("TRN2", target_bir_lowering=False)
    em = Em(nc)
    hold["nc"] = nc; hold["em"] = em
    D, S, KC, NKB, TQ, BS, NBLK, TB, NT, TW = cf.D, cf.S, cf.KC, cf.NKB, cf.TQ, cf.BS, cf.NBLK, cf.TB, cf.NT, cf.TW
    NHS, NHD, SBW, DSW, IH, ID, FC, DFF = cf.NHS, cf.NHD, cf.SBW, cf.DSW, cf.IH, cf.ID, cf.FC, cf.DFF
    NSP, NBIS, OWN, NOB = cf.NSP, cf.NBIS, cf.OWN, cf.NOB
    NM = NHS + NHD

    def din(name, shape, dt=F32):
        return nc.dram_tensor(name, list(shape), dt, kind="ExternalInput").ap()

    def dscr(name, shape, dt):
        return nc.dram_tensor(name, list(shape), dt, kind="Internal").ap()

    xkv = din("xkv", [S, D]); xq = din("xq", [TQ, D])
    posk = din("posk", [128, NKB], I32); posq = din("posq", [BS, NBLK], I32)
    qcol = din("qcol", [BS, NBLK]); qrel = din("qrel", [BS, NBLK * NSP]); qrow = din("qrow", [128, TQ])
    cb = din("cb", [128, KC])
    w_ada = din("w_ada", [D, 6 * D]); b_ada = din("b_ada", [1, 6 * D])
    w_in = din("w_in", [D, cf.INC]); w_out = din("w_out", [D, D])
    w_up = din("w_up", [D, 2 * DFF]); w_down = din("w_down", [DFF, D])
    n1g = din("n1g", [128, KC]); n2g = din("n2g", [128, KC])
    sbg = din("sbg", [128, NHS]); dsg = din("dsg", [128, NHD]); fng = din("fng", [128, D])
    convw = din("convw", [128, 3 * 2 * FC]); convb = din("convb", [128, 2 * FC])
    identf = din("identf", [128, 128]); cmat = din("cmat", [128, 5 * 128])
    iota = din("iota", [128, 512]); kcolc = din("kcolc", [128, 1]); pow2 = din("pow2", [128, NBIS + 1])
    invf = din("invf", [128, 16]); flag = din("flag", [128, 1])
    out = nc.dram_tensor("out", [OWN, D], F32, kind="ExternalOutput").ap()
    modrow = dscr("modrow", [1, 6 * D], F32)
    kTsb = dscr("kTsb", [NHS, 128, S], BF16)
    vsb = dscr("vsb", [NHS, 128, NKB * 128], BF16)
    xmid = dscr("xmid", [TQ, D], F32)
    yscr = dscr("yscr", [OWN, D], F32)
    h2Ts = dscr("h2Ts", [128, KC * TQ], BF16)

    def mm(o, lhsT, rhs, start, stop, r, w):
        em.op("pe", lambda e: e.matmul(o, lhsT=lhsT, rhs=rhs, start=start, stop=stop), r, w)

    def tr(o, in_, ident, r, w):
        em.op("pe", lambda e: e.transpose(o, in_, ident), r, w)

    def act(o, in_, func, r, w, **kw):
        em.op("act", lambda e: e.activation(out=o, in_=in_, func=func, **kw), r, w)

    def ts(eng, o, in0, s1, s2, op0, op1, r, w, accum_out=None):
        if op1 is None:
            em.op(eng, lambda e: e.tensor_scalar(out=o, in0=in0, scalar1=s1, scalar2=None, op0=op0), r, w)
        elif accum_out is None:
            em.op(eng, lambda e: e.tensor_scalar(out=o, in0=in0, scalar1=s1, scalar2=s2, op0=op0, op1=op1), r, w)
        else:
            em.op(eng, lambda e: e.tensor_scalar(out=o, in0=in0, scalar1=s1, scalar2=s2, op0=op0, op1=op1,
                                                 accum_out=accum_out), r, w)

    def tt(eng, o, in0, in1, op, r, w):
        em.op(eng, lambda e: e.tensor_tensor(out=o, in0=in0, in1=in1, op=op), r, w)

    def stt(o, in0, sc, in1, op0, op1, r, w):
        em.op("dve", lambda e: e.scalar_tensor_tensor(out=o, in0=in0, scalar=sc, in1=in1, op0=op0, op1=op1), r, w)

    def cp(eng, o, in_, r, w):
        if eng == "act":
            em.op("act", lambda e: e.copy(out=o, in_=in_), r, w)
        else:
            em.op(eng, lambda e: e.tensor_copy(out=o, in_=in_), r, w)

    def memset(eng, o, val, w):
        em.op(eng, lambda e: e.memset(o, val), (), w)

    def dma(q, o, in_, r, w, stream, slow=False):
        if slow:
            em.op(q, lambda e: e.dma_start(out=o, in_=in_, allow_slow_non_contiguous=True), r, w, dma=True, stream=stream)
        else:
            em.op(q, lambda e: e.dma_start(out=o, in_=in_), r, w, dma=True, stream=stream)

    def chk(tag):
        if cf.stop == tag:
            raise _Stop()

    with contextlib.ExitStack() as gst:
        def sbt(st, name, shape, dt):
            return st.enter_context(nc.sbuf_tensor("s_" + name, list(shape), dt))

        PS = gst.enter_context(nc.psum_tensor("PS", [128, 8, 512], F32))
        pb = [PS[:, i, :] for i in range(8)]
        bpb = [Buf("pb%d" % i) for i in range(8)]

        identf_t = sbt(gst, "identf", [128, 128], F32)
        cm_t = sbt(gst, "cmat", [128, 5, 128], BF16)
        iota_t = sbt(gst, "iota", [128, 512], F32)
        kcol_t = sbt(gst, "kcolc", [128, 1], F32)
        pow2_t = sbt(gst, "pow2", [128, NBIS + 1], F32)
        flag_t = sbt(gst, "flag", [128, 1], F32)
        vec_t = sbt(gst, "vecs", [128, 8, KC], F32)
        sbg_t = sbt(gst, "sbg", [128, NHS], F32)
        dsg_t = sbt(gst, "dsg", [128, NHD], F32)
        cvw_t = sbt(gst, "cvw", [128, 3, 2 * FC], F32)
        cvb_t = sbt(gst, "cvb", [128, 2 * FC], F32)
        qcol_t = sbt(gst, "qcol", [BS, NBLK], F32)
        qrel_t = sbt(gst, "qrel", [BS, NBLK, NSP], F32)
        cosK = sbt(gst, "cosK", [128, NKB, 16], F32); sinK = sbt(gst, "sinK", [128, NKB, 16], F32)
        cosQ = sbt(gst, "cosQ", [BS, NBLK, 16], F32); sinQ = sbt(gst, "sinQ", [BS, NBLK, 16], F32)
        kTds = sbt(gst, "kTds", [128, S], BF16)
        vds = sbt(gst, "vds", [128, NKB, 128], BF16)
        kTix = sbt(gst, "kTix", [128, S], BF16)
        bc = Buf("consts")
        b_vec = Buf("vecs"); b_rope = Buf("rope")
        b_kTds = Buf("kTds"); b_vds = Buf("vds"); b_kTix = Buf("kTix")

        for (t_, src, nm) in ((identf_t[:], identf[:, :], "c0"), (iota_t[:], iota[:, :], "c1"),
                              (kcol_t[:], kcolc[:, :], "c2"), (pow2_t[:], pow2[:, :], "c3"),
                              (flag_t[:], flag[:, :], "c4"), (sbg_t[:], sbg[:, :], "c5"), (dsg_t[:], dsg[:, :], "c6"),
                              (cvw_t[:], convw.rearrange("p (a f) -> p a f", a=3), "c7"), (cvb_t[:], convb[:, :], "c8"),
                              (qcol_t[:], qcol[:, :], "c9"),
                              (qrel_t[:], qrel.rearrange("p (a f) -> p a f", a=NBLK), "c10"),
                              (vec_t[:, 4, :], n1g[:, :], "c11"), (vec_t[:, 5, :], n2g[:, :], "c12")):
            dma("sp", t_, src, (), [bc], nm)
        dma("pool", cm_t[:], cmat.rearrange("p (a f) -> p a f", a=5), (), [bc], "c13")
        identb = cm_t[:, 0, :]; ntri = cm_t[:, 1, :]; nones = cm_t[:, 2, :]; umat = cm_t[:, 3, :]; onesb = cm_t[:, 4, :]

        def rope_tables(st, pos_ap, P, n, cos_t, sin_t, tag):
            pi_ = sbt(st, "pi" + tag, [P, n], I32)
            pf = sbt(st, "pf" + tag, [P, n], F32)
            ang = sbt(st, "ang" + tag, [P, n, 16], F32)
            ki = sbt(st, "ki" + tag, [P, n, 16], I32)
            kf = sbt(st, "kf" + tag, [P, n, 16], F32)
            tm = sbt(st, "tm" + tag, [P, n, 16], F32)
            ivf = sbt(st, "ivf" + tag, [P, 16], F32)
            b = Buf("ropetmp" + tag)
            dma("sp", pi_[:], pos_ap, (), [b], "rp" + tag)
            dma("sp", ivf[:], invf[0:P, :], (), [b], "rp" + tag)
            cp("dve", pf[:], pi_[:], [b], [b])
            for c in range(n):
                ts("dve", ang[:, c, :], ivf[:], pf[:, c:c + 1], None, ALU.mult, None, [b], [b])

            def reduce_sin(dst, shift):
                a2 = ang[:]
                if shift != 0.0:
                    ts("dve", tm[:], ang[:], shift, None, ALU.add, None, [b], [b])
                    a2 = tm[:]
                ts("dve", kf[:], a2, 1.0 / TWO_PI, None, ALU.mult, None, [b], [b])
                cp("dve", ki[:], kf[:], [b], [b])
                cp("dve", kf[:], ki[:], [b], [b])
                stt(tm[:], kf[:], -TWO_PI, a2, ALU.mult, ALU.add, [b], [b])
                ts("dve", kf[:], tm[:], PI, -TWO_PI, ALU.is_gt, ALU.mult, [b], [b])
                tt("dve", tm[:], tm[:], kf[:], ALU.add, [b], [b])
                ts("dve", kf[:], tm[:], -PI, TWO_PI, ALU.is_lt, ALU.mult, [b], [b])
                tt("dve", tm[:], tm[:], kf[:], ALU.add, [b], [b])
                act(dst, tm[:], AF.Sin, [b], [b_rope])

            reduce_sin(sin_t[:], 0.0)
            reduce_sin(cos_t[:], PI / 2)

        with contextlib.ExitStack() as st:
            rope_tables(st, posk[:, :], 128, NKB, cosK, sinK, "k")
            rope_tables(st, posq[:, :], BS, NBLK, cosQ, sinQ, "q")
        em.barrier()

        rope_bufs = {}

        def rope_apply(x1, x2, cos_ap, sin_ap, tmp, P, h, r, w):
            bt_ = rope_bufs.setdefault(id(tmp), Buf("ropetmp"))
            r = list(r) + [bt_]; w = list(w) + [bt_]
            tt("dve", tmp[:P, 0, :h], x1, cos_ap, ALU.mult, r, w)
            tt("dve", tmp[:P, 1, :h], x2, sin_ap, ALU.mult, r, w)
            tt("dve", tmp[:P, 2, :h], x2, cos_ap, ALU.mult, r, w)
            tt("dve", tmp[:P, 3, :h], x1, sin_ap, ALU.mult, r, w)
            tt("dve", x1, tmp[:P, 0, :h], tmp[:P, 1, :h], ALU.subtract, r, w)
            tt("dve", x2, tmp[:P, 2, :h], tmp[:P, 3, :h], ALU.add, r, w)

        nrm_ctr = [0]

        def norm_T(xt_ap, P, bx, dst_fn, bdst, G_ap, sh_ap, tmps, pbank, xs_out=None, bxs=None):
            junk, ssq, bt = tmps
            act(junk[:P, :], xt_ap, AF.Square, [bx], [bt], accum_out=ssq[:P, 0:1])
            act(ssq[:P, 1:2], ssq[:P, 0:1], AF.Sqrt, [bt], [bt], scale=1.0 / D, bias=1e-6)
            em.op("dve", lambda e: e.reciprocal(out=ssq[:P, 2:3], in_=ssq[:P, 1:2]), [bt], [bt])
            if xs_out is None:
                ts("dve", xt_ap, xt_ap, ssq[:P, 2:3], None, ALU.mult, None, [bx, bt], [bx])
            else:
                ts("dve", xs_out, xt_ap, ssq[:P, 2:3], None, ALU.mult, None, [bx, bt], [bxs])
                xt_ap = xs_out; bx = bxs
            for k0 in range(0, KC, 4):
                nj = min(4, KC - k0)
                pbi = pbank[nrm_ctr[0] % len(pbank)]
                nrm_ctr[0] += 1
                for j in range(nj):
                    tr(pb[pbi][:, j * 128:j * 128 + P], xt_ap[:, (k0 + j) * 128:(k0 + j + 1) * 128],
                       identf_t[:P, :P], [bx, bc], [bpb[pbi]])
                for j in range(nj):
                    k = k0 + j
                    if j % 2 == 0:
                        act(dst_fn(k), pb[pbi][:, j * 128:j * 128 + P], AF.Identity, [bpb[pbi], b_vec], [bdst],
                            scale=G_ap[:, k:k + 1], bias=sh_ap[:, k:k + 1])
                    else:
                        ts("dve", dst_fn(k), pb[pbi][:, j * 128:j * 128 + P], G_ap[:, k:k + 1], sh_ap[:, k:k + 1],
                           ALU.mult, ALU.add, [bpb[pbi], b_vec], [bdst])

        winv = w_in.rearrange("(k p) n -> p k n", p=128)
        G1 = vec_t[:, 0, :]; SH1 = vec_t[:, 1, :]; G2 = vec_t[:, 2, :]; SH2 = vec_t[:, 3, :]
        with contextlib.ExitStack() as st:
            MG = 256
            NG = (6 * D) // MG
            NG1 = (2 * D) // MG
            wg = [sbt(st, "wg%d" % i, [128, KC, MG], BF16) for i in range(2)]
            bwg = [Buf("wg%d" % i) for i in range(2)]
            cb_t = sbt(st, "cb", [128, KC], F32); cs_t = sbt(st, "cs", [128, KC], BF16)
            brow = [sbt(st, "brow%d" % i, [1, MG], F32) for i in range(2)]
            mrow = [sbt(st, "mrow%d" % i, [1, MG], F32) for i in range(2)]
            bbr = [Buf("brow%d" % i) for i in range(2)]; bmr = [Buf("mrow%d" % i) for i in range(2)]
            bcs = Buf("cs"); bmodg = [Buf("modrow%d" % i) for i in range(NG)]
            mrows = sbt(st, "mrows", [KC, 4, 128], F32); bmrows = Buf("mrows")
            dma("sp", cb_t[:], cb[:, :], (), [bcs], "cb")
            act(cs_t[:], cb_t[:], AF.Silu, [bcs], [bcs])
            wav = w_ada.rearrange("(k p) n -> p k n", p=128)

            def mod_load(g):
                s2 = g % 2
                dma("pool", wg[s2][:], wav[:, :, g * MG:(g + 1) * MG], (), [bwg[s2]], "wg%d" % s2)
                dma("pool", brow[s2][:], b_ada[0:1, g * MG:(g + 1) * MG], (), [bbr[s2]], "brow%d" % s2)

            def mod_compute(g, pbi):
                s2 = g % 2
                for k in range(KC):
                    mm(pb[pbi][0:1, 0:MG], cs_t[:, k:k + 1], wg[s2][:, k, :], k == 0, k == KC - 1,
                       [bcs, bwg[s2]], [bpb[pbi]])
                tt("dve", mrow[s2][:], pb[pbi][0:1, 0:MG], brow[s2][:], ALU.add, [bpb[pbi], bbr[s2]], [bmr[s2]])
                dma("pool", modrow[0:1, g * MG:(g + 1) * MG], mrow[s2][:], [bmr[s2]], [bmodg[g]], "modw%d" % s2)

            def mod_vec(i_, a_, slot, pbi):
                gs = [bmodg[g] for g in range(a_ * D // MG, (a_ + 1) * D // MG)]
                dma("sp", mrows[:, i_, :], modrow[0:1, a_ * D:(a_ + 1) * D].rearrange("o (k p) -> (o k) p", p=128),
                    gs, [bmrows], "mv")
                tr(pb[pbi][:, i_ * KC:(i_ + 1) * KC], mrows[:, i_, :], identf_t[:KC, :KC], [bmrows, bc], [bpb[pbi]])
                cp("dve", vec_t[:, slot, :], pb[pbi][:, i_ * KC:(i_ + 1) * KC], [bpb[pbi]], [b_vec])

            WAW = 2 * SBW + 320
            wA = sbt(st, "wA", [128, KC, WAW], BF16); bwA = Buf("wA")
            mod_load(0)
            if NG1 > 1:
                mod_load(1)
            for g in range(NG1):
                mod_compute(g, 7)
                if g + 2 < NG1:
                    mod_load(g + 2)
            c0 = 0
            for (src0, n) in ((cf.o_ksb, SBW), (cf.o_vsb, SBW), (cf.o_kds, 256), (cf.o_kix, 64)):
                for a in range(0, n, 512):
                    m = min(512, n - a)
                    dma("pool", wA[:, :, c0 + a:c0 + a + m], winv[:, :, src0 + a:src0 + a + m], (), [bwA], "wA")
                c0 += n
            mod_vec(0, 0, 1, 6)
            mod_vec(1, 1, 6, 6)
            stt(vec_t[:, 0, :], vec_t[:, 6, :], 1.0, vec_t[:, 4, :], ALU.add, ALU.mult, [b_vec, bc], [b_vec])
            gnext = [NG1]
            if gnext[0] < NG:
                mod_load(gnext[0])
            if gnext[0] + 1 < NG:
                mod_load(gnext[0] + 1)

            def mod_step():
                g = gnext[0]
                if g >= NG:
                    return
                mod_compute(g, 7)
                if g + 2 < NG:
                    mod_load(g + 2)
                gnext[0] += 1

            xt = [sbt(st, "xt%d" % i, [128, D], F32) for i in range(3)]; bxt = [Buf("xt%d" % i) for i in range(3)]
            junk = sbt(st, "junkA", [128, D], BF16); ssq = sbt(st, "ssqA", [128, 4], F32)
            bt = Buf("nrmtmpA")
            hTa = [sbt(st, "hTa%d" % i, [128, KC, 512], BF16) for i in range(2)]; bhT = [Buf("hTa%d" % i) for i in range(2)]
            vst = [sbt(st, "vst%d" % i, [128, SBW], BF16) for i in range(2)]; bvst = [Buf("vst%d" % i) for i in range(2)]
            kst = [sbt(st, "kst%d" % i, [128, 512], BF16) for i in range(2)]; bkst = [Buf("kst%d" % i) for i in range(2)]
            sm = [sbt(st, "smA%d" % i, [128, 384], F32) for i in range(2)]; bsm = [Buf("smA%d" % i) for i in range(2)]
            rtmp = sbt(st, "rtmpA", [128, 4, 16], F32)
            vsbv = vsb.rearrange("h p x -> p h x")
            kctr = 0
            per_blk = -(-(NG - NG1) // NKB)

            def prep(c):
                s3 = c % 3; ti = (c // 4) % 2; cc = c % 4
                dma("sp", xt[s3][:], xkv[c * 128:(c + 1) * 128, :], (), [bxt[s3]], "xt%d" % s3)
                norm_T(xt[s3][:], 128, bxt[s3], lambda k, ti=ti, cc=cc: hTa[ti][:, k, cc * 128:(cc + 1) * 128],
                       bhT[ti], G1, SH1, (junk, ssq, bt), [0, 1, 6])

            def ktr(c):
                tr(pb[5][:, 0:128], sm[c % 2][:, 0:128], identf_t[:], [bsm[c % 2], bc], [bpb[5]])
                tr(pb[5][:, 128:256], sm[c % 2][:, 256:384], identf_t[:], [bsm[c % 2], bc], [bpb[5]])
                cp("act", kTds[:, c * 128:(c + 1) * 128], pb[5][:, 0:128], [bpb[5]], [b_kTds])
                cp("act", kTix[:, c * 128:(c + 1) * 128], pb[5][:, 128:256], [bpb[5]], [b_kTix])

            prep(0)
            if NKB > 1:
                prep(1)
            for c in range(NKB):
                s2 = c % 2
                ti = (c // 4) % 2
                cc = c % 4
                for g in range(0, SBW, 512):
                    pbi = 2 + (g // 512) % 2
                    n = min(512, SBW - g)
                    for k in range(KC):
                        mm(pb[pbi][:, 0:n], hTa[ti][:, k, cc * 128:(cc + 1) * 128], wA[:, k, SBW + g:SBW + g + n],
                           k == 0, k == KC - 1, [bhT[ti], bwA], [bpb[pbi]])
                    cp("act", vst[s2][:, g:g + n], pb[pbi][:, 0:n], [bpb[pbi]], [bvst[s2]])
                dma("act", vsbv[:, :, c * 128:(c + 1) * 128], vst[s2][:].rearrange("p (h d) -> p h d", h=NHS),
                    [bvst[s2]], (), "vsbw%d" % s2)
                for k in range(KC):
                    mm(pb[4][:, 0:320], hTa[ti][:, k, cc * 128:(cc + 1) * 128], wA[:, k, 2 * SBW:2 * SBW + 320],
                       k == 0, k == KC - 1, [bhT[ti], bwA], [bpb[4]])
                smc = sm[c % 2]; bsmc = bsm[c % 2]
                cp("act", smc[:, 0:320], pb[4][:, 0:320], [bpb[4]], [bsmc])
                cp("pool", vds[:, c, :], smc[:, 128:256], [bsmc], [b_vds])
                rope_apply(smc[:, 0:16], smc[:, 16:32], cosK[:, c, :], sinK[:, c, :], rtmp, 128, 16, [bsmc, b_rope], [bsmc])
                rope_apply(smc[:, 256:264], smc[:, 264:272], cosK[:, c, 0:16:2], sinK[:, c, 0:16:2], rtmp, 128, 8,
                           [bsmc, b_rope], [bsmc])
                cp("dve", smc[:, 320:384], smc[:, 256:320], [bsmc], [bsmc])
                if c > 0:
                    ktr(c - 1)
                if cc == 3:
                    t0 = (c // 4) * 512
                    for h in range(NHS):
                        pbi = 2 + h % 2
                        for k in range(KC):
                            mm(pb[pbi][:, :], wA[:, k, h * 128:(h + 1) * 128], hTa[ti][:, k, :], k == 0, k == KC - 1,
                               [bhT[ti], bwA], [bpb[pbi]])
                        ks = kctr % 2; kctr += 1
                        cp("act", kst[ks][:], pb[pbi][:, :], [bpb[pbi]], [bkst[ks]])
                        dma("act", kTsb[h, :, t0:t0 + 512], kst[ks][:], [bkst[ks]], (), "ktw%d" % ks)
                for _ in range(per_blk):
                    mod_step()
                if c + 2 < NKB:
                    prep(c + 2)
            ktr(NKB - 1)
            while gnext[0] < NG:
                mod_step()
            mod_vec(2, 3, 3, 6)
            mod_vec(3, 4, 7, 6)
            stt(vec_t[:, 2, :], vec_t[:, 7, :], 1.0, vec_t[:, 5, :], ALU.add, ALU.mult, [b_vec, bc], [b_vec])
        em.barrier()
        chk("A")
        wov = w_out.rearrange("(k p) n -> p k n", p=128)
        h2v = h2Ts.rearrange("p (k t) -> p k t", k=KC)
        U8 = mybir.dt.uint8
        with contextlib.ExitStack() as st:
            hTt = sbt(st, "hTt", [128, KC, TW], BF16); bhTt = Buf("hTt")
            xA = [sbt(st, "xA%d" % i, [BS, D], F32) for i in range(2)]; bxA = [Buf("xA%d" % i) for i in range(2)]
            junk = sbt(st, "junkT", [BS, D], BF16); ssq = sbt(st, "ssqT", [BS, 4], F32); bt = Buf("nrmtmpT")
            wq = [sbt(st, "wq%d" % i, [128, KC, 256], BF16) for i in range(2)]; bwq = [Buf("wq%d" % i) for i in range(2)]
            qTsb = sbt(st, "qTsb", [128, NHS, TW], BF16); bqsb = Buf("qTsb")
            qTds = sbt(st, "qTds", [128, NHD, TW], BF16); bqds = Buf("qTds")
            qTix = sbt(st, "qTix", [128, IH // 2, TW], BF16); bqix = Buf("qTix")
            wix = sbt(st, "wix", [BS, TB, IH], F32); bwix = Buf("wix")
            qtm = sbt(st, "qtm", [BS, 256], F32); bqtm = Buf("qtm")
            qtm2 = [sbt(st, "qtm2_%d" % i, [BS, 256], F32) for i in range(2)]; bqtm2 = [Buf("qtm2_%d" % i) for i in range(2)]
            rtmp = sbt(st, "rtmpT", [BS, 4, 16], F32)
            Mb = sbt(st, "Mb", [BS, TB, S], BF16); bMb = [Buf("Mb%d" % i) for i in range(TB)]
            SCRB = max(S * 4 + 8192, NKB * TW * 2, TB * D * 4)
            scr = sbt(st, "scr", [128, SCRB], U8)
            kvh = sbt(st, "kvh", [128, 4 * S], U8)
            kTh = kvh[:, 0:2 * S].bitcast(BF16); bkTh = Buf("kTh")
            vh = kvh[:, 2 * S:4 * S].bitcast(BF16); bvh = Buf("vh")
            scoreb = [scr[:BS, 0:S * 4].bitcast(F32), kvh[:BS, 0:S * 4].bitcast(F32)]
            bscb = [Buf("score0"), Buf("score1")]
            relb = [scr[:BS, S * 4 + 4096 * i:S * 4 + 4096 * i + 2048].bitcast(BF16) for i in range(2)]
            relb.append(sbt(st, "relb2", [BS, 1024], BF16)[:, :])
            brel = [Buf("rel%d" % i) for i in range(3)]
            biasT = [scr[:BS, S * 4 + 4096 * i + 2048:S * 4 + 4096 * (i + 1)].bitcast(F32) for i in range(2)]
            bbias = [Buf("biasT%d" % i) for i in range(2)]
            diagw = sbt(st, "diagw", [BS, IH, BS], BF16); bdiag = Buf("diagw")
            Yt = scr[:, 0:NKB * TW * 2].bitcast(BF16); bY = Buf("Yt")
            xqb = [scr[:BS, D * 4 * i:D * 4 * (i + 1)].bitcast(F32) for i in range(TB)]; bxq = [Buf("xqb%d" % i) for i in range(TB)]
            bis = [sbt(st, "bis%d" % i, [BS, 8 + NSP], F32) for i in range(2)]; bbis = [Buf("bis%d" % i) for i in range(2)]
            dtab = [sbt(st, "dtab%d" % i, [BS, NBIS + 1], F32) for i in range(2)]
            ndtab = [sbt(st, "ndtab%d" % i, [BS, NBIS + 1], F32) for i in range(2)]
            mrg = sbt(st, "mrg", [128, NM, TW], BF16); bmrg = [Buf("mrg%d" % i) for i in range(NM)]
            ytmp = sbt(st, "ytmp", [128, TW], F32); bytmp = Buf("ytmp")
            qrow_t = sbt(st, "qrow", [128, TW], F32); bqrow = Buf("qrow")
            ebuf = [sbt(st, "ebuf%d" % i, [128, TW], F32) for i in range(2)]; beb = [Buf("ebuf%d" % i) for i in range(2)]
            spb = [sbt(st, "spb%d" % i, [128, TW], BF16) for i in range(2)]; bspb = [Buf("spb%d" % i) for i in range(2)]
            Ab = [sbt(st, "Ab%d" % i, [128, TW], BF16) for i in range(2)]; bAb = [Buf("Ab%d" % i) for i in range(2)]
            wb = [sbt(st, "wb%d" % i, [128, TW], BF16) for i in range(2)]; bwb = [Buf("wb%d" % i) for i in range(2)]
            pbuf = [sbt(st, "pbuf%d" % i, [128, TW], BF16) for i in range(2)]; bpbuf = [Buf("pbuf%d" % i) for i in range(2)]
            rden = sbt(st, "rden", [128, TW], F32); brden = Buf("rden")
            g1t = [sbt(st, "g1t%d" % i, [BS, 256], F32) for i in range(2)]; bg1 = [Buf("g1t%d" % i) for i in range(2)]
            gsq = sbt(st, "gsq", [128, TW], BF16); bgsq = Buf("gsq")
            grs = sbt(st, "grs", [128, 2, TW], F32); bgrs = Buf("grs")
            h2st = sbt(st, "h2st", [128, KC, BS], BF16); bh2st = Buf("h2st")
            wqctr = [0]

            def wq_load(src_v, col0, n):
                s = wqctr[0] % 2; wqctr[0] += 1
                dma("pool", wq[s][:, :, 0:n], src_v[:, :, col0:col0 + n], (), [bwq[s]], "wq%d" % s)
                return s

            for t in range(NT):
                tc0 = t * TW
                St = cf.tile_keys(t); NKBt = St // 128; NSPt = St // 512
                for bi in range(TB):
                    blk = t * TB + bi
                    xa = blk % 2
                    dma("sp", xA[xa][:], xq[blk * BS:(blk + 1) * BS, :], (), [bxA[xa]], "xA%d" % xa)
                    norm_T(xA[xa][:], BS, bxA[xa], lambda k, bi=bi: hTt[:, k, bi * BS:(bi + 1) * BS], bhTt,
                           G1, SH1, (junk, ssq, bt), [0, 1])
                dma("sp", qrow_t[:], qrow[:, tc0:tc0 + TW], (), [bqrow], "qrow")
                chk("T1")
                for g in range(0, SBW, 256):
                    s = wq_load(winv, cf.o_qsb + g, 256)
                    for hh in range(2):
                        h = g // 128 + hh
                        pbi = 2 + h % 2
                        for k in range(KC):
                            mm(pb[pbi][:, 0:TW], wq[s][:, k, hh * 128:(hh + 1) * 128], hTt[:, k, :], k == 0, k == KC - 1,
                               [bwq[s], bhTt], [bpb[pbi]])
                        act(qTsb[:, h, :], pb[pbi][:, 0:TW], AF.Copy, [bpb[pbi]], [bqsb], scale=cf.qk_scale)
                qsteps = [("ds", g) for g in range(0, DSW, 256)] + [("ix", g) for g in range(0, IH * ID, 256)]
                pend = []
                sctr = [0]

                def q_mm(kind, s, bi):
                    blk = t * TB + bi
                    sl = sctr[0] % 2; sctr[0] += 1
                    pm = 4 if sl == 0 else 6
                    for k in range(KC):
                        mm(pb[pm][:BS, 0:256], hTt[:, k, bi * BS:(bi + 1) * BS], wq[s][:, k, :], k == 0, k == KC - 1,
                           [bwq[s], bhTt], [bpb[pm]])
                    cp("act", qtm2[sl][:, :], pb[pm][:BS, 0:256], [bpb[pm]], [bqtm2[sl]])
                    if kind == "ds":
                        for hh in range(2):
                            o_ = hh * 128
                            rope_apply(qtm2[sl][:, o_:o_ + 16], qtm2[sl][:, o_ + 16:o_ + 32], cosQ[:, blk, :], sinQ[:, blk, :],
                                       rtmp, BS, 16, [bqtm2[sl], b_rope], [bqtm2[sl]])
                    else:
                        for hh in range(256 // ID):
                            o_ = hh * ID
                            rope_apply(qtm2[sl][:, o_:o_ + 8], qtm2[sl][:, o_ + 8:o_ + 16], cosQ[:, blk, 0:16:2],
                                       sinQ[:, blk, 0:16:2], rtmp, BS, 8, [bqtm2[sl], b_rope], [bqtm2[sl]])
                    return sl

                def q_tr(kind, g, bi, sl):
                    pt = 5 if sl == 0 else 7
                    for pp in range(2):
                        tr(pb[pt][:, pp * 128:pp * 128 + BS], qtm2[sl][:, pp * 128:(pp + 1) * 128], identf_t[:BS, :BS],
                           [bqtm2[sl], bc], [bpb[pt]])
                    for pp in range(2):
                        if kind == "ds":
                            act(qTds[:, g // 128 + pp, bi * BS:(bi + 1) * BS], pb[pt][:, pp * 128:pp * 128 + BS], AF.Copy,
                                [bpb[pt]], [bqds], scale=cf.qk_scale)
                        else:
                            cp("act", qTix[:, g // 128 + pp, bi * BS:(bi + 1) * BS], pb[pt][:, pp * 128:pp * 128 + BS],
                               [bpb[pt]], [bqix])

                for (kind, g) in qsteps:
                    s = wq_load(winv, (cf.o_qds if kind == "ds" else cf.o_qix) + g, 256)
                    for bi in range(TB):
                        sl = q_mm(kind, s, bi)
                        if pend:
                            q_tr(*pend.pop())
                        pend.append((kind, g, bi, sl))
                if pend:
                    q_tr(*pend.pop())
                s = wq_load(winv, cf.o_wix, IH)
                for bi in range(TB):
                    for k in range(KC):
                        mm(pb[4][:BS, 0:IH], hTt[:, k, bi * BS:(bi + 1) * BS], wq[s][:, k, 0:IH], k == 0, k == KC - 1,
                           [bwq[s], bhTt], [bpb[4]])
                    act(wix[:, bi, :], pb[4][:BS, 0:IH], AF.Copy, [bpb[4]], [bwix], scale=cf.idx_scale)
                wo_slots = [wq_load(wov, g_, 256) for g_ in range(0, min(D, 512), 256)]

                chk("T2")
                def idx_block(bi):
                    blk = t * TB + bi
                    sb_ = bi % 2; sc = scoreb[sb_]; bs_ = bscb[sb_]; B_ = bis[sb_]; bB = bbis[sb_]
                    for h in range(IH):
                        ts("pool", diagw[:, h, :], identb[:BS, :BS], wix[:, bi, h:h + 1], None, ALU.mult, None,
                           [bc, bwix], [bdiag])
                    gctr = 0; spctr = 0
                    for sp_ in range(0, St, 1024):
                        nsp = min(1024, St - sp_); nb = nsp // 512
                        ab = 6; spctr += 1
                        def idx_mm(h, pg):
                            po = (h % 2) * 64
                            for q_ in range(nb):
                                mm(pb[pg + q_][:BS, :], qTix[po:po + 64, h // 2, bi * BS:(bi + 1) * BS],
                                   kTix[po:po + 64, sp_ + q_ * 512:sp_ + (q_ + 1) * 512], True, True,
                                   [bqix, b_kTix], [bpb[pg + q_]])

                        pgs = []
                        for h in range(IH):
                            pgs.append((gctr % 3) * 2); gctr += 1
                        idx_mm(0, pgs[0])
                        if IH > 1:
                            idx_mm(1, pgs[1])
                        for h in range(IH):
                            pg = pgs[h]
                            if h + 2 < IH:
                                idx_mm(h + 2, pgs[h + 2])
                            rs = (gctr + h) % 3
                            em.op("dve", lambda e, rs=rs, pg=pg, nb=nb, nsp=nsp: e.tensor_scalar(
                                out=relb[rs][:, 0:nsp].rearrange("p (a f) -> p a f", a=nb), in0=PS[:BS, pg:pg + nb, :],
                                scalar1=0.0, scalar2=None, op0=ALU.max),
                                [bpb[pg + q_] for q_ in range(nb)], [brel[rs]])
                            for q_ in range(nb):
                                mm(pb[ab + q_][:BS, :], diagw[:, h, :], relb[rs][:, q_ * 512:(q_ + 1) * 512], h == 0, h == IH - 1,
                                   [bdiag, brel[rs]], [bpb[ab + q_]])
                        for q_ in range(nb):
                            kt = sp_ // 512 + q_
                            em.op("dve", lambda e, kt=kt, ab=ab, q_=q_, B_=B_: e.tensor_reduce(
                                out=B_[:, 8 + kt:9 + kt], in_=pb[ab + q_][:BS, :], axis=AX.X, op=ALU.min),
                                [bpb[ab + q_]], [bB])
                            ts("dve", biasT[q_ % 2], iota_t[:BS, :], qrel_t[:, blk, kt:kt + 1], -1e30, ALU.is_gt, ALU.mult,
                               [bc], [bbias[q_ % 2]])
                            tt("dve", sc[:, kt * 512:(kt + 1) * 512], pb[ab + q_][:BS, :], biasT[q_ % 2], ALU.add,
                               [bpb[ab + q_], bbias[q_ % 2]], [bs_])
                    em.op("dve", lambda e, St=St, sc=sc, B_=B_: e.tensor_reduce(out=B_[:, 0:1], in_=sc[:, 0:St], axis=AX.X, op=ALU.max), [bs_], [bB])
                    em.op("dve", lambda e, NSPt=NSPt, B_=B_: e.tensor_reduce(out=B_[:, 1:2], in_=B_[:, 8:8 + NSPt], axis=AX.X, op=ALU.min), [bB], [bB])
                    stt(B_[:, 6:7], B_[:, 0:1], 2.0, B_[:, 1:2], ALU.add, ALU.subtract, [bB], [bB])
                    ts("dve", dtab[sb_][:], pow2_t[:BS, :], B_[:, 6:7], None, ALU.mult, None, [bB, bc], [bB])
                    ts("dve", ndtab[sb_][:], dtab[sb_][:], -1.0, None, ALU.mult, None, [bB], [bB])
                    ts("dve", B_[:, 2:3], B_[:, 1:2], -1.0, 1.0, ALU.mult, ALU.add, [bB], [bB])
                    tt("dve", B_[:, 2:3], B_[:, 2:3], dtab[sb_][:, 0:1], ALU.subtract, [bB], [bB])

                def bisect(bi):
                    sb_ = bi % 2; sc = scoreb[sb_]; bs_ = bscb[sb_]; B_ = bis[sb_]; bB = bbis[sb_]
                    for r_ in range(NBIS):
                        act(Mb[:, bi, 0:St], sc[:, 0:St], AF.Sign, [bs_, bB], [bMb[bi], bB], bias=B_[:, 2:3], accum_out=B_[:, 3:4])
                        act(B_[:, 4:5], B_[:, 3:4], AF.Sign, [bB], [bB], bias=float(St - 2 * cf.TOPK) + 0.5)
                        act(B_[:, 2:3], B_[:, 4:5], AF.Identity, [bB], [bB], scale=ndtab[sb_][:, r_ + 1:r_ + 2], bias=B_[:, 2:3])

                def finalize(bi):
                    sb_ = bi % 2; sc = scoreb[sb_]; bs_ = bscb[sb_]; B_ = bis[sb_]; bB = bbis[sb_]
                    stt(B_[:, 5:6], B_[:, 2:3], -1.0, dtab[sb_][:, NBIS:NBIS + 1], ALU.mult, ALU.subtract, [bB], [bB])
                    ts("dve", Mb[:, bi, 0:St], sc[:, 0:St], B_[:, 5:6], NEGB, ALU.is_le, ALU.mult, [bs_, bB], [bMb[bi]])

                idx_block(0)
                for bi in range(TB):
                    bisect(bi)
                    if bi + 1 < TB:
                        idx_block(bi + 1)
                    finalize(bi)

                chk("T3")
                for h in range(NHD):
                    def S_step(c):
                        pz = c % 2
                        mm(pb[pz][:, 0:TW], kTds[:, c * 128:(c + 1) * 128], qTds[:, h, :], True, False,
                           [b_kTds, bqds], [bpb[pz]])
                        for bi in range(TB):
                            mm(pb[pz][:, bi * BS:(bi + 1) * BS], Mb[:, bi, c * 128:(c + 1) * 128], identb[:BS, :BS],
                               False, bi == TB - 1, [bMb[bi], bc], [bpb[pz]])
                        act(pbuf[pz][:], pb[pz][:, 0:TW], AF.Exp, [bpb[pz]], [bpbuf[pz]])

                    def PV_step(c):
                        pz = c % 2
                        mm(pb[2][:, 0:TW], vds[:, c, :], pbuf[pz][:], c == 0, c == NKBt - 1, [b_vds, bpbuf[pz]], [bpb[2]])
                        mm(pb[3][:, 0:TW], onesb, pbuf[pz][:], c == 0, c == NKBt - 1, [bc, bpbuf[pz]], [bpb[3]])

                    S_step(0)
                    for c in range(NKBt):
                        if c + 1 < NKBt:
                            S_step(c + 1)
                        PV_step(c)
                    em.op("dve", lambda e: e.reciprocal(out=rden[:], in_=pb[3][:, 0:TW]), [bpb[3]], [brden])
                    tt("dve", mrg[:, NHS + h, :], pb[2][:, 0:TW], rden[:], ALU.mult, [bpb[2], brden], [bmrg[NHS + h]])

                chk("T4")
                em.barrier()
                for c in range(NKBt):
                    ts("dve", ytmp[:], qrow_t[:], float(-128 * c), 0.0, ALU.add, ALU.max, [bqrow], [bytmp])
                    ts("dve", Yt[:, c * TW:(c + 1) * TW], ytmp[:], kcol_t[:, 0:1], None, ALU.is_equal, None, [bytmp, bc], [bY])
                for h in range(NHS):
                    dma("sp", kTh[:, 0:St], kTsb[h, :, 0:St], (), [bkTh], "kTh")
                    dma("sp", vh[:, 0:St], vsb[h, :, 0:St], (), [bvh], "vh")
                    order = list(range(NKBt - 1, -1, -1))

                    def Z_step(i):
                        c = order[i]; pz = 4 + i % 2
                        mm(pb[pz][:, 0:TW], kTh[:, c * 128:(c + 1) * 128], qTsb[:, h, :], True, False,
                           [bkTh, bqsb], [bpb[pz]])
                        mm(pb[pz][:, 0:TW], umat, Yt[:, c * TW:(c + 1) * TW], False, True, [bc, bY], [bpb[pz]])
                        act(ebuf[i % 2][:], pb[pz][:, 0:TW], AF.Exp, [bpb[pz]], [beb[i % 2]])
                        act(spb[i % 2][:], ebuf[i % 2][:], AF.Ln, [beb[i % 2]], [bspb[i % 2]], bias=1.0)

                    def X_step(i):
                        c = order[i]; px = 6 + i % 2
                        mm(pb[px][:, 0:TW], kTh[:, c * 128:(c + 1) * 128], qTsb[:, h, :], True, False,
                           [bkTh, bqsb], [bpb[px]])
                        mm(pb[px][:, 0:TW], umat, Yt[:, c * TW:(c + 1) * TW], False, False, [bc, bY], [bpb[px]])
                        if i > 0:
                            mm(pb[px][:, 0:TW], nones, Ab[i % 2][:], False, False, [bc, bAb[i % 2]], [bpb[px]])
                        mm(pb[px][:, 0:TW], ntri, spb[i % 2][:], False, True, [bc, bspb[i % 2]], [bpb[px]])
                        if i == 0:
                            cp("pool", Ab[1][:], spb[0][:], [bspb[0]], [bAb[1]])
                        elif i + 1 < NKBt:
                            tt("pool", Ab[(i + 1) % 2][:], Ab[i % 2][:], spb[i % 2][:], ALU.add,
                               [bAb[i % 2], bspb[i % 2]], [bAb[(i + 1) % 2]])
                        act(wb[i % 2][:], pb[px][:, 0:TW], AF.Exp, [bpb[px]], [bwb[i % 2]])

                    def PVs(i):
                        c = order[i]
                        mm(pb[3][:, 0:TW], vh[:, c * 128:(c + 1) * 128], wb[i % 2][:], i == 0, i == NKBt - 1, [bvh, bwb[i % 2]], [bpb[3]])

                    Z_step(0)
                    for i in range(NKBt):
                        if i + 1 < NKBt:
                            Z_step(i + 1)
                        X_step(i)
                        if i > 0:
                            PVs(i - 1)
                    PVs(NKBt - 1)
                    cp("dve", mrg[:, h, :], pb[3][:, 0:TW], [bpb[3]], [bmrg[h]])

                chk("T5")
                for gi, (h0, nh_, g_t, W_) in enumerate(((0, NHS, sbg_t, SBW), (NHS, NHD, dsg_t, DSW))):
                    for hh in range(nh_):
                        act(gsq[:], mrg[:, h0 + hh, :], AF.Square, [bmrg[h0 + hh]], [bgsq])
                        mm(pb[0][:, 0:TW], onesb, gsq[:], hh == 0, hh == nh_ - 1, [bc, bgsq], [bpb[0]])
                    act(grs[:, 0, :], pb[0][:, 0:TW], AF.Sqrt, [bpb[0]], [bgrs], scale=1.0 / W_, bias=1e-6)
                    em.op("dve", lambda e: e.reciprocal(out=grs[:, 1, :], in_=grs[:, 0, :]), [bgrs], [bgrs])
                    for hh in range(nh_):
                        stt(mrg[:, h0 + hh, :], mrg[:, h0 + hh, :], g_t[:, hh:hh + 1], grs[:, 1, :], ALU.mult, ALU.mult,
                            [bmrg[h0 + hh], bgrs, bc], [bmrg[h0 + hh]])

                chk("T6")
                em.barrier()
                for bi in range(TB):
                    blk = t * TB + bi
                    dma("sp", xqb[bi], xq[blk * BS:(blk + 1) * BS, :], (), [bxq[bi]], "xqb%d" % bi)
                for gi, g in enumerate(range(0, D, 256)):
                    s = wo_slots[gi]
                    gs = gi % 2
                    dma("sp", g1t[gs][:], modrow[0:1, 2 * D + g:2 * D + g + 256].partition_broadcast(BS), (), [bg1[gs]], "g1t%d" % gs)
                    for bi in range(TB):
                        pbi = bi % 2
                        for k in range(NM):
                            mm(pb[pbi][:BS, 0:256], mrg[:, k, bi * BS:(bi + 1) * BS], wq[s][:, k, :], k == 0, k == NM - 1,
                               bmrg + [bwq[s]], [bpb[pbi]])
                        tt("dve", qtm[:, :], pb[pbi][:BS, 0:256], g1t[gs][:], ALU.mult, [bpb[pbi], bg1[gs]], [bqtm])
                        tt("dve", xqb[bi][:, g:g + 256], xqb[bi][:, g:g + 256], qtm[:, :], ALU.add, [bqtm, bxq[bi]], [bxq[bi]])
                    if g + 512 < D:
                        wo_slots.append(wq_load(wov, g + 512, 256))
                for bi in range(TB):
                    blk = t * TB + bi
                    dma("pool", xmid[blk * BS:(blk + 1) * BS, :], xqb[bi], [bxq[bi]], (), "xmidw%d" % bi)
                    norm_T(xqb[bi], BS, bxq[bi], lambda k: h2st[:, k, :], bh2st, G2, SH2, (junk, ssq, bt), [2, 3],
                           xs_out=xA[blk % 2][:], bxs=bxA[blk % 2])
                    dma("act", h2v[:, :, blk * BS:(blk + 1) * BS], h2st[:], [bh2st], (), "h2w")
                em.barrier()
        em.barrier()

        chk("T")
        wuv = w_up.rearrange("(k p) n -> p k n", p=128)
        wdv = w_down.rearrange("(k p) n -> p k n", p=128)
        with contextlib.ExitStack() as st:
            mT = sbt(st, "mT", [128, FC, OWN], BF16); bmT = Buf("mT")
            h2T = sbt(st, "h2T", [128, KC, TQ], BF16); b_h2T = Buf("h2T")
            dma("sp", h2T[:], h2Ts.rearrange("p (k t) -> p k t", k=KC), (), [b_h2T], "h2l")
            with contextlib.ExitStack() as st2:
                wu = [sbt(st2, "wu%d" % i, [128, KC, 256], BF16) for i in range(3)]; bwu = [Buf("wu%d" % i) for i in range(3)]
                usb = [sbt(st2, "usb%d" % i, [128, TQ], F32) for i in range(2)]; busb = [Buf("usb%d" % i) for i in range(2)]
                ucg = sbt(st2, "ucg", [128, OWN], F32); bucg = Buf("ucg")
                ucv = sbt(st2, "ucv", [128, OWN], F32); bucv = Buf("ucv")
                for f in range(FC):
                    s = f % 3
                    dma("pool", wu[s][:, :, 0:128], wuv[:, :, f * 128:(f + 1) * 128], (), [bwu[s]], "wu%d" % s)
                    dma("pool", wu[s][:, :, 128:256], wuv[:, :, DFF + f * 128:DFF + (f + 1) * 128], (), [bwu[s]], "wu%d" % s)
                    for half in range(2):
                        ci = f + half * FC
                        ub = usb[half]; bub = busb[half]
                        for t in range(NT):
                            pbi = (2 * t + half) % 4
                            for k in range(KC):
                                mm(pb[pbi][:, 0:TW], wu[s][:, k, half * 128:(half + 1) * 128], h2T[:, k, t * TW:(t + 1) * TW],
                                   k == 0, k == KC - 1, [bwu[s], b_h2T], [bpb[pbi]])
                            cp("act", ub[:, t * TW:(t + 1) * TW], pb[pbi][:, 0:TW], [bpb[pbi]], [bub])
                        ts("dve", ub[:, 0:2], ub[:, 0:2], flag_t[:, 0:1], None, ALU.mult, None, [bub, bc], [bub])
                        uc = ucg if half == 0 else ucv
                        buc = bucg if half == 0 else bucv
                        ubv = ub[:].rearrange("p (s w) -> p s w", s=NT)
                        uc3 = uc[:].rearrange("p (s w) -> p s w", s=NT)
                        act(uc3, ubv[:, :, 2:TW], AF.Identity, [bub, bc], [buc], scale=cvw_t[:, 2, ci:ci + 1], bias=cvb_t[:, ci:ci + 1])
                        stt(uc3, ubv[:, :, 1:TW - 1], cvw_t[:, 1, ci:ci + 1], uc3, ALU.mult, ALU.add, [bub, bc, buc], [buc])
                        stt(uc3, ubv[:, :, 0:TW - 2], cvw_t[:, 0, ci:ci + 1], uc3, ALU.mult, ALU.add, [bub, bc, buc], [buc])
                    act(ucg[:], ucg[:], AF.Silu, [bucg], [bucg])
                    tt("dve", mT[:, f, :], ucg[:], ucv[:], ALU.mult, [bucg, bucv], [bmT])
            em.barrier()
            with contextlib.ExitStack() as st2:
                wd = [sbt(st2, "wd%d" % i, [128, FC, 256], BF16) for i in range(2)]; bwd = [Buf("wd%d" % i) for i in range(2)]
                yst = [sbt(st2, "yst%d" % i, [128, 256], F32) for i in range(2)]; byst = [Buf("yst%d" % i) for i in range(2)]
                yc = 0
                for gi, g in enumerate(range(0, D, 256)):
                    s = gi % 2
                    for f0 in range(0, FC, 16):
                        f1 = min(FC, f0 + 16)
                        dma("pool", wd[s][:, f0:f1, :], wdv[:, f0:f1, g:g + 256], (), [bwd[s]], "wd%d" % s)
                    for tb in range(NOB):
                        pbi = tb % 2
                        for f in range(FC):
                            mm(pb[pbi][:, 0:256], mT[:, f, tb * 128:(tb + 1) * 128], wd[s][:, f, :], f == 0, f == FC - 1,
                               [bmT, bwd[s]], [bpb[pbi]])
                        ys = yc % 2; yc += 1
                        cp("act", yst[ys][:], pb[pbi][:, 0:256], [bpb[pbi]], [byst[ys]])
                        dma("act", yscr[tb * 128:(tb + 1) * 128, g:g + 256], yst[ys][:], [byst[ys]], (), "yscrw%d" % ys)
        em.barrier()
        with contextlib.ExitStack() as st:
            g2bc = sbt(st, "g2bc", [128, D], F32); fngt = sbt(st, "fngt", [128, D], F32); bgg = Buf("g2fng")
            xm = [sbt(st, "xm%d" % i, [128, D], F32) for i in range(2)]; bxm = [Buf("xm%d" % i) for i in range(2)]
            yy = [sbt(st, "yy%d" % i, [128, D], F32) for i in range(2)]; byy = [Buf("yy%d" % i) for i in range(2)]
            junk = sbt(st, "junkF", [128, D], BF16); ssq = sbt(st, "ssqF", [128, 4], F32); bt = Buf("nrmF")
            dma("sp", g2bc[:], modrow[0:1, 5 * D:6 * D].partition_broadcast(128), (), [bgg], "g2bc")
            dma("sp", fngt[:], fng[:, :], (), [bgg], "g2bc")
            for tb in range(NOB):
                s = tb % 2
                r0_ = (tb // cf.BPS) * TW + 2 + (tb % cf.BPS) * 128
                dma("sp", xm[s][:], xmid[r0_:r0_ + 128, :], (), [bxm[s]], "xm%d" % s)
                dma("sp", yy[s][:], yscr[tb * 128:(tb + 1) * 128, :], (), [byy[s]], "yy%d" % s)
                tt("dve", yy[s][:], yy[s][:], g2bc[:], ALU.mult, [byy[s], bgg], [byy[s]])
                tt("dve", xm[s][:], xm[s][:], yy[s][:], ALU.add, [byy[s], bxm[s]], [bxm[s]])
                act(junk[:], xm[s][:], AF.Square, [bxm[s]], [bt], accum_out=ssq[:, 0:1])
                act(ssq[:, 1:2], ssq[:, 0:1], AF.Sqrt, [bt], [bt], scale=1.0 / D, bias=1e-6)
                em.op("dve", lambda e: e.reciprocal(out=ssq[:, 2:3], in_=ssq[:, 1:2]), [bt], [bt])
                stt(yy[s][:], xm[s][:], ssq[:, 2:3], fngt[:], ALU.mult, ALU.mult, [bxm[s], bt, bgg], [byy[s]])
                dma("pool", out[tb * 128:(tb + 1) * 128, :], yy[s][:], [byy[s]], (), "outw%d" % s)
        em.emit(final_waits=["outw0", "outw1"] if NOB > 1 else ["outw0"])
    return nc


def host_inputs(cf, inp, core):
    D, S, KC, NKB, TQ, BS, NBLK, NSP, FC = cf.D, cf.S, cf.KC, cf.NKB, cf.TQ, cf.BS, cf.NBLK, cf.NSP, cf.FC
    b = core // cf.CPB; j = core % cf.CPB
    f32 = np.float32
    x = np.asarray(inp["x"], f32); pos = np.asarray(inp["positions"], np.int32)
    tok = np.concatenate([np.arange(cf.seg_tokens(j, t) - 2, cf.seg_tokens(j, t) + cf.SEG) for t in range(cf.NT)])
    assert tok.shape[0] == TQ
    tokc = np.maximum(tok, 0)
    m = {}
    m["xkv"] = np.ascontiguousarray(x[b])
    m["xq"] = np.ascontiguousarray(x[b][tokc])
    m["posk"] = np.ascontiguousarray(pos[b].reshape(NKB, 128).T)
    m["posq"] = np.ascontiguousarray(pos[b][tokc].reshape(NBLK, BS).T)
    qc = tokc.astype(f32).reshape(NBLK, BS).T
    m["qcol"] = np.ascontiguousarray(qc)
    qr = qc[:, :, None] - (512.0 * np.arange(NSP, dtype=f32))[None, None, :]
    m["qrel"] = np.ascontiguousarray(qr.reshape(BS, NBLK * NSP).astype(f32))
    m["qrow"] = np.ascontiguousarray(np.broadcast_to(tokc.astype(f32)[None, :], (128, TQ)))
    pk = lambda v: np.ascontiguousarray(np.asarray(v, f32).reshape(-1, 128).T)
    m["cb"] = pk(np.asarray(inp["c"], f32)[b])
    m["w_ada"] = np.ascontiguousarray(np.asarray(inp["w_ada"], f32)[0])
    m["b_ada"] = np.ascontiguousarray(np.asarray(inp["b_ada"], f32)[0][None, :])
    m["w_in"] = np.ascontiguousarray(np.asarray(inp["w_in"], f32)[0])
    m["w_out"] = np.ascontiguousarray(np.asarray(inp["w_out"], f32)[0])
    m["w_up"] = np.ascontiguousarray(np.asarray(inp["w_up"], f32)[0])
    m["w_down"] = np.ascontiguousarray(np.asarray(inp["w_down"], f32)[0])
    m["n1g"] = pk(np.asarray(inp["norm1_g"])[0]); m["n2g"] = pk(np.asarray(inp["norm2_g"])[0])
    m["sbg"] = pk(np.asarray(inp["sb_norm_g"])[0]); m["dsg"] = pk(np.asarray(inp["dsa_norm_g"])[0])
    m["fng"] = np.ascontiguousarray(np.broadcast_to(np.asarray(inp["final_norm_g"], f32)[None, :], (128, D)))
    cw = np.asarray(inp["conv_w"], f32)[0]
    m["convw"] = np.ascontiguousarray(np.stack([pk(cw[i]) for i in range(3)], axis=1).reshape(128, 3 * 2 * FC))
    m["convb"] = pk(np.asarray(inp["conv_b"], f32)[0])
    m["identf"] = np.eye(128, dtype=f32)
    jj = np.arange(128)[:, None]; ss = np.arange(128)[None, :]
    cm = np.stack([np.eye(128, dtype=f32), -(jj >= ss).astype(f32), -np.ones((128, 128), f32),
                   NEGB * (ss >= jj).astype(f32), np.ones((128, 128), f32)], axis=1)
    m["cmat"] = np.ascontiguousarray(cm.reshape(128, 5 * 128))
    m["iota"] = np.ascontiguousarray(np.broadcast_to(np.arange(512, dtype=f32)[None, :], (128, 512)))
    m["kcolc"] = np.arange(128, dtype=f32)[:, None].copy()
    m["pow2"] = np.ascontiguousarray(np.broadcast_to((0.5 ** np.arange(1, cf.NBIS + 2)).astype(f32)[None, :], (128, cf.NBIS + 1)))
    ivf = (np.float32(500000.0) ** (-(np.arange(16, dtype=f32) / np.float32(16)))).astype(f32)
    m["invf"] = np.ascontiguousarray(np.broadcast_to(ivf[None, :], (128, 16)))
    m["flag"] = np.full((128, 1), 0.0 if j == 0 else 1.0, f32)
    return m


_CACHE = {}


def kernel(x, c, positions, w_ada, b_ada, norm1_g, w_in, sb_norm_g, dsa_norm_g, w_out, norm2_g,
           w_up, conv_w, conv_b, w_down, final_norm_g):
    inp = dict(x=x, c=c, positions=positions, w_ada=w_ada, b_ada=b_ada, norm1_g=norm1_g, w_in=w_in,
               sb_norm_g=sb_norm_g, dsa_norm_g=dsa_norm_g, w_out=w_out, norm2_g=norm2_g, w_up=w_up,
               conv_w=conv_w, conv_b=conv_b, w_down=w_down, final_norm_g=final_norm_g)
    cf = Cfg()
    nc = build_program(cf)
    in_maps = [host_inputs(cf, inp, core) for core in range(8)]
    res = run_bass_kernel_spmd(nc, in_maps, core_ids=list(range(8)))
    outp = np.zeros((2, cf.S, cf.D), np.float32)
    for core in range(8):
        b = core // cf.CPB; j = core % cf.CPB
        o_ = np.asarray(res.results[core]["out"], np.float32)
        for t in range(cf.NT):
            g0 = cf.seg_tokens(j, t)
            outp[b, g0:g0 + cf.SEG, :] = o_[t * cf.SEG:(t + 1) * cf.SEG]
    return outp
```

```python
import contextlib
import numpy as np
import concourse.bass as bass
import concourse.mybir as mybir
from concourse.bass_utils import run_bass_kernel_spmd

F32 = mybir.dt.float32
BF16 = mybir.dt.bfloat16
I32 = mybir.dt.int32
AF = mybir.ActivationFunctionType
ALU = mybir.AluOpType
AX = mybir.AxisListType

ENGS = ("pe", "act", "dve", "pool", "sp")


class Buf:
    __slots__ = ("name", "lw", "rs")

    def __init__(self, name):
        self.name = name
        self.lw = None
        self.rs = []


class Op:
    __slots__ = ("eng", "idx", "fn", "deps", "dma", "stream", "gen", "flag", "incidx")

    def __init__(self, eng, idx, fn, dma, stream):
        self.eng = eng; self.idx = idx; self.fn = fn; self.deps = []
        self.dma = dma; self.stream = stream; self.gen = 0
        self.flag = False; self.incidx = 0


class Em:
    def __init__(self, nc):
        self.nc = nc
        self.ops = {e: [] for e in ENGS}
        self.streams = {}
        self.last_dma = {}
        self.pending = {e: None for e in ENGS}
        self.pool_dmas = []

    def barrier(self):
        lasts = [self.ops[e][-1] for e in ENGS if self.ops[e]]
        lasts += list(self.last_dma.values())
        for e in ENGS:
            self.pending[e] = lasts

    def op(self, eng, fn, reads=(), writes=(), dma=False, stream=None):
        o = Op(eng, len(self.ops[eng]), fn, dma, stream)
        deps = []
        if self.pending[eng] is not None:
            deps.extend(self.pending[eng])
            self.pending[eng] = None
        if dma and eng == "pool":
            if len(self.pool_dmas) >= 4:
                deps.append(self.pool_dmas[-4])
            self.pool_dmas.append(o)
        for b in reads:
            if b.lw is not None:
                deps.append(b.lw)
        for b in writes:
            if b.lw is not None:
                deps.append(b.lw)
            deps.extend(b.rs)
        for b in reads:
            b.rs.append(o)
        for b in writes:
            b.lw = o
            b.rs = []
        if dma:
            g = self.streams.get(stream, 0) + 1
            self.streams[stream] = g
            o.gen = g
            self.last_dma[stream] = o
        best = {}
        for d in deps:
            if d is o:
                continue
            if d.dma:
                key = ("dma", d.stream)
                if key not in best or best[key].gen < d.gen:
                    best[key] = d
            else:
                if d.eng == eng and eng == "pe":
                    continue
                key = ("eng", d.eng)
                if key not in best or best[key].idx < d.idx:
                    best[key] = d
        o.deps = list(best.values())
        self.ops[eng].append(o)
        return o

    def emit(self, final_waits=()):
        nc = self.nc
        for e in ENGS:
            seen = {}
            for o in self.ops[e]:
                nd = []
                for d in o.deps:
                    key = ("dma", d.stream) if d.dma else ("eng", d.eng)
                    val = d.gen if d.dma else d.idx
                    if seen.get(key, -1) >= val:
                        continue
                    seen[key] = val
                    nd.append(d)
                    if not d.dma:
                        d.flag = True
                o.deps = nd
        for e in ENGS:
            c = 0
            for o in self.ops[e]:
                if o.flag and not o.dma:
                    c += 1
                    o.incidx = c
        with contextlib.ExitStack() as st:
            esem = {e: st.enter_context(nc.semaphore("s_" + e)) for e in ENGS}
            dsem = {s: st.enter_context(nc.semaphore("d_%d" % i)) for i, s in enumerate(self.streams)}
            block = st.enter_context(nc.Block())

            def run(e, eng):
                for o in self.ops[e]:
                    for d in o.deps:
                        if d.dma:
                            eng.wait_ge(dsem[d.stream], 16 * d.gen)
                        else:
                            eng.wait_ge(esem[d.eng], d.incidx)
                    ins = o.fn(eng)
                    if o.dma:
                        ins.then_inc(dsem[o.stream], 16)
                    elif o.flag:
                        ins.then_inc(esem[e], 1)
                if e == "sp":
                    for s in final_waits:
                        eng.wait_ge(dsem[s], 16 * self.streams[s])

            @block.tensor
            def _(eng):
                run("pe", eng)

            @block.scalar
            def _(eng):
                run("act", eng)

            @block.vector
            def _(eng):
                run("dve", eng)

            @block.gpsimd
            def _(eng):
                run("pool", eng)

            @block.sync
            def _(eng):
                run("sp", eng)


class Cfg:
    def __init__(self, D=2048, S=4096, DFF=5504, BS=86, TB=3, NSEG=4, NBIS=24, IH=16, ID=64,
                 TOPK_MAX=256, CPB=4):
        self.D, self.S, self.DFF = D, S, DFF
        self.HD = 128
        nh = D // 128
        self.NHS = nh // 2
        self.NHD = nh - self.NHS
        self.SBW = self.NHS * 128
        self.DSW = self.NHD * 128
        self.IH, self.ID = IH, ID
        self.TOPK = min(TOPK_MAX, S // 4)
        self.CPB = CPB
        self.OWN = S // CPB
        self.NSEG = NSEG
        self.SEG = self.OWN // NSEG
        self.TW = self.SEG + 2
        self.BS, self.TB = BS, TB
        assert BS * TB == self.TW and BS <= 128 and self.TW <= 512
        self.NT = NSEG
        self.NBLK = NSEG * TB
        self.TQ = NSEG * self.TW
        self.KC = D // 128
        self.NKB = S // 128
        self.FC = DFF // 128
        assert DFF % 128 == 0 and S % 512 == 0 and self.SEG % 128 == 0
        self.NOB = self.OWN // 128
        self.BPS = self.SEG // 128
        self.NBIS = NBIS
        o = 0
        self.o_qsb = o; o += self.SBW
        self.o_ksb = o; o += self.SBW
        self.o_vsb = o; o += self.SBW
        self.o_qds = o; o += self.DSW
        self.o_kds = o; o += 128
        self.o_vds = o; o += 128
        self.o_qix = o; o += IH * ID
        self.o_kix = o; o += ID
        self.o_wix = o; o += IH
        self.INC = o
        self.NSP = S // 512
        self.idx_scale = (IH ** -0.5) * (ID ** -0.5)
        self.qk_scale = 128 ** -0.5
        self.stop = None

    def tile_keys(self, t):
        k = (self.CPB * t + self.CPB) * self.SEG
        assert k % 512 == 0 and k <= self.S
        return k

    def seg_tokens(self, j, t):
        gt = self.CPB * t + j
        return gt * self.SEG


class _Stop(Exception):
    pass


TWO_PI = 6.283185307179586
PI = 3.141592653589793
NEGB = -30000.0


def build_program(cf):
    hold = {}
    try:
        return _build(cf, hold)
    except _Stop:
        hold["em"].emit(final_waits=[])
        return hold["nc"]


def _build(cf, hold):
    nc = bass.Bass("TRN2", target_bir_lowering=False)
    em = Em(nc)
    hold["nc"] = nc; hold["em"] = em
    D, S, KC, NKB, TQ, BS, NBLK, TB, NT, TW = cf.D, cf.S, cf.KC, cf.NKB, cf.TQ, cf.BS, cf.NBLK, cf.TB, cf.NT, cf.TW
    NHS, NHD, SBW, DSW, IH, ID, FC, DFF = cf.NHS, cf.NHD, cf.SBW, cf.DSW, cf.IH, cf.ID, cf.FC, cf.DFF
    NSP, NBIS, OWN, NOB = cf.NSP, cf.NBIS, cf.OWN, cf.NOB
    NM = NHS + NHD

    def din(name, shape, dt=F32):
        return nc.dram_tensor(name, list(shape), dt, kind="ExternalInput").ap()

    def dscr(name, shape, dt):
        return nc.dram_tensor(name, list(shape), dt, kind="Internal").ap()

    xkv = din("xkv", [S, D]); xq = din("xq", [TQ, D])
    posk = din("posk", [128, NKB], I32); posq = din("posq", [BS, NBLK], I32)
    qcol = din("qcol", [BS, NBLK]); qrel = din("qrel", [BS, NBLK * NSP]); qrow = din("qrow", [128, TQ])
    cb = din("cb", [128, KC])
    w_ada = din("w_ada", [D, 6 * D]); b_ada = din("b_ada", [1, 6 * D])
    w_in = din("w_in", [D, cf.INC]); w_out = din("w_out", [D, D])
    w_up = din("w_up", [D, 2 * DFF]); w_down = din("w_down", [DFF, D])
    n1g = din("n1g", [128, KC]); n2g = din("n2g", [128, KC])
    sbg = din("sbg", [128, NHS]); dsg = din("dsg", [128, NHD]); fng = din("fng", [128, D])
    convw = din("convw", [128, 3 * 2 * FC]); convb = din("convb", [128, 2 * FC])
    identf = din("identf", [128, 128]); cmat = din("cmat", [128, 5 * 128])
    iota = din("iota", [128, 512]); kcolc = din("kcolc", [128, 1]); pow2 = din("pow2", [128, NBIS + 1])
    invf = din("invf", [128, 16]); flag = din("flag", [128, 1])
    out = nc.dram_tensor("out", [OWN, D], F32, kind="ExternalOutput").ap()
    modrow = dscr("modrow", [1, 6 * D], F32)
    kTsb = dscr("kTsb", [NHS, 128, S], BF16)
    vsb = dscr("vsb", [NHS, 128, NKB * 128], BF16)
    xmid = dscr("xmid", [TQ, D], F32)
    yscr = dscr("yscr", [OWN, D], F32)
    h2Ts = dscr("h2Ts", [128, KC * TQ], BF16)

    def mm(o, lhsT, rhs, start, stop, r, w):
        em.op("pe", lambda e: e.matmul(o, lhsT=lhsT, rhs=rhs, start=start, stop=stop), r, w)

    def tr(o, in_, ident, r, w):
        em.op("pe", lambda e: e.transpose(o, in_, ident), r, w)

    def act(o, in_, func, r, w, **kw):
        em.op("act", lambda e: e.activation(out=o, in_=in_, func=func, **kw), r, w)

    def ts(eng, o, in0, s1, s2, op0, op1, r, w, accum_out=None):
        if op1 is None:
            em.op(eng, lambda e: e.tensor_scalar(out=o, in0=in0, scalar1=s1, scalar2=None, op0=op0), r, w)
        elif accum_out is None:
            em.op(eng, lambda e: e.tensor_scalar(out=o, in0=in0, scalar1=s1, scalar2=s2, op0=op0, op1=op1), r, w)
        else:
            em.op(eng, lambda e: e.tensor_scalar(out=o, in0=in0, scalar1=s1, scalar2=s2, op0=op0, op1=op1,
                                                 accum_out=accum_out), r, w)

    def tt(eng, o, in0, in1, op, r, w):
        em.op(eng, lambda e: e.tensor_tensor(out=o, in0=in0, in1=in1, op=op), r, w)

    def stt(o, in0, sc, in1, op0, op1, r, w):
        em.op("dve", lambda e: e.scalar_tensor_tensor(out=o, in0=in0, scalar=sc, in1=in1, op0=op0, op1=op1), r, w)

    def cp(eng, o, in_, r, w):
        if eng == "act":
            em.op("act", lambda e: e.copy(out=o, in_=in_), r, w)
        else:
            em.op(eng, lambda e: e.tensor_copy(out=o, in_=in_), r, w)

    def memset(eng, o, val, w):
        em.op(eng, lambda e: e.memset(o, val), (), w)

    def dma(q, o, in_, r, w, stream, slow=False):
        if slow:
            em.op(q, lambda e: e.dma_start(out=o, in_=in_, allow_slow_non_contiguous=True), r, w, dma=True, stream=stream)
        else:
            em.op(q, lambda e: e.dma_start(out=o, in_=in_), r, w, dma=True, stream=stream)

    def chk(tag):
        if cf.stop == tag:
            raise _Stop()

    with contextlib.ExitStack() as gst:
        def sbt(st, name, shape, dt):
            return st.enter_context(nc.sbuf_tensor("s_" + name, list(shape), dt))

        PS = gst.enter_context(nc.psum_tensor("PS", [128, 8, 512], F32))
        pb = [PS[:, i, :] for i in range(8)]
        bpb = [Buf("pb%d" % i) for i in range(8)]

        identf_t = sbt(gst, "identf", [128, 128], F32)
        cm_t = sbt(gst, "cmat", [128, 5, 128], BF16)
        iota_t = sbt(gst, "iota", [128, 512], F32)
        kcol_t = sbt(gst, "kcolc", [128, 1], F32)
        pow2_t = sbt(gst, "pow2", [128, NBIS + 1], F32)
        flag_t = sbt(gst, "flag", [128, 1], F32)
        vec_t = sbt(gst, "vecs", [128, 8, KC], F32)
        sbg_t = sbt(gst, "sbg", [128, NHS], F32)
        dsg_t = sbt(gst, "dsg", [128, NHD], F32)
        cvw_t = sbt(gst, "cvw", [128, 3, 2 * FC], F32)
        cvb_t = sbt(gst, "cvb", [128, 2 * FC], F32)
        qcol_t = sbt(gst, "qcol", [BS, NBLK], F32)
        qrel_t = sbt(gst, "qrel", [BS, NBLK, NSP], F32)
        cosK = sbt(gst, "cosK", [128, NKB, 16], F32); sinK = sbt(gst, "sinK", [128, NKB, 16], F32)
        cosQ = sbt(gst, "cosQ", [BS, NBLK, 16], F32); sinQ = sbt(gst, "sinQ", [BS, NBLK, 16], F32)
        kTds = sbt(gst, "kTds", [128, S], BF16)
        vds = sbt(gst, "vds", [128, NKB, 128], BF16)
        kTix = sbt(gst, "kTix", [128, S], BF16)
        bc = Buf("consts")
        b_vec = Buf("vecs"); b_rope = Buf("rope")
        b_kTds = Buf("kTds"); b_vds = Buf("vds"); b_kTix = Buf("kTix")

        for (t_, src, nm) in ((identf_t[:], identf[:, :], "c0"), (iota_t[:], iota[:, :], "c1"),
                              (kcol_t[:], kcolc[:, :], "c2"), (pow2_t[:], pow2[:, :], "c3"),
                              (flag_t[:], flag[:, :], "c4"), (sbg_t[:], sbg[:, :], "c5"), (dsg_t[:], dsg[:, :], "c6"),
                              (cvw_t[:], convw.rearrange("p (a f) -> p a f", a=3), "c7"), (cvb_t[:], convb[:, :], "c8"),
                              (qcol_t[:], qcol[:, :], "c9"),
                              (qrel_t[:], qrel.rearrange("p (a f) -> p a f", a=NBLK), "c10"),
                              (vec_t[:, 4, :], n1g[:, :], "c11"), (vec_t[:, 5, :], n2g[:, :], "c12")):
            dma("sp", t_, src, (), [bc], nm)
        dma("pool", cm_t[:], cmat.rearrange("p (a f) -> p a f", a=5), (), [bc], "c13")
        identb = cm_t[:, 0, :]; ntri = cm_t[:, 1, :]; nones = cm_t[:, 2, :]; umat = cm_t[:, 3, :]; onesb = cm_t[:, 4, :]

        def rope_tables(st, pos_ap, P, n, cos_t, sin_t, tag):
            pi_ = sbt(st, "pi" + tag, [P, n], I32)
            pf = sbt(st, "pf" + tag, [P, n], F32)
            ang = sbt(st, "ang" + tag, [P, n, 16], F32)
            ki = sbt(st, "ki" + tag, [P, n, 16], I32)
            kf = sbt(st, "kf" + tag, [P, n, 16], F32)
            tm = sbt(st, "tm" + tag, [P, n, 16], F32)
            ivf = sbt(st, "ivf" + tag, [P, 16], F32)
            b = Buf("ropetmp" + tag)
            dma("sp", pi_[:], pos_ap, (), [b], "rp" + tag)
            dma("sp", ivf[:], invf[0:P, :], (), [b], "rp" + tag)
            cp("dve", pf[:], pi_[:], [b], [b])
            for c in range(n):
                ts("dve", ang[:, c, :], ivf[:], pf[:, c:c + 1], None, ALU.mult, None, [b], [b])

            def reduce_sin(dst, shift):
                a2 = ang[:]
                if shift != 0.0:
                    ts("dve", tm[:], ang[:], shift, None, ALU.add, None, [b], [b])
                    a2 = tm[:]
                ts("dve", kf[:], a2, 1.0 / TWO_PI, None, ALU.mult, None, [b], [b])
                cp("dve", ki[:], kf[:], [b], [b])
                cp("dve", kf[:], ki[:], [b], [b])
                stt(tm[:], kf[:], -TWO_PI, a2, ALU.mult, ALU.add, [b], [b])
                ts("dve", kf[:], tm[:], PI, -TWO_PI, ALU.is_gt, ALU.mult, [b], [b])
                tt("dve", tm[:], tm[:], kf[:], ALU.add, [b], [b])
                ts("dve", kf[:], tm[:], -PI, TWO_PI, ALU.is_lt, ALU.mult, [b], [b])
                tt("dve", tm[:], tm[:], kf[:], ALU.add, [b], [b])
                act(dst, tm[:], AF.Sin, [b], [b_rope])

            reduce_sin(sin_t[:], 0.0)
            reduce_sin(cos_t[:], PI / 2)

        with contextlib.ExitStack() as st:
            rope_tables(st, posk[:, :], 128, NKB, cosK, sinK, "k")
            rope_tables(st, posq[:, :], BS, NBLK, cosQ, sinQ, "q")
        em.barrier()

        rope_bufs = {}

        def rope_apply(x1, x2, cos_ap, sin_ap, tmp, P, h, r, w):
            bt_ = rope_bufs.setdefault(id(tmp), Buf("ropetmp"))
            r = list(r) + [bt_]; w = list(w) + [bt_]
            tt("dve", tmp[:P, 0, :h], x1, cos_ap, ALU.mult, r, w)
            tt("dve", tmp[:P, 1, :h], x2, sin_ap, ALU.mult, r, w)
            tt("dve", tmp[:P, 2, :h], x2, cos_ap, ALU.mult, r, w)
            tt("dve", tmp[:P, 3, :h], x1, sin_ap, ALU.mult, r, w)
            tt("dve", x1, tmp[:P, 0, :h], tmp[:P, 1, :h], ALU.subtract, r, w)
            tt("dve", x2, tmp[:P, 2, :h], tmp[:P, 3, :h], ALU.add, r, w)

        nrm_ctr = [0]

        def norm_T(xt_ap, P, bx, dst_fn, bdst, G_ap, sh_ap, tmps, pbank, xs_out=None, bxs=None):
            junk, ssq, bt = tmps
            act(junk[:P, :], xt_ap, AF.Square, [bx], [bt], accum_out=ssq[:P, 0:1])
            act(ssq[:P, 1:2], ssq[:P, 0:1], AF.Sqrt, [bt], [bt], scale=1.0 / D, bias=1e-6)
            em.op("dve", lambda e: e.reciprocal(out=ssq[:P, 2:3], in_=ssq[:P, 1:2]), [bt], [bt])
            if xs_out is None:
                ts("dve", xt_ap, xt_ap, ssq[:P, 2:3], None, ALU.mult, None, [bx, bt], [bx])
            else:
                ts("dve", xs_out, xt_ap, ssq[:P, 2:3], None, ALU.mult, None, [bx, bt], [bxs])
                xt_ap = xs_out; bx = bxs
            for k0 in range(0, KC, 4):
                nj = min(4, KC - k0)
                pbi = pbank[nrm_ctr[0] % len(pbank)]
                nrm_ctr[0] += 1
                for j in range(nj):
                    tr(pb[pbi][:, j * 128:j * 128 + P], xt_ap[:, (k0 + j) * 128:(k0 + j + 1) * 128],
                       identf_t[:P, :P], [bx, bc], [bpb[pbi]])
                for j in range(nj):
                    k = k0 + j
                    if j % 2 == 0:
                        act(dst_fn(k), pb[pbi][:, j * 128:j * 128 + P], AF.Identity, [bpb[pbi], b_vec], [bdst],
                            scale=G_ap[:, k:k + 1], bias=sh_ap[:, k:k + 1])
                    else:
                        ts("dve", dst_fn(k), pb[pbi][:, j * 128:j * 128 + P], G_ap[:, k:k + 1], sh_ap[:, k:k + 1],
                           ALU.mult, ALU.add, [bpb[pbi], b_vec], [bdst])

        winv = w_in.rearrange("(k p) n -> p k n", p=128)
        G1 = vec_t[:, 0, :]; SH1 = vec_t[:, 1, :]; G2 = vec_t[:, 2, :]; SH2 = vec_t[:, 3, :]
        with contextlib.ExitStack() as st:
            MG = 256
            NG = (6 * D) // MG
            NG1 = (2 * D) // MG
            wg = [sbt(st, "wg%d" % i, [128, KC, MG], BF16) for i in range(2)]
            bwg = [Buf("wg%d" % i) for i in range(2)]
            cb_t = sbt(st, "cb", [128, KC], F32); cs_t = sbt(st, "cs", [128, KC], BF16)
            brow = [sbt(st, "brow%d" % i, [1, MG], F32) for i in range(2)]
            mrow = [sbt(st, "mrow%d" % i, [1, MG], F32) for i in range(2)]
            bbr = [Buf("brow%d" % i) for i in range(2)]; bmr = [Buf("mrow%d" % i) for i in range(2)]
            bcs = Buf("cs"); bmodg = [Buf("modrow%d" % i) for i in range(NG)]
            mrows = sbt(st, "mrows", [KC, 4, 128], F32); bmrows = Buf("mrows")
            dma("sp", cb_t[:], cb[:, :], (), [bcs], "cb")
            act(cs_t[:], cb_t[:], AF.Silu, [bcs], [bcs])
            wav = w_ada.rearrange("(k p) n -> p k n", p=128)

            def mod_load(g):
                s2 = g % 2
                dma("pool", wg[s2][:], wav[:, :, g * MG:(g + 1) * MG], (), [bwg[s2]], "wg%d" % s2)
                dma("pool", brow[s2][:], b_ada[0:1, g * MG:(g + 1) * MG], (), [bbr[s2]], "brow%d" % s2)

            def mod_compute(g, pbi):
                s2 = g % 2
                for k in range(KC):
                    mm(pb[pbi][0:1, 0:MG], cs_t[:, k:k + 1], wg[s2][:, k, :], k == 0, k == KC - 1,
                       [bcs, bwg[s2]], [bpb[pbi]])
                tt("dve", mrow[s2][:], pb[pbi][0:1, 0:MG], brow[s2][:], ALU.add, [bpb[pbi], bbr[s2]], [bmr[s2]])
                dma("pool", modrow[0:1, g * MG:(g + 1) * MG], mrow[s2][:], [bmr[s2]], [bmodg[g]], "modw%d" % s2)

            def mod_vec(i_, a_, slot, pbi):
                gs = [bmodg[g] for g in range(a_ * D // MG, (a_ + 1) * D // MG)]
                dma("sp", mrows[:, i_, :], modrow[0:1, a_ * D:(a_ + 1) * D].rearrange("o (k p) -> (o k) p", p=128),
                    gs, [bmrows], "mv")
                tr(pb[pbi][:, i_ * KC:(i_ + 1) * KC], mrows[:, i_, :], identf_t[:KC, :KC], [bmrows, bc], [bpb[pbi]])
                cp("dve", vec_t[:, slot, :], pb[pbi][:, i_ * KC:(i_ + 1) * KC], [bpb[pbi]], [b_vec])

            WAW = 2 * SBW + 320
            wA = sbt(st, "wA", [128, KC, WAW], BF16); bwA = Buf("wA")
            mod_load(0)
            if NG1 > 1:
                mod_load(1)
            for g in range(NG1):
                mod_compute(g, 7)
                if g + 2 < NG1:
                    mod_load(g + 2)
            c0 = 0
            for (src0, n) in ((cf.o_ksb, SBW), (cf.o_vsb, SBW), (cf.o_kds, 256), (cf.o_kix, 64)):
                for a in range(0, n, 512):
                    m = min(512, n - a)
                    dma("pool", wA[:, :, c0 + a:c0 + a + m], winv[:, :, src0 + a:src0 + a + m], (), [bwA], "wA")
                c0 += n
            mod_vec(0, 0, 1, 6)
            mod_vec(1, 1, 6, 6)
            stt(vec_t[:, 0, :], vec_t[:, 6, :], 1.0, vec_t[:, 4, :], ALU.add, ALU.mult, [b_vec, bc], [b_vec])
            gnext = [NG1]
            if gnext[0] < NG:
                mod_load(gnext[0])
            if gnext[0] + 1 < NG:
                mod_load(gnext[0] + 1)

            def mod_step():
                g = gnext[0]
                if g >= NG:
                    return
                mod_compute(g, 7)
                if g + 2 < NG:
                    mod_load(g + 2)
                gnext[0] += 1

            xt = [sbt(st, "xt%d" % i, [128, D], F32) for i in range(3)]; bxt = [Buf("xt%d" % i) for i in range(3)]
            junk = sbt(st, "junkA", [128, D], BF16); ssq = sbt(st, "ssqA", [128, 4], F32)
            bt = Buf("nrmtmpA")
            hTa = [sbt(st, "hTa%d" % i, [128, KC, 512], BF16) for i in range(2)]
            bhT = [[Buf("hTa%d_%d" % (i, j_)) for j_ in range(4)] for i in range(2)]
            vst = [sbt(st, "vst%d" % i, [128, SBW], BF16) for i in range(2)]; bvst = [Buf("vst%d" % i) for i in range(2)]
            kst = [sbt(st, "kst%d" % i, [128, 512], BF16) for i in range(2)]; bkst = [Buf("kst%d" % i) for i in range(2)]
            sm = [sbt(st, "smA%d" % i, [128, 384], F32) for i in range(2)]; bsm = [Buf("smA%d" % i) for i in range(2)]
            rtmp = sbt(st, "rtmpA", [128, 4, 16], F32)
            vsbv = vsb.rearrange("h p x -> p h x")
            kctr = 0
            per_blk = -(-(NG - NG1) // NKB)

            def prep(c):
                s3 = c % 3; ti = (c // 4) % 2; cc = c % 4
                dma("sp", xt[s3][:], xkv[c * 128:(c + 1) * 128, :], (), [bxt[s3]], "xt%d" % s3)
                norm_T(xt[s3][:], 128, bxt[s3], lambda k, ti=ti, cc=cc: hTa[ti][:, k, cc * 128:(cc + 1) * 128],
                       bhT[ti][cc], G1, SH1, (junk, ssq, bt), [0, 1, 6])

            def ktr(c):
                tr(pb[5][:, 0:128], sm[c % 2][:, 0:128], identf_t[:], [bsm[c % 2], bc], [bpb[5]])
                tr(pb[5][:, 128:256], sm[c % 2][:, 256:384], identf_t[:], [bsm[c % 2], bc], [bpb[5]])
                cp("act", kTds[:, c * 128:(c + 1) * 128], pb[5][:, 0:128], [bpb[5]], [b_kTds])
                cp("act", kTix[:, c * 128:(c + 1) * 128], pb[5][:, 128:256], [bpb[5]], [b_kTix])

            prep(0)
            if NKB > 1:
                prep(1)
            for c in range(NKB):
                s2 = c % 2
                ti = (c // 4) % 2
                cc = c % 4
                for g in range(0, SBW, 512):
                    pbi = 2 + (g // 512) % 2
                    n = min(512, SBW - g)
                    for k in range(KC):
                        mm(pb[pbi][:, 0:n], hTa[ti][:, k, cc * 128:(cc + 1) * 128], wA[:, k, SBW + g:SBW + g + n],
                           k == 0, k == KC - 1, [bhT[ti][cc], bwA], [bpb[pbi]])
                    cp("act", vst[s2][:, g:g + n], pb[pbi][:, 0:n], [bpb[pbi]], [bvst[s2]])
                dma("act", vsbv[:, :, c * 128:(c + 1) * 128], vst[s2][:].rearrange("p (h d) -> p h d", h=NHS),
                    [bvst[s2]], (), "vsbw%d" % s2)
                for k in range(KC):
                    mm(pb[4][:, 0:320], hTa[ti][:, k, cc * 128:(cc + 1) * 128], wA[:, k, 2 * SBW:2 * SBW + 320],
                       k == 0, k == KC - 1, [bhT[ti][cc], bwA], [bpb[4]])
                smc = sm[c % 2]; bsmc = bsm[c % 2]
                cp("act", smc[:, 0:320], pb[4][:, 0:320], [bpb[4]], [bsmc])
                cp("pool", vds[:, c, :], smc[:, 128:256], [bsmc], [b_vds])
                rope_apply(smc[:, 0:16], smc[:, 16:32], cosK[:, c, :], sinK[:, c, :], rtmp, 128, 16, [bsmc, b_rope], [bsmc])
                rope_apply(smc[:, 256:264], smc[:, 264:272], cosK[:, c, 0:16:2], sinK[:, c, 0:16:2], rtmp, 128, 8,
                           [bsmc, b_rope], [bsmc])
                cp("dve", smc[:, 320:384], smc[:, 256:320], [bsmc], [bsmc])
                if c > 0:
                    ktr(c - 1)
                if cc == 3:
                    t0 = (c // 4) * 512
                    for h in range(NHS):
                        pbi = 2 + h % 2
                        for k in range(KC):
                            mm(pb[pbi][:, :], wA[:, k, h * 128:(h + 1) * 128], hTa[ti][:, k, :], k == 0, k == KC - 1,
                               bhT[ti] + [bwA], [bpb[pbi]])
                        ks = kctr % 2; kctr += 1
                        cp("act", kst[ks][:], pb[pbi][:, :], [bpb[pbi]], [bkst[ks]])
                        dma("act", kTsb[h, :, t0:t0 + 512], kst[ks][:], [bkst[ks]], (), "ktw%d" % ks)
                for _ in range(per_blk):
                    mod_step()
                if c + 2 < NKB:
                    prep(c + 2)
            ktr(NKB - 1)
            while gnext[0] < NG:
                mod_step()
            mod_vec(2, 3, 3, 6)
            mod_vec(3, 4, 7, 6)
            stt(vec_t[:, 2, :], vec_t[:, 7, :], 1.0, vec_t[:, 5, :], ALU.add, ALU.mult, [b_vec, bc], [b_vec])
        em.barrier()
        chk("A")
        wov = w_out.rearrange("(k p) n -> p k n", p=128)
        h2v = h2Ts.rearrange("p (k t) -> p k t", k=KC)
        U8 = mybir.dt.uint8
        with contextlib.ExitStack() as st:
            hTt = sbt(st, "hTt", [128, KC, TW], BF16); bhTt = Buf("hTt")
            xA = [sbt(st, "xA%d" % i, [BS, D], F32) for i in range(2)]; bxA = [Buf("xA%d" % i) for i in range(2)]
            junk = sbt(st, "junkT", [BS, D], BF16); ssq = sbt(st, "ssqT", [BS, 4], F32); bt = Buf("nrmtmpT")
            wq = [sbt(st, "wq%d" % i, [128, KC, 256], BF16) for i in range(2)]; bwq = [Buf("wq%d" % i) for i in range(2)]
            qTsb = sbt(st, "qTsb", [128, NHS, TW], BF16); bqsb = Buf("qTsb")
            qTds = sbt(st, "qTds", [128, NHD, TW], BF16); bqds = Buf("qTds")
            qTix = sbt(st, "qTix", [128, IH // 2, TW], BF16); bqix = Buf("qTix")
            wix = sbt(st, "wix", [BS, TB, IH], F32); bwix = Buf("wix")
            qtm = sbt(st, "qtm", [BS, 256], F32); bqtm = Buf("qtm")
            qtm2 = [sbt(st, "qtm2_%d" % i, [BS, 256], F32) for i in range(2)]; bqtm2 = [Buf("qtm2_%d" % i) for i in range(2)]
            rtmp = sbt(st, "rtmpT", [BS, 4, 16], F32)
            Mb = sbt(st, "Mb", [BS, TB, S], BF16); bMb = [Buf("Mb%d" % i) for i in range(TB)]
            SCRB = max(S * 4 + 8192, NKB * TW * 2, TB * D * 4)
            scr = sbt(st, "scr", [128, SCRB], U8)
            kvh = sbt(st, "kvh", [128, 4 * S], U8)
            kTh = kvh[:, 0:2 * S].bitcast(BF16); bkTh = Buf("kTh")
            vh = kvh[:, 2 * S:4 * S].bitcast(BF16); bvh = Buf("vh")
            scoreb = [scr[:BS, 0:S * 4].bitcast(F32), kvh[:BS, 0:S * 4].bitcast(F32)]
            bscb = [Buf("score0"), Buf("score1")]
            relb = [scr[:BS, S * 4 + 4096 * i:S * 4 + 4096 * i + 2048].bitcast(BF16) for i in range(2)]
            relb.append(sbt(st, "relb2", [BS, 1024], BF16)[:, :])
            brel = [Buf("rel%d" % i) for i in range(3)]
            biasT = [scr[:BS, S * 4 + 4096 * i + 2048:S * 4 + 4096 * (i + 1)].bitcast(F32) for i in range(2)]
            bbias = [Buf("biasT%d" % i) for i in range(2)]
            diagw = sbt(st, "diagw", [BS, IH, BS], BF16); bdiag = Buf("diagw")
            Yt = scr[:, 0:NKB * TW * 2].bitcast(BF16); bY = Buf("Yt")
            xqb = [scr[:BS, D * 4 * i:D * 4 * (i + 1)].bitcast(F32) for i in range(TB)]; bxq = [Buf("xqb%d" % i) for i in range(TB)]
            bis = [sbt(st, "bis%d" % i, [BS, 8 + NSP], F32) for i in range(2)]; bbis = [Buf("bis%d" % i) for i in range(2)]
            dtab = [sbt(st, "dtab%d" % i, [BS, NBIS + 1], F32) for i in range(2)]
            ndtab = [sbt(st, "ndtab%d" % i, [BS, NBIS + 1], F32) for i in range(2)]
            mrg = sbt(st, "mrg", [128, NM, TW], BF16); bmrg = [Buf("mrg%d" % i) for i in range(NM)]
            ytmp = sbt(st, "ytmp", [128, TW], F32); bytmp = Buf("ytmp")
            qrow_t = sbt(st, "qrow", [128, TW], F32); bqrow = Buf("qrow")
            ebuf = [sbt(st, "ebuf%d" % i, [128, TW], F32) for i in range(2)]; beb = [Buf("ebuf%d" % i) for i in range(2)]
            spb = [sbt(st, "spb%d" % i, [128, TW], BF16) for i in range(2)]; bspb = [Buf("spb%d" % i) for i in range(2)]
            Ab = [sbt(st, "Ab%d" % i, [128, TW], BF16) for i in range(2)]; bAb = [Buf("Ab%d" % i) for i in range(2)]
            wb = [sbt(st, "wb%d" % i, [128, TW], BF16) for i in range(2)]; bwb = [Buf("wb%d" % i) for i in range(2)]
            pbuf = [sbt(st, "pbuf%d" % i, [128, TW], BF16) for i in range(2)]; bpbuf = [Buf("pbuf%d" % i) for i in range(2)]
            rden = sbt(st, "rden", [128, TW], F32); brden = Buf("rden")
            g1t = [sbt(st, "g1t%d" % i, [BS, 256], F32) for i in range(2)]; bg1 = [Buf("g1t%d" % i) for i in range(2)]
            gsq = sbt(st, "gsq", [128, TW], BF16); bgsq = Buf("gsq")
            grs = sbt(st, "grs", [128, 2, TW], F32); bgrs = Buf("grs")
            h2st = sbt(st, "h2st", [128, KC, BS], BF16); bh2st = Buf("h2st")
            wqctr = [0]

            def wq_load(src_v, col0, n):
                s = wqctr[0] % 2; wqctr[0] += 1
                dma("pool", wq[s][:, :, 0:n], src_v[:, :, col0:col0 + n], (), [bwq[s]], "wq%d" % s)
                return s

            for t in range(NT):
                tc0 = t * TW
                St = cf.tile_keys(t); NKBt = St // 128; NSPt = St // 512
                for bi in range(TB):
                    blk = t * TB + bi
                    xa = blk % 2
                    dma("sp", xA[xa][:], xq[blk * BS:(blk + 1) * BS, :], (), [bxA[xa]], "xA%d" % xa)
                    norm_T(xA[xa][:], BS, bxA[xa], lambda k, bi=bi: hTt[:, k, bi * BS:(bi + 1) * BS], bhTt,
                           G1, SH1, (junk, ssq, bt), [0, 1])
                dma("sp", qrow_t[:], qrow[:, tc0:tc0 + TW], (), [bqrow], "qrow")
                chk("T1")
                for g in range(0, SBW, 256):
                    s = wq_load(winv, cf.o_qsb + g, 256)
                    for hh in range(2):
                        h = g // 128 + hh
                        pbi = 2 + h % 2
                        for k in range(KC):
                            mm(pb[pbi][:, 0:TW], wq[s][:, k, hh * 128:(hh + 1) * 128], hTt[:, k, :], k == 0, k == KC - 1,
                               [bwq[s], bhTt], [bpb[pbi]])
                        act(qTsb[:, h, :], pb[pbi][:, 0:TW], AF.Copy, [bpb[pbi]], [bqsb], scale=cf.qk_scale)
                qsteps = [("ds", g) for g in range(0, DSW, 256)] + [("ix", g) for g in range(0, IH * ID, 256)]
                pend = []
                sctr = [0]

                def q_mm(kind, s, bi):
                    blk = t * TB + bi
                    sl = sctr[0] % 2; sctr[0] += 1
                    pm = 4 if sl == 0 else 6
                    for k in range(KC):
                        mm(pb[pm][:BS, 0:256], hTt[:, k, bi * BS:(bi + 1) * BS], wq[s][:, k, :], k == 0, k == KC - 1,
                           [bwq[s], bhTt], [bpb[pm]])
                    cp("act", qtm2[sl][:, :], pb[pm][:BS, 0:256], [bpb[pm]], [bqtm2[sl]])
                    if kind == "ds":
                        for hh in range(2):
                            o_ = hh * 128
                            rope_apply(qtm2[sl][:, o_:o_ + 16], qtm2[sl][:, o_ + 16:o_ + 32], cosQ[:, blk, :], sinQ[:, blk, :],
                                       rtmp, BS, 16, [bqtm2[sl], b_rope], [bqtm2[sl]])
                    else:
                        for hh in range(256 // ID):
                            o_ = hh * ID
                            rope_apply(qtm2[sl][:, o_:o_ + 8], qtm2[sl][:, o_ + 8:o_ + 16], cosQ[:, blk, 0:16:2],
                                       sinQ[:, blk, 0:16:2], rtmp, BS, 8, [bqtm2[sl], b_rope], [bqtm2[sl]])
                    return sl

                def q_tr(kind, g, bi, sl):
                    pt = 5 if sl == 0 else 7
                    for pp in range(2):
                        tr(pb[pt][:, pp * 128:pp * 128 + BS], qtm2[sl][:, pp * 128:(pp + 1) * 128], identf_t[:BS, :BS],
                           [bqtm2[sl], bc], [bpb[pt]])
                    for pp in range(2):
                        if kind == "ds":
                            act(qTds[:, g // 128 + pp, bi * BS:(bi + 1) * BS], pb[pt][:, pp * 128:pp * 128 + BS], AF.Copy,
                                [bpb[pt]], [bqds], scale=cf.qk_scale)
                        else:
                            cp("act", qTix[:, g // 128 + pp, bi * BS:(bi + 1) * BS], pb[pt][:, pp * 128:pp * 128 + BS],
                               [bpb[pt]], [bqix])

                for (kind, g) in qsteps:
                    s = wq_load(winv, (cf.o_qds if kind == "ds" else cf.o_qix) + g, 256)
                    for bi in range(TB):
                        sl = q_mm(kind, s, bi)
                        if pend:
                            q_tr(*pend.pop())
                        pend.append((kind, g, bi, sl))
                if pend:
                    q_tr(*pend.pop())
                s = wq_load(winv, cf.o_wix, IH)
                for bi in range(TB):
                    for k in range(KC):
                        mm(pb[4][:BS, 0:IH], hTt[:, k, bi * BS:(bi + 1) * BS], wq[s][:, k, 0:IH], k == 0, k == KC - 1,
                           [bwq[s], bhTt], [bpb[4]])
                    act(wix[:, bi, :], pb[4][:BS, 0:IH], AF.Copy, [bpb[4]], [bwix], scale=cf.idx_scale)
                wo_slots = [wq_load(wov, g_, 256) for g_ in range(0, min(D, 512), 256)]

                chk("T2")
                def idx_block(bi):
                    blk = t * TB + bi
                    sb_ = bi % 2; sc = scoreb[sb_]; bs_ = bscb[sb_]; B_ = bis[sb_]; bB = bbis[sb_]
                    for h in range(IH):
                        ts("pool", diagw[:, h, :], identb[:BS, :BS], wix[:, bi, h:h + 1], None, ALU.mult, None,
                           [bc, bwix], [bdiag])
                    gctr = 0; spctr = 0
                    for sp_ in range(0, St, 1024):
                        nsp = min(1024, St - sp_); nb = nsp // 512
                        ab = 6; spctr += 1
                        def idx_mm(h, pg):
                            po = (h % 2) * 64
                            for q_ in range(nb):
                                mm(pb[pg + q_][:BS, :], qTix[po:po + 64, h // 2, bi * BS:(bi + 1) * BS],
                                   kTix[po:po + 64, sp_ + q_ * 512:sp_ + (q_ + 1) * 512], True, True,
                                   [bqix, b_kTix], [bpb[pg + q_]])

                        pgs = []
                        for h in range(IH):
                            pgs.append((gctr % 3) * 2); gctr += 1
                        idx_mm(0, pgs[0])
                        if IH > 1:
                            idx_mm(1, pgs[1])
                        for h in range(IH):
                            pg = pgs[h]
                            if h + 2 < IH:
                                idx_mm(h + 2, pgs[h + 2])
                            rs = (gctr + h) % 3
                            em.op("dve", lambda e, rs=rs, pg=pg, nb=nb, nsp=nsp: e.tensor_scalar(
                                out=relb[rs][:, 0:nsp].rearrange("p (a f) -> p a f", a=nb), in0=PS[:BS, pg:pg + nb, :],
                                scalar1=0.0, scalar2=None, op0=ALU.max),
                                [bpb[pg + q_] for q_ in range(nb)], [brel[rs]])
                            for q_ in range(nb):
                                mm(pb[ab + q_][:BS, :], diagw[:, h, :], relb[rs][:, q_ * 512:(q_ + 1) * 512], h == 0, h == IH - 1,
                                   [bdiag, brel[rs]], [bpb[ab + q_]])
                        for q_ in range(nb):
                            kt = sp_ // 512 + q_
                            em.op("dve", lambda e, kt=kt, ab=ab, q_=q_, B_=B_: e.tensor_reduce(
                                out=B_[:, 8 + kt:9 + kt], in_=pb[ab + q_][:BS, :], axis=AX.X, op=ALU.min),
                                [bpb[ab + q_]], [bB])
                            ts("dve", biasT[q_ % 2], iota_t[:BS, :], qrel_t[:, blk, kt:kt + 1], -1e30, ALU.is_gt, ALU.mult,
                               [bc], [bbias[q_ % 2]])
                            tt("dve", sc[:, kt * 512:(kt + 1) * 512], pb[ab + q_][:BS, :], biasT[q_ % 2], ALU.add,
                               [bpb[ab + q_], bbias[q_ % 2]], [bs_])
                    em.op("dve", lambda e, St=St, sc=sc, B_=B_: e.tensor_reduce(out=B_[:, 0:1], in_=sc[:, 0:St], axis=AX.X, op=ALU.max), [bs_], [bB])
                    em.op("dve", lambda e, NSPt=NSPt, B_=B_: e.tensor_reduce(out=B_[:, 1:2], in_=B_[:, 8:8 + NSPt], axis=AX.X, op=ALU.min), [bB], [bB])
                    stt(B_[:, 6:7], B_[:, 0:1], 2.0, B_[:, 1:2], ALU.add, ALU.subtract, [bB], [bB])
                    ts("dve", dtab[sb_][:], pow2_t[:BS, :], B_[:, 6:7], None, ALU.mult, None, [bB, bc], [bB])
                    ts("dve", ndtab[sb_][:], dtab[sb_][:], -1.0, None, ALU.mult, None, [bB], [bB])
                    ts("dve", B_[:, 2:3], B_[:, 1:2], -1.0, 1.0, ALU.mult, ALU.add, [bB], [bB])
                    tt("dve", B_[:, 2:3], B_[:, 2:3], dtab[sb_][:, 0:1], ALU.subtract, [bB], [bB])

                def bisect(bi):
                    sb_ = bi % 2; sc = scoreb[sb_]; bs_ = bscb[sb_]; B_ = bis[sb_]; bB = bbis[sb_]
                    for r_ in range(NBIS):
                        act(Mb[:, bi, 0:St], sc[:, 0:St], AF.Sign, [bs_, bB], [bMb[bi], bB], bias=B_[:, 2:3], accum_out=B_[:, 3:4])
                        act(B_[:, 4:5], B_[:, 3:4], AF.Sign, [bB], [bB], bias=float(St - 2 * cf.TOPK) + 0.5)
                        act(B_[:, 2:3], B_[:, 4:5], AF.Identity, [bB], [bB], scale=ndtab[sb_][:, r_ + 1:r_ + 2], bias=B_[:, 2:3])

                def finalize(bi):
                    sb_ = bi % 2; sc = scoreb[sb_]; bs_ = bscb[sb_]; B_ = bis[sb_]; bB = bbis[sb_]
                    stt(B_[:, 5:6], B_[:, 2:3], -1.0, dtab[sb_][:, NBIS:NBIS + 1], ALU.mult, ALU.subtract, [bB], [bB])
                    ts("dve", Mb[:, bi, 0:St], sc[:, 0:St], B_[:, 5:6], NEGB, ALU.is_le, ALU.mult, [bs_, bB], [bMb[bi]])

                idx_block(0)
                for bi in range(TB):
                    bisect(bi)
                    if bi + 1 < TB:
                        idx_block(bi + 1)
                    finalize(bi)

                chk("T3")
                for h in range(NHD):
                    def S_step(c):
                        pz = c % 2
                        mm(pb[pz][:, 0:TW], kTds[:, c * 128:(c + 1) * 128], qTds[:, h, :], True, False,
                           [b_kTds, bqds], [bpb[pz]])
                        for bi in range(TB):
                            mm(pb[pz][:, bi * BS:(bi + 1) * BS], Mb[:, bi, c * 128:(c + 1) * 128], identb[:BS, :BS],
                               False, bi == TB - 1, [bMb[bi], bc], [bpb[pz]])
                        act(pbuf[pz][:], pb[pz][:, 0:TW], AF.Exp, [bpb[pz]], [bpbuf[pz]])

                    def PV_step(c):
                        pz = c % 2
                        mm(pb[2][:, 0:TW], vds[:, c, :], pbuf[pz][:], c == 0, c == NKBt - 1, [b_vds, bpbuf[pz]], [bpb[2]])
                        mm(pb[3][:, 0:TW], onesb, pbuf[pz][:], c == 0, c == NKBt - 1, [bc, bpbuf[pz]], [bpb[3]])

                    S_step(0)
                    for c in range(NKBt):
                        if c + 1 < NKBt:
                            S_step(c + 1)
                        PV_step(c)
                    em.op("dve", lambda e: e.reciprocal(out=rden[:], in_=pb[3][:, 0:TW]), [bpb[3]], [brden])
                    tt("dve", mrg[:, NHS + h, :], pb[2][:, 0:TW], rden[:], ALU.mult, [bpb[2], brden], [bmrg[NHS + h]])

                chk("T4")
                em.barrier()
                for c in range(NKBt):
                    ts("dve", ytmp[:], qrow_t[:], float(-128 * c), 0.0, ALU.add, ALU.max, [bqrow], [bytmp])
                    ts("dve", Yt[:, c * TW:(c + 1) * TW], ytmp[:], kcol_t[:, 0:1], None, ALU.is_equal, None, [bytmp, bc], [bY])
                for h in range(NHS):
                    dma("sp", kTh[:, 0:St], kTsb[h, :, 0:St], (), [bkTh], "kTh")
                    dma("sp", vh[:, 0:St], vsb[h, :, 0:St], (), [bvh], "vh")
                    order = list(range(NKBt - 1, -1, -1))

                    def Z_step(i):
                        c = order[i]; pz = 4 + i % 2
                        mm(pb[pz][:, 0:TW], kTh[:, c * 128:(c + 1) * 128], qTsb[:, h, :], True, False,
                           [bkTh, bqsb], [bpb[pz]])
                        mm(pb[pz][:, 0:TW], umat, Yt[:, c * TW:(c + 1) * TW], False, True, [bc, bY], [bpb[pz]])
                        act(ebuf[i % 2][:], pb[pz][:, 0:TW], AF.Exp, [bpb[pz]], [beb[i % 2]])
                        act(spb[i % 2][:], ebuf[i % 2][:], AF.Ln, [beb[i % 2]], [bspb[i % 2]], bias=1.0)

                    def X_step(i):
                        c = order[i]; px = 6 + i % 2
                        mm(pb[px][:, 0:TW], kTh[:, c * 128:(c + 1) * 128], qTsb[:, h, :], True, False,
                           [bkTh, bqsb], [bpb[px]])
                        mm(pb[px][:, 0:TW], umat, Yt[:, c * TW:(c + 1) * TW], False, False, [bc, bY], [bpb[px]])
                        if i > 0:
                            mm(pb[px][:, 0:TW], nones, Ab[i % 2][:], False, False, [bc, bAb[i % 2]], [bpb[px]])
                        mm(pb[px][:, 0:TW], ntri, spb[i % 2][:], False, True, [bc, bspb[i % 2]], [bpb[px]])
                        if i == 0:
                            cp("pool", Ab[1][:], spb[0][:], [bspb[0]], [bAb[1]])
                        elif i + 1 < NKBt:
                            tt("pool", Ab[(i + 1) % 2][:], Ab[i % 2][:], spb[i % 2][:], ALU.add,
                               [bAb[i % 2], bspb[i % 2]], [bAb[(i + 1) % 2]])
                        act(wb[i % 2][:], pb[px][:, 0:TW], AF.Exp, [bpb[px]], [bwb[i % 2]])

                    def PVs(i):
                        c = order[i]
                        mm(pb[3][:, 0:TW], vh[:, c * 128:(c + 1) * 128], wb[i % 2][:], i == 0, i == NKBt - 1, [bvh, bwb[i % 2]], [bpb[3]])

                    Z_step(0)
                    for i in range(NKBt):
                        if i + 1 < NKBt:
                            Z_step(i + 1)
                        X_step(i)
                        if i > 0:
                            PVs(i - 1)
                    PVs(NKBt - 1)
                    cp("dve", mrg[:, h, :], pb[3][:, 0:TW], [bpb[3]], [bmrg[h]])

                chk("T5")
                for gi, (h0, nh_, g_t, W_) in enumerate(((0, NHS, sbg_t, SBW), (NHS, NHD, dsg_t, DSW))):
                    for hh in range(nh_):
                        act(gsq[:], mrg[:, h0 + hh, :], AF.Square, [bmrg[h0 + hh]], [bgsq])
                        mm(pb[0][:, 0:TW], onesb, gsq[:], hh == 0, hh == nh_ - 1, [bc, bgsq], [bpb[0]])
                    act(grs[:, 0, :], pb[0][:, 0:TW], AF.Sqrt, [bpb[0]], [bgrs], scale=1.0 / W_, bias=1e-6)
                    em.op("dve", lambda e: e.reciprocal(out=grs[:, 1, :], in_=grs[:, 0, :]), [bgrs], [bgrs])
                    for hh in range(nh_):
                        stt(mrg[:, h0 + hh, :], mrg[:, h0 + hh, :], g_t[:, hh:hh + 1], grs[:, 1, :], ALU.mult, ALU.mult,
                            [bmrg[h0 + hh], bgrs, bc], [bmrg[h0 + hh]])

                chk("T6")
                em.barrier()
                for bi in range(TB):
                    blk = t * TB + bi
                    dma("sp", xqb[bi], xq[blk * BS:(blk + 1) * BS, :], (), [bxq[bi]], "xqb%d" % bi)
                for gi, g in enumerate(range(0, D, 256)):
                    s = wo_slots[gi]
                    gs = gi % 2
                    dma("sp", g1t[gs][:], modrow[0:1, 2 * D + g:2 * D + g + 256].partition_broadcast(BS), (), [bg1[gs]], "g1t%d" % gs)
                    for bi in range(TB):
                        pbi = bi % 2
                        for k in range(NM):
                            mm(pb[pbi][:BS, 0:256], mrg[:, k, bi * BS:(bi + 1) * BS], wq[s][:, k, :], k == 0, k == NM - 1,
                               bmrg + [bwq[s]], [bpb[pbi]])
                        tt("dve", qtm[:, :], pb[pbi][:BS, 0:256], g1t[gs][:], ALU.mult, [bpb[pbi], bg1[gs]], [bqtm])
                        tt("dve", xqb[bi][:, g:g + 256], xqb[bi][:, g:g + 256], qtm[:, :], ALU.add, [bqtm, bxq[bi]], [bxq[bi]])
                    if g + 512 < D:
                        wo_slots.append(wq_load(wov, g + 512, 256))
                for bi in range(TB):
                    blk = t * TB + bi
                    dma("pool", xmid[blk * BS:(blk + 1) * BS, :], xqb[bi], [bxq[bi]], (), "xmidw%d" % bi)
                    norm_T(xqb[bi], BS, bxq[bi], lambda k: h2st[:, k, :], bh2st, G2, SH2, (junk, ssq, bt), [2, 3],
                           xs_out=xA[blk % 2][:], bxs=bxA[blk % 2])
                    dma("act", h2v[:, :, blk * BS:(blk + 1) * BS], h2st[:], [bh2st], (), "h2w")
                em.barrier()
        em.barrier()

        chk("T")
        wuv = w_up.rearrange("(k p) n -> p k n", p=128)
        wdv = w_down.rearrange("(k p) n -> p k n", p=128)
        with contextlib.ExitStack() as st:
            mT = sbt(st, "mT", [128, FC, OWN], BF16); bmT = Buf("mT")
            h2T = sbt(st, "h2T", [128, KC, TQ], BF16); b_h2T = Buf("h2T")
            dma("sp", h2T[:], h2Ts.rearrange("p (k t) -> p k t", k=KC), (), [b_h2T], "h2l")
            with contextlib.ExitStack() as st2:
                wu = [sbt(st2, "wu%d" % i, [128, KC, 256], BF16) for i in range(3)]; bwu = [Buf("wu%d" % i) for i in range(3)]
                usb = [sbt(st2, "usb%d" % i, [128, TQ], F32) for i in range(2)]; busb = [Buf("usb%d" % i) for i in range(2)]
                ucg = sbt(st2, "ucg", [128, OWN], F32); bucg = Buf("ucg")
                ucv = sbt(st2, "ucv", [128, OWN], F32); bucv = Buf("ucv")
                for f in range(FC):
                    s = f % 3
                    dma("pool", wu[s][:, :, 0:128], wuv[:, :, f * 128:(f + 1) * 128], (), [bwu[s]], "wu%d" % s)
                    dma("pool", wu[s][:, :, 128:256], wuv[:, :, DFF + f * 128:DFF + (f + 1) * 128], (), [bwu[s]], "wu%d" % s)
                    for half in range(2):
                        ci = f + half * FC
                        ub = usb[half]; bub = busb[half]
                        for t in range(NT):
                            pbi = (2 * t + half) % 4
                            for k in range(KC):
                                mm(pb[pbi][:, 0:TW], wu[s][:, k, half * 128:(half + 1) * 128], h2T[:, k, t * TW:(t + 1) * TW],
                                   k == 0, k == KC - 1, [bwu[s], b_h2T], [bpb[pbi]])
                            cp("act", ub[:, t * TW:(t + 1) * TW], pb[pbi][:, 0:TW], [bpb[pbi]], [bub])
                        ts("dve", ub[:, 0:2], ub[:, 0:2], flag_t[:, 0:1], None, ALU.mult, None, [bub, bc], [bub])
                        uc = ucg if half == 0 else ucv
                        buc = bucg if half == 0 else bucv
                        ubv = ub[:].rearrange("p (s w) -> p s w", s=NT)
                        uc3 = uc[:].rearrange("p (s w) -> p s w", s=NT)
                        act(uc3, ubv[:, :, 2:TW], AF.Identity, [bub, bc], [buc], scale=cvw_t[:, 2, ci:ci + 1], bias=cvb_t[:, ci:ci + 1])
                        stt(uc3, ubv[:, :, 1:TW - 1], cvw_t[:, 1, ci:ci + 1], uc3, ALU.mult, ALU.add, [bub, bc, buc], [buc])
                        stt(uc3, ubv[:, :, 0:TW - 2], cvw_t[:, 0, ci:ci + 1], uc3, ALU.mult, ALU.add, [bub, bc, buc], [buc])
                    act(ucg[:], ucg[:], AF.Silu, [bucg], [bucg])
                    tt("dve", mT[:, f, :], ucg[:], ucv[:], ALU.mult, [bucg, bucv], [bmT])
            em.barrier()
            with contextlib.ExitStack() as st2:
                wd = [sbt(st2, "wd%d" % i, [128, FC, 256], BF16) for i in range(2)]; bwd = [Buf("wd%d" % i) for i in range(2)]
                yst = [sbt(st2, "yst%d" % i, [128, 256], F32) for i in range(2)]; byst = [Buf("yst%d" % i) for i in range(2)]
                yc = 0
                for gi, g in enumerate(range(0, D, 256)):
                    s = gi % 2
                    for f0 in range(0, FC, 16):
                        f1 = min(FC, f0 + 16)
                        dma("pool", wd[s][:, f0:f1, :], wdv[:, f0:f1, g:g + 256], (), [bwd[s]], "wd%d" % s)
                    for tb in range(NOB):
                        pbi = tb % 2
                        for f in range(FC):
                            mm(pb[pbi][:, 0:256], mT[:, f, tb * 128:(tb + 1) * 128], wd[s][:, f, :], f == 0, f == FC - 1,
                               [bmT, bwd[s]], [bpb[pbi]])
                        ys = yc % 2; yc += 1
                        cp("act", yst[ys][:], pb[pbi][:, 0:256], [bpb[pbi]], [byst[ys]])
                        dma("act", yscr[tb * 128:(tb + 1) * 128, g:g + 256], yst[ys][:], [byst[ys]], (), "yscrw%d" % ys)
        em.barrier()
        with contextlib.ExitStack() as st:
            g2bc = sbt(st, "g2bc", [128, D], F32); fngt = sbt(st, "fngt", [128, D], F32); bgg = Buf("g2fng")
            xm = [sbt(st, "xm%d" % i, [128, D], F32) for i in range(2)]; bxm = [Buf("xm%d" % i) for i in range(2)]
            yy = [sbt(st, "yy%d" % i, [128, D], F32) for i in range(2)]; byy = [Buf("yy%d" % i) for i in range(2)]
            junk = sbt(st, "junkF", [128, D], BF16); ssq = sbt(st, "ssqF", [128, 4], F32); bt = Buf("nrmF")
            dma("sp", g2bc[:], modrow[0:1, 5 * D:6 * D].partition_broadcast(128), (), [bgg], "g2bc")
            dma("sp", fngt[:], fng[:, :], (), [bgg], "g2bc")
            for tb in range(NOB):
                s = tb % 2
                r0_ = (tb // cf.BPS) * TW + 2 + (tb % cf.BPS) * 128
                dma("sp", xm[s][:], xmid[r0_:r0_ + 128, :], (), [bxm[s]], "xm%d" % s)
                dma("sp", yy[s][:], yscr[tb * 128:(tb + 1) * 128, :], (), [byy[s]], "yy%d" % s)
                tt("dve", yy[s][:], yy[s][:], g2bc[:], ALU.mult, [byy[s], bgg], [byy[s]])
                tt("dve", xm[s][:], xm[s][:], yy[s][:], ALU.add, [byy[s], bxm[s]], [bxm[s]])
                act(junk[:], xm[s][:], AF.Square, [bxm[s]], [bt], accum_out=ssq[:, 0:1])
                act(ssq[:, 1:2], ssq[:, 0:1], AF.Sqrt, [bt], [bt], scale=1.0 / D, bias=1e-6)
                em.op("dve", lambda e: e.reciprocal(out=ssq[:, 2:3], in_=ssq[:, 1:2]), [bt], [bt])
                stt(yy[s][:], xm[s][:], ssq[:, 2:3], fngt[:], ALU.mult, ALU.mult, [bxm[s], bt, bgg], [byy[s]])
                dma("pool", out[tb * 128:(tb + 1) * 128, :], yy[s][:], [byy[s]], (), "outw%d" % s)
        em.emit(final_waits=["outw0", "outw1"] if NOB > 1 else ["outw0"])
    return nc


def host_inputs(cf, inp, core):
    D, S, KC, NKB, TQ, BS, NBLK, NSP, FC = cf.D, cf.S, cf.KC, cf.NKB, cf.TQ, cf.BS, cf.NBLK, cf.NSP, cf.FC
    b = core // cf.CPB; j = core % cf.CPB
    f32 = np.float32
    x = np.asarray(inp["x"], f32); pos = np.asarray(inp["positions"], np.int32)
    tok = np.concatenate([np.arange(cf.seg_tokens(j, t) - 2, cf.seg_tokens(j, t) + cf.SEG) for t in range(cf.NT)])
    assert tok.shape[0] == TQ
    tokc = np.maximum(tok, 0)
    m = {}
    m["xkv"] = np.ascontiguousarray(x[b])
    m["xq"] = np.ascontiguousarray(x[b][tokc])
    m["posk"] = np.ascontiguousarray(pos[b].reshape(NKB, 128).T)
    m["posq"] = np.ascontiguousarray(pos[b][tokc].reshape(NBLK, BS).T)
    qc = tokc.astype(f32).reshape(NBLK, BS).T
    m["qcol"] = np.ascontiguousarray(qc)
    qr = qc[:, :, None] - (512.0 * np.arange(NSP, dtype=f32))[None, None, :]
    m["qrel"] = np.ascontiguousarray(qr.reshape(BS, NBLK * NSP).astype(f32))
    m["qrow"] = np.ascontiguousarray(np.broadcast_to(tokc.astype(f32)[None, :], (128, TQ)))
    pk = lambda v: np.ascontiguousarray(np.asarray(v, f32).reshape(-1, 128).T)
    m["cb"] = pk(np.asarray(inp["c"], f32)[b])
    m["w_ada"] = np.ascontiguousarray(np.asarray(inp["w_ada"], f32)[0])
    m["b_ada"] = np.ascontiguousarray(np.asarray(inp["b_ada"], f32)[0][None, :])
    m["w_in"] = np.ascontiguousarray(np.asarray(inp["w_in"], f32)[0])
    m["w_out"] = np.ascontiguousarray(np.asarray(inp["w_out"], f32)[0])
    m["w_up"] = np.ascontiguousarray(np.asarray(inp["w_up"], f32)[0])
    m["w_down"] = np.ascontiguousarray(np.asarray(inp["w_down"], f32)[0])
    m["n1g"] = pk(np.asarray(inp["norm1_g"])[0]); m["n2g"] = pk(np.asarray(inp["norm2_g"])[0])
    m["sbg"] = pk(np.asarray(inp["sb_norm_g"])[0]); m["dsg"] = pk(np.asarray(inp["dsa_norm_g"])[0])
    m["fng"] = np.ascontiguousarray(np.broadcast_to(np.asarray(inp["final_norm_g"], f32)[None, :], (128, D)))
    cw = np.asarray(inp["conv_w"], f32)[0]
    m["convw"] = np.ascontiguousarray(np.stack([pk(cw[i]) for i in range(3)], axis=1).reshape(128, 3 * 2 * FC))
    m["convb"] = pk(np.asarray(inp["conv_b"], f32)[0])
    m["identf"] = np.eye(128, dtype=f32)
    jj = np.arange(128)[:, None]; ss = np.arange(128)[None, :]
    cm = np.stack([np.eye(128, dtype=f32), -(jj >= ss).astype(f32), -np.ones((128, 128), f32),
                   NEGB * (ss >= jj).astype(f32), np.ones((128, 128), f32)], axis=1)
    m["cmat"] = np.ascontiguousarray(cm.reshape(128, 5 * 128))
    m["iota"] = np.ascontiguousarray(np.broadcast_to(np.arange(512, dtype=f32)[None, :], (128, 512)))
    m["kcolc"] = np.arange(128, dtype=f32)[:, None].copy()
    m["pow2"] = np.ascontiguousarray(np.broadcast_to((0.5 ** np.arange(1, cf.NBIS + 2)).astype(f32)[None, :], (128, cf.NBIS + 1)))
    ivf = (np.float32(500000.0) ** (-(np.arange(16, dtype=f32) / np.float32(16)))).astype(f32)
    m["invf"] = np.ascontiguousarray(np.broadcast_to(ivf[None, :], (128, 16)))
    m["flag"] = np.full((128, 1), 0.0 if j == 0 else 1.0, f32)
    return m


_CACHE = {}


def kernel(x, c, positions, w_ada, b_ada, norm1_g, w_in, sb_norm_g, dsa_norm_g, w_out, norm2_g,
           w_up, conv_w, conv_b, w_down, final_norm_g):
    inp = dict(x=x, c=c, positions=positions, w_ada=w_ada, b_ada=b_ada, norm1_g=norm1_g, w_in=w_in,
               sb_norm_g=sb_norm_g, dsa_norm_g=dsa_norm_g, w_out=w_out, norm2_g=norm2_g, w_up=w_up,
               conv_w=conv_w, conv_b=conv_b, w_down=w_down, final_norm_g=final_norm_g)
    cf = Cfg()
    nc = build_program(cf)
    in_maps = [host_inputs(cf, inp, core) for core in range(8)]
    res = run_bass_kernel_spmd(nc, in_maps, core_ids=list(range(8)))
    outp = np.zeros((2, cf.S, cf.D), np.float32)
    for core in range(8):
        b = core // cf.CPB; j = core % cf.CPB
        o_ = np.asarray(res.results[core]["out"], np.float32)
        for t in range(cf.NT):
            g0 = cf.seg_tokens(j, t)
            outp[b, g0:g0 + cf.SEG, :] = o_[t * cf.SEG:(t + 1) * cf.SEG]
    return outp
```
